# Optimizing a Trainium2 kernel written in Bass

```python
import math
import jax, jax.numpy as jnp
from jax import lax
import numpy as np

D_MODEL = 1024
BATCH = 8
SEQ = 4096
DEPTH = 1

MIX_WIDTH = D_MODEL
MLA_WIDTH = MIX_WIDTH // 2
RET_WIDTH = MIX_WIDTH - MLA_WIDTH
MLA_HEADS = 4
MLA_NOPE_DIM = 128
MLA_ROPE_DIM = 64
MLA_QK_DIM = MLA_NOPE_DIM + MLA_ROPE_DIM
MLA_V_DIM = MLA_WIDTH // MLA_HEADS
MLA_Q_RANK = 256
MLA_KV_RANK = 128
Q_BLOCK = 128
RET_HEADS = 4
RET_HEAD_DIM = RET_WIDTH // RET_HEADS
RET_CHUNK = 128
MEM_TOKENS = 256
CROSS_HEADS = 4
CROSS_HEAD_DIM = D_MODEL // CROSS_HEADS
N_GROUPS = 4
EXPERTS_PER_GROUP = 8
N_EXPERTS = N_GROUPS * EXPERTS_PER_GROUP
TOP_K = 2
EXPERT_FF = 256
ROPE_BASE = 10000.0
EPS = 1e-6
NEG_INF = -1e30
IN_SPLITS = (MLA_Q_RANK, MLA_KV_RANK, MLA_ROPE_DIM, RET_WIDTH, RET_WIDTH, RET_WIDTH, RET_WIDTH)
IN_COLS = sum(IN_SPLITS)

kernel_name = 'hybrid_mla_retention_hmoe_block'


def rms_norm(x, g):
    xf = x.astype(jnp.float32)
    y = xf * lax.rsqrt(jnp.mean(xf * xf, axis=-1, keepdims=True) + EPS)
    return (y * g.astype(jnp.float32)).astype(x.dtype)


def rope(x, positions):
    half = x.shape[-1] // 2
    inv_freq = ROPE_BASE ** (-jnp.arange(half, dtype=jnp.float32) / half)
    ang = positions.astype(jnp.float32)[:, :, None] * inv_freq
    cos = jnp.cos(ang)[:, :, None, :]
    sin = jnp.sin(ang)[:, :, None, :]
    xf = x.astype(jnp.float32)
    x1, x2 = xf[..., :half], xf[..., half:]
    return jnp.concatenate([x1 * cos - x2 * sin, x2 * cos + x1 * sin], axis=-1).astype(x.dtype)


def causal_block_attention(q, k, v):
    B, S, H, dq = q.shape
    dv = v.shape[-1]
    nb = S // Q_BLOCK
    scale = dq ** -0.5
    qb = q.reshape(B, nb, Q_BLOCK, H, dq).transpose(1, 0, 2, 3, 4)
    k_idx = jnp.arange(S)

    def one_block(args):
        q_blk, b = args
        s = jnp.einsum('bqhd,bkhd->bhqk', q_blk, k, preferred_element_type=jnp.float32) * scale
        q_idx = b * Q_BLOCK + jnp.arange(Q_BLOCK)
        s = jnp.where(k_idx[None, :] <= q_idx[:, None], s, NEG_INF)
        p = jax.nn.softmax(s, axis=-1).astype(v.dtype)
        return jnp.einsum('bhqk,bkhd->bqhd', p, v)

    out = lax.map(one_block, (qb, jnp.arange(nb)))
    return out.transpose(1, 0, 2, 3, 4).reshape(B, S, H, dv)


def retention_chunkwise(q, k, v):
    B, S, H, dk = q.shape
    dv = v.shape[-1]
    nc = S // RET_CHUNK

    def to_chunks(t):
        return t.astype(jnp.float32).reshape(B, nc, RET_CHUNK, H, t.shape[-1]).transpose(1, 0, 3, 2, 4)

    lg = jnp.log(1.0 - jnp.exp2(-5.0 - jnp.arange(H, dtype=jnp.float32)))
    idx = jnp.arange(RET_CHUNK, dtype=jnp.float32)
    diff = idx[:, None] - idx[None, :]
    intra = jnp.where(diff >= 0, jnp.exp(jnp.maximum(diff, 0.0)[None] * lg[:, None, None]), 0.0)
    q_decay = jnp.exp((idx + 1.0)[None] * lg[:, None])[:, :, None]
    k_decay = jnp.exp((RET_CHUNK - 1.0 - idx)[None] * lg[:, None])[:, :, None]
    chunk_decay = jnp.exp(RET_CHUNK * lg)[:, None, None]

    def step(state, qkv):
        qc, kc, vc = qkv
        scores = jnp.einsum('bhid,bhjd->bhij', qc, kc) * intra
        o = (jnp.einsum('bhij,bhjv->bhiv', scores, vc)
             + jnp.einsum('bhid,bhdv->bhiv', qc * q_decay, state))
        state = state * chunk_decay + jnp.einsum('bhjd,bhjv->bhdv', kc * k_decay, vc)
        return state, o

    state0 = jnp.zeros((B, H, dk, dv), jnp.float32)
    _, o = lax.scan(step, state0, (to_chunks(q), to_chunks(k), to_chunks(v)))
    return o.transpose(1, 0, 3, 2, 4).reshape(B, S, H, dv)


def mla_group(c_q, c_kv, k_pe, positions, q_norm_g, w_uq, kv_norm_g, w_ukv, q_qk_g, k_qk_g):
    B, S, _ = c_q.shape
    q = (rms_norm(c_q, q_norm_g) @ w_uq).reshape(B, S, MLA_HEADS, MLA_QK_DIM)
    kv = (rms_norm(c_kv, kv_norm_g) @ w_ukv).reshape(B, S, MLA_HEADS, MLA_NOPE_DIM + MLA_V_DIM)
    k_nope, v = kv[..., :MLA_NOPE_DIM], kv[..., MLA_NOPE_DIM:]
    k_rope = jnp.broadcast_to(k_pe[:, :, None, :], (B, S, MLA_HEADS, MLA_ROPE_DIM))
    k = jnp.concatenate([k_nope, k_rope], axis=-1)
    q = rms_norm(q, q_qk_g)
    k = rms_norm(k, k_qk_g)
    q = jnp.concatenate([q[..., :MLA_NOPE_DIM], rope(q[..., MLA_NOPE_DIM:], positions)], axis=-1)
    k = jnp.concatenate([k[..., :MLA_NOPE_DIM], rope(k[..., MLA_NOPE_DIM:], positions)], axis=-1)
    o = causal_block_attention(q, k, v)
    return o.reshape(B, S, MLA_WIDTH)


def retention_group(rq, rk, rv, rg, positions, gn_g):
    B, S, _ = rq.shape
    q = rope(rq.reshape(B, S, RET_HEADS, RET_HEAD_DIM), positions)
    k = rope(rk.reshape(B, S, RET_HEADS, RET_HEAD_DIM), positions) * (RET_HEAD_DIM ** -0.5)
    v = rv.reshape(B, S, RET_HEADS, RET_HEAD_DIM)
    o = retention_chunkwise(q, k, v)
    mu = jnp.mean(o, axis=-1, keepdims=True)
    var = jnp.mean(jnp.square(o - mu), axis=-1, keepdims=True)
    o = ((o - mu) * lax.rsqrt(var + EPS)).reshape(B, S, RET_WIDTH) * gn_g.astype(jnp.float32)
    return (o * jax.nn.silu(rg.astype(jnp.float32))).astype(rq.dtype)


def memory_cross_attention(h, m, w_q, w_kv, q_qk_g, k_qk_g, w_o):
    B, S, _ = h.shape
    M = m.shape[1]
    q = rms_norm((h @ w_q).reshape(B, S, CROSS_HEADS, CROSS_HEAD_DIM), q_qk_g)
    kv = m @ w_kv
    k = rms_norm(kv[..., :D_MODEL].reshape(B, M, CROSS_HEADS, CROSS_HEAD_DIM), k_qk_g)
    v = kv[..., D_MODEL:].reshape(B, M, CROSS_HEADS, CROSS_HEAD_DIM)
    s = jnp.einsum('bshd,bmhd->bhsm', q, k, preferred_element_type=jnp.float32) * (CROSS_HEAD_DIM ** -0.5)
    p = jax.nn.softmax(s, axis=-1).astype(v.dtype)
    o = jnp.einsum('bhsm,bmhd->bshd', p, v).reshape(B, S, D_MODEL)
    return o @ w_o


def hierarchical_moe(h, w_group, b_group, w_expert, b_expert, w_gate, w_up, w_down):
    def per_row(hr):
        S = hr.shape[0]
        g_logits = jnp.einsum('sd,dg->sg', hr, w_group, preferred_element_type=jnp.float32)
        g_prob = jax.nn.softmax(g_logits, axis=-1)
        g_sel = jnp.argmax(g_logits + b_group.astype(jnp.float32), axis=-1)
        g_w = jnp.take_along_axis(g_prob, g_sel[:, None], axis=-1)
        e_all = jnp.einsum('sd,de->se', hr, w_expert, preferred_element_type=jnp.float32)
        e_all = e_all.reshape(S, N_GROUPS, EXPERTS_PER_GROUP)
        e_logits = jnp.take_along_axis(e_all, g_sel[:, None, None], axis=1)[:, 0]
        e_bias = b_expert.astype(jnp.float32).reshape(N_GROUPS, EXPERTS_PER_GROUP)[g_sel]
        e_prob = jax.nn.softmax(e_logits, axis=-1)
        _, top_idx = lax.top_k(e_logits + e_bias, TOP_K)
        top_p = jnp.take_along_axis(e_prob, top_idx, axis=-1)
        top_p = top_p / jnp.sum(top_p, axis=-1, keepdims=True)
        local_w = jnp.sum(jax.nn.one_hot(top_idx, EXPERTS_PER_GROUP, dtype=jnp.float32) * top_p[..., None], axis=1)
        gate = (jax.nn.one_hot(g_sel, N_GROUPS, dtype=jnp.float32)[:, :, None]
                * local_w[:, None, :] * g_w[:, :, None]).reshape(S, N_EXPERTS)
        a = jnp.einsum('sd,edf->sef', hr, w_gate)
        u = jnp.einsum('sd,edf->sef', hr, w_up)
        act = jax.nn.silu(a) * u * gate[:, :, None].astype(hr.dtype)
        return jnp.einsum('sef,efd->sd', act, w_down)

    return lax.map(per_row, h)


def setup_inputs(seed: int = 0) -> dict:
    key = jax.random.key(seed)
    ks = jax.random.split(key, 32)

    def w(k, shape, fan_in):
        return jax.random.normal(k, (DEPTH,) + shape, jnp.float32) * (fan_in ** -0.5)

    def gain(k, n):
        return 1.0 + 0.02 * jax.random.normal(k, (DEPTH, n), jnp.float32)

    def bias(k, n):
        return 0.01 * jax.random.normal(k, (DEPTH, n), jnp.float32)

    x = jax.random.normal(ks[0], (BATCH, SEQ, D_MODEL), jnp.float32)
    mem = jax.random.normal(ks[1], (BATCH, MEM_TOKENS, D_MODEL), jnp.float32)
    offsets = jax.random.randint(ks[2], (BATCH, 1), 0, 1024, dtype=jnp.int32)
    positions = offsets + jnp.arange(SEQ, dtype=jnp.int32)[None, :]
    return {
        'x': x,
        'mem': mem,
        'positions': positions,
        'attn_norm_g': gain(ks[3], D_MODEL),
        'w_in': w(ks[4], (D_MODEL, IN_COLS), D_MODEL),
        'mla_q_norm_g': gain(ks[5], MLA_Q_RANK),
        'mla_w_uq': w(ks[6], (MLA_Q_RANK, MLA_HEADS * MLA_QK_DIM), MLA_Q_RANK),
        'mla_kv_norm_g': gain(ks[7], MLA_KV_RANK),
        'mla_w_ukv': w(ks[8], (MLA_KV_RANK, MLA_HEADS * (MLA_NOPE_DIM + MLA_V_DIM)), MLA_KV_RANK),
        'mla_q_qk_g': gain(ks[9], MLA_QK_DIM),
        'mla_k_qk_g': gain(ks[10], MLA_QK_DIM),
        'ret_gn_g': gain(ks[11], RET_WIDTH),
        'w_out': w(ks[12], (MIX_WIDTH, D_MODEL), MIX_WIDTH),
        'cross_norm_g': gain(ks[13], D_MODEL),
        'mem_norm_g': gain(ks[14], D_MODEL),
        'cross_w_q': w(ks[15], (D_MODEL, D_MODEL), D_MODEL),
        'cross_w_kv': w(ks[16], (D_MODEL, 2 * D_MODEL), D_MODEL),
        'cross_q_qk_g': gain(ks[17], CROSS_HEAD_DIM),
        'cross_k_qk_g': gain(ks[18], CROSS_HEAD_DIM),
        'cross_w_o': w(ks[19], (D_MODEL, D_MODEL), D_MODEL),
        'moe_norm_g': gain(ks[20], D_MODEL),
        'router_w_group': w(ks[21], (D_MODEL, N_GROUPS), D_MODEL),
        'router_b_group': bias(ks[22], N_GROUPS),
        'router_w_expert': w(ks[23], (D_MODEL, N_EXPERTS), D_MODEL),
        'router_b_expert': bias(ks[24], N_EXPERTS),
        'expert_w_gate': w(ks[25], (N_EXPERTS, D_MODEL, EXPERT_FF), D_MODEL),
        'expert_w_up': w(ks[26], (N_EXPERTS, D_MODEL, EXPERT_FF), D_MODEL),
        'expert_w_down': w(ks[27], (N_EXPERTS, EXPERT_FF, D_MODEL), EXPERT_FF),
    }


def reference(x, mem, positions, attn_norm_g, w_in, mla_q_norm_g, mla_w_uq, mla_kv_norm_g, mla_w_ukv,
              mla_q_qk_g, mla_k_qk_g, ret_gn_g, w_out, cross_norm_g, mem_norm_g, cross_w_q, cross_w_kv,
              cross_q_qk_g, cross_k_qk_g, cross_w_o, moe_norm_g, router_w_group, router_b_group,
              router_w_expert, router_b_expert, expert_w_gate, expert_w_up, expert_w_down):
    split_points = [int(s) for s in np.cumsum(IN_SPLITS)[:-1]]
    for l in range(DEPTH):
        h = rms_norm(x, attn_norm_g[l])
        proj = h @ w_in[l]
        c_q, c_kv, k_pe, rq, rk, rv, rg = jnp.split(proj, split_points, axis=-1)
        a_out = mla_group(c_q, c_kv, k_pe, positions, mla_q_norm_g[l], mla_w_uq[l],
                          mla_kv_norm_g[l], mla_w_ukv[l], mla_q_qk_g[l], mla_k_qk_g[l])
        r_out = retention_group(rq, rk, rv, rg, positions, ret_gn_g[l])
        x = x + jnp.concatenate([a_out, r_out], axis=-1) @ w_out[l]
        hc = rms_norm(x, cross_norm_g[l])
        m = rms_norm(mem, mem_norm_g[l])
        x = x + memory_cross_attention(hc, m, cross_w_q[l], cross_w_kv[l], cross_q_qk_g[l],
                                       cross_k_qk_g[l], cross_w_o[l])
        hm = rms_norm(x, moe_norm_g[l])
        x = x + hierarchical_moe(hm, router_w_group[l], router_b_group[l], router_w_expert[l],
                                 router_b_expert[l], expert_w_gate[l], expert_w_up[l], expert_w_down[l])
    return x
```

```python
import math
from contextlib import ExitStack

import numpy as np
import concourse.bass as bass
import concourse.mybir as mybir
from concourse.bass_utils import run_bass_kernel_spmd

F32 = mybir.dt.float32
BF16 = mybir.dt.bfloat16
I32 = mybir.dt.int32
AF = mybir.ActivationFunctionType
ALU = mybir.AluOpType
AX = mybir.AxisListType

D = 1024
EPS = 1e-6
NDMA = 24
NCST = 904


class Res:
    __slots__ = ("name", "w", "rd", "excl")

    def __init__(self, name, excl=False):
        self.name = name
        self.w = None
        self.rd = []
        self.excl = excl


class Sched:
    ENGS = ("pe", "act", "dve", "pool", "sp")

    def __init__(self, nc, es):
        self.nc = nc
        self.eng = {"pe": nc.tensor, "act": nc.scalar, "dve": nc.vector, "pool": nc.gpsimd, "sp": nc.sync}
        self.sem = {e: es.enter_context(nc.semaphore("c_" + e)) for e in self.ENGS}
        self.cnt = {e: 0 for e in self.ENGS}
        self.waited = {e: {} for e in self.ENGS}
        self.dsem = {q: [es.enter_context(nc.semaphore("d_%s%d" % (q, i))) for i in range(NDMA)] for q in ("sp", "pool")}
        self.dcnt = {q: [0] * NDMA for q in ("sp", "pool")}
        self.dnext = {q: 0 for q in ("sp", "pool")}
        self.semname = {}
        self.out_tokens = []
        self.n_inst = 0
        self.n_wait = 0

    def res(self, name):
        return Res(name)

    def _wait(self, eng, tok):
        sem, val, src = tok
        if src == "pe" and eng == "pe":
            return
        k = id(sem)
        if self.waited[eng].get(k, 0) >= val:
            return
        self.eng[eng].wait_ge(sem, val)
        self.n_wait += 1
        self.waited[eng][k] = val

    def _deps(self, eng, r, w):
        w = list(w) + [x for x in r if x.excl]
        for x in r:
            if x.w is not None:
                self._wait(eng, x.w)
        for x in w:
            if x.w is not None:
                self._wait(eng, x.w)
            for t in x.rd:
                self._wait(eng, t)

    def _commit(self, tok, r, w):
        w = list(w) + [x for x in r if x.excl]
        r = [x for x in r if not x.excl]
        for x in r:
            x.rd = [t for t in x.rd if t[0] is not tok[0]] + [tok]
        for x in w:
            x.w = tok
            x.rd = []

    def op(self, eng, fn, r=(), w=()):
        self._deps(eng, r, w)
        inst = fn(self.eng[eng])
        self.cnt[eng] += 1
        self.n_inst += 1
        inst.then_inc(self.sem[eng], 1)
        tok = (self.sem[eng], self.cnt[eng], eng)
        self.waited[eng][id(self.sem[eng])] = max(self.waited[eng].get(id(self.sem[eng]), 0), 0)
        self._commit(tok, r, w)
        return tok

    def pe(self, fns, r=(), w=()):
        self._deps("pe", r, w)
        inst = None
        for fn in fns:
            inst = fn(self.eng["pe"])
            self.n_inst += 1
        self.cnt["pe"] += 1
        inst.then_inc(self.sem["pe"], 1)
        tok = (self.sem["pe"], self.cnt["pe"], "pe")
        self._commit(tok, r, w)
        return tok

    def dma(self, q, fn, r=(), w=(), is_out=False):
        self._deps(q, r, w)
        i = self.dnext[q] % NDMA
        self.dnext[q] += 1
        sem = self.dsem[q][i]
        if self.dcnt[q][i] > 0:
            self._wait(q, (sem, 16 * self.dcnt[q][i], "dma"))
        inst = fn(self.eng[q])
        self.n_inst += 1
        self.dcnt[q][i] += 1
        inst.then_inc(sem, 16)
        tok = (sem, 16 * self.dcnt[q][i], "dma")
        self._commit(tok, r, w)
        if is_out:
            self.out_tokens.append(tok)
        return tok

    def barrier(self):
        toks = [(self.sem[e], self.cnt[e], e) for e in self.ENGS if self.cnt[e] > 0]
        for q in ("sp", "pool"):
            for i in range(NDMA):
                if self.dcnt[q][i] > 0:
                    toks.append((self.dsem[q][i], 16 * self.dcnt[q][i], "dma"))
        for e in self.ENGS:
            for t in toks:
                if t[2] == e and e != "pe":
                    pass
                self._wait_force(e, t)

    def _wait_force(self, eng, tok):
        sem, val, src = tok
        k = id(sem)
        if self.waited[eng].get(k, 0) >= val:
            return
        self.eng[eng].wait_ge(sem, val)
        self.n_wait += 1
        self.waited[eng][k] = val

    def finish(self):
        for t in self.out_tokens:
            self._wait_force("sp", t)
        self.barrier()


def build_program(S, last_phase=6, dbg=()):
    NT = S // 128
    NB = S // 512
    nc = bass.Bass("TRN2", target_bir_lowering=False)

    def din(name, shape, dt=F32):
        return nc.dram_tensor(name, list(shape), dt, kind="ExternalInput").ap()

    x_d = din("x", [S, D])
    mem_d = din("mem", [256, D])
    pos_d = din("pos", [128, NT], I32)
    cst_d = din("cst", [128, NCST])
    w_in_d = din("w_in", [128, 8, 2496])
    g_attn_d = din("g_attn", [128, 8])
    w_uq_d = din("w_uq", [128, 2, 768])
    g_qn_d = din("g_qn", [128, 2])
    w_ukv_d = din("w_ukv", [128, 1024])
    g_kvn_d = din("g_kvn", [128, 1])
    gq_d = din("gq", [1, 192])
    gk_d = din("gk", [1, 192])
    gn_d = din("gn", [1, 512])
    w_out_d = din("w_out", [128, 8, 1024])
    g_cross_d = din("g_cross", [128, 8])
    g_mem_d = din("g_mem", [128, 8])
    cw_q_d = din("cw_q", [128, 8, 1024])
    cw_kv_d = din("cw_kv", [128, 8, 2048])
    cqg_d = din("cqg", [1, 256])
    ckg_d = din("ckg", [1, 256])
    cw_o_d = din("cw_o", [128, 8, 1024])
    mg_d = din("mg", [1, 1024])
    w_rt_d = din("w_rt", [128, 8, 36])
    b_rt_d = din("b_rt", [1, 36])
    if last_phase >= 5:
        ew_g_d = din("ew_g", [32 * 128, 2048])
        ew_u_d = din("ew_u", [32 * 128, 2048])
        ew_d_d = din("ew_d", [32 * 128, 2048])
    out_d = nc.dram_tensor("out", [S, D], F32, kind="ExternalOutput").ap()

    dbg_out = {}

    with ExitStack() as es:
        sc = Sched(nc, es)

        def sb(name, shape, dt=F32, stack=es):
            return stack.enter_context(nc.sbuf_tensor("s_" + name, list(shape), dt))

        banks = [es.enter_context(nc.psum_tensor("bank%d" % i, [128, 512], F32)) for i in range(8)]
        bank_res = [Res("bank%d" % i, excl=True) for i in range(8)]
        bstate = {"i": 0}

        def nbank():
            i = bstate["i"] % 8
            bstate["i"] += 1
            return banks[i], bank_res[i]

        def dump(name, ap, res, shape, dt=F32):
            if name not in dbg:
                return
            t = nc.dram_tensor("dbg_" + name, list(shape), dt, kind="ExternalOutput").ap()
            dbg_out[name] = t
            sc.dma("sp", lambda e: e.dma_start(out=t, in_=ap), r=[res], w=[], is_out=True)

        def nbank(lo=0, hi=8):
            key = (lo, hi)
            i = lo + bstate.get(key, 0) % (hi - lo)
            bstate[key] = bstate.get(key, 0) + 1
            return banks[i], bank_res[i]

        cst = sb("cst", [128, NCST])
        r_cst = sc.res("cst")
        sc.dma("sp", lambda e: e.dma_start(out=cst[:], in_=cst_d[:, :]), w=[r_cst])
        INVF = cst[:, 0:192]
        OFFS = cst[:, 192:384]
        QDc = cst[:, 384:388]
        KDc = cst[:, 388:392]
        CDEC = cst[:, 392:904]

        ident_bf = sb("ident_bf", [128, 128], BF16)
        ident_f = sb("ident_f", [128, 128], F32)
        maskT = sb("maskT", [128, 128], BF16)
        mask4 = sb("mask4", [128, 4, 128], F32)
        tri = sb("tri", [128, 128], BF16)
        ones_bf = sb("ones_bf", [128, 128], BF16)
        r_const = sc.res("consts")

        def mk_mask(t_ap, pattern, cmp):
            sc.op("pool", lambda e: e.memset(t_ap, 1.0), w=[r_const])
            sc.op("pool", lambda e: e.affine_select(out=t_ap, in_=t_ap, pattern=pattern, compare_op=cmp, fill=0.0,
                                                    base=0, channel_multiplier=-1), r=[r_const], w=[r_const])

        mk_mask(ident_bf[:], [[1, 128]], ALU.is_equal)
        mk_mask(ident_f[:], [[1, 128]], ALU.is_equal)
        mk_mask(maskT[:], [[1, 128]], ALU.is_ge)
        mk_mask(mask4[:], [[0, 4], [1, 128]], ALU.is_ge)
        mk_mask(tri[:], [[1, 128]], ALU.is_gt)
        sc.op("pool", lambda e: e.memset(ones_bf[:], 1.0), w=[r_const])

        def bload(name, src, n, stack=es):
            t = sb(name, [128, n], F32, stack)
            sc.dma("sp", lambda e: e.dma_start(out=t[:], in_=src.partition_broadcast(128)), w=[r_const])
            return t

        def pload(name, src, n, stack=es):
            t = sb(name, [128, n], F32, stack)
            sc.dma("sp", lambda e: e.dma_start(out=t[:], in_=src[:, :]), w=[r_const])
            return t

        gq = bload("gq", gq_d, 192)
        gk = bload("gk", gk_d, 192)
        gn = bload("gn", gn_d, 512)

        pos_i = sb("pos_i", [128, NT], I32)
        pos_f = sb("pos_f", [128, NT])
        SCm = sb("SCm", [128, NT, 64])
        rstd1 = sb("rstd1", [128, NT])
        r_sc = sc.res("SC")
        r_rstd1 = sc.res("rstd1")
        stage = [None, None]
        r_stage = [sc.res("stage%d" % i) for i in range(2)]
        st = {"i": 0}
        scale_engs = ("dve", "pool")

        def load_scaled(dst_fn, src_fn, gain_fn, C, N, rdst):
            for c in range(C):
                for n0 in range(0, N, 1024):
                    n1 = min(N, n0 + 1024)
                    i = st["i"] % 2
                    st["i"] += 1
                    stg, rs = stage[i], r_stage[i]
                    sc.dma("sp", lambda e: e.dma_start(out=stg[:, 0:n1 - n0], in_=src_fn(c, n0, n1)), w=[rs])
                    sc.op(scale_engs[i], lambda e: e.tensor_scalar(out=dst_fn(c, n0, n1), in0=stg[:, 0:n1 - n0], scalar1=gain_fn(c), scalar2=None,
                                                                   op0=ALU.mult), r=[rs, r_const], w=[rdst])

        def load_cast(dst_fn, src_fn, C, rdst):
            for c in range(C):
                sc.dma("pool", lambda e: e.dma_start(out=dst_fn(c), in_=src_fn(c)), w=[rdst])

        mhalf = sb("mhalf", [128, 8])
        sc.op("pool", lambda e: e.memset(mhalf[:], -0.5), w=[r_const])

        def rstd_from_ss(ss_ap, n, out_ap, r_in, r_out, tmp_ap, r_tmp):
            k = ss_ap.shape[1]
            sc.op("dve", lambda e: e.tensor_scalar(out=tmp_ap, in0=ss_ap, scalar1=1.0 / n, scalar2=EPS, op0=ALU.mult, op1=ALU.add),
                  r=[r_in], w=[r_tmp])
            sc.op("pool", lambda e: e.tensor_tensor(out=out_ap, in0=tmp_ap, in1=mhalf[:, 0:k], op=ALU.pow), r=[r_tmp, r_const], w=[r_out])

        sc.dma("sp", lambda e: e.dma_start(out=pos_i[:], in_=pos_d[:, :]), w=[r_sc])
        sc.op("dve", lambda e: e.tensor_copy(out=pos_f[:], in_=pos_i[:]), r=[r_sc], w=[r_sc])

        rout_d = nc.dram_tensor("rout_s", [S, 512], BF16).ap()
        x1_d = nc.dram_tensor("x1_s", [S, D], F32).ap()
        hm_d = nc.dram_tensor("hm_s", [S, D], BF16).ap()
        TS = 256
        NTS = (2 * S) // TS + 32
        NPOS = NTS * TS
        xs_d = nc.dram_tensor("xs_s", [NPOS, D], BF16).ap()
        ys_d = nc.dram_tensor("ys_s", [NPOS, D], F32).ap()
        r_xs = sc.res("xs_d")

        def run_pipelined(gen_fn, n_items, depth):
            active = []
            nxt = 0
            while nxt < n_items or active:
                if nxt < n_items and len(active) < depth:
                    active.append(gen_fn(nxt))
                    nxt += 1
                for g in list(active):
                    try:
                        next(g)
                    except StopIteration:
                        active.remove(g)

        def mkres(n, k=2):
            return [sc.res("%s%d" % (n, i)) for i in range(k)]

        if last_phase >= 1:
            p1s = es.enter_context(ExitStack())
            import os as _os
            stop = int(_os.environ.get('P1_STOP', '99'))
            SCr = sb("SCr", [128, NT, 128], F32, p1s)
            w_r = sb("w_r", [128, 8, 2048], BF16, p1s)
            r_wr = sc.res("w_r")
            stage[:] = [sb("stage1_%d" % i, [128, 1024], F32, p1s) for i in range(2)]
            g_attn = pload("g_attn", g_attn_d, 8, p1s)
            load_scaled(lambda c, a, b_: w_r[:, c, a:b_], lambda c, a, b_: w_in_d[:, c, 448 + a:448 + b_], lambda c: g_attn[:, c:c + 1], 8, 2048, r_wr)

            NTsc = NT if stop >= 2 else 0
            tr_t = sb("tr_t", [128, 192], F32, p1s)
            tr_i = sb("tr_i", [128, 192], I32, p1s)
            tr_f = sb("tr_f", [128, 192], F32, p1s)
            r_tr = sc.res("tr")
            for t in range(NTsc):
                sc.op("dve", lambda e: e.scalar_tensor_tensor(out=tr_t[:], in0=INVF, scalar=pos_f[:, t:t + 1], in1=OFFS,
                                                              op0=ALU.mult, op1=ALU.add), r=[r_cst, r_sc], w=[r_tr])
                sc.op("dve", lambda e: e.tensor_copy(out=tr_i[:], in_=tr_t[:]), r=[r_tr], w=[r_tr])
                sc.op("dve", lambda e: e.tensor_copy(out=tr_f[:], in_=tr_i[:]), r=[r_tr], w=[r_tr])
                sc.op("dve", lambda e: e.tensor_tensor(out=tr_t[:], in0=tr_t[:], in1=tr_f[:], op=ALU.subtract), r=[r_tr], w=[r_tr])
                sc.op("dve", lambda e: e.scalar_tensor_tensor(out=tr_f[:], in0=tr_t[:], scalar=0.5, in1=tr_t[:],
                                                              op0=ALU.is_gt, op1=ALU.subtract), r=[r_tr], w=[r_tr])
                sc.op("dve", lambda e: e.scalar_tensor_tensor(out=tr_t[:], in0=tr_f[:], scalar=0.5, in1=tr_f[:],
                                                              op0=ALU.is_gt, op1=ALU.subtract), r=[r_tr], w=[r_tr])
                sc.op("act", lambda e: e.activation(out=SCr[:, t, :], in_=tr_t[:, 0:128], func=AF.Sin, scale=6.28318), r=[r_tr], w=[r_sc])
                sc.op("act", lambda e: e.activation(out=SCm[:, t, :], in_=tr_t[:, 128:192], func=AF.Sin, scale=6.28318), r=[r_tr], w=[r_sc])
            dump("SCr", SCr[:], r_sc, [128, NT, 128])
            dump("SCm", SCm[:], r_sc, [128, NT, 64])

            do_pc = last_phase >= 5
            if do_pc:
                ewb_d = [nc.dram_tensor("ewb%d" % k, [32 * 128, 2048], BF16).ap() for k in range(3)]
                ew_src = [ew_g_d, ew_u_d, ew_d_d]
                r_ewb = sc.res("ewb")
                pcs = [sb("pcs%d" % i, [128, 2048], BF16, p1s) for i in range(3)]
                r_pcs = mkres("pcs", 3)
                pc_state = {"i": 0}
                PC_PER_TILE = (96 + NT - 1) // NT

                def precast_step():
                    for _ in range(PC_PER_TILE):
                        i = pc_state["i"]
                        if i >= 96:
                            return
                        pc_state["i"] += 1
                        e_, k_ = i // 3, i % 3
                        bi = i % 3
                        rows = slice(e_ * 128, (e_ + 1) * 128)
                        sc.dma("pool", lambda e: e.dma_start(out=pcs[bi][:], in_=ew_src[k_][rows, :]), w=[r_pcs[bi]])
                        sc.dma("sp", lambda e: e.dma_start(out=ewb_d[k_][rows, :], in_=pcs[bi][:]), r=[r_pcs[bi]], w=[r_ewb])

            zt = sb("zt", [128, 2, 1024], BF16, p1s)
            r_zt = sc.res("zt")
            sc.op("pool", lambda e: e.memset(zt[:], 0.0), w=[r_zt])
            ROWS_PER = NPOS // NT

            def zero_step(t):
                if last_phase < 4:
                    return
                for r0_ in range(t * ROWS_PER, (t + 1) * ROWS_PER, 256):
                    sc.dma("sp", lambda e: e.dma_start(out=xs_d[r0_:r0_ + 256, :].rearrange("(p a) d -> p a d", a=2), in_=zt[:]), r=[r_zt], w=[r_xs])

            xt = [sb("xt%d" % i, [128, 1024], F32, p1s) for i in range(4)]
            xb = [sb("xb%d" % i, [128, 1024], BF16, p1s) for i in range(4)]
            xT = [sb("xT%d" % i, [128, 8, 128], BF16, p1s) for i in range(4)]
            junk = sb("junk", [128, 1024], BF16, p1s)
            rq_f = [sb("rq_f%d" % i, [128, 512], F32, p1s) for i in range(4)]
            rk_f = [sb("rk_f%d" % i, [128, 512], F32, p1s) for i in range(4)]
            v_b = [sb("v_b%d" % i, [128, 512], BF16, p1s) for i in range(4)]
            sg = [sb("sg%d" % i, [128, 512], F32, p1s) for i in range(6)]
            sm1 = [sb("sm1_%d" % i, [128, 32], F32, p1s) for i in range(4)]
            rp = [sb("rp%d" % i, [128, 2, 512], F32, p1s) for i in range(2)]
            qp_b = [sb("qp_b%d" % i, [128, 512], BF16, p1s) for i in range(4)]
            kp_b = [sb("kp_b%d" % i, [128, 512], BF16, p1s) for i in range(4)]
            qpT = [sb("qpT%d" % i, [128, 4, 128], BF16, p1s) for i in range(4)]
            kpT = [sb("kpT%d" % i, [128, 4, 128], BF16, p1s) for i in range(4)]
            PT = [sb("PT%d" % i, [128, 4, 128], BF16, p1s) for i in range(3)]
            Tst = sb("Tst", [128, 4, 128], F32, p1s)
            Tst_b = sb("Tst_b", [128, 4, 128], BF16, p1s)
            Ttmp = sb("Ttmp", [128, 4, 128], F32, p1s)
            o_f = [sb("o_f%d" % i, [128, 4, 128], F32, p1s) for i in range(4)]
            bnst = [sb("bnst%d" % i, [128, 4, 6], F32, p1s) for i in range(4)]
            bnag = [sb("bnag%d" % i, [128, 4, 2], F32, p1s) for i in range(4)]
            ro_b = [sb("ro_b%d" % i, [128, 512], BF16, p1s) for i in range(3)]

            r_xt, r_xb, r_xT = mkres("xt", 4), mkres("xb", 4), mkres("xT", 4)
            r_rq, r_rk, r_vb, r_sg, r_sm1 = mkres("rq", 4), mkres("rk", 4), mkres("vb", 4), mkres("sg", 6), mkres("sm1", 4)
            r_rp = mkres("rp")
            r_qpb, r_kpb, r_qpT, r_kpT, r_PT, r_of, r_bn, r_rob = (mkres("qpb", 4), mkres("kpb", 4), mkres("qpT", 4), mkres("kpT", 4), mkres("PT", 3),
                                                                   mkres("of", 4), mkres("bn", 4), mkres("rob", 3))
            r_junk = sc.res("junk")
            r_T = sc.res("Tst")
            r_Tb = sc.res("Tst_b")
            r_Tt = sc.res("Ttmp")
            sc.op("dve", lambda e: e.memset(Tst[:], 0.0), w=[r_T])
            sc.op("dve", lambda e: e.memset(Tst_b[:], 0.0), w=[r_Tb])

            def rope_ret(eng, src, r_src, dst_b, r_dst, t, decay, scr, r_scr):
                cosB = SCr[:, t, 64:128].unsqueeze(1).broadcast_to([128, 8, 64])
                sinB = SCr[:, t, 0:64].unsqueeze(1).broadcast_to([128, 4, 64])
                sv = src[:].rearrange("p (h two d) -> p h two d", h=4, two=2)
                Pv = scr[:, 0, :]
                Qv = scr[:, 1, :].rearrange("p (h two d) -> p h two d", h=4, two=2)
                P4 = scr[:, 0, :].rearrange("p (h two d) -> p h two d", h=4, two=2)
                sc.op(eng, lambda e: e.tensor_tensor(out=Pv.rearrange("p (g d) -> p g d", g=8), in0=src[:].rearrange("p (g d) -> p g d", g=8),
                                                     in1=cosB, op=ALU.mult), r=[r_src, r_sc], w=[r_scr])
                sc.op(eng, lambda e: e.tensor_tensor(out=Qv[:, :, 0, :], in0=sv[:, :, 1, :], in1=sinB, op=ALU.mult), r=[r_src, r_sc], w=[r_scr])
                sc.op(eng, lambda e: e.tensor_tensor(out=Qv[:, :, 1, :], in0=sv[:, :, 0, :], in1=sinB, op=ALU.mult), r=[r_src, r_sc], w=[r_scr])
                sc.op(eng, lambda e: e.tensor_tensor(out=P4[:, :, 0, :], in0=P4[:, :, 0, :], in1=Qv[:, :, 0, :], op=ALU.subtract), r=[r_scr], w=[r_scr])
                sc.op(eng, lambda e: e.tensor_tensor(out=P4[:, :, 1, :], in0=P4[:, :, 1, :], in1=Qv[:, :, 1, :], op=ALU.add), r=[r_scr], w=[r_scr])
                decB = decay.unsqueeze(2).broadcast_to([128, 4, 128])
                sc.op(eng, lambda e: e.tensor_tensor(out=dst_b[:].rearrange("p (h d) -> p h d", h=4), in0=Pv.rearrange("p (h d) -> p h d", h=4),
                                                     in1=decB, op=ALU.mult), r=[r_scr, r_cst], w=[r_dst])

            def p1_tile(t):
                b = t % 4
                b6 = t % 6
                b3 = t % 3
                ts_ = slice(t * 128, (t + 1) * 128)
                s1 = sm1[b]
                sc.dma("sp", lambda e: e.dma_start(out=xt[b][:], in_=x_d[ts_, :]), w=[r_xt[b]])
                if do_pc:
                    precast_step()
                zero_step(t)
                sc.op("act", lambda e: e.activation(out=junk[:], in_=xt[b][:], func=AF.Square, accum_out=s1[:, 0:1]),
                      r=[r_xt[b]], w=[r_junk, r_sm1[b]])
                rstd_from_ss(s1[:, 0:1], 1024.0, rstd1[:, t:t + 1], r_sm1[b], r_rstd1, s1[:, 1:2], r_sm1[b])
                yield
                sc.op("pool", lambda e: e.tensor_copy(out=xb[b][:], in_=xt[b][:]), r=[r_xt[b]], w=[r_xb[b]])
                bk, rb = nbank()
                pv = bk[:].bitcast(BF16).rearrange("p (c n) -> p c n", n=128)
                sc.pe([(lambda e, c=c: e.transpose(out=pv[:, c, :], in_=xb[b][:, c * 128:(c + 1) * 128], identity=ident_bf[:])) for c in range(8)],
                      r=[r_xb[b], r_const], w=[rb])
                sc.op("dve", lambda e: e.tensor_copy(out=xT[b][:], in_=pv), r=[rb], w=[r_xT[b]])
                rs1 = rstd1[:, t:t + 1]
                yield
                pb = []
                for k4 in range(4):
                    bk, rb = nbank()
                    sc.pe([(lambda e, c=c: e.matmul(bk[:], lhsT=xT[b][:, c, :], rhs=w_r[:, c, k4 * 512:(k4 + 1) * 512], start=(c == 0), stop=(c == 7)))
                           for c in range(8)], r=[r_xT[b], r_wr], w=[rb])
                    pb.append((bk, rb))
                sc.op("act", lambda e: e.activation(out=rq_f[b][:], in_=pb[0][0][:], func=AF.Copy, scale=rs1), r=[pb[0][1], r_rstd1], w=[r_rq[b]])
                sc.op("dve", lambda e: e.tensor_scalar(out=rk_f[b][:], in0=pb[1][0][:], scalar1=rs1, scalar2=None, op0=ALU.mult),
                      r=[pb[1][1], r_rstd1], w=[r_rk[b]])
                sc.op("act", lambda e: e.activation(out=v_b[b][:], in_=pb[2][0][:], func=AF.Copy, scale=rs1), r=[pb[2][1], r_rstd1], w=[r_vb[b]])
                sc.op("act", lambda e: e.activation(out=sg[b6][:], in_=pb[3][0][:], func=AF.Silu, scale=rs1), r=[pb[3][1], r_rstd1], w=[r_sg[b6]])

                yield
                rope_ret("dve", rq_f[b], r_rq[b], qp_b[b], r_qpb[b], t, QDc, rp[0], r_rp[0])
                rope_ret("pool", rk_f[b], r_rk[b], kp_b[b], r_kpb[b], t, KDc, rp[1], r_rp[1])
                yield
                bk, rb = nbank()
                pv = bk[:].bitcast(BF16).rearrange("p (c n) -> p c n", n=128)
                sc.pe([(lambda e, h=h: e.transpose(out=pv[:, h, :], in_=qp_b[b][:, h * 128:(h + 1) * 128], identity=ident_bf[:])) for h in range(4)]
                      + [(lambda e, h=h: e.transpose(out=pv[:, 4 + h, :], in_=kp_b[b][:, h * 128:(h + 1) * 128], identity=ident_bf[:])) for h in range(4)],
                      r=[r_qpb[b], r_kpb[b], r_const], w=[rb])
                sc.op("act", lambda e: e.copy(out=qpT[b][:], in_=pv[:, 0:4, :]), r=[rb], w=[r_qpT[b]])
                sc.op("dve", lambda e: e.tensor_copy(out=kpT[b][:], in_=pv[:, 4:8, :]), r=[rb], w=[r_kpT[b]])
                bk, rb = nbank()
                sv_ = bk[:].rearrange("p (h n) -> p h n", h=4)
                sc.pe([(lambda e, h=h: e.matmul(sv_[:, h, :], lhsT=kpT[b][:, h, :], rhs=qpT[b][:, h, :], start=True, stop=True)) for h in range(4)],
                      r=[r_kpT[b], r_qpT[b]], w=[rb])
                sc.op("dve", lambda e: e.tensor_tensor(out=PT[b3][:], in0=sv_, in1=mask4[:], op=ALU.mult), r=[rb, r_const], w=[r_PT[b3]])
                yield
                bk_o, rb_o = nbank()
                ov = bk_o[:].rearrange("p (h n) -> p h n", h=4)
                fns = []
                for h in range(4):
                    fns.append(lambda e, h=h: e.matmul(ov[:, h, :], lhsT=PT[b3][:, h, :], rhs=v_b[b][:, h * 128:(h + 1) * 128], start=True, stop=False))
                    fns.append(lambda e, h=h: e.matmul(ov[:, h, :], lhsT=qpT[b][:, h, :], rhs=Tst_b[:, h, :], start=False, stop=True))
                sc.pe(fns, r=[r_PT[b3], r_vb[b], r_qpT[b], r_Tb], w=[rb_o])
                bk_s, rb_s = nbank()
                stv = bk_s[:].rearrange("p (h n) -> p h n", h=4)
                sc.pe([(lambda e, h=h: e.matmul(stv[:, h, :], lhsT=kp_b[b][:, h * 128:(h + 1) * 128], rhs=v_b[b][:, h * 128:(h + 1) * 128],
                                                start=True, stop=True)) for h in range(4)], r=[r_kpb[b], r_vb[b]], w=[rb_s])
                sc.op("dve", lambda e: e.tensor_tensor(out=Ttmp[:], in0=stv, in1=Tst[:], op=ALU.add), r=[rb_s, r_T], w=[r_Tt])
                sc.op("pool", lambda e: e.tensor_tensor(out=Tst[:], in0=Ttmp[:], in1=CDEC.rearrange("p (h n) -> p h n", h=4), op=ALU.mult),
                      r=[r_Tt, r_cst], w=[r_T])
                sc.op("pool", lambda e: e.tensor_copy(out=Tst_b[:], in_=Tst[:]), r=[r_T], w=[r_Tb])
                yield
                sc.op("act", lambda e: e.copy(out=o_f[b][:], in_=ov), r=[rb_o], w=[r_of[b]])
                for h in range(4):
                    sc.op("dve", lambda e: e.bn_stats(out=bnst[b][:, h, :], in_=o_f[b][:, h, :]), r=[r_of[b]], w=[r_bn[b]])
                for h in range(4):
                    sc.op("dve", lambda e: e.bn_aggr(out=bnag[b][:, h, :], in_=bnst[b][:, h, :]), r=[r_bn[b]], w=[r_bn[b]])
                sc.op("dve", lambda e: e.tensor_scalar(out=s1[:, 20:24], in0=bnag[b][:, :, 1], scalar1=EPS, scalar2=None, op0=ALU.add),
                      r=[r_bn[b]], w=[r_sm1[b]])
                sc.op("pool", lambda e: e.tensor_tensor(out=s1[:, 24:28], in0=s1[:, 20:24], in1=mhalf[:, 0:4], op=ALU.pow), r=[r_sm1[b], r_const], w=[r_sm1[b]])
                for h in range(4):
                    sc.op("dve", lambda e: e.tensor_scalar(out=o_f[b][:, h, :], in0=o_f[b][:, h, :], scalar1=bnag[b][:, h, 0:1], scalar2=s1[:, 24 + h:25 + h],
                                                           op0=ALU.subtract, op1=ALU.mult), r=[r_of[b], r_bn[b], r_sm1[b]], w=[r_of[b]])
                ofl = o_f[b][:].rearrange("p h n -> p (h n)")
                sc.op("pool", lambda e: e.tensor_tensor(out=ofl, in0=ofl, in1=gn[:], op=ALU.mult), r=[r_of[b], r_const], w=[r_of[b]])
                sc.op("pool", lambda e: e.tensor_tensor(out=ro_b[b3][:], in0=ofl, in1=sg[b6][:], op=ALU.mult), r=[r_of[b], r_sg[b6]], w=[r_rob[b3]])
                r_routd = sc.res("rout_d%d" % t)
                sc.dma("sp", lambda e: e.dma_start(out=rout_d[ts_, :], in_=ro_b[b3][:]), r=[r_rob[b3]], w=[r_routd])
                if t == NT - 1:
                    dump("ro_last", ro_b[b3][:], r_rob[b3], [128, 512], BF16)
            run_pipelined(p1_tile, NT, 4)
            dump("rstd1", rstd1[:], r_rstd1, [128, NT])
            sc.barrier()
            p1s.close()

        if last_phase >= 2:
            p2s = es.enter_context(ExitStack())
            KTn = sb("KTn", [128, 4, S], BF16, p2s)
            KTr = sb("KTr", [128, 2, S], BF16, p2s)
            Vaug = sb("Vaug", [128, NT, 4, 129], BF16, p2s)
            r_KT = [sc.res("KT%d" % t) for t in range(NT)]
            r_V = sc.res("Vaug")
            sc.op("pool", lambda e: e.memset(Vaug[:], 1.0), w=[r_V])
            w_a = sb("w_a", [128, 8, 448], BF16, p2s)
            w_uq = sb("w_uq", [128, 2, 768], BF16, p2s)
            w_ukv = sb("w_ukv", [128, 1024], BF16, p2s)
            w_out = sb("w_out", [128, 8, 1024], BF16, p2s)
            r_w2 = sc.res("w2")
            g_attn2 = pload("g_attn2", g_attn_d, 8, p2s)
            g_qn = pload("g_qn", g_qn_d, 2, p2s)
            g_kvn = pload("g_kvn", g_kvn_d, 1, p2s)
            pw2 = es.enter_context(ExitStack())
            stage[:] = [sb("stage2_%d" % i, [128, 1024], F32, pw2) for i in range(2)]
            load_scaled(lambda c, a, b_: w_a[:, c, a:b_], lambda c, a, b_: w_in_d[:, c, a:b_], lambda c: g_attn2[:, c:c + 1], 8, 448, r_w2)
            load_scaled(lambda c, a, b_: w_uq[:, c, a:b_], lambda c, a, b_: w_uq_d[:, c, a:b_], lambda c: g_qn[:, c:c + 1], 2, 768, r_w2)
            load_scaled(lambda c, a, b_: w_ukv[:, a:b_], lambda c, a, b_: w_ukv_d[:, a:b_], lambda c: g_kvn[:, 0:1], 1, 1024, r_w2)
            load_cast(lambda c: w_out[:, c, :], lambda c: w_out_d[:, c, :], 8, r_w2)
            sc.barrier()
            pw2.close()

            xt = [sb("x2t%d" % i, [128, 1024], F32, p2s) for i in range(2)]
            xb = [sb("x2b%d" % i, [128, 1024], BF16, p2s) for i in range(2)]
            xT = [sb("x2T%d" % i, [128, 8, 128], BF16, p2s) for i in range(2)]
            junk = sb("junk2", [128, 768], F32, p2s)
            cq_f = [sb("cq_f%d" % i, [128, 256], F32, p2s) for i in range(3)]
            cq_b = [sb("cq_b%d" % i, [128, 256], BF16, p2s) for i in range(3)]
            cqT = [sb("cqT%d" % i, [128, 2, 128], BF16, p2s) for i in range(3)]
            ckv_f = [sb("ckv_f%d" % i, [128, 192], F32, p2s) for i in range(3)]
            ckv_b = [sb("ckv_b%d" % i, [128, 128], BF16, p2s) for i in range(3)]
            ckvT = [sb("ckvT%d" % i, [128, 128], BF16, p2s) for i in range(3)]
            kn_f = [sb("kn_f0", [128, 4, 128], F32, p2s)] * 3
            kn_b = [sb("kn_b%d" % i, [128, 4, 128], BF16, p2s) for i in range(3)]
            kpe = [sb("kpe%d" % i, [128, 3, 64], F32, p2s) for i in range(3)]
            kr = [sb("kr%d" % i, [128, 64], F32, p2s) for i in range(3)]
            krn_b = [sb("krn_b%d" % i, [128, 4, 64], BF16, p2s) for i in range(3)]
            q_f = [sb("q_f%d" % i, [128, 4, 192], F32, p2s) for i in range(2)]
            qn_b = [sb("qn_b%d" % i, [128, 4, 128], BF16, p2s) for i in range(3)]
            qr = [sb("qr0", [128, 4, 4, 64], F32, p2s)] * 3
            qrn_b = [sb("qrn_b%d" % i, [128, 4, 64], BF16, p2s) for i in range(3)]
            sm2 = [sb("sm2_%d" % i, [128, 48], F32, p2s) for i in range(3)]
            QTn = [sb("QTn%d" % i, [128, 4, 512], BF16, p2s) for i in range(2)]
            QTr = [sb("QTr%d" % i, [128, 2, 512], BF16, p2s) for i in range(2)]
            PTt = [sb("PTt%d" % i, [128, 512], BF16, p2s) for i in range(3)]
            a_b = [sb("a_b0", [128, 4, 512], BF16, p2s)] * 2
            rcp = [sb("rcp%d" % i, [128, 4], F32, p2s) for i in range(2)]
            r_b = [sb("r_b%d" % i, [128, 512], BF16, p2s) for i in range(2)]
            aoT = [sb("aoT%d" % i, [128, 8, 128], BF16, p2s) for i in range(2)]

            r_xt, r_xb, r_xT = mkres("x2t"), mkres("x2b"), mkres("x2T")
            r_junk = sc.res("junk2")
            r_cq, r_cqb, r_cqT, r_ckv, r_ckvb, r_ckvT = mkres("cq", 3), mkres("cqb", 3), mkres("cqT", 3), mkres("ckv", 3), mkres("ckvb", 3), mkres("ckvT", 3)
            r_kn, r_knb, r_kpe, r_kr, r_krn = mkres("kn", 1) * 3, mkres("knb", 3), mkres("kpe", 3), mkres("kr", 3), mkres("krn", 3)
            r_qf, r_qnb, r_qr, r_qrn, r_sm2 = mkres("qf"), mkres("qnb", 3), mkres("qr", 1) * 3, mkres("qrn", 3), mkres("sm2", 3)
            r_QT = mkres("QT")
            r_PTt = mkres("PTt", 3)
            r_ab, r_rcp, r_rb, r_aoT = mkres("ab", 1) * 2, mkres("rcp"), mkres("rb"), mkres("aoT")
            SCALE = 192.0 ** -0.5

            def prep_tile(t, qb):
                b = t % 3
                bx = t % 2
                ts_ = slice(t * 128, (t + 1) * 128)
                tl = slice((t % 4) * 128, (t % 4 + 1) * 128)
                s2 = sm2[b]
                rs1 = rstd1[:, t:t + 1]
                sc.dma("sp", lambda e: e.dma_start(out=xt[bx][:], in_=x_d[ts_, :]), w=[r_xt[bx]])
                sc.op("dve", lambda e: e.tensor_copy(out=xb[bx][:], in_=xt[bx][:]), r=[r_xt[bx]], w=[r_xb[bx]])
                yield
                bk, rb = nbank(4, 8)
                pv = bk[:].bitcast(BF16).rearrange("p (c n) -> p c n", n=128)
                sc.pe([(lambda e, c=c: e.transpose(out=pv[:, c, :], in_=xb[bx][:, c * 128:(c + 1) * 128], identity=ident_bf[:])) for c in range(8)],
                      r=[r_xb[bx], r_const], w=[rb])
                sc.op("dve", lambda e: e.tensor_copy(out=xT[bx][:], in_=pv), r=[rb], w=[r_xT[bx]])
                yield
                bk, rb = nbank(4, 8)
                sc.pe([(lambda e, c=c: e.matmul(bk[:, 0:448], lhsT=xT[bx][:, c, :], rhs=w_a[:, c, :], start=(c == 0), stop=(c == 7))) for c in range(8)],
                      r=[r_xT[bx], r_w2], w=[rb])
                sc.op("act", lambda e: e.activation(out=cq_f[b][:], in_=bk[:, 0:256], func=AF.Copy, scale=rs1), r=[rb, r_rstd1], w=[r_cq[b]])
                sc.op("act", lambda e: e.activation(out=ckv_f[b][:], in_=bk[:, 256:448], func=AF.Copy, scale=rs1), r=[rb, r_rstd1], w=[r_ckv[b]])
                yield
                sc.op("act", lambda e: e.activation(out=junk[:, 0:128], in_=ckv_f[b][:, 0:128], func=AF.Square, accum_out=s2[:, 2:3]), r=[r_ckv[b]], w=[r_junk, r_sm2[b]])
                rstd_from_ss(s2[:, 2:3], 128.0, s2[:, 3:4], r_sm2[b], r_sm2[b], s2[:, 4:5], r_sm2[b])
                sc.op("act", lambda e: e.activation(out=junk[:, 128:192], in_=ckv_f[b][:, 128:192], func=AF.Square, accum_out=s2[:, 5:6]), r=[r_ckv[b]], w=[r_junk, r_sm2[b]])
                sc.op("pool", lambda e: e.tensor_copy(out=ckv_b[b][:], in_=ckv_f[b][:, 0:128]), r=[r_ckv[b]], w=[r_ckvb[b]])
                bk, rb = nbank(4, 8)
                pv = bk[:].bitcast(BF16)
                sc.pe([lambda e: e.transpose(out=pv[:, 0:128], in_=ckv_b[b][:], identity=ident_bf[:])], r=[r_ckvb[b], r_const], w=[rb])
                sc.op("act", lambda e: e.copy(out=ckvT[b][:], in_=pv[:, 0:128]), r=[rb], w=[r_ckvT[b]])
                yield
                for hh in range(2):
                    bk, rb = nbank(4, 8)
                    sc.pe([lambda e: e.matmul(bk[:], lhsT=ckvT[b][:], rhs=w_ukv[:, hh * 512:(hh + 1) * 512], start=True, stop=True)],
                          r=[r_ckvT[b], r_w2], w=[rb])
                    kvv = bk[:].rearrange("p (h two d) -> p h two d", h=2, two=2)
                    sc.op("dve", lambda e: e.tensor_scalar(out=kn_f[b][:, 2 * hh:2 * hh + 2, :], in0=kvv[:, :, 0, :], scalar1=s2[:, 3:4], scalar2=None,
                                                           op0=ALU.mult), r=[rb, r_sm2[b]], w=[r_kn[b]])
                    sc.op("act", lambda e: e.activation(out=Vaug[:, t, 2 * hh:2 * hh + 2, 0:128], in_=kvv[:, :, 1, :], func=AF.Copy, scale=s2[:, 3:4]),
                          r=[rb, r_sm2[b]], w=[r_V])
                yield
                jk = junk[:, 0:512].rearrange("p (h d) -> p h d", h=4)
                sc.op("dve", lambda e: e.tensor_tensor(out=jk, in0=kn_f[b][:], in1=kn_f[b][:], op=ALU.mult), r=[r_kn[b]], w=[r_junk])
                sc.op("dve", lambda e: e.tensor_reduce(out=s2[:, 8:12], in_=jk, axis=AX.X, op=ALU.add), r=[r_junk], w=[r_sm2[b]])
                sc.op("dve", lambda e: e.tensor_scalar(out=s2[:, 8:12], in0=s2[:, 8:12], scalar1=s2[:, 5:6], scalar2=None, op0=ALU.add),
                      r=[r_sm2[b]], w=[r_sm2[b]])
                rstd_from_ss(s2[:, 8:12], 192.0, s2[:, 12:16], r_sm2[b], r_sm2[b], s2[:, 16:20], r_sm2[b])
                for h in range(4):
                    sc.op("dve", lambda e: e.scalar_tensor_tensor(out=kn_b[b][:, h, :], in0=kn_f[b][:, h, :], scalar=s2[:, 12 + h:13 + h], in1=gk[:, 0:128],
                                                                  op0=ALU.mult, op1=ALU.mult), r=[r_kn[b], r_sm2[b], r_const], w=[r_knb[b]])
                yield
                kp = kpe[b]
                sc.op("pool", lambda e: e.tensor_tensor(out=kp[:, 0, :], in0=ckv_f[b][:, 128:192], in1=gk[:, 128:192], op=ALU.mult),
                      r=[r_ckv[b], r_const], w=[r_kpe[b]])
                cosM2 = SCm[:, t, 32:64].unsqueeze(1).broadcast_to([128, 2, 32])
                sinM2 = SCm[:, t, 0:32].unsqueeze(1).broadcast_to([128, 2, 32])
                k0 = kp[:, 0, :].rearrange("p (two d) -> p two d", two=2)
                kA = kp[:, 1, :].rearrange("p (two d) -> p two d", two=2)
                kB = kp[:, 2, :].rearrange("p (two d) -> p two d", two=2)
                sc.op("pool", lambda e: e.tensor_tensor(out=kA, in0=k0, in1=cosM2, op=ALU.mult), r=[r_kpe[b], r_sc], w=[r_kpe[b]])
                sc.op("pool", lambda e: e.tensor_tensor(out=kB, in0=k0, in1=sinM2, op=ALU.mult), r=[r_kpe[b], r_sc], w=[r_kpe[b]])
                sc.op("pool", lambda e: e.tensor_tensor(out=kr[b][:, 0:32], in0=kp[:, 1, 0:32], in1=kp[:, 2, 32:64], op=ALU.subtract),
                      r=[r_kpe[b]], w=[r_kr[b]])
                sc.op("pool", lambda e: e.tensor_tensor(out=kr[b][:, 32:64], in0=kp[:, 1, 32:64], in1=kp[:, 2, 0:32], op=ALU.add),
                      r=[r_kpe[b]], w=[r_kr[b]])
                for h in range(4):
                    sc.op("dve", lambda e: e.tensor_scalar(out=krn_b[b][:, h, :], in0=kr[b][:], scalar1=s2[:, 12 + h:13 + h], scalar2=None, op0=ALU.mult),
                          r=[r_kr[b], r_sm2[b]], w=[r_krn[b]])
                bk, rb = nbank(4, 8)
                pv = bk[:].bitcast(BF16).rearrange("p (c n) -> p c n", n=128)
                sc.pe([(lambda e, h=h: e.transpose(out=pv[:, h, :], in_=kn_b[b][:, h, :], identity=ident_bf[:])) for h in range(4)]
                      + [(lambda e, pr=pr: e.transpose(out=pv[:, 4 + pr, :], in_=krn_b[b][:, 2 * pr:2 * pr + 2, :].rearrange("p a d -> p (a d)"),
                                                       identity=ident_bf[:])) for pr in range(2)],
                      r=[r_knb[b], r_krn[b], r_const], w=[rb])
                sc.op("act", lambda e: e.copy(out=KTn[:, :, ts_], in_=pv[:, 0:4, :]), r=[rb], w=[r_KT[t]])
                sc.op("dve", lambda e: e.tensor_copy(out=KTr[:, :, ts_], in_=pv[:, 4:6, :]), r=[rb], w=[r_KT[t]])
                yield
                sc.op("act", lambda e: e.activation(out=junk[:, 0:256], in_=cq_f[b][:], func=AF.Square, accum_out=s2[:, 20:21]), r=[r_cq[b]], w=[r_junk, r_sm2[b]])
                rstd_from_ss(s2[:, 20:21], 256.0, s2[:, 21:22], r_sm2[b], r_sm2[b], s2[:, 22:23], r_sm2[b])
                sc.op("pool", lambda e: e.tensor_copy(out=cq_b[b][:], in_=cq_f[b][:]), r=[r_cq[b]], w=[r_cqb[b]])
                bk, rb = nbank(4, 8)
                pv = bk[:].bitcast(BF16).rearrange("p (c n) -> p c n", n=128)
                sc.pe([(lambda e, c=c: e.transpose(out=pv[:, c, :], in_=cq_b[b][:, c * 128:(c + 1) * 128], identity=ident_bf[:])) for c in range(2)],
                      r=[r_cqb[b], r_const], w=[rb])
                sc.op("act", lambda e: e.copy(out=cqT[b][:], in_=pv[:, 0:2, :]), r=[rb], w=[r_cqT[b]])
                yield
                for hh in range(2):
                    bk, rb = nbank(4, 8)
                    sc.pe([(lambda e, c=c: e.matmul(bk[:, 0:384], lhsT=cqT[b][:, c, :], rhs=w_uq[:, c, hh * 384:(hh + 1) * 384], start=(c == 0), stop=(c == 1)))
                           for c in range(2)], r=[r_cqT[b], r_w2], w=[rb])
                    sc.op("act", lambda e: e.activation(out=q_f[bx][:, 2 * hh:2 * hh + 2, :], in_=bk[:, 0:384].rearrange("p (h d) -> p h d", h=2),
                                                        func=AF.Copy, scale=s2[:, 21:22]), r=[rb, r_sm2[b]], w=[r_qf[bx]])
                yield
                jq = junk[:, 0:768].rearrange("p (h d) -> p h d", h=4)
                sc.op("dve", lambda e: e.tensor_tensor(out=jq, in0=q_f[bx][:], in1=q_f[bx][:], op=ALU.mult), r=[r_qf[bx]], w=[r_junk])
                sc.op("dve", lambda e: e.tensor_reduce(out=s2[:, 24:28], in_=jq, axis=AX.X, op=ALU.add), r=[r_junk], w=[r_sm2[b]])
                rstd_from_ss(s2[:, 24:28], 192.0, s2[:, 28:32], r_sm2[b], r_sm2[b], s2[:, 32:36], r_sm2[b])
                for h in range(4):
                    sc.op("dve", lambda e: e.scalar_tensor_tensor(out=qn_b[b][:, h, :], in0=q_f[bx][:, h, 0:128], scalar=s2[:, 28 + h:29 + h], in1=gq[:, 0:128],
                                                                  op0=ALU.mult, op1=ALU.mult), r=[r_qf[bx], r_sm2[b], r_const], w=[r_qnb[b]])
                yield
                qq = qr[b]
                gqB = gq[:, 128:192].unsqueeze(1).broadcast_to([128, 4, 64])
                sc.op("pool", lambda e: e.tensor_tensor(out=qq[:, 0, :, :], in0=q_f[bx][:, :, 128:192], in1=gqB, op=ALU.mult), r=[r_qf[bx], r_const], w=[r_qr[b]])
                cosM8 = SCm[:, t, 32:64].unsqueeze(1).broadcast_to([128, 8, 32])
                sinM8 = SCm[:, t, 0:32].unsqueeze(1).broadcast_to([128, 8, 32])
                q0 = qq[:, 0, :, :].rearrange("p h (two d) -> p (h two) d", two=2)
                qA = qq[:, 1, :, :].rearrange("p h (two d) -> p (h two) d", two=2)
                qB = qq[:, 2, :, :].rearrange("p h (two d) -> p (h two) d", two=2)
                sc.op("pool", lambda e: e.tensor_tensor(out=qA, in0=q0, in1=cosM8, op=ALU.mult), r=[r_qr[b], r_sc], w=[r_qr[b]])
                sc.op("pool", lambda e: e.tensor_tensor(out=qB, in0=q0, in1=sinM8, op=ALU.mult), r=[r_qr[b], r_sc], w=[r_qr[b]])
                sc.op("pool", lambda e: e.tensor_tensor(out=qq[:, 3, :, 0:32], in0=qq[:, 1, :, 0:32], in1=qq[:, 2, :, 32:64], op=ALU.subtract),
                      r=[r_qr[b]], w=[r_qr[b]])
                sc.op("pool", lambda e: e.tensor_tensor(out=qq[:, 3, :, 32:64], in0=qq[:, 1, :, 32:64], in1=qq[:, 2, :, 0:32], op=ALU.add),
                      r=[r_qr[b]], w=[r_qr[b]])
                yield
                rsB = s2[:, 28:32].unsqueeze(2).broadcast_to([128, 4, 64])
                sc.op("dve", lambda e: e.tensor_tensor(out=qrn_b[b][:], in0=qq[:, 3, :, :], in1=rsB, op=ALU.mult), r=[r_qr[b], r_sm2[b]], w=[r_qrn[b]])
                bk, rb = nbank(4, 8)
                pv = bk[:].bitcast(BF16).rearrange("p (c n) -> p c n", n=128)
                sc.pe([(lambda e, h=h: e.transpose(out=pv[:, h, :], in_=qn_b[b][:, h, :], identity=ident_bf[:])) for h in range(4)]
                      + [(lambda e, pr=pr: e.transpose(out=pv[:, 4 + pr, :], in_=qrn_b[b][:, 2 * pr:2 * pr + 2, :].rearrange("p a d -> p (a d)"),
                                                       identity=ident_bf[:])) for pr in range(2)],
                      r=[r_qnb[b], r_qrn[b], r_const], w=[rb])
                sc.op("act", lambda e: e.copy(out=QTn[qb][:, :, tl], in_=pv[:, 0:4, :]), r=[rb], w=[r_QT[qb]])
                sc.op("dve", lambda e: e.tensor_copy(out=QTr[qb][:, :, tl], in_=pv[:, 4:6, :]), r=[rb], w=[r_QT[qb]])

            def attention_block(i, qb):
                ab = a_b[i % 2]
                its = [(h, j) for h in range(4) for j in range(4 * i + 4)]

                def qk(h, j):
                    pair, hp = h // 2, h % 2
                    psl = slice(hp * 64, (hp + 1) * 64)
                    r0 = max(0, j - 4 * i)
                    n = 512 - r0 * 128
                    ks = slice(j * 128, (j + 1) * 128)
                    bk, rb = nbank(4, 8)
                    sc.pe([lambda e: e.matmul(bk[:, 0:n], lhsT=KTn[:, h, ks], rhs=QTn[qb][:, h, r0 * 128:512], start=True, stop=False),
                           lambda e: e.matmul(bk[:, 0:n], lhsT=KTr[psl, pair, ks], rhs=QTr[qb][psl, pair, r0 * 128:512], start=False, stop=True)],
                          r=[r_KT[j], r_QT[qb]], w=[rb])
                    pi = (h * 64 + j) % 3
                    pt, rpt = PTt[pi], r_PTt[pi]
                    sc.op("act", lambda e: e.activation(out=pt[:, 0:n], in_=bk[:, 0:n], func=AF.Exp, scale=SCALE), r=[rb], w=[rpt])
                    if j >= 4 * i:
                        sc.op("pool", lambda e: e.tensor_tensor(out=pt[:, 0:128], in0=pt[:, 0:128], in1=maskT[:], op=ALU.mult), r=[rpt, r_const], w=[rpt])
                    return pt, rpt, r0

                def pv_(h, j, pt, rpt, r0):
                    fns = []
                    for s in range(r0, 4):
                        fns.append(lambda e, s=s: e.matmul(banks[s][:, 0:129], lhsT=pt[:, (s - r0) * 128:(s - r0 + 1) * 128], rhs=Vaug[:, j, h, :],
                                                           start=(j == 0), stop=(j == 4 * i + s)))
                    sc.pe(fns, r=[rpt, r_V, r_KT[j]], w=[bank_res[s] for s in range(r0, 4)])
                    if j >= 4 * i:
                        s = j - 4 * i
                        sc.op("dve", lambda e: e.reciprocal(out=rcp[i % 2][:, s:s + 1], in_=banks[s][:, 128:129]), r=[bank_res[s]], w=[r_rcp[i % 2]])
                        sc.op("dve", lambda e: e.tensor_scalar(out=ab[:, s, h * 128:(h + 1) * 128], in0=banks[s][:, 0:128], scalar1=rcp[i % 2][:, s:s + 1],
                                                               scalar2=None, op0=ALU.mult), r=[bank_res[s], r_rcp[i % 2]], w=[r_ab[i % 2]])

                pend = []
                for k_, (h, j) in enumerate(its):
                    pend.append((h, j) + qk(h, j))
                    if len(pend) > 1:
                        pv_(*pend.pop(0))
                    if k_ % 8 == 7:
                        yield
                while pend:
                    pv_(*pend.pop(0))

            def out_tile(t):
                i, s = t // 4, t % 4
                ab = a_b[i % 2]
                if True:
                    b = t % 2
                    ts_ = slice(t * 128, (t + 1) * 128)
                    sc.dma("sp", lambda e: e.dma_start(out=r_b[b][:], in_=rout_d[ts_, :]), w=[r_rb[b]])
                    sc.dma("sp", lambda e: e.dma_start(out=x1t[b][:], in_=x_d[ts_, :]), w=[r_x1t[b]])
                    yield
                    bk, rb = nbank(4, 8)
                    pv = bk[:].bitcast(BF16).rearrange("p (c n) -> p c n", n=128)
                    sc.pe([(lambda e, c=c: e.transpose(out=pv[:, c, :], in_=ab[:, s, c * 128:(c + 1) * 128], identity=ident_bf[:])) for c in range(4)]
                          + [(lambda e, c=c: e.transpose(out=pv[:, 4 + c, :], in_=r_b[b][:, c * 128:(c + 1) * 128], identity=ident_bf[:])) for c in range(4)],
                          r=[r_ab[i % 2], r_rb[b], r_const], w=[rb])
                    sc.op("dve", lambda e: e.tensor_copy(out=aoT[b][:], in_=pv), r=[rb], w=[r_aoT[b]])
                    yield
                    for hh in range(2):
                        bk, rb = nbank(4, 8)
                        sc.pe([(lambda e, c=c: e.matmul(bk[:], lhsT=aoT[b][:, c, :], rhs=w_out[:, c, hh * 512:(hh + 1) * 512], start=(c == 0), stop=(c == 7)))
                               for c in range(8)], r=[r_aoT[b], r_w2], w=[rb])
                        sc.op("dve", lambda e: e.tensor_tensor(out=x1t[b][:, hh * 512:(hh + 1) * 512], in0=bk[:], in1=x1t[b][:, hh * 512:(hh + 1) * 512], op=ALU.add),
                              r=[rb], w=[r_x1t[b]])
                    r_x1d = sc.res("x1d")
                    sc.dma("sp", lambda e: e.dma_start(out=x1_d[ts_, :], in_=x1t[b][:]), r=[r_x1t[b]], w=[r_x1d])

            def drive(gens):
                gens = list(gens)
                while gens:
                    for g in list(gens):
                        try:
                            next(g)
                        except StopIteration:
                            gens.remove(g)

            x1t = xt
            r_x1t = r_xt
            run_pipelined(lambda t: prep_tile(t, 0), 4, 3)
            for i in range(NB):
                qb = i % 2

                def side(i=i):
                    if i + 1 < NB:
                        act_ = []
                        nx = 4 * (i + 1)
                        while nx < 4 * (i + 2) or act_:
                            if nx < 4 * (i + 2) and len(act_) < 3:
                                act_.append(prep_tile(nx, (i + 1) % 2))
                                nx += 1
                            for g in list(act_):
                                try:
                                    next(g)
                                except StopIteration:
                                    act_.remove(g)
                            yield

                drive([attention_block(i, qb), side()])
                run_pipelined(lambda k_, i=i: out_tile(4 * i + k_), 4, 2)
            if "x1" in dbg:
                sc.barrier()
                t_ = nc.dram_tensor("dbg_x1", [S, D], F32, kind="ExternalOutput").ap()
                dbg_out["x1"] = t_
                sc.dma("sp", lambda e: e.dma_start(out=t_, in_=x1_d), is_out=True)
            dump("KTn", KTn[:], r_KT[NT - 1], [128, 4, S], BF16)
            dump("KTr", KTr[:], r_KT[NT - 1], [128, 2, S], BF16)
            dump("Vaug", Vaug[:], r_V, [128, NT, 4, 129], BF16)
            sc.barrier()
            p2s.close()

        if last_phase >= 3:
            p36 = es.enter_context(ExitStack())
            lg_all = sb("lg_all", [128, NT, 36], F32, p36)
            r_lg = sc.res("lg_all")
            pos12 = sb("pos12", [128, 2, NT], I32, p36)
            w12 = sb("w12", [128, 2, NT], F32, p36)
            widx = sb("widx", [128, NTS], I32, p36)
            r_route = sc.res("route")
            b_rt = bload("b_rt", b_rt_d, 36, p36)

            p3s = es.enter_context(ExitStack())
            cw_q = sb("cw_q", [128, 8, 1024], BF16, p3s)
            cw_o = sb("cw_o", [128, 8, 1024], BF16, p3s)
            KcT = sb("KcT", [128, 4, 2, 256], BF16, p3s)
            Vc = sb("Vc", [128, 2, 4, 257], BF16, p3s)
            mg = bload("mg", mg_d, 1024, p3s)
            cqg = bload("cqg", cqg_d, 256, p3s)
            ckg = bload("ckg", ckg_d, 256, p3s)
            stage[:] = [sb("stage3_%d" % i, [128, 1024], F32, p3s) for i in range(2)]
            g_cross = pload("g_cross", g_cross_d, 8, p3s)
            g_mem = pload("g_mem", g_mem_d, 8, p3s)
            w_rt = sb("w_rt", [128, 8, 36], F32, p3s)
            r_w3 = sc.res("w3")
            sc.dma("sp", lambda e: e.dma_start(out=w_rt[:], in_=w_rt_d[:, :, :]), w=[r_w3])
            load_scaled(lambda c, a, b_: cw_q[:, c, a:b_], lambda c, a, b_: cw_q_d[:, c, a:b_], lambda c: g_cross[:, c:c + 1], 8, 1024, r_w3)
            load_cast(lambda c: cw_o[:, c, :], lambda c: cw_o_d[:, c, :], 8, r_w3)
            r_kvc = sc.res("kvc")
            sc.op("pool", lambda e: e.memset(Vc[:], 1.0), w=[r_kvc])

            pm = es.enter_context(ExitStack())
            cw_kv = sb("cw_kv", [128, 8, 2048], BF16, pm)
            r_cwkv = sc.res("cw_kv")
            load_scaled(lambda c, a, b_: cw_kv[:, c, a:b_], lambda c, a, b_: cw_kv_d[:, c, a:b_], lambda c: g_mem[:, c:c + 1], 8, 2048, r_cwkv)
            m_f = sb("m_f", [128, 1024], F32, pm)
            m_b = sb("m_b", [128, 1024], BF16, pm)
            m_T = sb("m_T", [128, 8, 128], BF16, pm)
            kc_f = sb("kc_f", [128, 4, 256], F32, pm)
            kc_sq = sb("kc_sq", [128, 4, 256], F32, pm)
            kc_b = sb("kc_b", [128, 4, 256], BF16, pm)
            sm = sb("sm0", [128, 16], F32, pm)
            r_m = sc.res("m")
            r_sm = sc.res("sm0")
            for mt in range(2):
                sc.dma("sp", lambda e: e.dma_start(out=m_f[:], in_=mem_d[mt * 128:(mt + 1) * 128, :]), w=[r_m])
                sc.op("act", lambda e: e.activation(out=m_b[:], in_=m_f[:], func=AF.Square, accum_out=sm[:, 0:1]), r=[r_m], w=[r_m, r_sm])
                rstd_from_ss(sm[:, 0:1], 1024.0, sm[:, 1:2], r_sm, r_sm, sm[:, 2:3], r_sm)
                sc.op("pool", lambda e: e.tensor_copy(out=m_b[:], in_=m_f[:]), r=[r_m], w=[r_m])
                bk, rb = nbank()
                pv = bk[:].bitcast(BF16).rearrange("p (c n) -> p c n", n=128)
                sc.pe([(lambda e, c=c: e.transpose(out=pv[:, c, :], in_=m_b[:, c * 128:(c + 1) * 128], identity=ident_bf[:])) for c in range(8)],
                      r=[r_m, r_const], w=[rb])
                sc.op("dve", lambda e: e.tensor_copy(out=m_T[:], in_=pv), r=[rb], w=[r_m])
                for nchunk in range(4):
                    bk, rb = nbank()
                    sc.pe([(lambda e, c=c: e.matmul(bk[:], lhsT=m_T[:, c, :], rhs=cw_kv[:, c, nchunk * 512:(nchunk + 1) * 512],
                                                    start=(c == 0), stop=(c == 7))) for c in range(8)], r=[r_m, r_cwkv], w=[rb])
                    if nchunk < 2:
                        sc.op("act", lambda e: e.activation(out=kc_f[:, 2 * nchunk:2 * nchunk + 2, :], in_=bk[:].rearrange("p (h d) -> p h d", h=2),
                                                            func=AF.Copy, scale=sm[:, 1:2]), r=[rb, r_sm], w=[r_m])
                    else:
                        hh = 2 * (nchunk - 2)
                        sc.op("act", lambda e: e.activation(out=Vc[:, mt, hh:hh + 2, 0:256], in_=bk[:].rearrange("p (h d) -> p h d", h=2),
                                                            func=AF.Copy, scale=sm[:, 1:2]), r=[rb, r_sm], w=[r_kvc])
                sc.op("dve", lambda e: e.tensor_tensor(out=kc_sq[:], in0=kc_f[:], in1=kc_f[:], op=ALU.mult), r=[r_m], w=[r_m])
                sc.op("dve", lambda e: e.tensor_reduce(out=sm[:, 4:8], in_=kc_sq[:], axis=AX.X, op=ALU.add), r=[r_m], w=[r_sm])
                rstd_from_ss(sm[:, 4:8], 256.0, sm[:, 8:12], r_sm, r_sm, sm[:, 12:16], r_sm)
                for h in range(4):
                    sc.op("dve", lambda e: e.scalar_tensor_tensor(out=kc_b[:, h, :], in0=kc_f[:, h, :], scalar=sm[:, 8 + h:9 + h], in1=ckg[:],
                                                                  op0=ALU.mult, op1=ALU.mult), r=[r_m, r_sm, r_const], w=[r_m])
                bk, rb = nbank()
                pv = bk[:].bitcast(BF16).rearrange("p (h c n) -> p h c n", h=4, c=2)
                sc.pe([(lambda e, h=h, c=c: e.transpose(out=pv[:, h, c, :], in_=kc_b[:, h, c * 128:(c + 1) * 128], identity=ident_bf[:]))
                       for h in range(4) for c in range(2)], r=[r_m, r_const], w=[rb])
                sc.op("dve", lambda e: e.tensor_copy(out=KcT[:, :, :, mt * 128:(mt + 1) * 128], in_=pv), r=[rb], w=[r_kvc])
            dump("KcT", KcT[:], r_kvc, [128, 4, 2, 256], BF16)
            dump("Vc", Vc[:], r_kvc, [128, 2, 4, 257], BF16)
            sc.barrier()
            pm.close()

            x1t = [sb("x3t%d" % i, [128, 1024], F32, p3s) for i in range(3)]
            xb = [sb("x3b%d" % i, [128, 1024], BF16, p3s) for i in range(2)]
            hcT = [sb("hcT%d" % i, [128, 8, 128], BF16, p3s) for i in range(2)]
            junk = sb("junk3", [128, 1024], F32, p3s)
            qc_f = sb("qc_f", [128, 4, 256], F32, p3s)
            qc_b = sb("qc_b", [128, 4, 256], BF16, p3s)
            qcT = [sb("qcT%d" % i, [128, 4, 2, 128], BF16, p3s) for i in range(2)]
            PTc = [sb("PTc%d" % i, [128, 8, 128], BF16, p3s) for i in range(2)]
            oc_b = sb("oc_b", [128, 4, 256], BF16, p3s)
            ocT = [sb("ocT%d" % i, [128, 8, 128], BF16, p3s) for i in range(2)]
            hm_f = [sb("hm_f%d" % i, [128, 1024], F32, p3s) for i in range(2)]
            hm_b = [sb("hm_b%d" % i, [128, 1024], BF16, p3s) for i in range(2)]
            hmT_f = sb("hmT_f", [128, 8, 128], F32, p3s)
            sm3 = [sb("sm3_%d" % i, [128, 32], F32, p3s) for i in range(3)]
            r_x1t, r_xb, r_hcT = mkres("x3t", 3), mkres("x3b"), mkres("hcT")
            r_junk = sc.res("junk3")
            r_qcf, r_qcb = sc.res("qcf"), sc.res("qcb")
            r_qcT, r_PTc, r_ocT, r_hmf, r_hmb, r_sm3 = mkres("qcT"), mkres("PTc"), mkres("ocT"), mkres("hmf"), mkres("hmb"), mkres("sm3", 3)
            r_ocb = sc.res("ocb")
            r_hmT = sc.res("hmT")
            r_x2d = [sc.res("x2d%d" % t) for t in range(NT)]
            r_hmd = [sc.res("hmd%d" % t) for t in range(NT)]
            CSCALE = 256.0 ** -0.5

            def p3_tile(t):
                b = t % 2
                b3 = t % 3
                ts_ = slice(t * 128, (t + 1) * 128)
                s3 = sm3[b3]
                sc.dma("sp", lambda e: e.dma_start(out=x1t[b3][:], in_=x1_d[ts_, :]), w=[r_x1t[b3]])
                sc.op("act", lambda e: e.activation(out=junk[:], in_=x1t[b3][:], func=AF.Square, accum_out=s3[:, 0:1]), r=[r_x1t[b3]], w=[r_junk, r_sm3[b3]])
                rstd_from_ss(s3[:, 0:1], 1024.0, s3[:, 1:2], r_sm3[b3], r_sm3[b3], s3[:, 2:3], r_sm3[b3])
                sc.op("act", lambda e: e.copy(out=xb[b][:], in_=x1t[b3][:]), r=[r_x1t[b3]], w=[r_xb[b]])
                yield
                bk, rb = nbank()
                pv = bk[:].bitcast(BF16).rearrange("p (c n) -> p c n", n=128)
                sc.pe([(lambda e, c=c: e.transpose(out=pv[:, c, :], in_=xb[b][:, c * 128:(c + 1) * 128], identity=ident_bf[:])) for c in range(8)],
                      r=[r_xb[b], r_const], w=[rb])
                sc.op("dve", lambda e: e.tensor_copy(out=hcT[b][:], in_=pv), r=[rb], w=[r_hcT[b]])
                yield
                for hh in range(2):
                    bk, rb = nbank()
                    sc.pe([(lambda e, c=c: e.matmul(bk[:], lhsT=hcT[b][:, c, :], rhs=cw_q[:, c, hh * 512:(hh + 1) * 512], start=(c == 0), stop=(c == 7)))
                           for c in range(8)], r=[r_hcT[b], r_w3], w=[rb])
                    sc.op("act", lambda e: e.activation(out=qc_f[:, 2 * hh:2 * hh + 2, :], in_=bk[:].rearrange("p (h d) -> p h d", h=2), func=AF.Copy,
                                                        scale=s3[:, 1:2]), r=[rb, r_sm3[b3]], w=[r_qcf])
                yield
                jq = junk[:].rearrange("p (h d) -> p h d", h=4)
                sc.op("dve", lambda e: e.tensor_tensor(out=jq, in0=qc_f[:], in1=qc_f[:], op=ALU.mult), r=[r_qcf], w=[r_junk])
                sc.op("dve", lambda e: e.tensor_reduce(out=s3[:, 4:8], in_=jq, axis=AX.X, op=ALU.add), r=[r_junk], w=[r_sm3[b3]])
                rstd_from_ss(s3[:, 4:8], 256.0, s3[:, 8:12], r_sm3[b3], r_sm3[b3], s3[:, 12:16], r_sm3[b3])
                for h in range(4):
                    sc.op("dve", lambda e: e.scalar_tensor_tensor(out=qc_b[:, h, :], in0=qc_f[:, h, :], scalar=s3[:, 8 + h:9 + h], in1=cqg[:],
                                                                  op0=ALU.mult, op1=ALU.mult), r=[r_qcf, r_sm3[b3], r_const], w=[r_qcb])
                yield
                bk, rb = nbank()
                pv = bk[:].bitcast(BF16).rearrange("p (h c n) -> p h c n", h=4, c=2)
                sc.pe([(lambda e, h=h, c=c: e.transpose(out=pv[:, h, c, :], in_=qc_b[:, h, c * 128:(c + 1) * 128], identity=ident_bf[:]))
                       for h in range(4) for c in range(2)], r=[r_qcb, r_const], w=[rb])
                sc.op("act", lambda e: e.copy(out=qcT[b][:], in_=pv), r=[rb], w=[r_qcT[b]])
                yield
                for hp in range(2):
                    bk, rb = nbank()
                    sv_ = bk[:].rearrange("p (a n) -> p a n", a=4)
                    fns = []
                    for hl in range(2):
                        h = 2 * hp + hl
                        for mc in range(2):
                            for dc in range(2):
                                fns.append(lambda e, h=h, mc=mc, dc=dc, hl=hl: e.matmul(sv_[:, hl * 2 + mc, :], lhsT=KcT[:, h, dc, mc * 128:(mc + 1) * 128],
                                                                                        rhs=qcT[b][:, h, dc, :], start=(dc == 0), stop=(dc == 1)))
                    sc.pe(fns, r=[r_kvc, r_qcT[b]], w=[rb])
                    sc.op("act", lambda e: e.activation(out=PTc[b][:, 4 * hp:4 * hp + 4, :], in_=sv_, func=AF.Exp, scale=CSCALE), r=[rb], w=[r_PTc[b]])
                yield
                for h in range(4):
                    bk, rb = nbank()
                    sc.pe([(lambda e, mc=mc: e.matmul(bk[:, 0:257], lhsT=PTc[b][:, 2 * h + mc, :], rhs=Vc[:, mc, h, :], start=(mc == 0), stop=(mc == 1)))
                           for mc in range(2)], r=[r_PTc[b], r_kvc], w=[rb])
                    sc.op("dve", lambda e: e.reciprocal(out=s3[:, 16 + h:17 + h], in_=bk[:, 256:257]), r=[rb], w=[r_sm3[b3]])
                    sc.op("dve", lambda e: e.tensor_scalar(out=oc_b[:, h, :], in0=bk[:, 0:256], scalar1=s3[:, 16 + h:17 + h], scalar2=None, op0=ALU.mult),
                          r=[rb, r_sm3[b3]], w=[r_ocb])
                yield
                bk, rb = nbank()
                pv = bk[:].bitcast(BF16).rearrange("p (c n) -> p c n", n=128)
                ocf = oc_b[:].rearrange("p h d -> p (h d)")
                sc.pe([(lambda e, c=c: e.transpose(out=pv[:, c, :], in_=ocf[:, c * 128:(c + 1) * 128], identity=ident_bf[:])) for c in range(8)],
                      r=[r_ocb, r_const], w=[rb])
                sc.op("act", lambda e: e.copy(out=ocT[b][:], in_=pv), r=[rb], w=[r_ocT[b]])
                yield
                for hh in range(2):
                    bk, rb = nbank()
                    sc.pe([(lambda e, c=c: e.matmul(bk[:], lhsT=ocT[b][:, c, :], rhs=cw_o[:, c, hh * 512:(hh + 1) * 512], start=(c == 0), stop=(c == 7)))
                           for c in range(8)], r=[r_ocT[b], r_w3], w=[rb])
                    sc.op("dve", lambda e: e.tensor_tensor(out=x1t[b3][:, hh * 512:(hh + 1) * 512], in0=bk[:], in1=x1t[b3][:, hh * 512:(hh + 1) * 512], op=ALU.add),
                          r=[rb], w=[r_x1t[b3]])
                sc.dma("sp", lambda e: e.dma_start(out=out_d[ts_, :], in_=x1t[b3][:]), r=[r_x1t[b3]], w=[r_x2d[t]])
                yield
                sc.op("act", lambda e: e.activation(out=junk[:], in_=x1t[b3][:], func=AF.Square, accum_out=s3[:, 20:21]), r=[r_x1t[b3]], w=[r_junk, r_sm3[b3]])
                rstd_from_ss(s3[:, 20:21], 1024.0, s3[:, 21:22], r_sm3[b3], r_sm3[b3], s3[:, 22:23], r_sm3[b3])
                sc.op("dve", lambda e: e.scalar_tensor_tensor(out=hm_f[b][:], in0=x1t[b3][:], scalar=s3[:, 21:22], in1=mg[:], op0=ALU.mult, op1=ALU.mult),
                      r=[r_x1t[b3], r_sm3[b3], r_const], w=[r_hmf[b]])
                sc.op("pool", lambda e: e.tensor_copy(out=hm_b[b][:], in_=hm_f[b][:]), r=[r_hmf[b]], w=[r_hmb[b]])
                sc.dma("sp", lambda e: e.dma_start(out=hm_d[ts_, :], in_=hm_b[b][:]), r=[r_hmb[b]], w=[r_hmd[t]])
                yield
                bkA, rbA = nbank()
                bkB, rbB = nbank()
                sc.pe([(lambda e, c=c: e.transpose(out=(bkA if c < 4 else bkB)[:, (c % 4) * 128:(c % 4 + 1) * 128], in_=hm_f[b][:, c * 128:(c + 1) * 128],
                                                   identity=ident_f[:])) for c in range(8)], r=[r_hmf[b], r_const], w=[rbA, rbB])
                sc.op("dve", lambda e: e.tensor_copy(out=hmT_f[:, 0:4, :], in_=bkA[:].rearrange("p (c n) -> p c n", c=4)), r=[rbA], w=[r_hmT])
                sc.op("act", lambda e: e.copy(out=hmT_f[:, 4:8, :], in_=bkB[:].rearrange("p (c n) -> p c n", c=4)), r=[rbB], w=[r_hmT])
                yield
                bk, rb = nbank()
                sc.pe([(lambda e, c=c: e.matmul(bk[:, 0:36], lhsT=hmT_f[:, c, :], rhs=w_rt[:, c, :], start=(c == 0), stop=(c == 7))) for c in range(8)],
                      r=[r_hmT, r_w3], w=[rb])
                sc.op("act", lambda e: e.copy(out=lg_all[:, t, :], in_=bk[:, 0:36]), r=[rb], w=[r_lg])
            run_pipelined(p3_tile, NT, 3)
            dump("logits", lg_all[:], r_lg, [128, NT, 36])
            if "x2" in dbg:
                sc.barrier()
                t_ = nc.dram_tensor("dbg_x2", [S, D], F32, kind="ExternalOutput").ap()
                dbg_out["x2"] = t_
                sc.dma("sp", lambda e: e.dma_start(out=t_, in_=out_d), is_out=True)
                t2_ = nc.dram_tensor("dbg_hm", [S, D], BF16, kind="ExternalOutput").ap()
                dbg_out["hm"] = t2_
                sc.dma("sp", lambda e: e.dma_start(out=t2_, in_=hm_d), is_out=True)
            sc.barrier()
            p3s.close()

        if last_phase >= 4:
            p4s = es.enter_context(ExitStack())
            reg_npos = nc.gpsimd.to_reg(NPOS - 1)
            reg_ew = nc.gpsimd.to_reg(32 * 128 - 1)
            r4 = sc.res("r4")

            def T4(name, shape, dt=F32):
                return sb("r4_" + name, shape, dt, p4s)

            def V(fn, r=(), w=(), eng="dve"):
                sc.op(eng, fn, r=[r4, r_lg, r_const] + list(r), w=[r4] + list(w))

            L = lg_all
            GL = L[:, :, 0:4]
            EL = L[:, :, 4:36].rearrange("p t (g j) -> p t g j", g=4)
            gb, goh, ge = T4("gb", [128, NT, 4]), T4("goh", [128, NT, 4]), T4("ge", [128, NT, 4])
            gmax, gm, gsum, gnum, gw = (T4(n, [128, NT]) for n in ("gmax", "gm", "gsum", "gnum", "gw"))
            t48 = T4("t48", [128, NT, 4, 8])
            esel, bsel, eb, oh1, eb2, oh2, ex, t8 = (T4(n, [128, NT, 8]) for n in ("esel", "bsel", "eb", "oh1", "eb2", "oh2", "ex", "t8"))
            m1, m2, em, a1, a2, den, ff = (T4(n, [128, NT]) for n in ("m1", "m2", "em", "a1", "a2", "den", "ff"))
            OH1, OH2 = T4("OH1", [128, NT, 32]), T4("OH2", [128, NT, 32])
            C_bf = T4("C_bf", [128, NT, 32], BF16)
            TT, PP, cA, cB, base, tmp32 = (T4(n, [128, NT, 32]) for n in ("TT", "PP", "cA", "cB", "base", "tmp32"))
            npad_i = T4("npad_i", [128, 32], I32)
            npad, eA, eB, off = (T4(n, [128, 32]) for n in ("npad", "eA", "eB", "off"))
            posf = T4("posf", [128, 2, NT])
            tpos_i = T4("tpos_i", [128, NTS], I32)
            tpos_f, eid_f = T4("tpos_f", [128, NTS]), T4("eid_f", [128, NTS])
            cmp_ = T4("cmp", [128, NTS, 32])
            pidx_i = T4("pidx_i", [128, 1], I32)
            pidx_f = T4("pidx_f", [128, 1])

            def bc(ap2, n):
                return ap2.unsqueeze(2).broadcast_to([128, NT, n])

            bg = b_rt[:, 0:4].unsqueeze(1).broadcast_to([128, NT, 4])
            be = b_rt[:, 4:36].rearrange("p (g j) -> p g j", g=4).unsqueeze(1).broadcast_to([128, NT, 4, 8])
            V(lambda e: e.tensor_tensor(out=gb[:], in0=GL, in1=bg, op=ALU.add))
            V(lambda e: e.tensor_reduce(out=gmax[:], in_=gb[:], axis=AX.X, op=ALU.max))
            V(lambda e: e.tensor_tensor(out=goh[:], in0=gb[:], in1=bc(gmax[:], 4), op=ALU.is_equal))
            V(lambda e: e.tensor_reduce(out=gm[:], in_=GL, axis=AX.X, op=ALU.max))
            V(lambda e: e.tensor_tensor(out=ge[:], in0=GL, in1=bc(gm[:], 4), op=ALU.subtract))
            V(lambda e: e.activation(out=ge[:].rearrange("p t g -> p (t g)"), in_=ge[:].rearrange("p t g -> p (t g)"), func=AF.Exp), eng="act")
            V(lambda e: e.tensor_reduce(out=gsum[:], in_=ge[:], axis=AX.X, op=ALU.add))
            V(lambda e: e.tensor_tensor(out=gb[:], in0=goh[:], in1=ge[:], op=ALU.mult))
            V(lambda e: e.tensor_reduce(out=gnum[:], in_=gb[:], axis=AX.X, op=ALU.add))
            V(lambda e: e.reciprocal(out=gsum[:], in_=gsum[:]))
            V(lambda e: e.tensor_tensor(out=gw[:], in0=gnum[:], in1=gsum[:], op=ALU.mult))
            goh_b = goh[:].unsqueeze(3).broadcast_to([128, NT, 4, 8])
            V(lambda e: e.tensor_tensor(out=t48[:], in0=EL, in1=goh_b, op=ALU.mult))
            V(lambda e: e.tensor_reduce(out=esel[:], in_=t48[:].rearrange("p t g j -> p t j g"), axis=AX.X, op=ALU.add))
            V(lambda e: e.tensor_tensor(out=t48[:], in0=be, in1=goh_b, op=ALU.mult))
            V(lambda e: e.tensor_reduce(out=bsel[:], in_=t48[:].rearrange("p t g j -> p t j g"), axis=AX.X, op=ALU.add))
            V(lambda e: e.tensor_tensor(out=eb[:], in0=esel[:], in1=bsel[:], op=ALU.add))
            V(lambda e: e.tensor_reduce(out=m1[:], in_=eb[:], axis=AX.X, op=ALU.max))
            V(lambda e: e.tensor_tensor(out=oh1[:], in0=eb[:], in1=bc(m1[:], 8), op=ALU.is_equal))
            V(lambda e: e.scalar_tensor_tensor(out=eb2[:].rearrange("p t j -> p (t j)"), in0=oh1[:].rearrange("p t j -> p (t j)"), scalar=-1e30,
                                               in1=eb[:].rearrange("p t j -> p (t j)"), op0=ALU.mult, op1=ALU.add))
            V(lambda e: e.tensor_reduce(out=m2[:], in_=eb2[:], axis=AX.X, op=ALU.max))
            V(lambda e: e.tensor_tensor(out=oh2[:], in0=eb2[:], in1=bc(m2[:], 8), op=ALU.is_equal))
            V(lambda e: e.tensor_reduce(out=em[:], in_=esel[:], axis=AX.X, op=ALU.max))
            V(lambda e: e.tensor_tensor(out=ex[:], in0=esel[:], in1=bc(em[:], 8), op=ALU.subtract))
            V(lambda e: e.activation(out=ex[:].rearrange("p t j -> p (t j)"), in_=ex[:].rearrange("p t j -> p (t j)"), func=AF.Exp), eng="act")
            V(lambda e: e.tensor_tensor(out=t8[:], in0=oh1[:], in1=ex[:], op=ALU.mult))
            V(lambda e: e.tensor_reduce(out=a1[:], in_=t8[:], axis=AX.X, op=ALU.add))
            V(lambda e: e.tensor_tensor(out=t8[:], in0=oh2[:], in1=ex[:], op=ALU.mult))
            V(lambda e: e.tensor_reduce(out=a2[:], in_=t8[:], axis=AX.X, op=ALU.add))
            V(lambda e: e.tensor_tensor(out=den[:], in0=a1[:], in1=a2[:], op=ALU.add))
            V(lambda e: e.reciprocal(out=den[:], in_=den[:]))
            V(lambda e: e.tensor_tensor(out=ff[:], in0=den[:], in1=gw[:], op=ALU.mult))
            V(lambda e: e.tensor_tensor(out=w12[:, 0, :], in0=a1[:], in1=ff[:], op=ALU.mult), w=[r_route])
            V(lambda e: e.tensor_tensor(out=w12[:, 1, :], in0=a2[:], in1=ff[:], op=ALU.mult), w=[r_route])
            V(lambda e: e.tensor_tensor(out=OH1[:].rearrange("p t (g j) -> p t g j", g=4), in0=goh_b,
                                        in1=oh1[:].unsqueeze(2).broadcast_to([128, NT, 4, 8]), op=ALU.mult))
            V(lambda e: e.tensor_tensor(out=OH2[:].rearrange("p t (g j) -> p t g j", g=4), in0=goh_b,
                                        in1=oh2[:].unsqueeze(2).broadcast_to([128, NT, 4, 8]), op=ALU.mult))
            V(lambda e: e.tensor_tensor(out=C_bf[:], in0=OH1[:], in1=OH2[:], op=ALU.add))
            W = NT * 32
            Cf = C_bf[:].rearrange("p t e -> p (t e)")
            TTf = TT[:].rearrange("p t e -> p (t e)")
            PPf = PP[:].rearrange("p t e -> p (t e)")
            for c0 in range(0, W, 512):
                c1 = min(W, c0 + 512)
                bk, rb = nbank()
                sc.pe([lambda e: e.matmul(bk[:, 0:c1 - c0], lhsT=ones_bf[:], rhs=Cf[:, c0:c1], start=True, stop=True)], r=[r4, r_const], w=[rb])
                sc.op("act", lambda e: e.copy(out=TTf[:, c0:c1], in_=bk[:, 0:c1 - c0]), r=[rb], w=[r4])
                bk, rb = nbank()
                sc.pe([lambda e: e.matmul(bk[:, 0:c1 - c0], lhsT=tri[:], rhs=Cf[:, c0:c1], start=True, stop=True)], r=[r4, r_const], w=[rb])
                sc.op("act", lambda e: e.copy(out=PPf[:, c0:c1], in_=bk[:, 0:c1 - c0]), r=[rb], w=[r4])
            V(lambda e: e.tensor_copy(out=cA[:], in_=TT[:]))
            cur, nxt = cA, cB
            s_ = 1
            while s_ < NT:
                V(lambda e: e.tensor_tensor(out=nxt[:, s_:NT, :], in0=cur[:, s_:NT, :], in1=cur[:, 0:NT - s_, :], op=ALU.add))
                V(lambda e: e.tensor_copy(out=nxt[:, 0:s_, :], in_=cur[:, 0:s_, :]))
                cur, nxt = nxt, cur
                s_ *= 2
            incl = cur
            V(lambda e: e.tensor_scalar(out=npad[:], in0=incl[:, NT - 1, :], scalar1=float(TS - 1), scalar2=None, op0=ALU.add))
            V(lambda e: e.tensor_copy(out=npad_i[:], in_=npad[:]))
            V(lambda e: e.tensor_scalar(out=npad_i[:], in0=npad_i[:], scalar1=8, scalar2=8, op0=ALU.arith_shift_right, op1=ALU.logical_shift_left))
            V(lambda e: e.tensor_copy(out=npad[:], in_=npad_i[:]))
            V(lambda e: e.tensor_copy(out=eA[:], in_=npad[:]))
            cur2, nxt2 = eA, eB
            s_ = 1
            while s_ < 32:
                V(lambda e: e.tensor_tensor(out=nxt2[:, s_:32], in0=cur2[:, s_:32], in1=cur2[:, 0:32 - s_], op=ALU.add))
                V(lambda e: e.tensor_copy(out=nxt2[:, 0:s_], in_=cur2[:, 0:s_]))
                cur2, nxt2 = nxt2, cur2
                s_ *= 2
            endI = cur2
            V(lambda e: e.tensor_tensor(out=off[:], in0=endI[:], in1=npad[:], op=ALU.subtract))
            V(lambda e: e.tensor_tensor(out=base[:], in0=incl[:], in1=TT[:], op=ALU.subtract))
            V(lambda e: e.tensor_tensor(out=base[:], in0=base[:], in1=PP[:], op=ALU.add))
            V(lambda e: e.tensor_tensor(out=base[:], in0=base[:], in1=off[:].unsqueeze(1).broadcast_to([128, NT, 32]), op=ALU.add))
            V(lambda e: e.tensor_tensor(out=tmp32[:], in0=OH1[:], in1=base[:], op=ALU.mult))
            V(lambda e: e.tensor_reduce(out=posf[:, 0, :], in_=tmp32[:], axis=AX.X, op=ALU.add))
            V(lambda e: e.tensor_tensor(out=tmp32[:], in0=OH2[:], in1=base[:], op=ALU.mult))
            V(lambda e: e.tensor_reduce(out=posf[:, 1, :], in_=tmp32[:], axis=AX.X, op=ALU.add))
            V(lambda e: e.tensor_copy(out=pos12[:], in_=posf[:]), w=[r_route])
            V(lambda e: e.iota(tpos_i[:], pattern=[[TS, NTS]], base=0, channel_multiplier=0), eng="pool")
            V(lambda e: e.iota(pidx_i[:], pattern=[[0, 1]], base=0, channel_multiplier=1), eng="pool")
            V(lambda e: e.tensor_copy(out=tpos_f[:], in_=tpos_i[:]))
            V(lambda e: e.tensor_copy(out=pidx_f[:], in_=pidx_i[:]))
            V(lambda e: e.tensor_tensor(out=cmp_[:], in0=endI[:].unsqueeze(1).broadcast_to([128, NTS, 32]),
                                        in1=tpos_f[:].unsqueeze(2).broadcast_to([128, NTS, 32]), op=ALU.is_le))
            V(lambda e: e.tensor_reduce(out=eid_f[:], in_=cmp_[:], axis=AX.X, op=ALU.add))
            V(lambda e: e.tensor_scalar(out=eid_f[:], in0=eid_f[:], scalar1=31.0, scalar2=128.0, op0=ALU.min, op1=ALU.mult))
            V(lambda e: e.tensor_scalar(out=eid_f[:], in0=eid_f[:], scalar1=pidx_f[:, 0:1], scalar2=None, op0=ALU.add))
            V(lambda e: e.tensor_copy(out=widx[:], in_=eid_f[:]), w=[r_route])
            dump("pos12", pos12[:], r_route, [128, 2, NT], I32)
            dump("w12", w12[:], r_route, [128, 2, NT])
            dump("widx", widx[:], r_route, [128, NTS], I32)

            hsb = [sb("hsb%d" % i, [128, 1024], BF16, p4s) for i in range(2)]
            r_hsb = mkres("hsb")
            for t in range(NT):
                b = t % 2
                ts_ = slice(t * 128, (t + 1) * 128)
                sc.dma("sp", lambda e: e.dma_start(out=hsb[b][:], in_=hm_d[ts_, :]), r=[r_hmd[t]], w=[r_hsb[b]])
                for k in range(2):
                    sc.dma("pool", lambda e: e.indirect_dma_start(out=xs_d[:, :], out_offset=bass.IndirectOffsetOnAxis(ap=pos12[:, k, t:t + 1], axis=0),
                                                                  in_=hsb[b][:], in_offset=None, bounds_check=reg_npos, oob_is_err=False),
                           r=[r_hsb[b], r_route], w=[r_xs])
            sc.barrier()
            p4s.close()

        if last_phase >= 5:
            p5s = es.enter_context(ExitStack())
            NWB = 3
            wg = [sb("wg%d" % i, [128, 2048], BF16, p5s) for i in range(NWB)]
            wu = [sb("wu%d" % i, [128, 2048], BF16, p5s) for i in range(NWB)]
            wd = [sb("wd%d" % i, [128, 2048], BF16, p5s) for i in range(NWB)]
            r_wg, r_wu, r_wd = mkres("wg", NWB), mkres("wu", NWB), mkres("wd", NWB)
            xrow = [sb("xrow%d" % i, [128, 1024], BF16, p5s) for i in range(4)]
            r_xrow = mkres("xrow", 4)
            XsT = [sb("XsT%d" % i, [128, 8, TS], BF16, p5s) for i in range(3)]
            r_XsT = mkres("XsT", 3)
            sa = [sb("sa%d" % i, [128, TS], F32, p5s) for i in range(2)]
            r_sa = mkres("sa")
            actT = [sb("actT%d" % i, [128, 2, TS], BF16, p5s) for i in range(3)]
            r_actT = mkres("actT", 3)
            yt = [sb("yt%d" % i, [128, 1024], F32, p5s) for i in range(3)]
            r_yt = mkres("yt", 3)
            r_ys = sc.res("ys_d")
            cnt5 = {"x": 0, "y": 0}
            def p5_tile(tp):
                wb = tp % NWB
                b = tp % 3
                for (dst, rdst, src) in ((wg[wb], r_wg[wb], ewb_d[0]), (wu[wb], r_wu[wb], ewb_d[1]), (wd[wb], r_wd[wb], ewb_d[2])):
                    sc.dma("pool", lambda e: e.indirect_dma_start(out=dst[:], out_offset=None, in_=src[:, :],
                                                                  in_offset=bass.IndirectOffsetOnAxis(ap=widx[:, tp:tp + 1], axis=0),
                                                                  bounds_check=reg_ew, oob_is_err=False), r=[r_route, r_ewb], w=[rdst])
                for s in range(TS // 128):
                    xi = (2 * tp + s) % 4
                    r0_ = tp * TS + s * 128
                    sc.dma("sp", lambda e: e.dma_start(out=xrow[xi][:], in_=xs_d[r0_:r0_ + 128, :]), r=[r_xs], w=[r_xrow[xi]])
                yield
                for s in range(TS // 128):
                    xi = (2 * tp + s) % 4
                    bk, rb = nbank()
                    pv = bk[:].bitcast(BF16).rearrange("p (c n) -> p c n", n=128)
                    sc.pe([(lambda e, c=c: e.transpose(out=pv[:, c, :], in_=xrow[xi][:, c * 128:(c + 1) * 128], identity=ident_bf[:])) for c in range(8)],
                          r=[r_xrow[xi], r_const], w=[rb])
                    sc.op("dve" if s % 2 == 0 else "act",
                          (lambda e: e.tensor_copy(out=XsT[b][:, :, s * 128:(s + 1) * 128], in_=pv)) if s % 2 == 0 else
                          (lambda e: e.copy(out=XsT[b][:, :, s * 128:(s + 1) * 128], in_=pv)), r=[rb], w=[r_XsT[b]])
                yield
                wgv = wg[wb][:].rearrange("p (c f) -> p c f", c=8)
                wuv = wu[wb][:].rearrange("p (c f) -> p c f", c=8)
                wdv = wd[wb][:].rearrange("p (c d) -> p c d", c=2)
                for fc in range(2):
                    bk, rb = nbank()
                    fns = [(lambda e, c=c: e.matmul(bk[:, 0:TS], lhsT=wgv[:, c, fc * 128:(fc + 1) * 128], rhs=XsT[b][:, c, :], start=(c == 0), stop=(c == 7)))
                           for c in range(8)]
                    fns += [(lambda e, c=c: e.matmul(bk[:, TS:2 * TS], lhsT=wuv[:, c, fc * 128:(fc + 1) * 128], rhs=XsT[b][:, c, :], start=(c == 0), stop=(c == 7)))
                            for c in range(8)]
                    sc.pe(fns, r=[r_wg[wb], r_wu[wb], r_XsT[b]], w=[rb])
                    sc.op("act", lambda e: e.activation(out=sa[fc][:], in_=bk[:, 0:TS], func=AF.Silu), r=[rb], w=[r_sa[fc]])
                    sc.op("dve", lambda e: e.tensor_tensor(out=actT[b][:, fc, :], in0=bk[:, TS:2 * TS], in1=sa[fc][:], op=ALU.mult),
                          r=[rb, r_sa[fc]], w=[r_actT[b]])
                    yield
                for s in range(TS // 128):
                    yi = cnt5["y"] % 3
                    cnt5["y"] += 1
                    for half in range(2):
                        bk, rb = nbank()
                        sc.pe([(lambda e, fc=fc: e.matmul(bk[:], lhsT=actT[b][:, fc, s * 128:(s + 1) * 128], rhs=wdv[:, fc, half * 512:(half + 1) * 512],
                                                          start=(fc == 0), stop=(fc == 1))) for fc in range(2)], r=[r_actT[b], r_wd[wb]], w=[rb])
                        if half == 0:
                            sc.op("act", lambda e: e.copy(out=yt[yi][:, 0:512], in_=bk[:]), r=[rb], w=[r_yt[yi]])
                        else:
                            sc.op("dve", lambda e: e.tensor_copy(out=yt[yi][:, 512:1024], in_=bk[:]), r=[rb], w=[r_yt[yi]])
                    r0_ = tp * TS + s * 128
                    sc.dma("sp", lambda e: e.dma_start(out=ys_d[r0_:r0_ + 128, :], in_=yt[yi][:]), r=[r_yt[yi]], w=[r_ys])
                    yield
            run_pipelined(p5_tile, NTS, 3)
            sc.barrier()
            p5s.close()

        if last_phase >= 6:
            p6s = es.enter_context(ExitStack())
            y1 = [sb("y1_%d" % i, [128, 1024], F32, p6s) for i in range(2)]
            y2 = [sb("y2_%d" % i, [128, 1024], F32, p6s) for i in range(2)]
            xo = [sb("xo_%d" % i, [128, 1024], F32, p6s) for i in range(2)]
            r_y1, r_y2, r_xo = mkres("y1"), mkres("y2"), mkres("xo")
            def p6_tile(t):
                b = t % 2
                ts_ = slice(t * 128, (t + 1) * 128)
                for k, (yy, ry) in enumerate(((y1[b], r_y1[b]), (y2[b], r_y2[b]))):
                    sc.dma("pool", lambda e: e.indirect_dma_start(out=yy[:], out_offset=None, in_=ys_d[:, :],
                                                                  in_offset=bass.IndirectOffsetOnAxis(ap=pos12[:, k, t:t + 1], axis=0),
                                                                  bounds_check=reg_npos, oob_is_err=False), r=[r_route, r_ys], w=[ry])
                sc.dma("sp", lambda e: e.dma_start(out=xo[b][:], in_=out_d[ts_, :]), r=[r_x2d[t]], w=[r_xo[b]])
                yield
                sc.op("dve", lambda e: e.scalar_tensor_tensor(out=xo[b][:], in0=y1[b][:], scalar=w12[:, 0, t:t + 1], in1=xo[b][:], op0=ALU.mult, op1=ALU.add),
                      r=[r_y1[b], r_route], w=[r_xo[b]])
                sc.op("pool", lambda e: e.scalar_tensor_tensor(out=xo[b][:], in0=y2[b][:], scalar=w12[:, 1, t:t + 1], in1=xo[b][:], op0=ALU.mult, op1=ALU.add),
                      r=[r_y2[b], r_route], w=[r_xo[b]]) if False else \
                    sc.op("dve", lambda e: e.scalar_tensor_tensor(out=xo[b][:], in0=y2[b][:], scalar=w12[:, 1, t:t + 1], in1=xo[b][:], op0=ALU.mult, op1=ALU.add),
                          r=[r_y2[b], r_route], w=[r_xo[b]])
                sc.dma("sp", lambda e: e.dma_start(out=out_d[ts_, :], in_=xo[b][:]), r=[r_xo[b]], w=[r_x2d[t]], is_out=True)
            run_pipelined(p6_tile, NT, 2)
            sc.barrier()
            p6s.close()

        sc.finish()
    print("program: %d instructions, %d waits" % (sc.n_inst, sc.n_wait))
    return nc, dbg_out


def _perm_rows(w, c):
    n = w.shape[1]
    return np.ascontiguousarray(w.reshape(c, 128, n).transpose(1, 0, 2))


def _consts():
    cst = np.zeros((128, NCST), np.float64)
    j64 = np.arange(64)
    j32 = np.arange(32)
    invR = 10000.0 ** (-j64 / 64.0) / (2 * np.pi)
    invM = 10000.0 ** (-j32 / 32.0) / (2 * np.pi)
    cst[:, 0:64] = invR
    cst[:, 64:128] = invR
    cst[:, 128:160] = invM
    cst[:, 160:192] = invM
    cst[:, 192 + 64:192 + 128] = 0.25
    cst[:, 192 + 160:192 + 192] = 0.25
    h = np.arange(4)
    lg = np.log(1.0 - np.exp2(-5.0 - h))
    p = np.arange(128)[:, None]
    cst[:, 384:388] = np.exp((p + 1.0) * lg[None, :])
    cst[:, 388:392] = np.exp(-(p + 1.0) * lg[None, :]) * (128.0 ** -0.5)
    cst[:, 392:904] = np.repeat(np.exp(128.0 * lg), 128)[None, :]
    return cst.astype(np.float32)


def make_in_maps(inputs, S, n_cores, last_phase=6):
    f = lambda a: np.ascontiguousarray(np.asarray(a), dtype=np.float32)
    l = 0
    shared = {
        "cst": _consts(),
        "w_in": _perm_rows(f(inputs["w_in"][l]), 8),
        "g_attn": np.ascontiguousarray(f(inputs["attn_norm_g"][l]).reshape(8, 128).T),
        "w_uq": _perm_rows(f(inputs["mla_w_uq"][l]), 2),
        "g_qn": np.ascontiguousarray(f(inputs["mla_q_norm_g"][l]).reshape(2, 128).T),
        "w_ukv": f(inputs["mla_w_ukv"][l]),
        "g_kvn": f(inputs["mla_kv_norm_g"][l]).reshape(128, 1),
        "gq": f(inputs["mla_q_qk_g"][l]).reshape(1, 192),
        "gk": f(inputs["mla_k_qk_g"][l]).reshape(1, 192),
        "gn": f(inputs["ret_gn_g"][l]).reshape(1, 512),
        "w_out": _perm_rows(f(inputs["w_out"][l]), 8),
        "g_cross": np.ascontiguousarray(f(inputs["cross_norm_g"][l]).reshape(8, 128).T),
        "g_mem": np.ascontiguousarray(f(inputs["mem_norm_g"][l]).reshape(8, 128).T),
        "cw_q": _perm_rows(f(inputs["cross_w_q"][l]), 8),
        "cw_kv": _perm_rows(f(inputs["cross_w_kv"][l]), 8),
        "cqg": f(inputs["cross_q_qk_g"][l]).reshape(1, 256),
        "ckg": f(inputs["cross_k_qk_g"][l]).reshape(1, 256),
        "cw_o": _perm_rows(f(inputs["cross_w_o"][l]), 8),
        "mg": f(inputs["moe_norm_g"][l]).reshape(1, 1024),
        "w_rt": _perm_rows(np.concatenate([f(inputs["router_w_group"][l]), f(inputs["router_w_expert"][l])], axis=1), 8),
        "b_rt": np.concatenate([f(inputs["router_b_group"][l]), f(inputs["router_b_expert"][l])]).reshape(1, 36),
        "ew_g": np.ascontiguousarray(f(inputs["expert_w_gate"][l]).reshape(32, 8, 128, 256).transpose(0, 2, 1, 3)).reshape(32 * 128, 2048),
        "ew_u": np.ascontiguousarray(f(inputs["expert_w_up"][l]).reshape(32, 8, 128, 256).transpose(0, 2, 1, 3)).reshape(32 * 128, 2048),
        "ew_d": np.ascontiguousarray(f(inputs["expert_w_down"][l]).reshape(32, 2, 128, 1024).transpose(0, 2, 1, 3)).reshape(32 * 128, 2048),
    }
    if last_phase < 5:
        for k in ("ew_g", "ew_u", "ew_d"):
            del shared[k]
    NT = S // 128
    maps = []
    for b in range(n_cores):
        m = dict(shared)
        m["x"] = f(inputs["x"][b])
        m["mem"] = f(inputs["mem"][b])
        m["pos"] = np.ascontiguousarray(np.asarray(inputs["positions"][b]).astype(np.int32).reshape(NT, 128).T)
        maps.append(m)
    return maps


def kernel(**inputs):
    B, S, _ = inputs["x"].shape
    nc, _ = build_program(S)
    maps = make_in_maps(inputs, S, B)
    res = run_bass_kernel_spmd(nc, maps, core_ids=list(range(B)))
    return np.stack([np.asarray(r["out"]) for r in res.results], axis=0).astype(np.float32)
```

```python
import math
from contextlib import ExitStack

import numpy as np
import concourse.bass as bass
import concourse.mybir as mybir
from concourse.bass_utils import run_bass_kernel_spmd

F32 = mybir.dt.float32
BF16 = mybir.dt.bfloat16
I32 = mybir.dt.int32
AF = mybir.ActivationFunctionType
ALU = mybir.AluOpType
AX = mybir.AxisListType

D = 1024
EPS = 1e-6
NDMA = 24
NCST = 904


class Res:
    __slots__ = ("name", "w", "rd", "excl")

    def __init__(self, name, excl=False):
        self.name = name
        self.w = None
        self.rd = []
        self.excl = excl


class Sched:
    ENGS = ("pe", "act", "dve", "pool", "sp")

    def __init__(self, nc, es):
        self.nc = nc
        self.eng = {"pe": nc.tensor, "act": nc.scalar, "dve": nc.vector, "pool": nc.gpsimd, "sp": nc.sync}
        self.sem = {e: es.enter_context(nc.semaphore("c_" + e)) for e in self.ENGS}
        self.cnt = {e: 0 for e in self.ENGS}
        self.waited = {e: {} for e in self.ENGS}
        self.dsem = {q: [es.enter_context(nc.semaphore("d_%s%d" % (q, i))) for i in range(NDMA)] for q in ("sp", "pool")}
        self.dcnt = {q: [0] * NDMA for q in ("sp", "pool")}
        self.dnext = {q: 0 for q in ("sp", "pool")}
        self.semname = {}
        self.out_tokens = []
        self.n_inst = 0
        self.n_wait = 0

    def res(self, name):
        return Res(name)

    def _wait(self, eng, tok):
        sem, val, src = tok
        if src == "pe" and eng == "pe":
            return
        k = id(sem)
        if self.waited[eng].get(k, 0) >= val:
            return
        self.eng[eng].wait_ge(sem, val)
        self.n_wait += 1
        self.waited[eng][k] = val

    def _deps(self, eng, r, w):
        w = list(w) + [x for x in r if x.excl]
        for x in r:
            if x.w is not None:
                self._wait(eng, x.w)
        for x in w:
            if x.w is not None:
                self._wait(eng, x.w)
            for t in x.rd:
                self._wait(eng, t)

    def _commit(self, tok, r, w):
        w = list(w) + [x for x in r if x.excl]
        r = [x for x in r if not x.excl]
        for x in r:
            x.rd = [t for t in x.rd if t[0] is not tok[0]] + [tok]
        for x in w:
            x.w = tok
            x.rd = []

    def op(self, eng, fn, r=(), w=()):
        self._deps(eng, r, w)
        inst = fn(self.eng[eng])
        self.cnt[eng] += 1
        self.n_inst += 1
        inst.then_inc(self.sem[eng], 1)
        tok = (self.sem[eng], self.cnt[eng], eng)
        self.waited[eng][id(self.sem[eng])] = max(self.waited[eng].get(id(self.sem[eng]), 0), 0)
        self._commit(tok, r, w)
        return tok

    def pe(self, fns, r=(), w=()):
        self._deps("pe", r, w)
        inst = None
        for fn in fns:
            inst = fn(self.eng["pe"])
            self.n_inst += 1
        self.cnt["pe"] += 1
        inst.then_inc(self.sem["pe"], 1)
        tok = (self.sem["pe"], self.cnt["pe"], "pe")
        self._commit(tok, r, w)
        return tok

    def dma(self, q, fn, r=(), w=(), is_out=False):
        self._deps(q, r, w)
        i = self.dnext[q] % NDMA
        self.dnext[q] += 1
        sem = self.dsem[q][i]
        if self.dcnt[q][i] > 0:
            self._wait(q, (sem, 16 * self.dcnt[q][i], "dma"))
        inst = fn(self.eng[q])
        self.n_inst += 1
        self.dcnt[q][i] += 1
        inst.then_inc(sem, 16)
        tok = (sem, 16 * self.dcnt[q][i], "dma")
        self._commit(tok, r, w)
        if is_out:
            self.out_tokens.append(tok)
        return tok

    def barrier(self):
        toks = [(self.sem[e], self.cnt[e], e) for e in self.ENGS if self.cnt[e] > 0]
        for q in ("sp", "pool"):
            for i in range(NDMA):
                if self.dcnt[q][i] > 0:
                    toks.append((self.dsem[q][i], 16 * self.dcnt[q][i], "dma"))
        for e in self.ENGS:
            for t in toks:
                if t[2] == e and e != "pe":
                    pass
                self._wait_force(e, t)

    def _wait_force(self, eng, tok):
        sem, val, src = tok
        k = id(sem)
        if self.waited[eng].get(k, 0) >= val:
            return
        self.eng[eng].wait_ge(sem, val)
        self.n_wait += 1
        self.waited[eng][k] = val

    def finish(self):
        for t in self.out_tokens:
            self._wait_force("sp", t)
        self.barrier()


def build_program(S, last_phase=6, dbg=()):
    NT = S // 128
    NB = S // 512
    nc = bass.Bass("TRN2", target_bir_lowering=False)

    def din(name, shape, dt=F32):
        return nc.dram_tensor(name, list(shape), dt, kind="ExternalInput").ap()

    x_d = din("x", [S, D])
    mem_d = din("mem", [256, D])
    pos_d = din("pos", [128, NT], I32)
    cst_d = din("cst", [128, NCST])
    w_in_d = din("w_in", [128, 8, 2496])
    g_attn_d = din("g_attn", [128, 8])
    w_uq_d = din("w_uq", [128, 2, 768])
    g_qn_d = din("g_qn", [128, 2])
    w_ukv_d = din("w_ukv", [128, 1024])
    g_kvn_d = din("g_kvn", [128, 1])
    gq_d = din("gq", [1, 192])
    gk_d = din("gk", [1, 192])
    gn_d = din("gn", [1, 512])
    w_out_d = din("w_out", [128, 8, 1024])
    g_cross_d = din("g_cross", [128, 8])
    g_mem_d = din("g_mem", [128, 8])
    cw_q_d = din("cw_q", [128, 8, 1024])
    cw_kv_d = din("cw_kv", [128, 8, 2048])
    cqg_d = din("cqg", [1, 256])
    ckg_d = din("ckg", [1, 256])
    cw_o_d = din("cw_o", [128, 8, 1024])
    mg_d = din("mg", [1, 1024])
    w_rt_d = din("w_rt", [128, 8, 36])
    b_rt_d = din("b_rt", [1, 36])
    if last_phase >= 5:
        ew_g_d = din("ew_g", [32 * 128, 2048])
        ew_u_d = din("ew_u", [32 * 128, 2048])
        ew_d_d = din("ew_d", [32 * 128, 2048])
    out_d = nc.dram_tensor("out", [S, D], F32, kind="ExternalOutput").ap()

    dbg_out = {}

    with ExitStack() as es:
        sc = Sched(nc, es)

        def sb(name, shape, dt=F32, stack=es):
            return stack.enter_context(nc.sbuf_tensor("s_" + name, list(shape), dt))

        banks = [es.enter_context(nc.psum_tensor("bank%d" % i, [128, 512], F32)) for i in range(8)]
        bank_res = [Res("bank%d" % i, excl=True) for i in range(8)]
        bstate = {"i": 0}

        def nbank():
            i = bstate["i"] % 8
            bstate["i"] += 1
            return banks[i], bank_res[i]

        def dump(name, ap, res, shape, dt=F32):
            if name not in dbg:
                return
            t = nc.dram_tensor("dbg_" + name, list(shape), dt, kind="ExternalOutput").ap()
            dbg_out[name] = t
            sc.dma("sp", lambda e: e.dma_start(out=t, in_=ap), r=[res], w=[], is_out=True)

        def nbank(lo=0, hi=8):
            key = (lo, hi)
            i = lo + bstate.get(key, 0) % (hi - lo)
            bstate[key] = bstate.get(key, 0) + 1
            return banks[i], bank_res[i]

        cst = sb("cst", [128, NCST])
        r_cst = sc.res("cst")
        sc.dma("sp", lambda e: e.dma_start(out=cst[:], in_=cst_d[:, :]), w=[r_cst])
        INVF = cst[:, 0:192]
        OFFS = cst[:, 192:384]
        QDc = cst[:, 384:388]
        KDc = cst[:, 388:392]
        CDEC = cst[:, 392:904]

        ident_bf = sb("ident_bf", [128, 128], BF16)
        ident_f = sb("ident_f", [128, 128], F32)
        maskT = sb("maskT", [128, 128], BF16)
        mask4 = sb("mask4", [128, 4, 128], F32)
        tri = sb("tri", [128, 128], BF16)
        ones_bf = sb("ones_bf", [128, 128], BF16)
        r_const = sc.res("consts")

        def mk_mask(t_ap, pattern, cmp):
            sc.op("pool", lambda e: e.memset(t_ap, 1.0), w=[r_const])
            sc.op("pool", lambda e: e.affine_select(out=t_ap, in_=t_ap, pattern=pattern, compare_op=cmp, fill=0.0,
                                                    base=0, channel_multiplier=-1), r=[r_const], w=[r_const])

        mk_mask(ident_bf[:], [[1, 128]], ALU.is_equal)
        mk_mask(ident_f[:], [[1, 128]], ALU.is_equal)
        mk_mask(maskT[:], [[1, 128]], ALU.is_ge)
        mk_mask(mask4[:], [[0, 4], [1, 128]], ALU.is_ge)
        mk_mask(tri[:], [[1, 128]], ALU.is_gt)
        negm = sb("negm", [128, 128], BF16)
        sc.op("pool", lambda e: e.memset(negm[:], -30000.0), w=[r_const])
        sc.op("pool", lambda e: e.affine_select(out=negm[:], in_=negm[:], pattern=[[-1, 128]], compare_op=ALU.is_gt, fill=0.0,
                                                base=0, channel_multiplier=1), r=[r_const], w=[r_const])
        sc.op("pool", lambda e: e.memset(ones_bf[:], 1.0), w=[r_const])

        def bload(name, src, n, stack=es):
            t = sb(name, [128, n], F32, stack)
            sc.dma("sp", lambda e: e.dma_start(out=t[:], in_=src.partition_broadcast(128)), w=[r_const])
            return t

        def pload(name, src, n, stack=es):
            t = sb(name, [128, n], F32, stack)
            sc.dma("sp", lambda e: e.dma_start(out=t[:], in_=src[:, :]), w=[r_const])
            return t

        gq = bload("gq", gq_d, 192)
        gk = bload("gk", gk_d, 192)
        gn = bload("gn", gn_d, 512)

        pos_i = sb("pos_i", [128, NT], I32)
        pos_f = sb("pos_f", [128, NT])
        SCm = sb("SCm", [128, NT, 64])
        rstd1 = sb("rstd1", [128, NT])
        r_sc = sc.res("SC")
        r_rstd1 = sc.res("rstd1")
        stage = [None, None]
        r_stage = [sc.res("stage%d" % i) for i in range(2)]
        st = {"i": 0}
        scale_engs = ("dve", "act")

        def load_scaled(dst_fn, src_fn, gain_fn, C, N, rdst):
            for c in range(C):
                for n0 in range(0, N, 1024):
                    n1 = min(N, n0 + 1024)
                    i = st["i"] % 2
                    st["i"] += 1
                    stg, rs = stage[i], r_stage[i]
                    sc.dma("sp", lambda e: e.dma_start(out=stg[:, 0:n1 - n0], in_=src_fn(c, n0, n1)), w=[rs])
                    if scale_engs[i] == "act":
                        sc.op("act", lambda e: e.activation(out=dst_fn(c, n0, n1), in_=stg[:, 0:n1 - n0], func=AF.Copy, scale=gain_fn(c)),
                              r=[rs, r_const], w=[rdst])
                    else:
                        sc.op("dve", lambda e: e.tensor_scalar(out=dst_fn(c, n0, n1), in0=stg[:, 0:n1 - n0], scalar1=gain_fn(c), scalar2=None,
                                                               op0=ALU.mult), r=[rs, r_const], w=[rdst])

        def load_cast(dst_fn, src_fn, C, rdst):
            for c in range(C):
                sc.dma("pool", lambda e: e.dma_start(out=dst_fn(c), in_=src_fn(c)), w=[rdst])

        mhalf = sb("mhalf", [128, 8])
        sc.op("pool", lambda e: e.memset(mhalf[:], -0.5), w=[r_const])

        def rstd_from_ss(ss_ap, n, out_ap, r_in, r_out, tmp_ap, r_tmp):
            k = ss_ap.shape[1]
            sc.op("dve", lambda e: e.tensor_scalar(out=tmp_ap, in0=ss_ap, scalar1=1.0 / n, scalar2=EPS, op0=ALU.mult, op1=ALU.add),
                  r=[r_in], w=[r_tmp])
            sc.op("pool", lambda e: e.tensor_tensor(out=out_ap, in0=tmp_ap, in1=mhalf[:, 0:k], op=ALU.pow), r=[r_tmp, r_const], w=[r_out])

        sc.dma("sp", lambda e: e.dma_start(out=pos_i[:], in_=pos_d[:, :]), w=[r_sc])
        sc.op("dve", lambda e: e.tensor_copy(out=pos_f[:], in_=pos_i[:]), r=[r_sc], w=[r_sc])

        rout_d = nc.dram_tensor("rout_s", [S, 512], BF16).ap()
        x1_d = nc.dram_tensor("x1_s", [S, D], F32).ap()
        hm_d = nc.dram_tensor("hm_s", [S, D], BF16).ap()
        TS = 256
        NTS = (2 * S) // TS + 32
        NPOS = NTS * TS
        xs_d = nc.dram_tensor("xs_s", [NPOS, D], BF16).ap()
        ys_d = nc.dram_tensor("ys_s", [NPOS, D], F32).ap()
        r_xs = sc.res("xs_d")

        def run_pipelined(gen_fn, n_items, depth):
            active = []
            nxt = 0
            while nxt < n_items or active:
                if nxt < n_items and len(active) < depth:
                    active.append(gen_fn(nxt))
                    nxt += 1
                for g in list(active):
                    try:
                        next(g)
                    except StopIteration:
                        active.remove(g)

        def mkres(n, k=2):
            return [sc.res("%s%d" % (n, i)) for i in range(k)]

        if last_phase >= 1:
            p1s = es.enter_context(ExitStack())
            import os as _os
            stop = int(_os.environ.get('P1_STOP', '99'))
            SCr = sb("SCr", [128, NT, 128], F32, p1s)
            w_r = sb("w_r", [128, 8, 2048], BF16, p1s)
            r_wr = sc.res("w_r")
            stage[:] = [sb("stage1_%d" % i, [128, 1024], F32, p1s) for i in range(2)]
            g_attn = pload("g_attn", g_attn_d, 8, p1s)
            load_scaled(lambda c, a, b_: w_r[:, c, a:b_], lambda c, a, b_: w_in_d[:, c, 448 + a:448 + b_], lambda c: g_attn[:, c:c + 1], 8, 2048, r_wr)

            NTsc = NT if stop >= 2 else 0
            tr_t = sb("tr_t", [128, 192], F32, p1s)
            tr_i = sb("tr_i", [128, 192], I32, p1s)
            tr_f = sb("tr_f", [128, 192], F32, p1s)
            r_tr = sc.res("tr")
            for t in range(NTsc):
                sc.op("dve", lambda e: e.scalar_tensor_tensor(out=tr_t[:], in0=INVF, scalar=pos_f[:, t:t + 1], in1=OFFS,
                                                              op0=ALU.mult, op1=ALU.add), r=[r_cst, r_sc], w=[r_tr])
                sc.op("dve", lambda e: e.tensor_copy(out=tr_i[:], in_=tr_t[:]), r=[r_tr], w=[r_tr])
                sc.op("dve", lambda e: e.tensor_copy(out=tr_f[:], in_=tr_i[:]), r=[r_tr], w=[r_tr])
                sc.op("dve", lambda e: e.tensor_tensor(out=tr_t[:], in0=tr_t[:], in1=tr_f[:], op=ALU.subtract), r=[r_tr], w=[r_tr])
                sc.op("dve", lambda e: e.scalar_tensor_tensor(out=tr_f[:], in0=tr_t[:], scalar=0.5, in1=tr_t[:],
                                                              op0=ALU.is_gt, op1=ALU.subtract), r=[r_tr], w=[r_tr])
                sc.op("dve", lambda e: e.scalar_tensor_tensor(out=tr_t[:], in0=tr_f[:], scalar=0.5, in1=tr_f[:],
                                                              op0=ALU.is_gt, op1=ALU.subtract), r=[r_tr], w=[r_tr])
                sc.op("act", lambda e: e.activation(out=SCr[:, t, :], in_=tr_t[:, 0:128], func=AF.Sin, scale=6.28318), r=[r_tr], w=[r_sc])
                sc.op("act", lambda e: e.activation(out=SCm[:, t, :], in_=tr_t[:, 128:192], func=AF.Sin, scale=6.28318), r=[r_tr], w=[r_sc])
            dump("SCr", SCr[:], r_sc, [128, NT, 128])
            dump("SCm", SCm[:], r_sc, [128, NT, 64])

            do_pc = last_phase >= 5
            if do_pc:
                ewb_all = nc.dram_tensor("ewb_all", [32 * 128, 6144], BF16).ap()
                ewb_d = [ewb_all[:, k * 2048:(k + 1) * 2048] for k in range(3)]
                ew_src = [ew_g_d, ew_u_d, ew_d_d]
                r_ewb = sc.res("ewb")
                pcs = [sb("pcs%d" % i, [128, 2048], BF16, p1s) for i in range(3)]
                r_pcs = mkres("pcs", 3)
                pc_state = {"i": 0}
                PC_PER_TILE = (96 + NT - 1) // NT

                def precast_step():
                    for _ in range(PC_PER_TILE):
                        i = pc_state["i"]
                        if i >= 96:
                            return
                        pc_state["i"] += 1
                        e_, k_ = i // 3, i % 3
                        bi = i % 3
                        rows = slice(e_ * 128, (e_ + 1) * 128)
                        sc.dma("pool", lambda e: e.dma_start(out=pcs[bi][:], in_=ew_src[k_][rows, :]), w=[r_pcs[bi]])
                        sc.dma("sp", lambda e: e.dma_start(out=ewb_d[k_][rows, :], in_=pcs[bi][:]), r=[r_pcs[bi]], w=[r_ewb])

            zt = sb("zt", [128, 2, 1024], BF16, p1s)
            r_zt = sc.res("zt")
            sc.op("pool", lambda e: e.memset(zt[:], 0.0), w=[r_zt])
            ROWS_PER = NPOS // NT

            def zero_step(t):
                if last_phase < 4:
                    return
                for r0_ in range(t * ROWS_PER, (t + 1) * ROWS_PER, 256):
                    sc.dma("sp", lambda e: e.dma_start(out=xs_d[r0_:r0_ + 256, :].rearrange("(p a) d -> p a d", a=2), in_=zt[:]), r=[r_zt], w=[r_xs])

            xt = [sb("xt%d" % i, [128, 1024], F32, p1s) for i in range(4)]
            xb = [sb("xb%d" % i, [128, 1024], BF16, p1s) for i in range(4)]
            xT = [sb("xT%d" % i, [128, 8, 128], BF16, p1s) for i in range(4)]
            junk = sb("junk", [128, 1024], BF16, p1s)
            rq_f = [sb("rq_f%d" % i, [128, 512], F32, p1s) for i in range(4)]
            rk_f = [sb("rk_f%d" % i, [128, 512], F32, p1s) for i in range(4)]
            v_b = [sb("v_b%d" % i, [128, 512], BF16, p1s) for i in range(4)]
            sg = [sb("sg%d" % i, [128, 512], F32, p1s) for i in range(6)]
            sm1 = [sb("sm1_%d" % i, [128, 32], F32, p1s) for i in range(4)]
            rp = [sb("rp%d" % i, [128, 2, 512], F32, p1s) for i in range(2)]
            qp_b = [sb("qp_b%d" % i, [128, 512], BF16, p1s) for i in range(4)]
            kp_b = [sb("kp_b%d" % i, [128, 512], BF16, p1s) for i in range(4)]
            qpT = [sb("qpT%d" % i, [128, 4, 128], BF16, p1s) for i in range(4)]
            kpT = [sb("kpT%d" % i, [128, 4, 128], BF16, p1s) for i in range(4)]
            PT = [sb("PT%d" % i, [128, 4, 128], BF16, p1s) for i in range(3)]
            Tst = sb("Tst", [128, 4, 128], F32, p1s)
            Tst_b = sb("Tst_b", [128, 4, 128], BF16, p1s)
            Ttmp = sb("Ttmp", [128, 4, 128], F32, p1s)
            o_f = [sb("o_f%d" % i, [128, 4, 128], F32, p1s) for i in range(4)]
            bnst = [sb("bnst%d" % i, [128, 4, 6], F32, p1s) for i in range(4)]
            bnag = [sb("bnag%d" % i, [128, 4, 2], F32, p1s) for i in range(4)]
            ro_b = [sb("ro_b%d" % i, [128, 512], BF16, p1s) for i in range(3)]

            r_xt, r_xb, r_xT = mkres("xt", 4), mkres("xb", 4), mkres("xT", 4)
            r_rq, r_rk, r_vb, r_sg, r_sm1 = mkres("rq", 4), mkres("rk", 4), mkres("vb", 4), mkres("sg", 6), mkres("sm1", 4)
            r_rp = mkres("rp")
            r_qpb, r_kpb, r_qpT, r_kpT, r_PT, r_of, r_bn, r_rob = (mkres("qpb", 4), mkres("kpb", 4), mkres("qpT", 4), mkres("kpT", 4), mkres("PT", 3),
                                                                   mkres("of", 4), mkres("bn", 4), mkres("rob", 3))
            r_junk = sc.res("junk")
            r_T = sc.res("Tst")
            r_Tb = sc.res("Tst_b")
            r_Tt = sc.res("Ttmp")
            sc.op("dve", lambda e: e.memset(Tst[:], 0.0), w=[r_T])
            sc.op("dve", lambda e: e.memset(Tst_b[:], 0.0), w=[r_Tb])

            def rope_ret(eng, src, r_src, dst_b, r_dst, t, decay, scr, r_scr):
                cosB = SCr[:, t, 64:128].unsqueeze(1).broadcast_to([128, 8, 64])
                sinB = SCr[:, t, 0:64].unsqueeze(1).broadcast_to([128, 4, 64])
                sv = src[:].rearrange("p (h two d) -> p h two d", h=4, two=2)
                Pv = scr[:, 0, :]
                Qv = scr[:, 1, :].rearrange("p (h two d) -> p h two d", h=4, two=2)
                P4 = scr[:, 0, :].rearrange("p (h two d) -> p h two d", h=4, two=2)
                sc.op(eng, lambda e: e.tensor_tensor(out=Pv.rearrange("p (g d) -> p g d", g=8), in0=src[:].rearrange("p (g d) -> p g d", g=8),
                                                     in1=cosB, op=ALU.mult), r=[r_src, r_sc], w=[r_scr])
                sc.op(eng, lambda e: e.tensor_tensor(out=Qv[:, :, 0, :], in0=sv[:, :, 1, :], in1=sinB, op=ALU.mult), r=[r_src, r_sc], w=[r_scr])
                sc.op(eng, lambda e: e.tensor_tensor(out=Qv[:, :, 1, :], in0=sv[:, :, 0, :], in1=sinB, op=ALU.mult), r=[r_src, r_sc], w=[r_scr])
                sc.op(eng, lambda e: e.tensor_tensor(out=P4[:, :, 0, :], in0=P4[:, :, 0, :], in1=Qv[:, :, 0, :], op=ALU.subtract), r=[r_scr], w=[r_scr])
                sc.op(eng, lambda e: e.tensor_tensor(out=P4[:, :, 1, :], in0=P4[:, :, 1, :], in1=Qv[:, :, 1, :], op=ALU.add), r=[r_scr], w=[r_scr])
                decB = decay.unsqueeze(2).broadcast_to([128, 4, 128])
                sc.op(eng, lambda e: e.tensor_tensor(out=dst_b[:].rearrange("p (h d) -> p h d", h=4), in0=Pv.rearrange("p (h d) -> p h d", h=4),
                                                     in1=decB, op=ALU.mult), r=[r_scr, r_cst], w=[r_dst])

            def p1_tile(t):
                b = t % 4
                b6 = t % 6
                b3 = t % 3
                ts_ = slice(t * 128, (t + 1) * 128)
                s1 = sm1[b]
                sc.dma("sp", lambda e: e.dma_start(out=xt[b][:], in_=x_d[ts_, :]), w=[r_xt[b]])
                if do_pc:
                    precast_step()
                zero_step(t)
                sc.op("act", lambda e: e.activation(out=junk[:], in_=xt[b][:], func=AF.Square, accum_out=s1[:, 0:1]),
                      r=[r_xt[b]], w=[r_junk, r_sm1[b]])
                rstd_from_ss(s1[:, 0:1], 1024.0, rstd1[:, t:t + 1], r_sm1[b], r_rstd1, s1[:, 1:2], r_sm1[b])
                yield
                sc.op("act", lambda e: e.copy(out=xb[b][:], in_=xt[b][:]), r=[r_xt[b]], w=[r_xb[b]])
                bk, rb = nbank()
                pv = bk[:].bitcast(BF16).rearrange("p (c n) -> p c n", n=128)
                sc.pe([(lambda e, c=c: e.transpose(out=pv[:, c, :], in_=xb[b][:, c * 128:(c + 1) * 128], identity=ident_bf[:])) for c in range(8)],
                      r=[r_xb[b], r_const], w=[rb])
                sc.op("dve", lambda e: e.tensor_copy(out=xT[b][:], in_=pv), r=[rb], w=[r_xT[b]])
                rs1 = rstd1[:, t:t + 1]
                yield
                pb = []
                for k4 in range(4):
                    bk, rb = nbank()
                    sc.pe([(lambda e, c=c: e.matmul(bk[:], lhsT=xT[b][:, c, :], rhs=w_r[:, c, k4 * 512:(k4 + 1) * 512], start=(c == 0), stop=(c == 7)))
                           for c in range(8)], r=[r_xT[b], r_wr], w=[rb])
                    pb.append((bk, rb))
                sc.op("act", lambda e: e.activation(out=rq_f[b][:], in_=pb[0][0][:], func=AF.Copy, scale=rs1), r=[pb[0][1], r_rstd1], w=[r_rq[b]])
                sc.op("dve", lambda e: e.tensor_scalar(out=rk_f[b][:], in0=pb[1][0][:], scalar1=rs1, scalar2=None, op0=ALU.mult),
                      r=[pb[1][1], r_rstd1], w=[r_rk[b]])
                sc.op("act", lambda e: e.activation(out=v_b[b][:], in_=pb[2][0][:], func=AF.Copy, scale=rs1), r=[pb[2][1], r_rstd1], w=[r_vb[b]])
                sc.op("act", lambda e: e.activation(out=sg[b6][:], in_=pb[3][0][:], func=AF.Silu, scale=rs1), r=[pb[3][1], r_rstd1], w=[r_sg[b6]])

                yield
                rope_ret("dve", rq_f[b], r_rq[b], qp_b[b], r_qpb[b], t, QDc, rp[0], r_rp[0])
                rope_ret("pool", rk_f[b], r_rk[b], kp_b[b], r_kpb[b], t, KDc, rp[1], r_rp[1])
                yield
                bk, rb = nbank()
                pv = bk[:].bitcast(BF16).rearrange("p (c n) -> p c n", n=128)
                sc.pe([(lambda e, h=h: e.transpose(out=pv[:, h, :], in_=qp_b[b][:, h * 128:(h + 1) * 128], identity=ident_bf[:])) for h in range(4)]
                      + [(lambda e, h=h: e.transpose(out=pv[:, 4 + h, :], in_=kp_b[b][:, h * 128:(h + 1) * 128], identity=ident_bf[:])) for h in range(4)],
                      r=[r_qpb[b], r_kpb[b], r_const], w=[rb])
                sc.op("act", lambda e: e.copy(out=qpT[b][:], in_=pv[:, 0:4, :]), r=[rb], w=[r_qpT[b]])
                sc.op("dve", lambda e: e.tensor_copy(out=kpT[b][:], in_=pv[:, 4:8, :]), r=[rb], w=[r_kpT[b]])
                bk, rb = nbank()
                sv_ = bk[:].rearrange("p (h n) -> p h n", h=4)
                sc.pe([(lambda e, h=h: e.matmul(sv_[:, h, :], lhsT=kpT[b][:, h, :], rhs=qpT[b][:, h, :], start=True, stop=True)) for h in range(4)],
                      r=[r_kpT[b], r_qpT[b]], w=[rb])
                sc.op("dve", lambda e: e.tensor_tensor(out=PT[b3][:], in0=sv_, in1=mask4[:], op=ALU.mult), r=[rb, r_const], w=[r_PT[b3]])
                yield
                bk_o, rb_o = nbank()
                ov = bk_o[:].rearrange("p (h n) -> p h n", h=4)
                fns = []
                for h in range(4):
                    fns.append(lambda e, h=h: e.matmul(ov[:, h, :], lhsT=PT[b3][:, h, :], rhs=v_b[b][:, h * 128:(h + 1) * 128], start=True, stop=False))
                    fns.append(lambda e, h=h: e.matmul(ov[:, h, :], lhsT=qpT[b][:, h, :], rhs=Tst_b[:, h, :], start=False, stop=True))
                sc.pe(fns, r=[r_PT[b3], r_vb[b], r_qpT[b], r_Tb], w=[rb_o])
                bk_s, rb_s = nbank()
                stv = bk_s[:].rearrange("p (h n) -> p h n", h=4)
                sc.pe([(lambda e, h=h: e.matmul(stv[:, h, :], lhsT=kp_b[b][:, h * 128:(h + 1) * 128], rhs=v_b[b][:, h * 128:(h + 1) * 128],
                                                start=True, stop=True)) for h in range(4)], r=[r_kpb[b], r_vb[b]], w=[rb_s])
                sc.op("dve", lambda e: e.tensor_tensor(out=Ttmp[:], in0=stv, in1=Tst[:], op=ALU.add), r=[rb_s, r_T], w=[r_Tt])
                sc.op("pool", lambda e: e.tensor_tensor(out=Tst[:], in0=Ttmp[:], in1=CDEC.rearrange("p (h n) -> p h n", h=4), op=ALU.mult),
                      r=[r_Tt, r_cst], w=[r_T])
                sc.op("pool", lambda e: e.tensor_copy(out=Tst_b[:], in_=Tst[:]), r=[r_T], w=[r_Tb])
                yield
                sc.op("act", lambda e: e.copy(out=o_f[b][:], in_=ov), r=[rb_o], w=[r_of[b]])
                for h in range(4):
                    sc.op("dve", lambda e: e.bn_stats(out=bnst[b][:, h, :], in_=o_f[b][:, h, :]), r=[r_of[b]], w=[r_bn[b]])
                for h in range(4):
                    sc.op("dve", lambda e: e.bn_aggr(out=bnag[b][:, h, :], in_=bnst[b][:, h, :]), r=[r_bn[b]], w=[r_bn[b]])
                sc.op("dve", lambda e: e.tensor_scalar(out=s1[:, 20:24], in0=bnag[b][:, :, 1], scalar1=EPS, scalar2=None, op0=ALU.add),
                      r=[r_bn[b]], w=[r_sm1[b]])
                sc.op("pool", lambda e: e.tensor_tensor(out=s1[:, 24:28], in0=s1[:, 20:24], in1=mhalf[:, 0:4], op=ALU.pow), r=[r_sm1[b], r_const], w=[r_sm1[b]])
                for h in range(4):
                    sc.op("dve", lambda e: e.tensor_scalar(out=o_f[b][:, h, :], in0=o_f[b][:, h, :], scalar1=bnag[b][:, h, 0:1], scalar2=s1[:, 24 + h:25 + h],
                                                           op0=ALU.subtract, op1=ALU.mult), r=[r_of[b], r_bn[b], r_sm1[b]], w=[r_of[b]])
                ofl = o_f[b][:].rearrange("p h n -> p (h n)")
                sc.op("pool", lambda e: e.tensor_tensor(out=ofl, in0=ofl, in1=gn[:], op=ALU.mult), r=[r_of[b], r_const], w=[r_of[b]])
                sc.op("pool", lambda e: e.tensor_tensor(out=ro_b[b3][:], in0=ofl, in1=sg[b6][:], op=ALU.mult), r=[r_of[b], r_sg[b6]], w=[r_rob[b3]])
                r_routd = sc.res("rout_d%d" % t)
                sc.dma("sp", lambda e: e.dma_start(out=rout_d[ts_, :], in_=ro_b[b3][:]), r=[r_rob[b3]], w=[r_routd])
                if t == NT - 1:
                    dump("ro_last", ro_b[b3][:], r_rob[b3], [128, 512], BF16)
            run_pipelined(p1_tile, NT, 4)
            dump("rstd1", rstd1[:], r_rstd1, [128, NT])
            sc.barrier()
            p1s.close()

        if last_phase >= 2:
            p2s = es.enter_context(ExitStack())
            KTn = sb("KTn", [128, 4, S], BF16, p2s)
            KTr = sb("KTr", [128, 2, S], BF16, p2s)
            Vaug = sb("Vaug", [128, NT, 4, 129], BF16, p2s)
            r_KT = [sc.res("KT%d" % t) for t in range(NT)]
            r_V = sc.res("Vaug")
            sc.op("pool", lambda e: e.memset(Vaug[:], 1.0), w=[r_V])
            w_a = sb("w_a", [128, 8, 448], BF16, p2s)
            w_uq = sb("w_uq", [128, 2, 768], BF16, p2s)
            w_ukv = sb("w_ukv", [128, 1024], BF16, p2s)
            w_out = sb("w_out", [128, 8, 1024], BF16, p2s)
            r_w2 = sc.res("w2")
            g_attn2 = pload("g_attn2", g_attn_d, 8, p2s)
            g_qn = pload("g_qn", g_qn_d, 2, p2s)
            g_kvn = pload("g_kvn", g_kvn_d, 1, p2s)
            pw2 = es.enter_context(ExitStack())
            stage[:] = [sb("stage2_%d" % i, [128, 1024], F32, pw2) for i in range(2)]
            load_scaled(lambda c, a, b_: w_a[:, c, a:b_], lambda c, a, b_: w_in_d[:, c, a:b_], lambda c: g_attn2[:, c:c + 1], 8, 448, r_w2)
            load_scaled(lambda c, a, b_: w_uq[:, c, a:b_], lambda c, a, b_: w_uq_d[:, c, a:b_], lambda c: g_qn[:, c:c + 1], 2, 768, r_w2)
            load_scaled(lambda c, a, b_: w_ukv[:, a:b_], lambda c, a, b_: w_ukv_d[:, a:b_], lambda c: g_kvn[:, 0:1], 1, 1024, r_w2)
            load_cast(lambda c: w_out[:, c, :], lambda c: w_out_d[:, c, :], 8, r_w2)
            sc.barrier()
            pw2.close()

            xt = [sb("x2t%d" % i, [128, 1024], F32, p2s) for i in range(2)]
            xb = [sb("x2b%d" % i, [128, 1024], BF16, p2s) for i in range(2)]
            xT = [sb("x2T%d" % i, [128, 8, 128], BF16, p2s) for i in range(2)]
            junk = sb("junk2", [128, 768], F32, p2s)
            cq_f = [sb("cq_f%d" % i, [128, 256], F32, p2s) for i in range(3)]
            cq_b = [sb("cq_b%d" % i, [128, 256], BF16, p2s) for i in range(3)]
            cqT = [sb("cqT%d" % i, [128, 2, 128], BF16, p2s) for i in range(3)]
            ckv_f = [sb("ckv_f%d" % i, [128, 192], F32, p2s) for i in range(3)]
            ckv_b = [sb("ckv_b%d" % i, [128, 128], BF16, p2s) for i in range(3)]
            ckvT = [sb("ckvT%d" % i, [128, 128], BF16, p2s) for i in range(3)]
            kn_f = [sb("kn_f0", [128, 4, 128], F32, p2s)] * 3
            kn_b = [sb("kn_b%d" % i, [128, 4, 128], BF16, p2s) for i in range(3)]
            kpe = [sb("kpe%d" % i, [128, 3, 64], F32, p2s) for i in range(3)]
            kr = [sb("kr%d" % i, [128, 64], F32, p2s) for i in range(3)]
            krn_b = [sb("krn_b%d" % i, [128, 4, 64], BF16, p2s) for i in range(3)]
            q_f = [sb("q_f%d" % i, [128, 4, 192], F32, p2s) for i in range(2)]
            qn_b = [sb("qn_b%d" % i, [128, 4, 128], BF16, p2s) for i in range(3)]
            qr = [sb("qr0", [128, 4, 4, 64], F32, p2s)] * 3
            qrn_b = [sb("qrn_b%d" % i, [128, 4, 64], BF16, p2s) for i in range(3)]
            sm2 = [sb("sm2_%d" % i, [128, 48], F32, p2s) for i in range(3)]
            QTn = [sb("QTn%d" % i, [128, 4, 512], BF16, p2s) for i in range(2)]
            QTr = [sb("QTr%d" % i, [128, 2, 512], BF16, p2s) for i in range(2)]
            PTt = [sb("PTt%d" % i, [128, 512], BF16, p2s) for i in range(3)]
            a_b = [sb("a_b0", [128, 4, 512], BF16, p2s)] * 2
            rcp = [sb("rcp%d" % i, [128, 4], F32, p2s) for i in range(2)]
            r_b = [sb("r_b%d" % i, [128, 512], BF16, p2s) for i in range(2)]
            aoT = [sb("aoT%d" % i, [128, 8, 128], BF16, p2s) for i in range(2)]

            r_xt, r_xb, r_xT = mkres("x2t"), mkres("x2b"), mkres("x2T")
            r_junk = sc.res("junk2")
            r_cq, r_cqb, r_cqT, r_ckv, r_ckvb, r_ckvT = mkres("cq", 3), mkres("cqb", 3), mkres("cqT", 3), mkres("ckv", 3), mkres("ckvb", 3), mkres("ckvT", 3)
            r_kn, r_knb, r_kpe, r_kr, r_krn = mkres("kn", 1) * 3, mkres("knb", 3), mkres("kpe", 3), mkres("kr", 3), mkres("krn", 3)
            r_qf, r_qnb, r_qr, r_qrn, r_sm2 = mkres("qf"), mkres("qnb", 3), mkres("qr", 1) * 3, mkres("qrn", 3), mkres("sm2", 3)
            r_QT = mkres("QT")
            r_PTt = mkres("PTt", 3)
            r_ab, r_rcp, r_rb, r_aoT = mkres("ab", 1) * 2, mkres("rcp"), mkres("rb"), mkres("aoT")
            SCALE = 192.0 ** -0.5

            def prep_tile(t, qb):
                b = t % 3
                bx = t % 2
                ts_ = slice(t * 128, (t + 1) * 128)
                tl = slice((t % 4) * 128, (t % 4 + 1) * 128)
                s2 = sm2[b]
                rs1 = rstd1[:, t:t + 1]
                sc.dma("sp", lambda e: e.dma_start(out=xt[bx][:], in_=x_d[ts_, :]), w=[r_xt[bx]])
                sc.op("dve", lambda e: e.tensor_copy(out=xb[bx][:], in_=xt[bx][:]), r=[r_xt[bx]], w=[r_xb[bx]])
                yield
                bk, rb = nbank(4, 8)
                pv = bk[:].bitcast(BF16).rearrange("p (c n) -> p c n", n=128)
                sc.pe([(lambda e, c=c: e.transpose(out=pv[:, c, :], in_=xb[bx][:, c * 128:(c + 1) * 128], identity=ident_bf[:])) for c in range(8)],
                      r=[r_xb[bx], r_const], w=[rb])
                sc.op("dve", lambda e: e.tensor_copy(out=xT[bx][:], in_=pv), r=[rb], w=[r_xT[bx]])
                yield
                bk, rb = nbank(4, 8)
                sc.pe([(lambda e, c=c: e.matmul(bk[:, 0:448], lhsT=xT[bx][:, c, :], rhs=w_a[:, c, :], start=(c == 0), stop=(c == 7))) for c in range(8)],
                      r=[r_xT[bx], r_w2], w=[rb])
                sc.op("act", lambda e: e.activation(out=cq_f[b][:], in_=bk[:, 0:256], func=AF.Copy, scale=rs1), r=[rb, r_rstd1], w=[r_cq[b]])
                sc.op("act", lambda e: e.activation(out=ckv_f[b][:], in_=bk[:, 256:448], func=AF.Copy, scale=rs1), r=[rb, r_rstd1], w=[r_ckv[b]])
                yield
                sc.op("act", lambda e: e.activation(out=junk[:, 0:128], in_=ckv_f[b][:, 0:128], func=AF.Square, accum_out=s2[:, 2:3]), r=[r_ckv[b]], w=[r_junk, r_sm2[b]])
                rstd_from_ss(s2[:, 2:3], 128.0, s2[:, 3:4], r_sm2[b], r_sm2[b], s2[:, 4:5], r_sm2[b])
                sc.op("act", lambda e: e.activation(out=junk[:, 128:192], in_=ckv_f[b][:, 128:192], func=AF.Square, accum_out=s2[:, 5:6]), r=[r_ckv[b]], w=[r_junk, r_sm2[b]])
                sc.op("pool", lambda e: e.tensor_copy(out=ckv_b[b][:], in_=ckv_f[b][:, 0:128]), r=[r_ckv[b]], w=[r_ckvb[b]])
                bk, rb = nbank(4, 8)
                pv = bk[:].bitcast(BF16)
                sc.pe([lambda e: e.transpose(out=pv[:, 0:128], in_=ckv_b[b][:], identity=ident_bf[:])], r=[r_ckvb[b], r_const], w=[rb])
                sc.op("act", lambda e: e.copy(out=ckvT[b][:], in_=pv[:, 0:128]), r=[rb], w=[r_ckvT[b]])
                yield
                for hh in range(2):
                    bk, rb = nbank(4, 8)
                    sc.pe([lambda e: e.matmul(bk[:], lhsT=ckvT[b][:], rhs=w_ukv[:, hh * 512:(hh + 1) * 512], start=True, stop=True)],
                          r=[r_ckvT[b], r_w2], w=[rb])
                    kvv = bk[:].rearrange("p (h two d) -> p h two d", h=2, two=2)
                    sc.op("dve", lambda e: e.tensor_scalar(out=kn_f[b][:, 2 * hh:2 * hh + 2, :], in0=kvv[:, :, 0, :], scalar1=s2[:, 3:4], scalar2=None,
                                                           op0=ALU.mult), r=[rb, r_sm2[b]], w=[r_kn[b]])
                    sc.op("act", lambda e: e.activation(out=Vaug[:, t, 2 * hh:2 * hh + 2, 0:128], in_=kvv[:, :, 1, :], func=AF.Copy, scale=s2[:, 3:4]),
                          r=[rb, r_sm2[b]], w=[r_V])
                yield
                jk = junk[:, 0:512].rearrange("p (h d) -> p h d", h=4)
                sc.op("dve", lambda e: e.tensor_tensor(out=jk, in0=kn_f[b][:], in1=kn_f[b][:], op=ALU.mult), r=[r_kn[b]], w=[r_junk])
                sc.op("dve", lambda e: e.tensor_reduce(out=s2[:, 8:12], in_=jk, axis=AX.X, op=ALU.add), r=[r_junk], w=[r_sm2[b]])
                sc.op("dve", lambda e: e.tensor_scalar(out=s2[:, 8:12], in0=s2[:, 8:12], scalar1=s2[:, 5:6], scalar2=None, op0=ALU.add),
                      r=[r_sm2[b]], w=[r_sm2[b]])
                rstd_from_ss(s2[:, 8:12], 192.0, s2[:, 12:16], r_sm2[b], r_sm2[b], s2[:, 16:20], r_sm2[b])
                for h in range(4):
                    sc.op("dve", lambda e: e.scalar_tensor_tensor(out=kn_b[b][:, h, :], in0=kn_f[b][:, h, :], scalar=s2[:, 12 + h:13 + h], in1=gk[:, 0:128],
                                                                  op0=ALU.mult, op1=ALU.mult), r=[r_kn[b], r_sm2[b], r_const], w=[r_knb[b]])
                yield
                kp = kpe[b]
                sc.op("pool", lambda e: e.tensor_tensor(out=kp[:, 0, :], in0=ckv_f[b][:, 128:192], in1=gk[:, 128:192], op=ALU.mult),
                      r=[r_ckv[b], r_const], w=[r_kpe[b]])
                cosM2 = SCm[:, t, 32:64].unsqueeze(1).broadcast_to([128, 2, 32])
                sinM2 = SCm[:, t, 0:32].unsqueeze(1).broadcast_to([128, 2, 32])
                k0 = kp[:, 0, :].rearrange("p (two d) -> p two d", two=2)
                kA = kp[:, 1, :].rearrange("p (two d) -> p two d", two=2)
                kB = kp[:, 2, :].rearrange("p (two d) -> p two d", two=2)
                sc.op("pool", lambda e: e.tensor_tensor(out=kA, in0=k0, in1=cosM2, op=ALU.mult), r=[r_kpe[b], r_sc], w=[r_kpe[b]])
                sc.op("pool", lambda e: e.tensor_tensor(out=kB, in0=k0, in1=sinM2, op=ALU.mult), r=[r_kpe[b], r_sc], w=[r_kpe[b]])
                sc.op("pool", lambda e: e.tensor_tensor(out=kr[b][:, 0:32], in0=kp[:, 1, 0:32], in1=kp[:, 2, 32:64], op=ALU.subtract),
                      r=[r_kpe[b]], w=[r_kr[b]])
                sc.op("pool", lambda e: e.tensor_tensor(out=kr[b][:, 32:64], in0=kp[:, 1, 32:64], in1=kp[:, 2, 0:32], op=ALU.add),
                      r=[r_kpe[b]], w=[r_kr[b]])
                for h in range(4):
                    sc.op("dve", lambda e: e.tensor_scalar(out=krn_b[b][:, h, :], in0=kr[b][:], scalar1=s2[:, 12 + h:13 + h], scalar2=None, op0=ALU.mult),
                          r=[r_kr[b], r_sm2[b]], w=[r_krn[b]])
                bk, rb = nbank(4, 8)
                pv = bk[:].bitcast(BF16).rearrange("p (c n) -> p c n", n=128)
                sc.pe([(lambda e, h=h: e.transpose(out=pv[:, h, :], in_=kn_b[b][:, h, :], identity=ident_bf[:])) for h in range(4)]
                      + [(lambda e, pr=pr: e.transpose(out=pv[:, 4 + pr, :], in_=krn_b[b][:, 2 * pr:2 * pr + 2, :].rearrange("p a d -> p (a d)"),
                                                       identity=ident_bf[:])) for pr in range(2)],
                      r=[r_knb[b], r_krn[b], r_const], w=[rb])
                sc.op("act", lambda e: e.copy(out=KTn[:, :, ts_], in_=pv[:, 0:4, :]), r=[rb], w=[r_KT[t]])
                sc.op("dve", lambda e: e.tensor_copy(out=KTr[:, :, ts_], in_=pv[:, 4:6, :]), r=[rb], w=[r_KT[t]])
                yield
                sc.op("act", lambda e: e.activation(out=junk[:, 0:256], in_=cq_f[b][:], func=AF.Square, accum_out=s2[:, 20:21]), r=[r_cq[b]], w=[r_junk, r_sm2[b]])
                rstd_from_ss(s2[:, 20:21], 256.0, s2[:, 21:22], r_sm2[b], r_sm2[b], s2[:, 22:23], r_sm2[b])
                sc.op("pool", lambda e: e.tensor_copy(out=cq_b[b][:], in_=cq_f[b][:]), r=[r_cq[b]], w=[r_cqb[b]])
                bk, rb = nbank(4, 8)
                pv = bk[:].bitcast(BF16).rearrange("p (c n) -> p c n", n=128)
                sc.pe([(lambda e, c=c: e.transpose(out=pv[:, c, :], in_=cq_b[b][:, c * 128:(c + 1) * 128], identity=ident_bf[:])) for c in range(2)],
                      r=[r_cqb[b], r_const], w=[rb])
                sc.op("act", lambda e: e.copy(out=cqT[b][:], in_=pv[:, 0:2, :]), r=[rb], w=[r_cqT[b]])
                yield
                for hh in range(2):
                    bk, rb = nbank(4, 8)
                    sc.pe([(lambda e, c=c: e.matmul(bk[:, 0:384], lhsT=cqT[b][:, c, :], rhs=w_uq[:, c, hh * 384:(hh + 1) * 384], start=(c == 0), stop=(c == 1)))
                           for c in range(2)], r=[r_cqT[b], r_w2], w=[rb])
                    sc.op("act", lambda e: e.activation(out=q_f[bx][:, 2 * hh:2 * hh + 2, :], in_=bk[:, 0:384].rearrange("p (h d) -> p h d", h=2),
                                                        func=AF.Copy, scale=s2[:, 21:22]), r=[rb, r_sm2[b]], w=[r_qf[bx]])
                yield
                jq = junk[:, 0:768].rearrange("p (h d) -> p h d", h=4)
                sc.op("dve", lambda e: e.tensor_tensor(out=jq, in0=q_f[bx][:], in1=q_f[bx][:], op=ALU.mult), r=[r_qf[bx]], w=[r_junk])
                sc.op("dve", lambda e: e.tensor_reduce(out=s2[:, 24:28], in_=jq, axis=AX.X, op=ALU.add), r=[r_junk], w=[r_sm2[b]])
                rstd_from_ss(s2[:, 24:28], 192.0, s2[:, 28:32], r_sm2[b], r_sm2[b], s2[:, 32:36], r_sm2[b])
                for h in range(4):
                    sc.op("dve", lambda e: e.scalar_tensor_tensor(out=qn_b[b][:, h, :], in0=q_f[bx][:, h, 0:128], scalar=s2[:, 28 + h:29 + h], in1=gq[:, 0:128],
                                                                  op0=ALU.mult, op1=ALU.mult), r=[r_qf[bx], r_sm2[b], r_const], w=[r_qnb[b]])
                yield
                qq = qr[b]
                gqB = gq[:, 128:192].unsqueeze(1).broadcast_to([128, 4, 64])
                sc.op("pool", lambda e: e.tensor_tensor(out=qq[:, 0, :, :], in0=q_f[bx][:, :, 128:192], in1=gqB, op=ALU.mult), r=[r_qf[bx], r_const], w=[r_qr[b]])
                cosM8 = SCm[:, t, 32:64].unsqueeze(1).broadcast_to([128, 8, 32])
                sinM8 = SCm[:, t, 0:32].unsqueeze(1).broadcast_to([128, 8, 32])
                q0 = qq[:, 0, :, :].rearrange("p h (two d) -> p (h two) d", two=2)
                qA = qq[:, 1, :, :].rearrange("p h (two d) -> p (h two) d", two=2)
                qB = qq[:, 2, :, :].rearrange("p h (two d) -> p (h two) d", two=2)
                sc.op("pool", lambda e: e.tensor_tensor(out=qA, in0=q0, in1=cosM8, op=ALU.mult), r=[r_qr[b], r_sc], w=[r_qr[b]])
                sc.op("pool", lambda e: e.tensor_tensor(out=qB, in0=q0, in1=sinM8, op=ALU.mult), r=[r_qr[b], r_sc], w=[r_qr[b]])
                sc.op("pool", lambda e: e.tensor_tensor(out=qq[:, 3, :, 0:32], in0=qq[:, 1, :, 0:32], in1=qq[:, 2, :, 32:64], op=ALU.subtract),
                      r=[r_qr[b]], w=[r_qr[b]])
                sc.op("pool", lambda e: e.tensor_tensor(out=qq[:, 3, :, 32:64], in0=qq[:, 1, :, 32:64], in1=qq[:, 2, :, 0:32], op=ALU.add),
                      r=[r_qr[b]], w=[r_qr[b]])
                yield
                rsB = s2[:, 28:32].unsqueeze(2).broadcast_to([128, 4, 64])
                sc.op("dve", lambda e: e.tensor_tensor(out=qrn_b[b][:], in0=qq[:, 3, :, :], in1=rsB, op=ALU.mult), r=[r_qr[b], r_sm2[b]], w=[r_qrn[b]])
                bk, rb = nbank(4, 8)
                pv = bk[:].bitcast(BF16).rearrange("p (c n) -> p c n", n=128)
                sc.pe([(lambda e, h=h: e.transpose(out=pv[:, h, :], in_=qn_b[b][:, h, :], identity=ident_bf[:])) for h in range(4)]
                      + [(lambda e, pr=pr: e.transpose(out=pv[:, 4 + pr, :], in_=qrn_b[b][:, 2 * pr:2 * pr + 2, :].rearrange("p a d -> p (a d)"),
                                                       identity=ident_bf[:])) for pr in range(2)],
                      r=[r_qnb[b], r_qrn[b], r_const], w=[rb])
                sc.op("act", lambda e: e.copy(out=QTn[qb][:, :, tl], in_=pv[:, 0:4, :]), r=[rb], w=[r_QT[qb]])
                sc.op("dve", lambda e: e.tensor_copy(out=QTr[qb][:, :, tl], in_=pv[:, 4:6, :]), r=[rb], w=[r_QT[qb]])

            def attention_block(i, qb):
                ab = a_b[i % 2]
                its = [(h, j) for h in range(4) for j in range(4 * i + 4)]

                def qk(h, j):
                    pair, hp = h // 2, h % 2
                    psl = slice(hp * 64, (hp + 1) * 64)
                    r0 = max(0, j - 4 * i)
                    n = 512 - r0 * 128
                    ks = slice(j * 128, (j + 1) * 128)
                    bk, rb = nbank(4, 8)
                    diag = j >= 4 * i
                    fq = [lambda e: e.matmul(bk[:, 0:n], lhsT=KTn[:, h, ks], rhs=QTn[qb][:, h, r0 * 128:512], start=True, stop=False),
                          lambda e: e.matmul(bk[:, 0:n], lhsT=KTr[psl, pair, ks], rhs=QTr[qb][psl, pair, r0 * 128:512], start=False, stop=not diag)]
                    if diag:
                        fq.append(lambda e: e.matmul(bk[:, 0:128], lhsT=ident_bf[:], rhs=negm[:], start=False, stop=True))
                    sc.pe(fq, r=[r_KT[j], r_QT[qb], r_const], w=[rb])
                    pi = (h * 64 + j) % 3
                    pt, rpt = PTt[pi], r_PTt[pi]
                    sc.op("act", lambda e: e.activation(out=pt[:, 0:n], in_=bk[:, 0:n], func=AF.Exp, scale=SCALE), r=[rb], w=[rpt])
                    return pt, rpt, r0

                def pv_(h, j, pt, rpt, r0):
                    fns = []
                    for s in range(r0, 4):
                        fns.append(lambda e, s=s: e.matmul(banks[s][:, 0:129], lhsT=pt[:, (s - r0) * 128:(s - r0 + 1) * 128], rhs=Vaug[:, j, h, :],
                                                           start=(j == 0), stop=(j == 4 * i + s)))
                    sc.pe(fns, r=[rpt, r_V, r_KT[j]], w=[bank_res[s] for s in range(r0, 4)])
                    if j >= 4 * i:
                        s = j - 4 * i
                        sc.op("dve", lambda e: e.reciprocal(out=rcp[i % 2][:, s:s + 1], in_=banks[s][:, 128:129]), r=[bank_res[s]], w=[r_rcp[i % 2]])
                        sc.op("dve", lambda e: e.tensor_scalar(out=ab[:, s, h * 128:(h + 1) * 128], in0=banks[s][:, 0:128], scalar1=rcp[i % 2][:, s:s + 1],
                                                               scalar2=None, op0=ALU.mult), r=[bank_res[s], r_rcp[i % 2]], w=[r_ab[i % 2]])

                pend = []
                ystep = max(1, len(its) // 30)
                for k_, (h, j) in enumerate(its):
                    pend.append((h, j) + qk(h, j))
                    if len(pend) > 1:
                        pv_(*pend.pop(0))
                    if k_ % ystep == ystep - 1:
                        yield
                while pend:
                    pv_(*pend.pop(0))

            def out_tile(t):
                i, s = t // 4, t % 4
                ab = a_b[i % 2]
                if True:
                    b = t % 2
                    ts_ = slice(t * 128, (t + 1) * 128)
                    sc.dma("sp", lambda e: e.dma_start(out=r_b[b][:], in_=rout_d[ts_, :]), w=[r_rb[b]])
                    sc.dma("sp", lambda e: e.dma_start(out=x1t[b][:], in_=x_d[ts_, :]), w=[r_x1t[b]])
                    yield
                    bk, rb = nbank(4, 8)
                    pv = bk[:].bitcast(BF16).rearrange("p (c n) -> p c n", n=128)
                    sc.pe([(lambda e, c=c: e.transpose(out=pv[:, c, :], in_=ab[:, s, c * 128:(c + 1) * 128], identity=ident_bf[:])) for c in range(4)]
                          + [(lambda e, c=c: e.transpose(out=pv[:, 4 + c, :], in_=r_b[b][:, c * 128:(c + 1) * 128], identity=ident_bf[:])) for c in range(4)],
                          r=[r_ab[i % 2], r_rb[b], r_const], w=[rb])
                    sc.op("dve", lambda e: e.tensor_copy(out=aoT[b][:], in_=pv), r=[rb], w=[r_aoT[b]])
                    yield
                    for hh in range(2):
                        bk, rb = nbank(4, 8)
                        sc.pe([(lambda e, c=c: e.matmul(bk[:], lhsT=aoT[b][:, c, :], rhs=w_out[:, c, hh * 512:(hh + 1) * 512], start=(c == 0), stop=(c == 7)))
                               for c in range(8)], r=[r_aoT[b], r_w2], w=[rb])
                        sc.op("dve", lambda e: e.tensor_tensor(out=x1t[b][:, hh * 512:(hh + 1) * 512], in0=bk[:], in1=x1t[b][:, hh * 512:(hh + 1) * 512], op=ALU.add),
                              r=[rb], w=[r_x1t[b]])
                    r_x1d = sc.res("x1d")
                    sc.dma("sp", lambda e: e.dma_start(out=x1_d[ts_, :], in_=x1t[b][:]), r=[r_x1t[b]], w=[r_x1d])

            def drive(gens):
                gens = list(gens)
                while gens:
                    for g in list(gens):
                        try:
                            next(g)
                        except StopIteration:
                            gens.remove(g)

            x1t = xt
            r_x1t = r_xt
            run_pipelined(lambda t: prep_tile(t, 0), 4, 3)
            for i in range(NB):
                qb = i % 2

                def side(i=i):
                    if i + 1 < NB:
                        act_ = []
                        nx = 4 * (i + 1)
                        while nx < 4 * (i + 2) or act_:
                            if nx < 4 * (i + 2) and len(act_) < 3:
                                act_.append(prep_tile(nx, (i + 1) % 2))
                                nx += 1
                            for g in list(act_):
                                try:
                                    next(g)
                                except StopIteration:
                                    act_.remove(g)
                            yield

                drive([attention_block(i, qb), side()])
                run_pipelined(lambda k_, i=i: out_tile(4 * i + k_), 4, 2)
            if "x1" in dbg:
                sc.barrier()
                t_ = nc.dram_tensor("dbg_x1", [S, D], F32, kind="ExternalOutput").ap()
                dbg_out["x1"] = t_
                sc.dma("sp", lambda e: e.dma_start(out=t_, in_=x1_d), is_out=True)
            dump("KTn", KTn[:], r_KT[NT - 1], [128, 4, S], BF16)
            dump("KTr", KTr[:], r_KT[NT - 1], [128, 2, S], BF16)
            dump("Vaug", Vaug[:], r_V, [128, NT, 4, 129], BF16)
            sc.barrier()
            p2s.close()

        if last_phase >= 3:
            p36 = es.enter_context(ExitStack())
            lg_all = sb("lg_all", [128, NT, 36], F32, p36)
            r_lg = sc.res("lg_all")
            pos12 = sb("pos12", [128, 2, NT], I32, p36)
            w12 = sb("w12", [128, 2, NT], F32, p36)
            widx = sb("widx", [128, NTS], I32, p36)
            r_route = sc.res("route")
            b_rt = bload("b_rt", b_rt_d, 36, p36)

            p3s = es.enter_context(ExitStack())
            cw_q = sb("cw_q", [128, 8, 1024], BF16, p3s)
            cw_o = sb("cw_o", [128, 8, 1024], BF16, p3s)
            KcT = sb("KcT", [128, 4, 2, 256], BF16, p3s)
            Vc = sb("Vc", [128, 2, 4, 257], BF16, p3s)
            mg = bload("mg", mg_d, 1024, p3s)
            cqg = bload("cqg", cqg_d, 256, p3s)
            ckg = bload("ckg", ckg_d, 256, p3s)
            stage[:] = [sb("stage3_%d" % i, [128, 1024], F32, p3s) for i in range(2)]
            g_cross = pload("g_cross", g_cross_d, 8, p3s)
            g_mem = pload("g_mem", g_mem_d, 8, p3s)
            w_rt = sb("w_rt", [128, 8, 36], F32, p3s)
            r_w3 = sc.res("w3")
            sc.dma("sp", lambda e: e.dma_start(out=w_rt[:], in_=w_rt_d[:, :, :]), w=[r_w3])
            load_scaled(lambda c, a, b_: cw_q[:, c, a:b_], lambda c, a, b_: cw_q_d[:, c, a:b_], lambda c: g_cross[:, c:c + 1], 8, 1024, r_w3)
            load_cast(lambda c: cw_o[:, c, :], lambda c: cw_o_d[:, c, :], 8, r_w3)
            r_kvc = sc.res("kvc")
            sc.op("pool", lambda e: e.memset(Vc[:], 1.0), w=[r_kvc])

            pm = es.enter_context(ExitStack())
            cw_kv = sb("cw_kv", [128, 8, 2048], BF16, pm)
            r_cwkv = sc.res("cw_kv")
            load_scaled(lambda c, a, b_: cw_kv[:, c, a:b_], lambda c, a, b_: cw_kv_d[:, c, a:b_], lambda c: g_mem[:, c:c + 1], 8, 2048, r_cwkv)
            m_f = sb("m_f", [128, 1024], F32, pm)
            m_b = sb("m_b", [128, 1024], BF16, pm)
            m_T = sb("m_T", [128, 8, 128], BF16, pm)
            kc_f = sb("kc_f", [128, 4, 256], F32, pm)
            kc_sq = sb("kc_sq", [128, 4, 256], F32, pm)
            kc_b = sb("kc_b", [128, 4, 256], BF16, pm)
            sm = sb("sm0", [128, 16], F32, pm)
            r_m = sc.res("m")
            r_sm = sc.res("sm0")
            for mt in range(2):
                sc.dma("sp", lambda e: e.dma_start(out=m_f[:], in_=mem_d[mt * 128:(mt + 1) * 128, :]), w=[r_m])
                sc.op("act", lambda e: e.activation(out=m_b[:], in_=m_f[:], func=AF.Square, accum_out=sm[:, 0:1]), r=[r_m], w=[r_m, r_sm])
                rstd_from_ss(sm[:, 0:1], 1024.0, sm[:, 1:2], r_sm, r_sm, sm[:, 2:3], r_sm)
                sc.op("pool", lambda e: e.tensor_copy(out=m_b[:], in_=m_f[:]), r=[r_m], w=[r_m])
                bk, rb = nbank()
                pv = bk[:].bitcast(BF16).rearrange("p (c n) -> p c n", n=128)
                sc.pe([(lambda e, c=c: e.transpose(out=pv[:, c, :], in_=m_b[:, c * 128:(c + 1) * 128], identity=ident_bf[:])) for c in range(8)],
                      r=[r_m, r_const], w=[rb])
                sc.op("dve", lambda e: e.tensor_copy(out=m_T[:], in_=pv), r=[rb], w=[r_m])
                for nchunk in range(4):
                    bk, rb = nbank()
                    sc.pe([(lambda e, c=c: e.matmul(bk[:], lhsT=m_T[:, c, :], rhs=cw_kv[:, c, nchunk * 512:(nchunk + 1) * 512],
                                                    start=(c == 0), stop=(c == 7))) for c in range(8)], r=[r_m, r_cwkv], w=[rb])
                    if nchunk < 2:
                        sc.op("act", lambda e: e.activation(out=kc_f[:, 2 * nchunk:2 * nchunk + 2, :], in_=bk[:].rearrange("p (h d) -> p h d", h=2),
                                                            func=AF.Copy, scale=sm[:, 1:2]), r=[rb, r_sm], w=[r_m])
                    else:
                        hh = 2 * (nchunk - 2)
                        sc.op("act", lambda e: e.activation(out=Vc[:, mt, hh:hh + 2, 0:256], in_=bk[:].rearrange("p (h d) -> p h d", h=2),
                                                            func=AF.Copy, scale=sm[:, 1:2]), r=[rb, r_sm], w=[r_kvc])
                sc.op("dve", lambda e: e.tensor_tensor(out=kc_sq[:], in0=kc_f[:], in1=kc_f[:], op=ALU.mult), r=[r_m], w=[r_m])
                sc.op("dve", lambda e: e.tensor_reduce(out=sm[:, 4:8], in_=kc_sq[:], axis=AX.X, op=ALU.add), r=[r_m], w=[r_sm])
                rstd_from_ss(sm[:, 4:8], 256.0, sm[:, 8:12], r_sm, r_sm, sm[:, 12:16], r_sm)
                for h in range(4):
                    sc.op("dve", lambda e: e.scalar_tensor_tensor(out=kc_b[:, h, :], in0=kc_f[:, h, :], scalar=sm[:, 8 + h:9 + h], in1=ckg[:],
                                                                  op0=ALU.mult, op1=ALU.mult), r=[r_m, r_sm, r_const], w=[r_m])
                bk, rb = nbank()
                pv = bk[:].bitcast(BF16).rearrange("p (h c n) -> p h c n", h=4, c=2)
                sc.pe([(lambda e, h=h, c=c: e.transpose(out=pv[:, h, c, :], in_=kc_b[:, h, c * 128:(c + 1) * 128], identity=ident_bf[:]))
                       for h in range(4) for c in range(2)], r=[r_m, r_const], w=[rb])
                sc.op("dve", lambda e: e.tensor_copy(out=KcT[:, :, :, mt * 128:(mt + 1) * 128], in_=pv), r=[rb], w=[r_kvc])
            dump("KcT", KcT[:], r_kvc, [128, 4, 2, 256], BF16)
            dump("Vc", Vc[:], r_kvc, [128, 2, 4, 257], BF16)
            sc.barrier()
            pm.close()

            x1t = [sb("x3t%d" % i, [128, 1024], F32, p3s) for i in range(3)]
            xb = [sb("x3b%d" % i, [128, 1024], BF16, p3s) for i in range(2)]
            hcT = [sb("hcT%d" % i, [128, 8, 128], BF16, p3s) for i in range(2)]
            junk = sb("junk3", [128, 1024], F32, p3s)
            qc_f = sb("qc_f", [128, 4, 256], F32, p3s)
            qc_b = sb("qc_b", [128, 4, 256], BF16, p3s)
            qcT = [sb("qcT%d" % i, [128, 4, 2, 128], BF16, p3s) for i in range(2)]
            PTc = [sb("PTc%d" % i, [128, 8, 128], BF16, p3s) for i in range(2)]
            oc_b = sb("oc_b", [128, 4, 256], BF16, p3s)
            ocT = [sb("ocT%d" % i, [128, 8, 128], BF16, p3s) for i in range(2)]
            hm_f = [sb("hm_f%d" % i, [128, 1024], F32, p3s) for i in range(2)]
            hm_b = [sb("hm_b%d" % i, [128, 1024], BF16, p3s) for i in range(2)]
            hmT_f = sb("hmT_f", [128, 8, 128], F32, p3s)
            sm3 = [sb("sm3_%d" % i, [128, 32], F32, p3s) for i in range(3)]
            r_x1t, r_xb, r_hcT = mkres("x3t", 3), mkres("x3b"), mkres("hcT")
            r_junk = sc.res("junk3")
            r_qcf, r_qcb = sc.res("qcf"), sc.res("qcb")
            r_qcT, r_PTc, r_ocT, r_hmf, r_hmb, r_sm3 = mkres("qcT"), mkres("PTc"), mkres("ocT"), mkres("hmf"), mkres("hmb"), mkres("sm3", 3)
            r_ocb = sc.res("ocb")
            r_hmT = sc.res("hmT")
            r_x2d = [sc.res("x2d%d" % t) for t in range(NT)]
            r_hmd = [sc.res("hmd%d" % t) for t in range(NT)]
            CSCALE = 256.0 ** -0.5

            def p3_tile(t):
                b = t % 2
                b3 = t % 3
                ts_ = slice(t * 128, (t + 1) * 128)
                s3 = sm3[b3]
                sc.dma("sp", lambda e: e.dma_start(out=x1t[b3][:], in_=x1_d[ts_, :]), w=[r_x1t[b3]])
                sc.op("act", lambda e: e.activation(out=junk[:], in_=x1t[b3][:], func=AF.Square, accum_out=s3[:, 0:1]), r=[r_x1t[b3]], w=[r_junk, r_sm3[b3]])
                rstd_from_ss(s3[:, 0:1], 1024.0, s3[:, 1:2], r_sm3[b3], r_sm3[b3], s3[:, 2:3], r_sm3[b3])
                sc.op("act", lambda e: e.copy(out=xb[b][:], in_=x1t[b3][:]), r=[r_x1t[b3]], w=[r_xb[b]])
                yield
                bk, rb = nbank()
                pv = bk[:].bitcast(BF16).rearrange("p (c n) -> p c n", n=128)
                sc.pe([(lambda e, c=c: e.transpose(out=pv[:, c, :], in_=xb[b][:, c * 128:(c + 1) * 128], identity=ident_bf[:])) for c in range(8)],
                      r=[r_xb[b], r_const], w=[rb])
                sc.op("dve", lambda e: e.tensor_copy(out=hcT[b][:], in_=pv), r=[rb], w=[r_hcT[b]])
                yield
                for hh in range(2):
                    bk, rb = nbank()
                    sc.pe([(lambda e, c=c: e.matmul(bk[:], lhsT=hcT[b][:, c, :], rhs=cw_q[:, c, hh * 512:(hh + 1) * 512], start=(c == 0), stop=(c == 7)))
                           for c in range(8)], r=[r_hcT[b], r_w3], w=[rb])
                    sc.op("act", lambda e: e.activation(out=qc_f[:, 2 * hh:2 * hh + 2, :], in_=bk[:].rearrange("p (h d) -> p h d", h=2), func=AF.Copy,
                                                        scale=s3[:, 1:2]), r=[rb, r_sm3[b3]], w=[r_qcf])
                yield
                jq = junk[:].rearrange("p (h d) -> p h d", h=4)
                sc.op("dve", lambda e: e.tensor_tensor(out=jq, in0=qc_f[:], in1=qc_f[:], op=ALU.mult), r=[r_qcf], w=[r_junk])
                sc.op("dve", lambda e: e.tensor_reduce(out=s3[:, 4:8], in_=jq, axis=AX.X, op=ALU.add), r=[r_junk], w=[r_sm3[b3]])
                rstd_from_ss(s3[:, 4:8], 256.0, s3[:, 8:12], r_sm3[b3], r_sm3[b3], s3[:, 12:16], r_sm3[b3])
                for h in range(4):
                    sc.op("dve", lambda e: e.scalar_tensor_tensor(out=qc_b[:, h, :], in0=qc_f[:, h, :], scalar=s3[:, 8 + h:9 + h], in1=cqg[:],
                                                                  op0=ALU.mult, op1=ALU.mult), r=[r_qcf, r_sm3[b3], r_const], w=[r_qcb])
                yield
                bk, rb = nbank()
                pv = bk[:].bitcast(BF16).rearrange("p (h c n) -> p h c n", h=4, c=2)
                sc.pe([(lambda e, h=h, c=c: e.transpose(out=pv[:, h, c, :], in_=qc_b[:, h, c * 128:(c + 1) * 128], identity=ident_bf[:]))
                       for h in range(4) for c in range(2)], r=[r_qcb, r_const], w=[rb])
                sc.op("act", lambda e: e.copy(out=qcT[b][:], in_=pv), r=[rb], w=[r_qcT[b]])
                yield
                for hp in range(2):
                    bk, rb = nbank()
                    sv_ = bk[:].rearrange("p (a n) -> p a n", a=4)
                    fns = []
                    for hl in range(2):
                        h = 2 * hp + hl
                        for mc in range(2):
                            for dc in range(2):
                                fns.append(lambda e, h=h, mc=mc, dc=dc, hl=hl: e.matmul(sv_[:, hl * 2 + mc, :], lhsT=KcT[:, h, dc, mc * 128:(mc + 1) * 128],
                                                                                        rhs=qcT[b][:, h, dc, :], start=(dc == 0), stop=(dc == 1)))
                    sc.pe(fns, r=[r_kvc, r_qcT[b]], w=[rb])
                    sc.op("act", lambda e: e.activation(out=PTc[b][:, 4 * hp:4 * hp + 4, :], in_=sv_, func=AF.Exp, scale=CSCALE), r=[rb], w=[r_PTc[b]])
                yield
                for h in range(4):
                    bk, rb = nbank()
                    sc.pe([(lambda e, mc=mc: e.matmul(bk[:, 0:257], lhsT=PTc[b][:, 2 * h + mc, :], rhs=Vc[:, mc, h, :], start=(mc == 0), stop=(mc == 1)))
                           for mc in range(2)], r=[r_PTc[b], r_kvc], w=[rb])
                    sc.op("dve", lambda e: e.reciprocal(out=s3[:, 16 + h:17 + h], in_=bk[:, 256:257]), r=[rb], w=[r_sm3[b3]])
                    sc.op("dve", lambda e: e.tensor_scalar(out=oc_b[:, h, :], in0=bk[:, 0:256], scalar1=s3[:, 16 + h:17 + h], scalar2=None, op0=ALU.mult),
                          r=[rb, r_sm3[b3]], w=[r_ocb])
                yield
                bk, rb = nbank()
                pv = bk[:].bitcast(BF16).rearrange("p (c n) -> p c n", n=128)
                ocf = oc_b[:].rearrange("p h d -> p (h d)")
                sc.pe([(lambda e, c=c: e.transpose(out=pv[:, c, :], in_=ocf[:, c * 128:(c + 1) * 128], identity=ident_bf[:])) for c in range(8)],
                      r=[r_ocb, r_const], w=[rb])
                sc.op("act", lambda e: e.copy(out=ocT[b][:], in_=pv), r=[rb], w=[r_ocT[b]])
                yield
                for hh in range(2):
                    bk, rb = nbank()
                    sc.pe([(lambda e, c=c: e.matmul(bk[:], lhsT=ocT[b][:, c, :], rhs=cw_o[:, c, hh * 512:(hh + 1) * 512], start=(c == 0), stop=(c == 7)))
                           for c in range(8)], r=[r_ocT[b], r_w3], w=[rb])
                    sc.op("dve", lambda e: e.tensor_tensor(out=x1t[b3][:, hh * 512:(hh + 1) * 512], in0=bk[:], in1=x1t[b3][:, hh * 512:(hh + 1) * 512], op=ALU.add),
                          r=[rb], w=[r_x1t[b3]])
                sc.dma("sp", lambda e: e.dma_start(out=out_d[ts_, :], in_=x1t[b3][:]), r=[r_x1t[b3]], w=[r_x2d[t]])
                yield
                sc.op("act", lambda e: e.activation(out=junk[:], in_=x1t[b3][:], func=AF.Square, accum_out=s3[:, 20:21]), r=[r_x1t[b3]], w=[r_junk, r_sm3[b3]])
                rstd_from_ss(s3[:, 20:21], 1024.0, s3[:, 21:22], r_sm3[b3], r_sm3[b3], s3[:, 22:23], r_sm3[b3])
                sc.op("dve", lambda e: e.scalar_tensor_tensor(out=hm_f[b][:], in0=x1t[b3][:], scalar=s3[:, 21:22], in1=mg[:], op0=ALU.mult, op1=ALU.mult),
                      r=[r_x1t[b3], r_sm3[b3], r_const], w=[r_hmf[b]])
                sc.op("pool", lambda e: e.tensor_copy(out=hm_b[b][:], in_=hm_f[b][:]), r=[r_hmf[b]], w=[r_hmb[b]])
                sc.dma("sp", lambda e: e.dma_start(out=hm_d[ts_, :], in_=hm_b[b][:]), r=[r_hmb[b]], w=[r_hmd[t]])
                yield
                bkA, rbA = nbank()
                bkB, rbB = nbank()
                sc.pe([(lambda e, c=c: e.transpose(out=(bkA if c < 4 else bkB)[:, (c % 4) * 128:(c % 4 + 1) * 128], in_=hm_f[b][:, c * 128:(c + 1) * 128],
                                                   identity=ident_f[:])) for c in range(8)], r=[r_hmf[b], r_const], w=[rbA, rbB])
                sc.op("dve", lambda e: e.tensor_copy(out=hmT_f[:, 0:4, :], in_=bkA[:].rearrange("p (c n) -> p c n", c=4)), r=[rbA], w=[r_hmT])
                sc.op("act", lambda e: e.copy(out=hmT_f[:, 4:8, :], in_=bkB[:].rearrange("p (c n) -> p c n", c=4)), r=[rbB], w=[r_hmT])
                yield
                bk, rb = nbank()
                sc.pe([(lambda e, c=c: e.matmul(bk[:, 0:36], lhsT=hmT_f[:, c, :], rhs=w_rt[:, c, :], start=(c == 0), stop=(c == 7))) for c in range(8)],
                      r=[r_hmT, r_w3], w=[rb])
                sc.op("act", lambda e: e.copy(out=lg_all[:, t, :], in_=bk[:, 0:36]), r=[rb], w=[r_lg])
            run_pipelined(p3_tile, NT, 3)
            dump("logits", lg_all[:], r_lg, [128, NT, 36])
            if "x2" in dbg:
                sc.barrier()
                t_ = nc.dram_tensor("dbg_x2", [S, D], F32, kind="ExternalOutput").ap()
                dbg_out["x2"] = t_
                sc.dma("sp", lambda e: e.dma_start(out=t_, in_=out_d), is_out=True)
                t2_ = nc.dram_tensor("dbg_hm", [S, D], BF16, kind="ExternalOutput").ap()
                dbg_out["hm"] = t2_
                sc.dma("sp", lambda e: e.dma_start(out=t2_, in_=hm_d), is_out=True)
            sc.barrier()
            p3s.close()

        if last_phase >= 4:
            p4s = es.enter_context(ExitStack())
            reg_npos = nc.gpsimd.to_reg(NPOS - 1)
            reg_ew = nc.gpsimd.to_reg(32 * 128 - 1)
            r4 = sc.res("r4")

            def T4(name, shape, dt=F32):
                return sb("r4_" + name, shape, dt, p4s)

            def V(fn, r=(), w=(), eng="dve"):
                sc.op(eng, fn, r=[r4, r_lg, r_const] + list(r), w=[r4] + list(w))

            L = lg_all
            GL = L[:, :, 0:4]
            EL = L[:, :, 4:36].rearrange("p t (g j) -> p t g j", g=4)
            gb, goh, ge = T4("gb", [128, NT, 4]), T4("goh", [128, NT, 4]), T4("ge", [128, NT, 4])
            gmax, gm, gsum, gnum, gw = (T4(n, [128, NT]) for n in ("gmax", "gm", "gsum", "gnum", "gw"))
            t48 = T4("t48", [128, NT, 4, 8])
            esel, bsel, eb, oh1, eb2, oh2, ex, t8 = (T4(n, [128, NT, 8]) for n in ("esel", "bsel", "eb", "oh1", "eb2", "oh2", "ex", "t8"))
            m1, m2, em, a1, a2, den, ff = (T4(n, [128, NT]) for n in ("m1", "m2", "em", "a1", "a2", "den", "ff"))
            OH1, OH2 = T4("OH1", [128, NT, 32]), T4("OH2", [128, NT, 32])
            C_bf = T4("C_bf", [128, NT, 32], BF16)
            TT, PP, cA, cB, base, tmp32 = (T4(n, [128, NT, 32]) for n in ("TT", "PP", "cA", "cB", "base", "tmp32"))
            npad_i = T4("npad_i", [128, 32], I32)
            npad, eA, eB, off = (T4(n, [128, 32]) for n in ("npad", "eA", "eB", "off"))
            posf = T4("posf", [128, 2, NT])
            tpos_i = T4("tpos_i", [128, NTS], I32)
            tpos_f, eid_f = T4("tpos_f", [128, NTS]), T4("eid_f", [128, NTS])
            cmp_ = T4("cmp", [128, NTS, 32])
            pidx_i = T4("pidx_i", [128, 1], I32)
            pidx_f = T4("pidx_f", [128, 1])

            def bc(ap2, n):
                return ap2.unsqueeze(2).broadcast_to([128, NT, n])

            bg = b_rt[:, 0:4].unsqueeze(1).broadcast_to([128, NT, 4])
            be = b_rt[:, 4:36].rearrange("p (g j) -> p g j", g=4).unsqueeze(1).broadcast_to([128, NT, 4, 8])
            V(lambda e: e.tensor_tensor(out=gb[:], in0=GL, in1=bg, op=ALU.add))
            V(lambda e: e.tensor_reduce(out=gmax[:], in_=gb[:], axis=AX.X, op=ALU.max))
            V(lambda e: e.tensor_tensor(out=goh[:], in0=gb[:], in1=bc(gmax[:], 4), op=ALU.is_equal))
            V(lambda e: e.tensor_reduce(out=gm[:], in_=GL, axis=AX.X, op=ALU.max))
            V(lambda e: e.tensor_tensor(out=ge[:], in0=GL, in1=bc(gm[:], 4), op=ALU.subtract))
            V(lambda e: e.activation(out=ge[:].rearrange("p t g -> p (t g)"), in_=ge[:].rearrange("p t g -> p (t g)"), func=AF.Exp), eng="act")
            V(lambda e: e.tensor_reduce(out=gsum[:], in_=ge[:], axis=AX.X, op=ALU.add))
            V(lambda e: e.tensor_tensor(out=gb[:], in0=goh[:], in1=ge[:], op=ALU.mult))
            V(lambda e: e.tensor_reduce(out=gnum[:], in_=gb[:], axis=AX.X, op=ALU.add))
            V(lambda e: e.reciprocal(out=gsum[:], in_=gsum[:]))
            V(lambda e: e.tensor_tensor(out=gw[:], in0=gnum[:], in1=gsum[:], op=ALU.mult))
            goh_b = goh[:].unsqueeze(3).broadcast_to([128, NT, 4, 8])
            V(lambda e: e.tensor_tensor(out=t48[:], in0=EL, in1=goh_b, op=ALU.mult))
            V(lambda e: e.tensor_reduce(out=esel[:], in_=t48[:].rearrange("p t g j -> p t j g"), axis=AX.X, op=ALU.add))
            V(lambda e: e.tensor_tensor(out=t48[:], in0=be, in1=goh_b, op=ALU.mult))
            V(lambda e: e.tensor_reduce(out=bsel[:], in_=t48[:].rearrange("p t g j -> p t j g"), axis=AX.X, op=ALU.add))
            V(lambda e: e.tensor_tensor(out=eb[:], in0=esel[:], in1=bsel[:], op=ALU.add))
            V(lambda e: e.tensor_reduce(out=m1[:], in_=eb[:], axis=AX.X, op=ALU.max))
            V(lambda e: e.tensor_tensor(out=oh1[:], in0=eb[:], in1=bc(m1[:], 8), op=ALU.is_equal))
            V(lambda e: e.scalar_tensor_tensor(out=eb2[:].rearrange("p t j -> p (t j)"), in0=oh1[:].rearrange("p t j -> p (t j)"), scalar=-1e30,
                                               in1=eb[:].rearrange("p t j -> p (t j)"), op0=ALU.mult, op1=ALU.add))
            V(lambda e: e.tensor_reduce(out=m2[:], in_=eb2[:], axis=AX.X, op=ALU.max))
            V(lambda e: e.tensor_tensor(out=oh2[:], in0=eb2[:], in1=bc(m2[:], 8), op=ALU.is_equal))
            V(lambda e: e.tensor_reduce(out=em[:], in_=esel[:], axis=AX.X, op=ALU.max))
            V(lambda e: e.tensor_tensor(out=ex[:], in0=esel[:], in1=bc(em[:], 8), op=ALU.subtract))
            V(lambda e: e.activation(out=ex[:].rearrange("p t j -> p (t j)"), in_=ex[:].rearrange("p t j -> p (t j)"), func=AF.Exp), eng="act")
            V(lambda e: e.tensor_tensor(out=t8[:], in0=oh1[:], in1=ex[:], op=ALU.mult))
            V(lambda e: e.tensor_reduce(out=a1[:], in_=t8[:], axis=AX.X, op=ALU.add))
            V(lambda e: e.tensor_tensor(out=t8[:], in0=oh2[:], in1=ex[:], op=ALU.mult))
            V(lambda e: e.tensor_reduce(out=a2[:], in_=t8[:], axis=AX.X, op=ALU.add))
            V(lambda e: e.tensor_tensor(out=den[:], in0=a1[:], in1=a2[:], op=ALU.add))
            V(lambda e: e.reciprocal(out=den[:], in_=den[:]))
            V(lambda e: e.tensor_tensor(out=ff[:], in0=den[:], in1=gw[:], op=ALU.mult))
            V(lambda e: e.tensor_tensor(out=w12[:, 0, :], in0=a1[:], in1=ff[:], op=ALU.mult), w=[r_route])
            V(lambda e: e.tensor_tensor(out=w12[:, 1, :], in0=a2[:], in1=ff[:], op=ALU.mult), w=[r_route])
            V(lambda e: e.tensor_tensor(out=OH1[:].rearrange("p t (g j) -> p t g j", g=4), in0=goh_b,
                                        in1=oh1[:].unsqueeze(2).broadcast_to([128, NT, 4, 8]), op=ALU.mult))
            V(lambda e: e.tensor_tensor(out=OH2[:].rearrange("p t (g j) -> p t g j", g=4), in0=goh_b,
                                        in1=oh2[:].unsqueeze(2).broadcast_to([128, NT, 4, 8]), op=ALU.mult))
            V(lambda e: e.tensor_tensor(out=C_bf[:], in0=OH1[:], in1=OH2[:], op=ALU.add))
            W = NT * 32
            Cf = C_bf[:].rearrange("p t e -> p (t e)")
            TTf = TT[:].rearrange("p t e -> p (t e)")
            PPf = PP[:].rearrange("p t e -> p (t e)")
            for c0 in range(0, W, 512):
                c1 = min(W, c0 + 512)
                bk, rb = nbank()
                sc.pe([lambda e: e.matmul(bk[:, 0:c1 - c0], lhsT=ones_bf[:], rhs=Cf[:, c0:c1], start=True, stop=True)], r=[r4, r_const], w=[rb])
                sc.op("act", lambda e: e.copy(out=TTf[:, c0:c1], in_=bk[:, 0:c1 - c0]), r=[rb], w=[r4])
                bk, rb = nbank()
                sc.pe([lambda e: e.matmul(bk[:, 0:c1 - c0], lhsT=tri[:], rhs=Cf[:, c0:c1], start=True, stop=True)], r=[r4, r_const], w=[rb])
                sc.op("act", lambda e: e.copy(out=PPf[:, c0:c1], in_=bk[:, 0:c1 - c0]), r=[rb], w=[r4])
            V(lambda e: e.tensor_copy(out=cA[:], in_=TT[:]))
            cur, nxt = cA, cB
            s_ = 1
            while s_ < NT:
                V(lambda e: e.tensor_tensor(out=nxt[:, s_:NT, :], in0=cur[:, s_:NT, :], in1=cur[:, 0:NT - s_, :], op=ALU.add))
                V(lambda e: e.tensor_copy(out=nxt[:, 0:s_, :], in_=cur[:, 0:s_, :]))
                cur, nxt = nxt, cur
                s_ *= 2
            incl = cur
            V(lambda e: e.tensor_scalar(out=npad[:], in0=incl[:, NT - 1, :], scalar1=float(TS - 1), scalar2=None, op0=ALU.add))
            V(lambda e: e.tensor_copy(out=npad_i[:], in_=npad[:]))
            V(lambda e: e.tensor_scalar(out=npad_i[:], in0=npad_i[:], scalar1=8, scalar2=8, op0=ALU.arith_shift_right, op1=ALU.logical_shift_left))
            V(lambda e: e.tensor_copy(out=npad[:], in_=npad_i[:]))
            V(lambda e: e.tensor_copy(out=eA[:], in_=npad[:]))
            cur2, nxt2 = eA, eB
            s_ = 1
            while s_ < 32:
                V(lambda e: e.tensor_tensor(out=nxt2[:, s_:32], in0=cur2[:, s_:32], in1=cur2[:, 0:32 - s_], op=ALU.add))
                V(lambda e: e.tensor_copy(out=nxt2[:, 0:s_], in_=cur2[:, 0:s_]))
                cur2, nxt2 = nxt2, cur2
                s_ *= 2
            endI = cur2
            V(lambda e: e.tensor_tensor(out=off[:], in0=endI[:], in1=npad[:], op=ALU.subtract))
            V(lambda e: e.tensor_tensor(out=base[:], in0=incl[:], in1=TT[:], op=ALU.subtract))
            V(lambda e: e.tensor_tensor(out=base[:], in0=base[:], in1=PP[:], op=ALU.add))
            V(lambda e: e.tensor_tensor(out=base[:], in0=base[:], in1=off[:].unsqueeze(1).broadcast_to([128, NT, 32]), op=ALU.add))
            V(lambda e: e.tensor_tensor(out=tmp32[:], in0=OH1[:], in1=base[:], op=ALU.mult))
            V(lambda e: e.tensor_reduce(out=posf[:, 0, :], in_=tmp32[:], axis=AX.X, op=ALU.add))
            V(lambda e: e.tensor_tensor(out=tmp32[:], in0=OH2[:], in1=base[:], op=ALU.mult))
            V(lambda e: e.tensor_reduce(out=posf[:, 1, :], in_=tmp32[:], axis=AX.X, op=ALU.add))
            V(lambda e: e.tensor_copy(out=pos12[:], in_=posf[:]), w=[r_route])
            V(lambda e: e.iota(tpos_i[:], pattern=[[TS, NTS]], base=0, channel_multiplier=0), eng="pool")
            V(lambda e: e.iota(pidx_i[:], pattern=[[0, 1]], base=0, channel_multiplier=1), eng="pool")
            V(lambda e: e.tensor_copy(out=tpos_f[:], in_=tpos_i[:]))
            V(lambda e: e.tensor_copy(out=pidx_f[:], in_=pidx_i[:]))
            V(lambda e: e.tensor_tensor(out=cmp_[:], in0=endI[:].unsqueeze(1).broadcast_to([128, NTS, 32]),
                                        in1=tpos_f[:].unsqueeze(2).broadcast_to([128, NTS, 32]), op=ALU.is_le))
            V(lambda e: e.tensor_reduce(out=eid_f[:], in_=cmp_[:], axis=AX.X, op=ALU.add))
            V(lambda e: e.tensor_scalar(out=eid_f[:], in0=eid_f[:], scalar1=31.0, scalar2=128.0, op0=ALU.min, op1=ALU.mult))
            V(lambda e: e.tensor_scalar(out=eid_f[:], in0=eid_f[:], scalar1=pidx_f[:, 0:1], scalar2=None, op0=ALU.add))
            V(lambda e: e.tensor_copy(out=widx[:], in_=eid_f[:]), w=[r_route])
            dump("pos12", pos12[:], r_route, [128, 2, NT], I32)
            dump("w12", w12[:], r_route, [128, 2, NT])
            dump("widx", widx[:], r_route, [128, NTS], I32)

            hsb = [sb("hsb%d" % i, [128, 1024], BF16, p4s) for i in range(2)]
            r_hsb = mkres("hsb")
            for t in range(NT):
                b = t % 2
                ts_ = slice(t * 128, (t + 1) * 128)
                sc.dma("sp", lambda e: e.dma_start(out=hsb[b][:], in_=hm_d[ts_, :]), r=[r_hmd[t]], w=[r_hsb[b]])
                for k in range(2):
                    sc.dma("pool", lambda e: e.indirect_dma_start(out=xs_d[:, :], out_offset=bass.IndirectOffsetOnAxis(ap=pos12[:, k, t:t + 1], axis=0),
                                                                  in_=hsb[b][:], in_offset=None, bounds_check=reg_npos, oob_is_err=False),
                           r=[r_hsb[b], r_route], w=[r_xs])
            sc.barrier()
            p4s.close()

        if last_phase >= 5:
            p5s = es.enter_context(ExitStack())
            NWB = 3
            wall = [sb("wall%d" % i, [128, 6144], BF16, p5s) for i in range(NWB)]
            wg = [w_[:, 0:2048] for w_ in wall]
            wu = [w_[:, 2048:4096] for w_ in wall]
            wd = [w_[:, 4096:6144] for w_ in wall]
            r_wg = mkres("wall", NWB)
            r_wu = r_wg
            r_wd = r_wg
            xrow = [sb("xrow%d" % i, [128, 1024], BF16, p5s) for i in range(4)]
            r_xrow = mkres("xrow", 4)
            XsT = [sb("XsT%d" % i, [128, 8, TS], BF16, p5s) for i in range(3)]
            r_XsT = mkres("XsT", 3)
            sa = [sb("sa%d" % i, [128, TS], F32, p5s) for i in range(2)]
            r_sa = mkres("sa")
            actT = [sb("actT%d" % i, [128, 2, TS], BF16, p5s) for i in range(3)]
            r_actT = mkres("actT", 3)
            yt = [sb("yt%d" % i, [128, 1024], F32, p5s) for i in range(3)]
            r_yt = mkres("yt", 3)
            r_ys = sc.res("ys_d")
            cnt5 = {"x": 0, "y": 0}
            def p5_tile(tp):
                wb = tp % NWB
                b = tp % 3
                sc.dma("pool", lambda e: e.indirect_dma_start(out=wall[wb][:], out_offset=None, in_=ewb_all[:, :],
                                                              in_offset=bass.IndirectOffsetOnAxis(ap=widx[:, tp:tp + 1], axis=0),
                                                              bounds_check=reg_ew, oob_is_err=False), r=[r_route, r_ewb], w=[r_wg[wb]])
                for s in range(TS // 128):
                    xi = (2 * tp + s) % 4
                    r0_ = tp * TS + s * 128
                    sc.dma("sp", lambda e: e.dma_start(out=xrow[xi][:], in_=xs_d[r0_:r0_ + 128, :]), r=[r_xs], w=[r_xrow[xi]])
                yield
                for s in range(TS // 128):
                    xi = (2 * tp + s) % 4
                    bk, rb = nbank()
                    pv = bk[:].bitcast(BF16).rearrange("p (c n) -> p c n", n=128)
                    sc.pe([(lambda e, c=c: e.transpose(out=pv[:, c, :], in_=xrow[xi][:, c * 128:(c + 1) * 128], identity=ident_bf[:])) for c in range(8)],
                          r=[r_xrow[xi], r_const], w=[rb])
                    sc.op("dve" if s % 2 == 0 else "act",
                          (lambda e: e.tensor_copy(out=XsT[b][:, :, s * 128:(s + 1) * 128], in_=pv)) if s % 2 == 0 else
                          (lambda e: e.copy(out=XsT[b][:, :, s * 128:(s + 1) * 128], in_=pv)), r=[rb], w=[r_XsT[b]])
                yield
                wgv = wg[wb].rearrange("p (c f) -> p c f", c=8)
                wuv = wu[wb].rearrange("p (c f) -> p c f", c=8)
                wdv = wd[wb].rearrange("p (c d) -> p c d", c=2)
                for fc in range(2):
                    bk, rb = nbank()
                    fns = [(lambda e, c=c: e.matmul(bk[:, 0:TS], lhsT=wgv[:, c, fc * 128:(fc + 1) * 128], rhs=XsT[b][:, c, :], start=(c == 0), stop=(c == 7)))
                           for c in range(8)]
                    fns += [(lambda e, c=c: e.matmul(bk[:, TS:2 * TS], lhsT=wuv[:, c, fc * 128:(fc + 1) * 128], rhs=XsT[b][:, c, :], start=(c == 0), stop=(c == 7)))
                            for c in range(8)]
                    sc.pe(fns, r=[r_wg[wb], r_wu[wb], r_XsT[b]], w=[rb])
                    sc.op("act", lambda e: e.activation(out=sa[fc][:], in_=bk[:, 0:TS], func=AF.Silu), r=[rb], w=[r_sa[fc]])
                    sc.op("dve", lambda e: e.tensor_tensor(out=actT[b][:, fc, :], in0=bk[:, TS:2 * TS], in1=sa[fc][:], op=ALU.mult),
                          r=[rb, r_sa[fc]], w=[r_actT[b]])
                    yield
                for s in range(TS // 128):
                    yi = cnt5["y"] % 3
                    cnt5["y"] += 1
                    for half in range(2):
                        bk, rb = nbank()
                        sc.pe([(lambda e, fc=fc: e.matmul(bk[:], lhsT=actT[b][:, fc, s * 128:(s + 1) * 128], rhs=wdv[:, fc, half * 512:(half + 1) * 512],
                                                          start=(fc == 0), stop=(fc == 1))) for fc in range(2)], r=[r_actT[b], r_wd[wb]], w=[rb])
                        if half == 0:
                            sc.op("act", lambda e: e.copy(out=yt[yi][:, 0:512], in_=bk[:]), r=[rb], w=[r_yt[yi]])
                        else:
                            sc.op("dve", lambda e: e.tensor_copy(out=yt[yi][:, 512:1024], in_=bk[:]), r=[rb], w=[r_yt[yi]])
                    r0_ = tp * TS + s * 128
                    sc.dma("sp", lambda e: e.dma_start(out=ys_d[r0_:r0_ + 128, :], in_=yt[yi][:]), r=[r_yt[yi]], w=[r_ys])
                    yield
            run_pipelined(p5_tile, NTS, 3)
            sc.barrier()
            p5s.close()

        if last_phase >= 6:
            p6s = es.enter_context(ExitStack())
            y1 = [sb("y1_%d" % i, [128, 1024], F32, p6s) for i in range(2)]
            y2 = [sb("y2_%d" % i, [128, 1024], F32, p6s) for i in range(2)]
            xo = [sb("xo_%d" % i, [128, 1024], F32, p6s) for i in range(2)]
            r_y1, r_y2, r_xo = mkres("y1"), mkres("y2"), mkres("xo")
            def p6_tile(t):
                b = t % 2
                ts_ = slice(t * 128, (t + 1) * 128)
                for k, (yy, ry) in enumerate(((y1[b], r_y1[b]), (y2[b], r_y2[b]))):
                    sc.dma("pool", lambda e: e.indirect_dma_start(out=yy[:], out_offset=None, in_=ys_d[:, :],
                                                                  in_offset=bass.IndirectOffsetOnAxis(ap=pos12[:, k, t:t + 1], axis=0),
                                                                  bounds_check=reg_npos, oob_is_err=False), r=[r_route, r_ys], w=[ry])
                sc.dma("sp", lambda e: e.dma_start(out=xo[b][:], in_=out_d[ts_, :]), r=[r_x2d[t]], w=[r_xo[b]])
                yield
                sc.op("dve", lambda e: e.scalar_tensor_tensor(out=xo[b][:], in0=y1[b][:], scalar=w12[:, 0, t:t + 1], in1=xo[b][:], op0=ALU.mult, op1=ALU.add),
                      r=[r_y1[b], r_route], w=[r_xo[b]])
                sc.op("pool", lambda e: e.scalar_tensor_tensor(out=xo[b][:], in0=y2[b][:], scalar=w12[:, 1, t:t + 1], in1=xo[b][:], op0=ALU.mult, op1=ALU.add),
                      r=[r_y2[b], r_route], w=[r_xo[b]]) if False else \
                    sc.op("dve", lambda e: e.scalar_tensor_tensor(out=xo[b][:], in0=y2[b][:], scalar=w12[:, 1, t:t + 1], in1=xo[b][:], op0=ALU.mult, op1=ALU.add),
                          r=[r_y2[b], r_route], w=[r_xo[b]])
                sc.dma("sp", lambda e: e.dma_start(out=out_d[ts_, :], in_=xo[b][:]), r=[r_xo[b]], w=[r_x2d[t]], is_out=True)
            run_pipelined(p6_tile, NT, 2)
            sc.barrier()
            p6s.close()

        sc.finish()
    print("program: %d instructions, %d waits" % (sc.n_inst, sc.n_wait))
    return nc, dbg_out


def _perm_rows(w, c):
    n = w.shape[1]
    return np.ascontiguousarray(w.reshape(c, 128, n).transpose(1, 0, 2))


def _consts():
    cst = np.zeros((128, NCST), np.float64)
    j64 = np.arange(64)
    j32 = np.arange(32)
    invR = 10000.0 ** (-j64 / 64.0) / (2 * np.pi)
    invM = 10000.0 ** (-j32 / 32.0) / (2 * np.pi)
    cst[:, 0:64] = invR
    cst[:, 64:128] = invR
    cst[:, 128:160] = invM
    cst[:, 160:192] = invM
    cst[:, 192 + 64:192 + 128] = 0.25
    cst[:, 192 + 160:192 + 192] = 0.25
    h = np.arange(4)
    lg = np.log(1.0 - np.exp2(-5.0 - h))
    p = np.arange(128)[:, None]
    cst[:, 384:388] = np.exp((p + 1.0) * lg[None, :])
    cst[:, 388:392] = np.exp(-(p + 1.0) * lg[None, :]) * (128.0 ** -0.5)
    cst[:, 392:904] = np.repeat(np.exp(128.0 * lg), 128)[None, :]
    return cst.astype(np.float32)


def make_in_maps(inputs, S, n_cores, last_phase=6):
    f = lambda a: np.ascontiguousarray(np.asarray(a), dtype=np.float32)
    l = 0
    shared = {
        "cst": _consts(),
        "w_in": _perm_rows(f(inputs["w_in"][l]), 8),
        "g_attn": np.ascontiguousarray(f(inputs["attn_norm_g"][l]).reshape(8, 128).T),
        "w_uq": _perm_rows(f(inputs["mla_w_uq"][l]), 2),
        "g_qn": np.ascontiguousarray(f(inputs["mla_q_norm_g"][l]).reshape(2, 128).T),
        "w_ukv": f(inputs["mla_w_ukv"][l]),
        "g_kvn": f(inputs["mla_kv_norm_g"][l]).reshape(128, 1),
        "gq": f(inputs["mla_q_qk_g"][l]).reshape(1, 192),
        "gk": f(inputs["mla_k_qk_g"][l]).reshape(1, 192),
        "gn": f(inputs["ret_gn_g"][l]).reshape(1, 512),
        "w_out": _perm_rows(f(inputs["w_out"][l]), 8),
        "g_cross": np.ascontiguousarray(f(inputs["cross_norm_g"][l]).reshape(8, 128).T),
        "g_mem": np.ascontiguousarray(f(inputs["mem_norm_g"][l]).reshape(8, 128).T),
        "cw_q": _perm_rows(f(inputs["cross_w_q"][l]), 8),
        "cw_kv": _perm_rows(f(inputs["cross_w_kv"][l]), 8),
        "cqg": f(inputs["cross_q_qk_g"][l]).reshape(1, 256),
        "ckg": f(inputs["cross_k_qk_g"][l]).reshape(1, 256),
        "cw_o": _perm_rows(f(inputs["cross_w_o"][l]), 8),
        "mg": f(inputs["moe_norm_g"][l]).reshape(1, 1024),
        "w_rt": _perm_rows(np.concatenate([f(inputs["router_w_group"][l]), f(inputs["router_w_expert"][l])], axis=1), 8),
        "b_rt": np.concatenate([f(inputs["router_b_group"][l]), f(inputs["router_b_expert"][l])]).reshape(1, 36),
        "ew_g": np.ascontiguousarray(f(inputs["expert_w_gate"][l]).reshape(32, 8, 128, 256).transpose(0, 2, 1, 3)).reshape(32 * 128, 2048),
        "ew_u": np.ascontiguousarray(f(inputs["expert_w_up"][l]).reshape(32, 8, 128, 256).transpose(0, 2, 1, 3)).reshape(32 * 128, 2048),
        "ew_d": np.ascontiguousarray(f(inputs["expert_w_down"][l]).reshape(32, 2, 128, 1024).transpose(0, 2, 1, 3)).reshape(32 * 128, 2048),
    }
    if last_phase < 5:
        for k in ("ew_g", "ew_u", "ew_d"):
            del shared[k]
    NT = S // 128
    maps = []
    for b in range(n_cores):
        m = dict(shared)
        m["x"] = f(inputs["x"][b])
        m["mem"] = f(inputs["mem"][b])
        m["pos"] = np.ascontiguousarray(np.asarray(inputs["positions"][b]).astype(np.int32).reshape(NT, 128).T)
        maps.append(m)
    return maps


def kernel(**inputs):
    B, S, _ = inputs["x"].shape
    nc, _ = build_program(S)
    maps = make_in_maps(inputs, S, B)
    res = run_bass_kernel_spmd(nc, maps, core_ids=list(range(B)))
    return np.stack([np.asarray(r["out"]) for r in res.results], axis=0).astype(np.float32)
```

```python
import math
from contextlib import ExitStack

import numpy as np
import concourse.bass as bass
import concourse.mybir as mybir
from concourse.bass_utils import run_bass_kernel_spmd

F32 = mybir.dt.float32
BF16 = mybir.dt.bfloat16
I32 = mybir.dt.int32
AF = mybir.ActivationFunctionType
ALU = mybir.AluOpType
AX = mybir.AxisListType

D = 1024
EPS = 1e-6
NDMA = 24
NCST = 904


class Res:
    __slots__ = ("name", "w", "rd", "excl")

    def __init__(self, name, excl=False):
        self.name = name
        self.w = None
        self.rd = []
        self.excl = excl


class Sched:
    ENGS = ("pe", "act", "dve", "pool", "sp")

    def __init__(self, nc, es):
        self.nc = nc
        self.eng = {"pe": nc.tensor, "act": nc.scalar, "dve": nc.vector, "pool": nc.gpsimd, "sp": nc.sync}
        self.sem = {e: es.enter_context(nc.semaphore("c_" + e)) for e in self.ENGS}
        self.cnt = {e: 0 for e in self.ENGS}
        self.waited = {e: {} for e in self.ENGS}
        self.dsem = {q: [es.enter_context(nc.semaphore("d_%s%d" % (q, i))) for i in range(NDMA)] for q in ("sp", "pool")}
        self.dcnt = {q: [0] * NDMA for q in ("sp", "pool")}
        self.dnext = {q: 0 for q in ("sp", "pool")}
        self.semname = {}
        self.out_tokens = []
        self.n_inst = 0
        self.n_wait = 0

    def res(self, name):
        return Res(name)

    def _wait(self, eng, tok):
        sem, val, src = tok
        if src == "pe" and eng == "pe":
            return
        k = id(sem)
        if self.waited[eng].get(k, 0) >= val:
            return
        self.eng[eng].wait_ge(sem, val)
        self.n_wait += 1
        self.waited[eng][k] = val

    def _deps(self, eng, r, w):
        w = list(w) + [x for x in r if x.excl]
        for x in r:
            if x.w is not None:
                self._wait(eng, x.w)
        for x in w:
            if x.w is not None:
                self._wait(eng, x.w)
            for t in x.rd:
                self._wait(eng, t)

    def _commit(self, tok, r, w):
        w = list(w) + [x for x in r if x.excl]
        r = [x for x in r if not x.excl]
        for x in r:
            x.rd = [t for t in x.rd if t[0] is not tok[0]] + [tok]
        for x in w:
            x.w = tok
            x.rd = []

    def op(self, eng, fn, r=(), w=()):
        self._deps(eng, r, w)
        inst = fn(self.eng[eng])
        self.cnt[eng] += 1
        self.n_inst += 1
        inst.then_inc(self.sem[eng], 1)
        tok = (self.sem[eng], self.cnt[eng], eng)
        self.waited[eng][id(self.sem[eng])] = max(self.waited[eng].get(id(self.sem[eng]), 0), 0)
        self._commit(tok, r, w)
        return tok

    def pe(self, fns, r=(), w=()):
        self._deps("pe", r, w)
        inst = None
        for fn in fns:
            inst = fn(self.eng["pe"])
            self.n_inst += 1
        self.cnt["pe"] += 1
        inst.then_inc(self.sem["pe"], 1)
        tok = (self.sem["pe"], self.cnt["pe"], "pe")
        self._commit(tok, r, w)
        return tok

    def dma(self, q, fn, r=(), w=(), is_out=False):
        self._deps(q, r, w)
        i = self.dnext[q] % NDMA
        self.dnext[q] += 1
        sem = self.dsem[q][i]
        if self.dcnt[q][i] > 0:
            self._wait(q, (sem, 16 * self.dcnt[q][i], "dma"))
        inst = fn(self.eng[q])
        self.n_inst += 1
        self.dcnt[q][i] += 1
        inst.then_inc(sem, 16)
        tok = (sem, 16 * self.dcnt[q][i], "dma")
        self._commit(tok, r, w)
        if is_out:
            self.out_tokens.append(tok)
        return tok

    def barrier(self):
        toks = [(self.sem[e], self.cnt[e], e) for e in self.ENGS if self.cnt[e] > 0]
        for q in ("sp", "pool"):
            for i in range(NDMA):
                if self.dcnt[q][i] > 0:
                    toks.append((self.dsem[q][i], 16 * self.dcnt[q][i], "dma"))
        for e in self.ENGS:
            for t in toks:
                if t[2] == e and e != "pe":
                    pass
                self._wait_force(e, t)

    def _wait_force(self, eng, tok):
        sem, val, src = tok
        k = id(sem)
        if self.waited[eng].get(k, 0) >= val:
            return
        self.eng[eng].wait_ge(sem, val)
        self.n_wait += 1
        self.waited[eng][k] = val

    def finish(self):
        for t in self.out_tokens:
            self._wait_force("sp", t)
        self.barrier()


def build_program(S, last_phase=6, dbg=()):
    NT = S // 128
    NB = S // 512
    nc = bass.Bass("TRN2", target_bir_lowering=False)

    def din(name, shape, dt=F32):
        return nc.dram_tensor(name, list(shape), dt, kind="ExternalInput").ap()

    x_d = din("x", [S, D])
    mem_d = din("mem", [256, D])
    pos_d = din("pos", [128, NT], I32)
    cst_d = din("cst", [128, NCST])
    w_in_d = din("w_in", [128, 8, 2496])
    g_attn_d = din("g_attn", [128, 8])
    w_uq_d = din("w_uq", [128, 2, 768])
    g_qn_d = din("g_qn", [128, 2])
    w_ukv_d = din("w_ukv", [128, 1024])
    g_kvn_d = din("g_kvn", [128, 1])
    gq_d = din("gq", [1, 192])
    gk_d = din("gk", [1, 192])
    gn_d = din("gn", [1, 512])
    w_out_d = din("w_out", [128, 8, 1024])
    g_cross_d = din("g_cross", [128, 8])
    g_mem_d = din("g_mem", [128, 8])
    cw_q_d = din("cw_q", [128, 8, 1024])
    cw_kv_d = din("cw_kv", [128, 8, 2048])
    cqg_d = din("cqg", [1, 256])
    ckg_d = din("ckg", [1, 256])
    cw_o_d = din("cw_o", [128, 8, 1024])
    mg_d = din("mg", [1, 1024])
    w_rt_d = din("w_rt", [128, 8, 36])
    b_rt_d = din("b_rt", [1, 36])
    if last_phase >= 5:
        ew_g_d = din("ew_g", [32 * 128, 2048])
        ew_u_d = din("ew_u", [32 * 128, 2048])
        ew_d_d = din("ew_d", [32 * 128, 2048])
    out_d = nc.dram_tensor("out", [S, D], F32, kind="ExternalOutput").ap()

    dbg_out = {}

    with ExitStack() as es:
        sc = Sched(nc, es)

        def sb(name, shape, dt=F32, stack=es):
            return stack.enter_context(nc.sbuf_tensor("s_" + name, list(shape), dt))

        banks = [es.enter_context(nc.psum_tensor("bank%d" % i, [128, 512], F32)) for i in range(8)]
        bank_res = [Res("bank%d" % i, excl=True) for i in range(8)]
        bstate = {"i": 0}

        def nbank():
            i = bstate["i"] % 8
            bstate["i"] += 1
            return banks[i], bank_res[i]

        def dump(name, ap, res, shape, dt=F32):
            if name not in dbg:
                return
            t = nc.dram_tensor("dbg_" + name, list(shape), dt, kind="ExternalOutput").ap()
            dbg_out[name] = t
            sc.dma("sp", lambda e: e.dma_start(out=t, in_=ap), r=[res], w=[], is_out=True)

        def nbank(lo=0, hi=8):
            key = (lo, hi)
            i = lo + bstate.get(key, 0) % (hi - lo)
            bstate[key] = bstate.get(key, 0) + 1
            return banks[i], bank_res[i]

        cst = sb("cst", [128, NCST])
        r_cst = sc.res("cst")
        sc.dma("sp", lambda e: e.dma_start(out=cst[:], in_=cst_d[:, :]), w=[r_cst])
        INVF = cst[:, 0:192]
        OFFS = cst[:, 192:384]
        QDc = cst[:, 384:388]
        KDc = cst[:, 388:392]
        CDEC = cst[:, 392:904]

        ident_bf = sb("ident_bf", [128, 128], BF16)
        ident_f = sb("ident_f", [128, 128], F32)
        maskT = sb("maskT", [128, 128], BF16)
        mask4 = sb("mask4", [128, 4, 128], F32)
        tri = sb("tri", [128, 128], BF16)
        ones_bf = sb("ones_bf", [128, 128], BF16)
        r_const = sc.res("consts")

        def mk_mask(t_ap, pattern, cmp):
            sc.op("pool", lambda e: e.memset(t_ap, 1.0), w=[r_const])
            sc.op("pool", lambda e: e.affine_select(out=t_ap, in_=t_ap, pattern=pattern, compare_op=cmp, fill=0.0,
                                                    base=0, channel_multiplier=-1), r=[r_const], w=[r_const])

        mk_mask(ident_bf[:], [[1, 128]], ALU.is_equal)
        mk_mask(ident_f[:], [[1, 128]], ALU.is_equal)
        mk_mask(maskT[:], [[1, 128]], ALU.is_ge)
        mk_mask(mask4[:], [[0, 4], [1, 128]], ALU.is_ge)
        mk_mask(tri[:], [[1, 128]], ALU.is_gt)
        negm = sb("negm", [128, 128], BF16)
        sc.op("pool", lambda e: e.memset(negm[:], -30000.0), w=[r_const])
        sc.op("pool", lambda e: e.affine_select(out=negm[:], in_=negm[:], pattern=[[-1, 128]], compare_op=ALU.is_gt, fill=0.0,
                                                base=0, channel_multiplier=1), r=[r_const], w=[r_const])
        sc.op("pool", lambda e: e.memset(ones_bf[:], 1.0), w=[r_const])

        def bload(name, src, n, stack=es):
            t = sb(name, [128, n], F32, stack)
            sc.dma("sp", lambda e: e.dma_start(out=t[:], in_=src.partition_broadcast(128)), w=[r_const])
            return t

        def pload(name, src, n, stack=es):
            t = sb(name, [128, n], F32, stack)
            sc.dma("sp", lambda e: e.dma_start(out=t[:], in_=src[:, :]), w=[r_const])
            return t

        gq = bload("gq", gq_d, 192)
        gk = bload("gk", gk_d, 192)
        gn = bload("gn", gn_d, 512)

        pos_i = sb("pos_i", [128, NT], I32)
        pos_f = sb("pos_f", [128, NT])
        SCm = sb("SCm", [128, NT, 64])
        rstd1 = sb("rstd1", [128, NT])
        r_sc = sc.res("SC")
        r_rstd1 = sc.res("rstd1")
        stage = [None, None]
        r_stage = [sc.res("stage%d" % i) for i in range(2)]
        st = {"i": 0}
        scale_engs = ("dve", "act")

        def load_scaled(dst_fn, src_fn, gain_fn, C, N, rdst):
            for c in range(C):
                for n0 in range(0, N, 1024):
                    n1 = min(N, n0 + 1024)
                    i = st["i"] % 2
                    st["i"] += 1
                    stg, rs = stage[i], r_stage[i]
                    sc.dma("sp", lambda e: e.dma_start(out=stg[:, 0:n1 - n0], in_=src_fn(c, n0, n1)), w=[rs])
                    if scale_engs[i] == "act":
                        sc.op("act", lambda e: e.activation(out=dst_fn(c, n0, n1), in_=stg[:, 0:n1 - n0], func=AF.Copy, scale=gain_fn(c)),
                              r=[rs, r_const], w=[rdst])
                    else:
                        sc.op("dve", lambda e: e.tensor_scalar(out=dst_fn(c, n0, n1), in0=stg[:, 0:n1 - n0], scalar1=gain_fn(c), scalar2=None,
                                                               op0=ALU.mult), r=[rs, r_const], w=[rdst])

        def load_cast(dst_fn, src_fn, C, rdst):
            for c in range(C):
                sc.dma("pool", lambda e: e.dma_start(out=dst_fn(c), in_=src_fn(c)), w=[rdst])

        mhalf = sb("mhalf", [128, 8])
        sc.op("pool", lambda e: e.memset(mhalf[:], -0.5), w=[r_const])

        def rstd_from_ss(ss_ap, n, out_ap, r_in, r_out, tmp_ap, r_tmp):
            k = ss_ap.shape[1]
            sc.op("dve", lambda e: e.tensor_scalar(out=tmp_ap, in0=ss_ap, scalar1=1.0 / n, scalar2=EPS, op0=ALU.mult, op1=ALU.add),
                  r=[r_in], w=[r_tmp])
            sc.op("pool", lambda e: e.tensor_tensor(out=out_ap, in0=tmp_ap, in1=mhalf[:, 0:k], op=ALU.pow), r=[r_tmp, r_const], w=[r_out])

        sc.dma("sp", lambda e: e.dma_start(out=pos_i[:], in_=pos_d[:, :]), w=[r_sc])
        sc.op("dve", lambda e: e.tensor_copy(out=pos_f[:], in_=pos_i[:]), r=[r_sc], w=[r_sc])

        rout_d = nc.dram_tensor("rout_s", [S, 512], BF16).ap()
        x1_d = nc.dram_tensor("x1_s", [S, D], F32).ap()
        hm_d = nc.dram_tensor("hm_s", [S, D], BF16).ap()
        TS = 256
        NTS = (2 * S) // TS + 32
        NPOS = NTS * TS
        xs_d = nc.dram_tensor("xs_s", [NPOS, D], BF16).ap()
        ys_d = nc.dram_tensor("ys_s", [NPOS, D], F32).ap()
        r_xs = sc.res("xs_d")

        def run_pipelined(gen_fn, n_items, depth):
            active = []
            nxt = 0
            while nxt < n_items or active:
                if nxt < n_items and len(active) < depth:
                    active.append(gen_fn(nxt))
                    nxt += 1
                for g in list(active):
                    try:
                        next(g)
                    except StopIteration:
                        active.remove(g)

        def mkres(n, k=2):
            return [sc.res("%s%d" % (n, i)) for i in range(k)]

        if last_phase >= 1:
            p1s = es.enter_context(ExitStack())
            import os as _os
            stop = int(_os.environ.get('P1_STOP', '99'))
            SCr = sb("SCr", [128, NT, 128], F32, p1s)
            w_r = sb("w_r", [128, 8, 2048], BF16, p1s)
            r_wr = sc.res("w_r")
            stage[:] = [sb("stage1_%d" % i, [128, 1024], F32, p1s) for i in range(2)]
            g_attn = pload("g_attn", g_attn_d, 8, p1s)
            load_scaled(lambda c, a, b_: w_r[:, c, a:b_], lambda c, a, b_: w_in_d[:, c, 448 + a:448 + b_], lambda c: g_attn[:, c:c + 1], 8, 2048, r_wr)

            NTsc = NT if stop >= 2 else 0
            tr_t = sb("tr_t", [128, 192], F32, p1s)
            tr_i = sb("tr_i", [128, 192], I32, p1s)
            tr_f = sb("tr_f", [128, 192], F32, p1s)
            r_tr = sc.res("tr")
            for t in range(NTsc):
                sc.op("dve", lambda e: e.scalar_tensor_tensor(out=tr_t[:], in0=INVF, scalar=pos_f[:, t:t + 1], in1=OFFS,
                                                              op0=ALU.mult, op1=ALU.add), r=[r_cst, r_sc], w=[r_tr])
                sc.op("dve", lambda e: e.tensor_copy(out=tr_i[:], in_=tr_t[:]), r=[r_tr], w=[r_tr])
                sc.op("dve", lambda e: e.tensor_copy(out=tr_f[:], in_=tr_i[:]), r=[r_tr], w=[r_tr])
                sc.op("dve", lambda e: e.tensor_tensor(out=tr_t[:], in0=tr_t[:], in1=tr_f[:], op=ALU.subtract), r=[r_tr], w=[r_tr])
                sc.op("dve", lambda e: e.scalar_tensor_tensor(out=tr_f[:], in0=tr_t[:], scalar=0.5, in1=tr_t[:],
                                                              op0=ALU.is_gt, op1=ALU.subtract), r=[r_tr], w=[r_tr])
                sc.op("dve", lambda e: e.scalar_tensor_tensor(out=tr_t[:], in0=tr_f[:], scalar=0.5, in1=tr_f[:],
                                                              op0=ALU.is_gt, op1=ALU.subtract), r=[r_tr], w=[r_tr])
                sc.op("act", lambda e: e.activation(out=SCr[:, t, :], in_=tr_t[:, 0:128], func=AF.Sin, scale=6.28318), r=[r_tr], w=[r_sc])
                sc.op("act", lambda e: e.activation(out=SCm[:, t, :], in_=tr_t[:, 128:192], func=AF.Sin, scale=6.28318), r=[r_tr], w=[r_sc])
            dump("SCr", SCr[:], r_sc, [128, NT, 128])
            dump("SCm", SCm[:], r_sc, [128, NT, 64])

            do_pc = last_phase >= 5
            if do_pc:
                ewb_all = nc.dram_tensor("ewb_all", [32 * 128, 6144], BF16).ap()
                ewb_d = [ewb_all[:, k * 2048:(k + 1) * 2048] for k in range(3)]
                ew_src = [ew_g_d, ew_u_d, ew_d_d]
                r_ewb = sc.res("ewb")
                pcs = [sb("pcs%d" % i, [128, 2048], BF16, p1s) for i in range(3)]
                r_pcs = mkres("pcs", 3)
                pc_state = {"i": 0}
                PC_PER_TILE = (96 + NT - 1) // NT

                def precast_step():
                    for _ in range(PC_PER_TILE):
                        i = pc_state["i"]
                        if i >= 96:
                            return
                        pc_state["i"] += 1
                        e_, k_ = i // 3, i % 3
                        bi = i % 3
                        rows = slice(e_ * 128, (e_ + 1) * 128)
                        sc.dma("pool", lambda e: e.dma_start(out=pcs[bi][:], in_=ew_src[k_][rows, :]), w=[r_pcs[bi]])
                        sc.dma("sp", lambda e: e.dma_start(out=ewb_d[k_][rows, :], in_=pcs[bi][:]), r=[r_pcs[bi]], w=[r_ewb])

            zt = sb("zt", [128, 2, 1024], BF16, p1s)
            r_zt = sc.res("zt")
            sc.op("pool", lambda e: e.memset(zt[:], 0.0), w=[r_zt])
            ROWS_PER = NPOS // NT

            def zero_step(t):
                if last_phase < 4:
                    return
                for r0_ in range(t * ROWS_PER, (t + 1) * ROWS_PER, 256):
                    sc.dma("sp", lambda e: e.dma_start(out=xs_d[r0_:r0_ + 256, :].rearrange("(p a) d -> p a d", a=2), in_=zt[:]), r=[r_zt], w=[r_xs])

            xt = [sb("xt%d" % i, [128, 1024], F32, p1s) for i in range(4)]
            xb = [sb("xb%d" % i, [128, 1024], BF16, p1s) for i in range(4)]
            xT = [sb("xT%d" % i, [128, 8, 128], BF16, p1s) for i in range(4)]
            junk = sb("junk", [128, 1024], BF16, p1s)
            rq_f = [sb("rq_f%d" % i, [128, 512], F32, p1s) for i in range(4)]
            rk_f = [sb("rk_f%d" % i, [128, 512], F32, p1s) for i in range(4)]
            v_b = [sb("v_b%d" % i, [128, 512], BF16, p1s) for i in range(4)]
            sg = [sb("sg%d" % i, [128, 512], F32, p1s) for i in range(6)]
            sm1 = [sb("sm1_%d" % i, [128, 32], F32, p1s) for i in range(4)]
            rp = [sb("rp%d" % i, [128, 2, 512], F32, p1s) for i in range(2)]
            qp_b = [sb("qp_b%d" % i, [128, 512], BF16, p1s) for i in range(4)]
            kp_b = [sb("kp_b%d" % i, [128, 512], BF16, p1s) for i in range(4)]
            qpT = [sb("qpT%d" % i, [128, 4, 128], BF16, p1s) for i in range(4)]
            kpT = [sb("kpT%d" % i, [128, 4, 128], BF16, p1s) for i in range(4)]
            PT = [sb("PT%d" % i, [128, 4, 128], BF16, p1s) for i in range(3)]
            Tst = sb("Tst", [128, 4, 128], F32, p1s)
            Tst_b = sb("Tst_b", [128, 4, 128], BF16, p1s)
            Ttmp = sb("Ttmp", [128, 4, 128], F32, p1s)
            o_f = [sb("o_f%d" % i, [128, 4, 128], F32, p1s) for i in range(4)]
            bnst = [sb("bnst%d" % i, [128, 4, 6], F32, p1s) for i in range(4)]
            bnag = [sb("bnag%d" % i, [128, 4, 2], F32, p1s) for i in range(4)]
            ro_b = [sb("ro_b%d" % i, [128, 512], BF16, p1s) for i in range(3)]

            r_xt, r_xb, r_xT = mkres("xt", 4), mkres("xb", 4), mkres("xT", 4)
            r_rq, r_rk, r_vb, r_sg, r_sm1 = mkres("rq", 4), mkres("rk", 4), mkres("vb", 4), mkres("sg", 6), mkres("sm1", 4)
            r_rp = mkres("rp")
            r_qpb, r_kpb, r_qpT, r_kpT, r_PT, r_of, r_bn, r_rob = (mkres("qpb", 4), mkres("kpb", 4), mkres("qpT", 4), mkres("kpT", 4), mkres("PT", 3),
                                                                   mkres("of", 4), mkres("bn", 4), mkres("rob", 3))
            r_junk = sc.res("junk")
            r_T = sc.res("Tst")
            r_Tb = sc.res("Tst_b")
            r_Tt = sc.res("Ttmp")
            sc.op("dve", lambda e: e.memset(Tst[:], 0.0), w=[r_T])
            sc.op("dve", lambda e: e.memset(Tst_b[:], 0.0), w=[r_Tb])

            def rope_ret(eng, src, r_src, dst_b, r_dst, t, decay, scr, r_scr):
                cosB = SCr[:, t, 64:128].unsqueeze(1).broadcast_to([128, 8, 64])
                sinB = SCr[:, t, 0:64].unsqueeze(1).broadcast_to([128, 4, 64])
                sv = src[:].rearrange("p (h two d) -> p h two d", h=4, two=2)
                Pv = scr[:, 0, :]
                Qv = scr[:, 1, :].rearrange("p (h two d) -> p h two d", h=4, two=2)
                P4 = scr[:, 0, :].rearrange("p (h two d) -> p h two d", h=4, two=2)
                sc.op(eng, lambda e: e.tensor_tensor(out=Pv.rearrange("p (g d) -> p g d", g=8), in0=src[:].rearrange("p (g d) -> p g d", g=8),
                                                     in1=cosB, op=ALU.mult), r=[r_src, r_sc], w=[r_scr])
                sc.op(eng, lambda e: e.tensor_tensor(out=Qv[:, :, 0, :], in0=sv[:, :, 1, :], in1=sinB, op=ALU.mult), r=[r_src, r_sc], w=[r_scr])
                sc.op(eng, lambda e: e.tensor_tensor(out=Qv[:, :, 1, :], in0=sv[:, :, 0, :], in1=sinB, op=ALU.mult), r=[r_src, r_sc], w=[r_scr])
                sc.op(eng, lambda e: e.tensor_tensor(out=P4[:, :, 0, :], in0=P4[:, :, 0, :], in1=Qv[:, :, 0, :], op=ALU.subtract), r=[r_scr], w=[r_scr])
                sc.op(eng, lambda e: e.tensor_tensor(out=P4[:, :, 1, :], in0=P4[:, :, 1, :], in1=Qv[:, :, 1, :], op=ALU.add), r=[r_scr], w=[r_scr])
                decB = decay.unsqueeze(2).broadcast_to([128, 4, 128])
                sc.op(eng, lambda e: e.tensor_tensor(out=dst_b[:].rearrange("p (h d) -> p h d", h=4), in0=Pv.rearrange("p (h d) -> p h d", h=4),
                                                     in1=decB, op=ALU.mult), r=[r_scr, r_cst], w=[r_dst])

            def p1_tile(t):
                b = t % 4
                b6 = t % 6
                b3 = t % 3
                ts_ = slice(t * 128, (t + 1) * 128)
                s1 = sm1[b]
                sc.dma("sp", lambda e: e.dma_start(out=xt[b][:], in_=x_d[ts_, :]), w=[r_xt[b]])
                if do_pc:
                    precast_step()
                zero_step(t)
                sc.op("act", lambda e: e.activation(out=junk[:], in_=xt[b][:], func=AF.Square, accum_out=s1[:, 0:1]),
                      r=[r_xt[b]], w=[r_junk, r_sm1[b]])
                rstd_from_ss(s1[:, 0:1], 1024.0, rstd1[:, t:t + 1], r_sm1[b], r_rstd1, s1[:, 1:2], r_sm1[b])
                yield
                sc.op("act", lambda e: e.copy(out=xb[b][:], in_=xt[b][:]), r=[r_xt[b]], w=[r_xb[b]])
                bk, rb = nbank()
                pv = bk[:].bitcast(BF16).rearrange("p (c n) -> p c n", n=128)
                sc.pe([(lambda e, c=c: e.transpose(out=pv[:, c, :], in_=xb[b][:, c * 128:(c + 1) * 128], identity=ident_bf[:])) for c in range(8)],
                      r=[r_xb[b], r_const], w=[rb])
                sc.op("dve", lambda e: e.tensor_copy(out=xT[b][:], in_=pv), r=[rb], w=[r_xT[b]])
                rs1 = rstd1[:, t:t + 1]
                yield
                pb = []
                for k4 in range(4):
                    bk, rb = nbank()
                    sc.pe([(lambda e, c=c: e.matmul(bk[:], lhsT=xT[b][:, c, :], rhs=w_r[:, c, k4 * 512:(k4 + 1) * 512], start=(c == 0), stop=(c == 7)))
                           for c in range(8)], r=[r_xT[b], r_wr], w=[rb])
                    pb.append((bk, rb))
                sc.op("act", lambda e: e.activation(out=rq_f[b][:], in_=pb[0][0][:], func=AF.Copy, scale=rs1), r=[pb[0][1], r_rstd1], w=[r_rq[b]])
                sc.op("dve", lambda e: e.tensor_scalar(out=rk_f[b][:], in0=pb[1][0][:], scalar1=rs1, scalar2=None, op0=ALU.mult),
                      r=[pb[1][1], r_rstd1], w=[r_rk[b]])
                sc.op("act", lambda e: e.activation(out=v_b[b][:], in_=pb[2][0][:], func=AF.Copy, scale=rs1), r=[pb[2][1], r_rstd1], w=[r_vb[b]])
                sc.op("act", lambda e: e.activation(out=sg[b6][:], in_=pb[3][0][:], func=AF.Silu, scale=rs1), r=[pb[3][1], r_rstd1], w=[r_sg[b6]])

                yield
                rope_ret("dve", rq_f[b], r_rq[b], qp_b[b], r_qpb[b], t, QDc, rp[0], r_rp[0])
                rope_ret("pool", rk_f[b], r_rk[b], kp_b[b], r_kpb[b], t, KDc, rp[1], r_rp[1])
                yield
                bk, rb = nbank()
                pv = bk[:].bitcast(BF16).rearrange("p (c n) -> p c n", n=128)
                sc.pe([(lambda e, h=h: e.transpose(out=pv[:, h, :], in_=qp_b[b][:, h * 128:(h + 1) * 128], identity=ident_bf[:])) for h in range(4)]
                      + [(lambda e, h=h: e.transpose(out=pv[:, 4 + h, :], in_=kp_b[b][:, h * 128:(h + 1) * 128], identity=ident_bf[:])) for h in range(4)],
                      r=[r_qpb[b], r_kpb[b], r_const], w=[rb])
                sc.op("act", lambda e: e.copy(out=qpT[b][:], in_=pv[:, 0:4, :]), r=[rb], w=[r_qpT[b]])
                sc.op("dve", lambda e: e.tensor_copy(out=kpT[b][:], in_=pv[:, 4:8, :]), r=[rb], w=[r_kpT[b]])
                bk, rb = nbank()
                sv_ = bk[:].rearrange("p (h n) -> p h n", h=4)
                sc.pe([(lambda e, h=h: e.matmul(sv_[:, h, :], lhsT=kpT[b][:, h, :], rhs=qpT[b][:, h, :], start=True, stop=True)) for h in range(4)],
                      r=[r_kpT[b], r_qpT[b]], w=[rb])
                sc.op("dve", lambda e: e.tensor_tensor(out=PT[b3][:], in0=sv_, in1=mask4[:], op=ALU.mult), r=[rb, r_const], w=[r_PT[b3]])
                yield
                bk_o, rb_o = nbank()
                ov = bk_o[:].rearrange("p (h n) -> p h n", h=4)
                fns = []
                for h in range(4):
                    fns.append(lambda e, h=h: e.matmul(ov[:, h, :], lhsT=PT[b3][:, h, :], rhs=v_b[b][:, h * 128:(h + 1) * 128], start=True, stop=False))
                    fns.append(lambda e, h=h: e.matmul(ov[:, h, :], lhsT=qpT[b][:, h, :], rhs=Tst_b[:, h, :], start=False, stop=True))
                sc.pe(fns, r=[r_PT[b3], r_vb[b], r_qpT[b], r_Tb], w=[rb_o])
                bk_s, rb_s = nbank()
                stv = bk_s[:].rearrange("p (h n) -> p h n", h=4)
                sc.pe([(lambda e, h=h: e.matmul(stv[:, h, :], lhsT=kp_b[b][:, h * 128:(h + 1) * 128], rhs=v_b[b][:, h * 128:(h + 1) * 128],
                                                start=True, stop=True)) for h in range(4)], r=[r_kpb[b], r_vb[b]], w=[rb_s])
                sc.op("dve", lambda e: e.tensor_tensor(out=Ttmp[:], in0=stv, in1=Tst[:], op=ALU.add), r=[rb_s, r_T], w=[r_Tt])
                sc.op("pool", lambda e: e.tensor_tensor(out=Tst[:], in0=Ttmp[:], in1=CDEC.rearrange("p (h n) -> p h n", h=4), op=ALU.mult),
                      r=[r_Tt, r_cst], w=[r_T])
                sc.op("pool", lambda e: e.tensor_copy(out=Tst_b[:], in_=Tst[:]), r=[r_T], w=[r_Tb])
                yield
                sc.op("act", lambda e: e.copy(out=o_f[b][:], in_=ov), r=[rb_o], w=[r_of[b]])
                for h in range(4):
                    sc.op("dve", lambda e: e.bn_stats(out=bnst[b][:, h, :], in_=o_f[b][:, h, :]), r=[r_of[b]], w=[r_bn[b]])
                for h in range(4):
                    sc.op("dve", lambda e: e.bn_aggr(out=bnag[b][:, h, :], in_=bnst[b][:, h, :]), r=[r_bn[b]], w=[r_bn[b]])
                sc.op("dve", lambda e: e.tensor_scalar(out=s1[:, 20:24], in0=bnag[b][:, :, 1], scalar1=EPS, scalar2=None, op0=ALU.add),
                      r=[r_bn[b]], w=[r_sm1[b]])
                sc.op("pool", lambda e: e.tensor_tensor(out=s1[:, 24:28], in0=s1[:, 20:24], in1=mhalf[:, 0:4], op=ALU.pow), r=[r_sm1[b], r_const], w=[r_sm1[b]])
                for h in range(4):
                    sc.op("dve", lambda e: e.tensor_scalar(out=o_f[b][:, h, :], in0=o_f[b][:, h, :], scalar1=bnag[b][:, h, 0:1], scalar2=s1[:, 24 + h:25 + h],
                                                           op0=ALU.subtract, op1=ALU.mult), r=[r_of[b], r_bn[b], r_sm1[b]], w=[r_of[b]])
                ofl = o_f[b][:].rearrange("p h n -> p (h n)")
                sc.op("pool", lambda e: e.tensor_tensor(out=ofl, in0=ofl, in1=gn[:], op=ALU.mult), r=[r_of[b], r_const], w=[r_of[b]])
                sc.op("pool", lambda e: e.tensor_tensor(out=ro_b[b3][:], in0=ofl, in1=sg[b6][:], op=ALU.mult), r=[r_of[b], r_sg[b6]], w=[r_rob[b3]])
                r_routd = sc.res("rout_d%d" % t)
                sc.dma("sp", lambda e: e.dma_start(out=rout_d[ts_, :], in_=ro_b[b3][:]), r=[r_rob[b3]], w=[r_routd])
                if t == NT - 1:
                    dump("ro_last", ro_b[b3][:], r_rob[b3], [128, 512], BF16)
            run_pipelined(p1_tile, NT, 4)
            dump("rstd1", rstd1[:], r_rstd1, [128, NT])
            sc.barrier()
            p1s.close()

        if last_phase >= 2:
            p2s = es.enter_context(ExitStack())
            KTn = sb("KTn", [128, 4, S], BF16, p2s)
            KTr = sb("KTr", [128, 2, S], BF16, p2s)
            Vaug = sb("Vaug", [128, NT, 4, 129], BF16, p2s)
            r_KT = [sc.res("KT%d" % t) for t in range(NT)]
            r_V = sc.res("Vaug")
            sc.op("pool", lambda e: e.memset(Vaug[:], 1.0), w=[r_V])
            w_a = sb("w_a", [128, 8, 448], BF16, p2s)
            w_uq = sb("w_uq", [128, 2, 768], BF16, p2s)
            w_ukv = sb("w_ukv", [128, 1024], BF16, p2s)
            w_out = sb("w_out", [128, 8, 1024], BF16, p2s)
            r_w2 = sc.res("w2")
            g_attn2 = pload("g_attn2", g_attn_d, 8, p2s)
            g_qn = pload("g_qn", g_qn_d, 2, p2s)
            g_kvn = pload("g_kvn", g_kvn_d, 1, p2s)
            pw2 = es.enter_context(ExitStack())
            stage[:] = [sb("stage2_%d" % i, [128, 1024], F32, pw2) for i in range(2)]
            load_scaled(lambda c, a, b_: w_a[:, c, a:b_], lambda c, a, b_: w_in_d[:, c, a:b_], lambda c: g_attn2[:, c:c + 1], 8, 448, r_w2)
            load_scaled(lambda c, a, b_: w_uq[:, c, a:b_], lambda c, a, b_: w_uq_d[:, c, a:b_], lambda c: g_qn[:, c:c + 1], 2, 768, r_w2)
            load_scaled(lambda c, a, b_: w_ukv[:, a:b_], lambda c, a, b_: w_ukv_d[:, a:b_], lambda c: g_kvn[:, 0:1], 1, 1024, r_w2)
            load_cast(lambda c: w_out[:, c, :], lambda c: w_out_d[:, c, :], 8, r_w2)
            sc.barrier()
            pw2.close()

            xt = [sb("x2t%d" % i, [128, 1024], F32, p2s) for i in range(2)]
            xb = [sb("x2b%d" % i, [128, 1024], BF16, p2s) for i in range(2)]
            xT = [sb("x2T%d" % i, [128, 8, 128], BF16, p2s) for i in range(2)]
            junk = sb("junk2", [128, 768], F32, p2s)
            cq_f = [sb("cq_f%d" % i, [128, 256], F32, p2s) for i in range(3)]
            cq_b = [sb("cq_b%d" % i, [128, 256], BF16, p2s) for i in range(3)]
            cqT = [sb("cqT%d" % i, [128, 2, 128], BF16, p2s) for i in range(3)]
            ckv_f = [sb("ckv_f%d" % i, [128, 192], F32, p2s) for i in range(3)]
            ckv_b = [sb("ckv_b%d" % i, [128, 128], BF16, p2s) for i in range(3)]
            ckvT = [sb("ckvT%d" % i, [128, 128], BF16, p2s) for i in range(3)]
            kn_f = [sb("kn_f0", [128, 4, 128], F32, p2s)] * 3
            kn_b = [sb("kn_b%d" % i, [128, 4, 128], BF16, p2s) for i in range(2)]
            kpe = [sb("kpe0", [128, 3, 64], F32, p2s)] * 3
            kr = [sb("kr%d" % i, [128, 64], F32, p2s) for i in range(3)]
            krn_b = [sb("krn_b%d" % i, [128, 4, 64], BF16, p2s) for i in range(3)]
            q_f = [sb("q_f%d" % i, [128, 4, 192], F32, p2s) for i in range(2)]
            qn_b = [sb("qn_b%d" % i, [128, 4, 128], BF16, p2s) for i in range(2)]
            qr = [sb("qr0", [128, 4, 4, 64], F32, p2s)] * 3
            qrn_b = [sb("qrn_b%d" % i, [128, 4, 64], BF16, p2s) for i in range(3)]
            sm2 = [sb("sm2_%d" % i, [128, 48], F32, p2s) for i in range(3)]
            QTn = [sb("QTn%d" % i, [128, 4, 512], BF16, p2s) for i in range(2)]
            QTr = [sb("QTr%d" % i, [128, 2, 512], BF16, p2s) for i in range(2)]
            PTt = [sb("PTt%d" % i, [128, 512], BF16, p2s) for i in range(3)]
            a_b = [sb("a_b%d" % i, [128, 4, 512], BF16, p2s) for i in range(2)]
            rcp = [sb("rcp%d" % i, [128, 4], F32, p2s) for i in range(2)]
            r_b = [sb("r_b%d" % i, [128, 512], BF16, p2s) for i in range(2)]
            aoT = [sb("aoT%d" % i, [128, 8, 128], BF16, p2s) for i in range(2)]

            r_xt, r_xb, r_xT = mkres("x2t"), mkres("x2b"), mkres("x2T")
            r_junk = sc.res("junk2")
            r_cq, r_cqb, r_cqT, r_ckv, r_ckvb, r_ckvT = mkres("cq", 3), mkres("cqb", 3), mkres("cqT", 3), mkres("ckv", 3), mkres("ckvb", 3), mkres("ckvT", 3)
            r_kn, r_knb, r_kpe, r_kr, r_krn = mkres("kn", 1) * 3, mkres("knb"), mkres("kpe", 1) * 3, mkres("kr", 3), mkres("krn", 3)
            r_qf, r_qnb, r_qr, r_qrn, r_sm2 = mkres("qf"), mkres("qnb"), mkres("qr", 1) * 3, mkres("qrn", 3), mkres("sm2", 3)
            r_QT = mkres("QT")
            r_PTt = mkres("PTt", 3)
            r_ab, r_rcp, r_rb, r_aoT = mkres("ab"), mkres("rcp"), mkres("rb"), mkres("aoT")
            SCALE = 192.0 ** -0.5

            def prep_tile(t, qb):
                b = t % 3
                bx = t % 2
                ts_ = slice(t * 128, (t + 1) * 128)
                tl = slice((t % 4) * 128, (t % 4 + 1) * 128)
                s2 = sm2[b]
                rs1 = rstd1[:, t:t + 1]
                sc.dma("sp", lambda e: e.dma_start(out=xt[0][:], in_=x_d[ts_, :]), w=[r_xt[0]])
                sc.op("dve", lambda e: e.tensor_copy(out=xb[bx][:], in_=xt[0][:]), r=[r_xt[0]], w=[r_xb[bx]])
                yield
                bk, rb = nbank(4, 8)
                pv = bk[:].bitcast(BF16).rearrange("p (c n) -> p c n", n=128)
                sc.pe([(lambda e, c=c: e.transpose(out=pv[:, c, :], in_=xb[bx][:, c * 128:(c + 1) * 128], identity=ident_bf[:])) for c in range(8)],
                      r=[r_xb[bx], r_const], w=[rb])
                sc.op("dve", lambda e: e.tensor_copy(out=xT[bx][:], in_=pv), r=[rb], w=[r_xT[bx]])
                yield
                bk, rb = nbank(4, 8)
                sc.pe([(lambda e, c=c: e.matmul(bk[:, 0:448], lhsT=xT[bx][:, c, :], rhs=w_a[:, c, :], start=(c == 0), stop=(c == 7))) for c in range(8)],
                      r=[r_xT[bx], r_w2], w=[rb])
                sc.op("act", lambda e: e.activation(out=cq_f[b][:], in_=bk[:, 0:256], func=AF.Copy, scale=rs1), r=[rb, r_rstd1], w=[r_cq[b]])
                sc.op("act", lambda e: e.activation(out=ckv_f[b][:], in_=bk[:, 256:448], func=AF.Copy, scale=rs1), r=[rb, r_rstd1], w=[r_ckv[b]])
                yield
                sc.op("act", lambda e: e.activation(out=junk[:, 0:128], in_=ckv_f[b][:, 0:128], func=AF.Square, accum_out=s2[:, 2:3]), r=[r_ckv[b]], w=[r_junk, r_sm2[b]])
                rstd_from_ss(s2[:, 2:3], 128.0, s2[:, 3:4], r_sm2[b], r_sm2[b], s2[:, 4:5], r_sm2[b])
                sc.op("act", lambda e: e.activation(out=junk[:, 128:192], in_=ckv_f[b][:, 128:192], func=AF.Square, accum_out=s2[:, 5:6]), r=[r_ckv[b]], w=[r_junk, r_sm2[b]])
                sc.op("pool", lambda e: e.tensor_copy(out=ckv_b[b][:], in_=ckv_f[b][:, 0:128]), r=[r_ckv[b]], w=[r_ckvb[b]])
                bk, rb = nbank(4, 8)
                pv = bk[:].bitcast(BF16)
                sc.pe([lambda e: e.transpose(out=pv[:, 0:128], in_=ckv_b[b][:], identity=ident_bf[:])], r=[r_ckvb[b], r_const], w=[rb])
                sc.op("act", lambda e: e.copy(out=ckvT[b][:], in_=pv[:, 0:128]), r=[rb], w=[r_ckvT[b]])
                yield
                for hh in range(2):
                    bk, rb = nbank(4, 8)
                    sc.pe([lambda e: e.matmul(bk[:], lhsT=ckvT[b][:], rhs=w_ukv[:, hh * 512:(hh + 1) * 512], start=True, stop=True)],
                          r=[r_ckvT[b], r_w2], w=[rb])
                    kvv = bk[:].rearrange("p (h two d) -> p h two d", h=2, two=2)
                    sc.op("dve", lambda e: e.tensor_scalar(out=kn_f[b][:, 2 * hh:2 * hh + 2, :], in0=kvv[:, :, 0, :], scalar1=s2[:, 3:4], scalar2=None,
                                                           op0=ALU.mult), r=[rb, r_sm2[b]], w=[r_kn[b]])
                    sc.op("act", lambda e: e.activation(out=Vaug[:, t, 2 * hh:2 * hh + 2, 0:128], in_=kvv[:, :, 1, :], func=AF.Copy, scale=s2[:, 3:4]),
                          r=[rb, r_sm2[b]], w=[r_V])
                yield
                jk = junk[:, 0:512].rearrange("p (h d) -> p h d", h=4)
                sc.op("dve", lambda e: e.tensor_tensor(out=jk, in0=kn_f[b][:], in1=kn_f[b][:], op=ALU.mult), r=[r_kn[b]], w=[r_junk])
                sc.op("dve", lambda e: e.tensor_reduce(out=s2[:, 8:12], in_=jk, axis=AX.X, op=ALU.add), r=[r_junk], w=[r_sm2[b]])
                sc.op("dve", lambda e: e.tensor_scalar(out=s2[:, 8:12], in0=s2[:, 8:12], scalar1=s2[:, 5:6], scalar2=None, op0=ALU.add),
                      r=[r_sm2[b]], w=[r_sm2[b]])
                rstd_from_ss(s2[:, 8:12], 192.0, s2[:, 12:16], r_sm2[b], r_sm2[b], s2[:, 16:20], r_sm2[b])
                for h in range(4):
                    sc.op("dve", lambda e: e.scalar_tensor_tensor(out=kn_b[bx][:, h, :], in0=kn_f[b][:, h, :], scalar=s2[:, 12 + h:13 + h], in1=gk[:, 0:128],
                                                                  op0=ALU.mult, op1=ALU.mult), r=[r_kn[b], r_sm2[b], r_const], w=[r_knb[bx]])
                yield
                kp = kpe[b]
                sc.op("pool", lambda e: e.tensor_tensor(out=kp[:, 0, :], in0=ckv_f[b][:, 128:192], in1=gk[:, 128:192], op=ALU.mult),
                      r=[r_ckv[b], r_const], w=[r_kpe[b]])
                cosM2 = SCm[:, t, 32:64].unsqueeze(1).broadcast_to([128, 2, 32])
                sinM2 = SCm[:, t, 0:32].unsqueeze(1).broadcast_to([128, 2, 32])
                k0 = kp[:, 0, :].rearrange("p (two d) -> p two d", two=2)
                kA = kp[:, 1, :].rearrange("p (two d) -> p two d", two=2)
                kB = kp[:, 2, :].rearrange("p (two d) -> p two d", two=2)
                sc.op("pool", lambda e: e.tensor_tensor(out=kA, in0=k0, in1=cosM2, op=ALU.mult), r=[r_kpe[b], r_sc], w=[r_kpe[b]])
                sc.op("pool", lambda e: e.tensor_tensor(out=kB, in0=k0, in1=sinM2, op=ALU.mult), r=[r_kpe[b], r_sc], w=[r_kpe[b]])
                sc.op("pool", lambda e: e.tensor_tensor(out=kr[b][:, 0:32], in0=kp[:, 1, 0:32], in1=kp[:, 2, 32:64], op=ALU.subtract),
                      r=[r_kpe[b]], w=[r_kr[b]])
                sc.op("pool", lambda e: e.tensor_tensor(out=kr[b][:, 32:64], in0=kp[:, 1, 32:64], in1=kp[:, 2, 0:32], op=ALU.add),
                      r=[r_kpe[b]], w=[r_kr[b]])
                for h in range(4):
                    sc.op("dve", lambda e: e.tensor_scalar(out=krn_b[b][:, h, :], in0=kr[b][:], scalar1=s2[:, 12 + h:13 + h], scalar2=None, op0=ALU.mult),
                          r=[r_kr[b], r_sm2[b]], w=[r_krn[b]])
                bk, rb = nbank(4, 8)
                pv = bk[:].bitcast(BF16).rearrange("p (c n) -> p c n", n=128)
                sc.pe([(lambda e, h=h: e.transpose(out=pv[:, h, :], in_=kn_b[bx][:, h, :], identity=ident_bf[:])) for h in range(4)]
                      + [(lambda e, pr=pr: e.transpose(out=pv[:, 4 + pr, :], in_=krn_b[b][:, 2 * pr:2 * pr + 2, :].rearrange("p a d -> p (a d)"),
                                                       identity=ident_bf[:])) for pr in range(2)],
                      r=[r_knb[bx], r_krn[b], r_const], w=[rb])
                sc.op("act", lambda e: e.copy(out=KTn[:, :, ts_], in_=pv[:, 0:4, :]), r=[rb], w=[r_KT[t]])
                sc.op("dve", lambda e: e.tensor_copy(out=KTr[:, :, ts_], in_=pv[:, 4:6, :]), r=[rb], w=[r_KT[t]])
                yield
                sc.op("act", lambda e: e.activation(out=junk[:, 0:256], in_=cq_f[b][:], func=AF.Square, accum_out=s2[:, 20:21]), r=[r_cq[b]], w=[r_junk, r_sm2[b]])
                rstd_from_ss(s2[:, 20:21], 256.0, s2[:, 21:22], r_sm2[b], r_sm2[b], s2[:, 22:23], r_sm2[b])
                sc.op("pool", lambda e: e.tensor_copy(out=cq_b[b][:], in_=cq_f[b][:]), r=[r_cq[b]], w=[r_cqb[b]])
                bk, rb = nbank(4, 8)
                pv = bk[:].bitcast(BF16).rearrange("p (c n) -> p c n", n=128)
                sc.pe([(lambda e, c=c: e.transpose(out=pv[:, c, :], in_=cq_b[b][:, c * 128:(c + 1) * 128], identity=ident_bf[:])) for c in range(2)],
                      r=[r_cqb[b], r_const], w=[rb])
                sc.op("act", lambda e: e.copy(out=cqT[b][:], in_=pv[:, 0:2, :]), r=[rb], w=[r_cqT[b]])
                yield
                for hh in range(2):
                    bk, rb = nbank(4, 8)
                    sc.pe([(lambda e, c=c: e.matmul(bk[:, 0:384], lhsT=cqT[b][:, c, :], rhs=w_uq[:, c, hh * 384:(hh + 1) * 384], start=(c == 0), stop=(c == 1)))
                           for c in range(2)], r=[r_cqT[b], r_w2], w=[rb])
                    sc.op("act", lambda e: e.activation(out=q_f[bx][:, 2 * hh:2 * hh + 2, :], in_=bk[:, 0:384].rearrange("p (h d) -> p h d", h=2),
                                                        func=AF.Copy, scale=s2[:, 21:22]), r=[rb, r_sm2[b]], w=[r_qf[bx]])
                yield
                jq = junk[:, 0:768].rearrange("p (h d) -> p h d", h=4)
                sc.op("dve", lambda e: e.tensor_tensor(out=jq, in0=q_f[bx][:], in1=q_f[bx][:], op=ALU.mult), r=[r_qf[bx]], w=[r_junk])
                sc.op("dve", lambda e: e.tensor_reduce(out=s2[:, 24:28], in_=jq, axis=AX.X, op=ALU.add), r=[r_junk], w=[r_sm2[b]])
                rstd_from_ss(s2[:, 24:28], 192.0, s2[:, 28:32], r_sm2[b], r_sm2[b], s2[:, 32:36], r_sm2[b])
                for h in range(4):
                    sc.op("dve", lambda e: e.scalar_tensor_tensor(out=qn_b[bx][:, h, :], in0=q_f[bx][:, h, 0:128], scalar=s2[:, 28 + h:29 + h], in1=gq[:, 0:128],
                                                                  op0=ALU.mult, op1=ALU.mult), r=[r_qf[bx], r_sm2[b], r_const], w=[r_qnb[bx]])
                yield
                qq = qr[b]
                gqB = gq[:, 128:192].unsqueeze(1).broadcast_to([128, 4, 64])
                sc.op("pool", lambda e: e.tensor_tensor(out=qq[:, 0, :, :], in0=q_f[bx][:, :, 128:192], in1=gqB, op=ALU.mult), r=[r_qf[bx], r_const], w=[r_qr[b]])
                cosM8 = SCm[:, t, 32:64].unsqueeze(1).broadcast_to([128, 8, 32])
                sinM8 = SCm[:, t, 0:32].unsqueeze(1).broadcast_to([128, 8, 32])
                q0 = qq[:, 0, :, :].rearrange("p h (two d) -> p (h two) d", two=2)
                qA = qq[:, 1, :, :].rearrange("p h (two d) -> p (h two) d", two=2)
                qB = qq[:, 2, :, :].rearrange("p h (two d) -> p (h two) d", two=2)
                sc.op("pool", lambda e: e.tensor_tensor(out=qA, in0=q0, in1=cosM8, op=ALU.mult), r=[r_qr[b], r_sc], w=[r_qr[b]])
                sc.op("pool", lambda e: e.tensor_tensor(out=qB, in0=q0, in1=sinM8, op=ALU.mult), r=[r_qr[b], r_sc], w=[r_qr[b]])
                sc.op("pool", lambda e: e.tensor_tensor(out=qq[:, 3, :, 0:32], in0=qq[:, 1, :, 0:32], in1=qq[:, 2, :, 32:64], op=ALU.subtract),
                      r=[r_qr[b]], w=[r_qr[b]])
                sc.op("pool", lambda e: e.tensor_tensor(out=qq[:, 3, :, 32:64], in0=qq[:, 1, :, 32:64], in1=qq[:, 2, :, 0:32], op=ALU.add),
                      r=[r_qr[b]], w=[r_qr[b]])
                yield
                rsB = s2[:, 28:32].unsqueeze(2).broadcast_to([128, 4, 64])
                sc.op("dve", lambda e: e.tensor_tensor(out=qrn_b[b][:], in0=qq[:, 3, :, :], in1=rsB, op=ALU.mult), r=[r_qr[b], r_sm2[b]], w=[r_qrn[b]])
                bk, rb = nbank(4, 8)
                pv = bk[:].bitcast(BF16).rearrange("p (c n) -> p c n", n=128)
                sc.pe([(lambda e, h=h: e.transpose(out=pv[:, h, :], in_=qn_b[bx][:, h, :], identity=ident_bf[:])) for h in range(4)]
                      + [(lambda e, pr=pr: e.transpose(out=pv[:, 4 + pr, :], in_=qrn_b[b][:, 2 * pr:2 * pr + 2, :].rearrange("p a d -> p (a d)"),
                                                       identity=ident_bf[:])) for pr in range(2)],
                      r=[r_qnb[bx], r_qrn[b], r_const], w=[rb])
                sc.op("act", lambda e: e.copy(out=QTn[qb][:, :, tl], in_=pv[:, 0:4, :]), r=[rb], w=[r_QT[qb]])
                sc.op("dve", lambda e: e.tensor_copy(out=QTr[qb][:, :, tl], in_=pv[:, 4:6, :]), r=[rb], w=[r_QT[qb]])

            def attention_block(i, qb):
                ab = a_b[i % 2]
                its = [(h, j) for h in range(4) for j in range(4 * i + 4)]

                def qk(h, j):
                    pair, hp = h // 2, h % 2
                    psl = slice(hp * 64, (hp + 1) * 64)
                    r0 = max(0, j - 4 * i)
                    n = 512 - r0 * 128
                    ks = slice(j * 128, (j + 1) * 128)
                    bk, rb = nbank(4, 8)
                    diag = j >= 4 * i
                    fq = [lambda e: e.matmul(bk[:, 0:n], lhsT=KTn[:, h, ks], rhs=QTn[qb][:, h, r0 * 128:512], start=True, stop=False),
                          lambda e: e.matmul(bk[:, 0:n], lhsT=KTr[psl, pair, ks], rhs=QTr[qb][psl, pair, r0 * 128:512], start=False, stop=not diag)]
                    if diag:
                        fq.append(lambda e: e.matmul(bk[:, 0:128], lhsT=ident_bf[:], rhs=negm[:], start=False, stop=True))
                    sc.pe(fq, r=[r_KT[j], r_QT[qb], r_const], w=[rb])
                    pi = (h * 64 + j) % 3
                    pt, rpt = PTt[pi], r_PTt[pi]
                    sc.op("act", lambda e: e.activation(out=pt[:, 0:n], in_=bk[:, 0:n], func=AF.Exp, scale=SCALE), r=[rb], w=[rpt])
                    return pt, rpt, r0

                def pv_(h, j, pt, rpt, r0):
                    fns = []
                    for s in range(r0, 4):
                        fns.append(lambda e, s=s: e.matmul(banks[s][:, 0:129], lhsT=pt[:, (s - r0) * 128:(s - r0 + 1) * 128], rhs=Vaug[:, j, h, :],
                                                           start=(j == 0), stop=(j == 4 * i + s)))
                    sc.pe(fns, r=[rpt, r_V, r_KT[j]], w=[bank_res[s] for s in range(r0, 4)])
                    if j >= 4 * i:
                        s = j - 4 * i
                        sc.op("dve", lambda e: e.reciprocal(out=rcp[i % 2][:, s:s + 1], in_=banks[s][:, 128:129]), r=[bank_res[s]], w=[r_rcp[i % 2]])
                        sc.op("dve", lambda e: e.tensor_scalar(out=ab[:, s, h * 128:(h + 1) * 128], in0=banks[s][:, 0:128], scalar1=rcp[i % 2][:, s:s + 1],
                                                               scalar2=None, op0=ALU.mult), r=[bank_res[s], r_rcp[i % 2]], w=[r_ab[i % 2]])

                pend = []
                ystep = max(1, len(its) // 30)
                for k_, (h, j) in enumerate(its):
                    pend.append((h, j) + qk(h, j))
                    if len(pend) > 1:
                        pv_(*pend.pop(0))
                    if k_ % ystep == ystep - 1:
                        yield
                while pend:
                    pv_(*pend.pop(0))

            def out_tile(t):
                i, s = t // 4, t % 4
                ab = a_b[i % 2]
                if True:
                    b = t % 2
                    ts_ = slice(t * 128, (t + 1) * 128)
                    sc.dma("sp", lambda e: e.dma_start(out=r_b[b][:], in_=rout_d[ts_, :]), w=[r_rb[b]])
                    sc.dma("sp", lambda e: e.dma_start(out=x1t[1][:], in_=x_d[ts_, :]), w=[r_x1t[1]])
                    yield
                    bk, rb = nbank(4, 8)
                    pv = bk[:].bitcast(BF16).rearrange("p (c n) -> p c n", n=128)
                    sc.pe([(lambda e, c=c: e.transpose(out=pv[:, c, :], in_=ab[:, s, c * 128:(c + 1) * 128], identity=ident_bf[:])) for c in range(4)]
                          + [(lambda e, c=c: e.transpose(out=pv[:, 4 + c, :], in_=r_b[b][:, c * 128:(c + 1) * 128], identity=ident_bf[:])) for c in range(4)],
                          r=[r_ab[i % 2], r_rb[b], r_const], w=[rb])
                    sc.op("dve", lambda e: e.tensor_copy(out=aoT[b][:], in_=pv), r=[rb], w=[r_aoT[b]])
                    yield
                    for hh in range(2):
                        bk, rb = nbank(4, 8)
                        sc.pe([(lambda e, c=c: e.matmul(bk[:], lhsT=aoT[b][:, c, :], rhs=w_out[:, c, hh * 512:(hh + 1) * 512], start=(c == 0), stop=(c == 7)))
                               for c in range(8)], r=[r_aoT[b], r_w2], w=[rb])
                        sc.op("dve", lambda e: e.tensor_tensor(out=x1t[1][:, hh * 512:(hh + 1) * 512], in0=bk[:], in1=x1t[1][:, hh * 512:(hh + 1) * 512], op=ALU.add),
                              r=[rb], w=[r_x1t[1]])
                    r_x1d = sc.res("x1d")
                    sc.dma("sp", lambda e: e.dma_start(out=x1_d[ts_, :], in_=x1t[1][:]), r=[r_x1t[1]], w=[r_x1d])

            def drive(gens):
                gens = list(gens)
                while gens:
                    for g in list(gens):
                        try:
                            next(g)
                        except StopIteration:
                            gens.remove(g)

            x1t = xt
            r_x1t = r_xt
            run_pipelined(lambda t: prep_tile(t, 0), 4, 3)
            for i in range(NB):
                qb = i % 2

                def side(i=i):
                    if i + 1 < NB:
                        act_ = []
                        nx = 4 * (i + 1)
                        while nx < 4 * (i + 2) or act_:
                            if nx < 4 * (i + 2) and len(act_) < 3:
                                act_.append(prep_tile(nx, (i + 1) % 2))
                                nx += 1
                            for g in list(act_):
                                try:
                                    next(g)
                                except StopIteration:
                                    act_.remove(g)
                            yield

                def outs(i=i):
                    if i == 0:
                        return
                    act_ = []
                    nx = 4 * (i - 1)
                    while nx < 4 * i or act_:
                        if nx < 4 * i and len(act_) < 1:
                            act_.append(out_tile(nx))
                            nx += 1
                        for g in list(act_):
                            try:
                                next(g)
                            except StopIteration:
                                act_.remove(g)
                        yield

                drive([attention_block(i, qb), outs(), side()])
            run_pipelined(lambda k_: out_tile(4 * (NB - 1) + k_), 4, 1)
            if "x1" in dbg:
                sc.barrier()
                t_ = nc.dram_tensor("dbg_x1", [S, D], F32, kind="ExternalOutput").ap()
                dbg_out["x1"] = t_
                sc.dma("sp", lambda e: e.dma_start(out=t_, in_=x1_d), is_out=True)
            dump("KTn", KTn[:], r_KT[NT - 1], [128, 4, S], BF16)
            dump("KTr", KTr[:], r_KT[NT - 1], [128, 2, S], BF16)
            dump("Vaug", Vaug[:], r_V, [128, NT, 4, 129], BF16)
            sc.barrier()
            p2s.close()

        if last_phase >= 3:
            p36 = es.enter_context(ExitStack())
            lg_all = sb("lg_all", [128, NT, 36], F32, p36)
            r_lg = sc.res("lg_all")
            pos12 = sb("pos12", [128, 2, NT], I32, p36)
            w12 = sb("w12", [128, 2, NT], F32, p36)
            widx = sb("widx", [128, NTS], I32, p36)
            r_route = sc.res("route")
            b_rt = bload("b_rt", b_rt_d, 36, p36)

            p3s = es.enter_context(ExitStack())
            cw_q = sb("cw_q", [128, 8, 1024], BF16, p3s)
            cw_o = sb("cw_o", [128, 8, 1024], BF16, p3s)
            KcT = sb("KcT", [128, 4, 2, 256], BF16, p3s)
            Vc = sb("Vc", [128, 2, 4, 257], BF16, p3s)
            mg = bload("mg", mg_d, 1024, p3s)
            cqg = bload("cqg", cqg_d, 256, p3s)
            ckg = bload("ckg", ckg_d, 256, p3s)
            stage[:] = [sb("stage3_%d" % i, [128, 1024], F32, p3s) for i in range(2)]
            g_cross = pload("g_cross", g_cross_d, 8, p3s)
            g_mem = pload("g_mem", g_mem_d, 8, p3s)
            w_rt = sb("w_rt", [128, 8, 36], F32, p3s)
            r_w3 = sc.res("w3")
            sc.dma("sp", lambda e: e.dma_start(out=w_rt[:], in_=w_rt_d[:, :, :]), w=[r_w3])
            load_scaled(lambda c, a, b_: cw_q[:, c, a:b_], lambda c, a, b_: cw_q_d[:, c, a:b_], lambda c: g_cross[:, c:c + 1], 8, 1024, r_w3)
            load_cast(lambda c: cw_o[:, c, :], lambda c: cw_o_d[:, c, :], 8, r_w3)
            r_kvc = sc.res("kvc")
            sc.op("pool", lambda e: e.memset(Vc[:], 1.0), w=[r_kvc])

            pm = es.enter_context(ExitStack())
            cw_kv = sb("cw_kv", [128, 8, 2048], BF16, pm)
            r_cwkv = sc.res("cw_kv")
            load_scaled(lambda c, a, b_: cw_kv[:, c, a:b_], lambda c, a, b_: cw_kv_d[:, c, a:b_], lambda c: g_mem[:, c:c + 1], 8, 2048, r_cwkv)
            m_f = sb("m_f", [128, 1024], F32, pm)
            m_b = sb("m_b", [128, 1024], BF16, pm)
            m_T = sb("m_T", [128, 8, 128], BF16, pm)
            kc_f = sb("kc_f", [128, 4, 256], F32, pm)
            kc_sq = sb("kc_sq", [128, 4, 256], F32, pm)
            kc_b = sb("kc_b", [128, 4, 256], BF16, pm)
            sm = sb("sm0", [128, 16], F32, pm)
            r_m = sc.res("m")
            r_sm = sc.res("sm0")
            for mt in range(2):
                sc.dma("sp", lambda e: e.dma_start(out=m_f[:], in_=mem_d[mt * 128:(mt + 1) * 128, :]), w=[r_m])
                sc.op("act", lambda e: e.activation(out=m_b[:], in_=m_f[:], func=AF.Square, accum_out=sm[:, 0:1]), r=[r_m], w=[r_m, r_sm])
                rstd_from_ss(sm[:, 0:1], 1024.0, sm[:, 1:2], r_sm, r_sm, sm[:, 2:3], r_sm)
                sc.op("pool", lambda e: e.tensor_copy(out=m_b[:], in_=m_f[:]), r=[r_m], w=[r_m])
                bk, rb = nbank()
                pv = bk[:].bitcast(BF16).rearrange("p (c n) -> p c n", n=128)
                sc.pe([(lambda e, c=c: e.transpose(out=pv[:, c, :], in_=m_b[:, c * 128:(c + 1) * 128], identity=ident_bf[:])) for c in range(8)],
                      r=[r_m, r_const], w=[rb])
                sc.op("dve", lambda e: e.tensor_copy(out=m_T[:], in_=pv), r=[rb], w=[r_m])
                for nchunk in range(4):
                    bk, rb = nbank()
                    sc.pe([(lambda e, c=c: e.matmul(bk[:], lhsT=m_T[:, c, :], rhs=cw_kv[:, c, nchunk * 512:(nchunk + 1) * 512],
                                                    start=(c == 0), stop=(c == 7))) for c in range(8)], r=[r_m, r_cwkv], w=[rb])
                    if nchunk < 2:
                        sc.op("act", lambda e: e.activation(out=kc_f[:, 2 * nchunk:2 * nchunk + 2, :], in_=bk[:].rearrange("p (h d) -> p h d", h=2),
                                                            func=AF.Copy, scale=sm[:, 1:2]), r=[rb, r_sm], w=[r_m])
                    else:
                        hh = 2 * (nchunk - 2)
                        sc.op("act", lambda e: e.activation(out=Vc[:, mt, hh:hh + 2, 0:256], in_=bk[:].rearrange("p (h d) -> p h d", h=2),
                                                            func=AF.Copy, scale=sm[:, 1:2]), r=[rb, r_sm], w=[r_kvc])
                sc.op("dve", lambda e: e.tensor_tensor(out=kc_sq[:], in0=kc_f[:], in1=kc_f[:], op=ALU.mult), r=[r_m], w=[r_m])
                sc.op("dve", lambda e: e.tensor_reduce(out=sm[:, 4:8], in_=kc_sq[:], axis=AX.X, op=ALU.add), r=[r_m], w=[r_sm])
                rstd_from_ss(sm[:, 4:8], 256.0, sm[:, 8:12], r_sm, r_sm, sm[:, 12:16], r_sm)
                for h in range(4):
                    sc.op("dve", lambda e: e.scalar_tensor_tensor(out=kc_b[:, h, :], in0=kc_f[:, h, :], scalar=sm[:, 8 + h:9 + h], in1=ckg[:],
                                                                  op0=ALU.mult, op1=ALU.mult), r=[r_m, r_sm, r_const], w=[r_m])
                bk, rb = nbank()
                pv = bk[:].bitcast(BF16).rearrange("p (h c n) -> p h c n", h=4, c=2)
                sc.pe([(lambda e, h=h, c=c: e.transpose(out=pv[:, h, c, :], in_=kc_b[:, h, c * 128:(c + 1) * 128], identity=ident_bf[:]))
                       for h in range(4) for c in range(2)], r=[r_m, r_const], w=[rb])
                sc.op("dve", lambda e: e.tensor_copy(out=KcT[:, :, :, mt * 128:(mt + 1) * 128], in_=pv), r=[rb], w=[r_kvc])
            dump("KcT", KcT[:], r_kvc, [128, 4, 2, 256], BF16)
            dump("Vc", Vc[:], r_kvc, [128, 2, 4, 257], BF16)
            sc.barrier()
            pm.close()

            x1t = [sb("x3t%d" % i, [128, 1024], F32, p3s) for i in range(5)]
            xb = [sb("x3b%d" % i, [128, 1024], BF16, p3s) for i in range(5)]
            hcT = [sb("hcT%d" % i, [128, 8, 128], BF16, p3s) for i in range(5)]
            junk = sb("junk3", [128, 1024], F32, p3s)
            qc_f = sb("qc_f", [128, 4, 256], F32, p3s)
            qc_b = sb("qc_b", [128, 4, 256], BF16, p3s)
            qcT = [sb("qcT%d" % i, [128, 4, 2, 128], BF16, p3s) for i in range(5)]
            PTc = [sb("PTc%d" % i, [128, 8, 128], BF16, p3s) for i in range(5)]
            oc_b = sb("oc_b", [128, 4, 256], BF16, p3s)
            ocT = [sb("ocT%d" % i, [128, 8, 128], BF16, p3s) for i in range(5)]
            hm_f = [sb("hm_f%d" % i, [128, 1024], F32, p3s) for i in range(5)]
            hm_b = [sb("hm_b%d" % i, [128, 1024], BF16, p3s) for i in range(5)]
            hmT_f = sb("hmT_f", [128, 8, 128], F32, p3s)
            sm3 = [sb("sm3_%d" % i, [128, 32], F32, p3s) for i in range(5)]
            r_x1t, r_xb, r_hcT = mkres("x3t", 5), mkres("x3b", 5), mkres("hcT", 5)
            r_junk = sc.res("junk3")
            r_qcf, r_qcb = sc.res("qcf"), sc.res("qcb")
            r_qcT, r_PTc, r_ocT, r_hmf, r_hmb, r_sm3 = mkres("qcT", 5), mkres("PTc", 5), mkres("ocT", 5), mkres("hmf", 5), mkres("hmb", 5), mkres("sm3", 5)
            r_ocb = sc.res("ocb")
            r_hmT = sc.res("hmT")
            r_x2d = [sc.res("x2d%d" % t) for t in range(NT)]
            r_hmd = [sc.res("hmd%d" % t) for t in range(NT)]
            CSCALE = 256.0 ** -0.5

            def p3_tile(t):
                b = t % 5
                b3 = t % 5
                ts_ = slice(t * 128, (t + 1) * 128)
                s3 = sm3[b3]
                sc.dma("sp", lambda e: e.dma_start(out=x1t[b3][:], in_=x1_d[ts_, :]), w=[r_x1t[b3]])
                sc.op("act", lambda e: e.activation(out=junk[:], in_=x1t[b3][:], func=AF.Square, accum_out=s3[:, 0:1]), r=[r_x1t[b3]], w=[r_junk, r_sm3[b3]])
                rstd_from_ss(s3[:, 0:1], 1024.0, s3[:, 1:2], r_sm3[b3], r_sm3[b3], s3[:, 2:3], r_sm3[b3])
                sc.op("act", lambda e: e.copy(out=xb[b][:], in_=x1t[b3][:]), r=[r_x1t[b3]], w=[r_xb[b]])
                yield
                bk, rb = nbank()
                pv = bk[:].bitcast(BF16).rearrange("p (c n) -> p c n", n=128)
                sc.pe([(lambda e, c=c: e.transpose(out=pv[:, c, :], in_=xb[b][:, c * 128:(c + 1) * 128], identity=ident_bf[:])) for c in range(8)],
                      r=[r_xb[b], r_const], w=[rb])
                sc.op("dve", lambda e: e.tensor_copy(out=hcT[b][:], in_=pv), r=[rb], w=[r_hcT[b]])
                yield
                for hh in range(2):
                    bk, rb = nbank()
                    sc.pe([(lambda e, c=c: e.matmul(bk[:], lhsT=hcT[b][:, c, :], rhs=cw_q[:, c, hh * 512:(hh + 1) * 512], start=(c == 0), stop=(c == 7)))
                           for c in range(8)], r=[r_hcT[b], r_w3], w=[rb])
                    sc.op("act", lambda e: e.activation(out=qc_f[:, 2 * hh:2 * hh + 2, :], in_=bk[:].rearrange("p (h d) -> p h d", h=2), func=AF.Copy,
                                                        scale=s3[:, 1:2]), r=[rb, r_sm3[b3]], w=[r_qcf])
                yield
                jq = junk[:].rearrange("p (h d) -> p h d", h=4)
                sc.op("dve", lambda e: e.tensor_tensor(out=jq, in0=qc_f[:], in1=qc_f[:], op=ALU.mult), r=[r_qcf], w=[r_junk])
                sc.op("dve", lambda e: e.tensor_reduce(out=s3[:, 4:8], in_=jq, axis=AX.X, op=ALU.add), r=[r_junk], w=[r_sm3[b3]])
                rstd_from_ss(s3[:, 4:8], 256.0, s3[:, 8:12], r_sm3[b3], r_sm3[b3], s3[:, 12:16], r_sm3[b3])
                for h in range(4):
                    sc.op("dve", lambda e: e.scalar_tensor_tensor(out=qc_b[:, h, :], in0=qc_f[:, h, :], scalar=s3[:, 8 + h:9 + h], in1=cqg[:],
                                                                  op0=ALU.mult, op1=ALU.mult), r=[r_qcf, r_sm3[b3], r_const], w=[r_qcb])
                yield
                bk, rb = nbank()
                pv = bk[:].bitcast(BF16).rearrange("p (h c n) -> p h c n", h=4, c=2)
                sc.pe([(lambda e, h=h, c=c: e.transpose(out=pv[:, h, c, :], in_=qc_b[:, h, c * 128:(c + 1) * 128], identity=ident_bf[:]))
                       for h in range(4) for c in range(2)], r=[r_qcb, r_const], w=[rb])
                sc.op("act", lambda e: e.copy(out=qcT[b][:], in_=pv), r=[rb], w=[r_qcT[b]])
                yield
                for hp in range(2):
                    bk, rb = nbank()
                    sv_ = bk[:].rearrange("p (a n) -> p a n", a=4)
                    fns = []
                    for hl in range(2):
                        h = 2 * hp + hl
                        for mc in range(2):
                            for dc in range(2):
                                fns.append(lambda e, h=h, mc=mc, dc=dc, hl=hl: e.matmul(sv_[:, hl * 2 + mc, :], lhsT=KcT[:, h, dc, mc * 128:(mc + 1) * 128],
                                                                                        rhs=qcT[b][:, h, dc, :], start=(dc == 0), stop=(dc == 1)))
                    sc.pe(fns, r=[r_kvc, r_qcT[b]], w=[rb])
                    sc.op("act", lambda e: e.activation(out=PTc[b][:, 4 * hp:4 * hp + 4, :], in_=sv_, func=AF.Exp, scale=CSCALE), r=[rb], w=[r_PTc[b]])
                yield
                for h in range(4):
                    bk, rb = nbank()
                    sc.pe([(lambda e, mc=mc: e.matmul(bk[:, 0:257], lhsT=PTc[b][:, 2 * h + mc, :], rhs=Vc[:, mc, h, :], start=(mc == 0), stop=(mc == 1)))
                           for mc in range(2)], r=[r_PTc[b], r_kvc], w=[rb])
                    sc.op("dve", lambda e: e.reciprocal(out=s3[:, 16 + h:17 + h], in_=bk[:, 256:257]), r=[rb], w=[r_sm3[b3]])
                    sc.op("dve", lambda e: e.tensor_scalar(out=oc_b[:, h, :], in0=bk[:, 0:256], scalar1=s3[:, 16 + h:17 + h], scalar2=None, op0=ALU.mult),
                          r=[rb, r_sm3[b3]], w=[r_ocb])
                yield
                bk, rb = nbank()
                pv = bk[:].bitcast(BF16).rearrange("p (c n) -> p c n", n=128)
                ocf = oc_b[:].rearrange("p h d -> p (h d)")
                sc.pe([(lambda e, c=c: e.transpose(out=pv[:, c, :], in_=ocf[:, c * 128:(c + 1) * 128], identity=ident_bf[:])) for c in range(8)],
                      r=[r_ocb, r_const], w=[rb])
                sc.op("act", lambda e: e.copy(out=ocT[b][:], in_=pv), r=[rb], w=[r_ocT[b]])
                yield
                for hh in range(2):
                    bk, rb = nbank()
                    sc.pe([(lambda e, c=c: e.matmul(bk[:], lhsT=ocT[b][:, c, :], rhs=cw_o[:, c, hh * 512:(hh + 1) * 512], start=(c == 0), stop=(c == 7)))
                           for c in range(8)], r=[r_ocT[b], r_w3], w=[rb])
                    sc.op("dve", lambda e: e.tensor_tensor(out=x1t[b3][:, hh * 512:(hh + 1) * 512], in0=bk[:], in1=x1t[b3][:, hh * 512:(hh + 1) * 512], op=ALU.add),
                          r=[rb], w=[r_x1t[b3]])
                sc.dma("sp", lambda e: e.dma_start(out=out_d[ts_, :], in_=x1t[b3][:]), r=[r_x1t[b3]], w=[r_x2d[t]])
                yield
                sc.op("act", lambda e: e.activation(out=junk[:], in_=x1t[b3][:], func=AF.Square, accum_out=s3[:, 20:21]), r=[r_x1t[b3]], w=[r_junk, r_sm3[b3]])
                rstd_from_ss(s3[:, 20:21], 1024.0, s3[:, 21:22], r_sm3[b3], r_sm3[b3], s3[:, 22:23], r_sm3[b3])
                sc.op("dve", lambda e: e.scalar_tensor_tensor(out=hm_f[b][:], in0=x1t[b3][:], scalar=s3[:, 21:22], in1=mg[:], op0=ALU.mult, op1=ALU.mult),
                      r=[r_x1t[b3], r_sm3[b3], r_const], w=[r_hmf[b]])
                sc.op("pool", lambda e: e.tensor_copy(out=hm_b[b][:], in_=hm_f[b][:]), r=[r_hmf[b]], w=[r_hmb[b]])
                sc.dma("sp", lambda e: e.dma_start(out=hm_d[ts_, :], in_=hm_b[b][:]), r=[r_hmb[b]], w=[r_hmd[t]])
                yield
                bkA, rbA = nbank()
                bkB, rbB = nbank()
                sc.pe([(lambda e, c=c: e.transpose(out=(bkA if c < 4 else bkB)[:, (c % 4) * 128:(c % 4 + 1) * 128], in_=hm_f[b][:, c * 128:(c + 1) * 128],
                                                   identity=ident_f[:])) for c in range(8)], r=[r_hmf[b], r_const], w=[rbA, rbB])
                sc.op("dve", lambda e: e.tensor_copy(out=hmT_f[:, 0:4, :], in_=bkA[:].rearrange("p (c n) -> p c n", c=4)), r=[rbA], w=[r_hmT])
                sc.op("act", lambda e: e.copy(out=hmT_f[:, 4:8, :], in_=bkB[:].rearrange("p (c n) -> p c n", c=4)), r=[rbB], w=[r_hmT])
                yield
                bk, rb = nbank()
                sc.pe([(lambda e, c=c: e.matmul(bk[:, 0:36], lhsT=hmT_f[:, c, :], rhs=w_rt[:, c, :], start=(c == 0), stop=(c == 7))) for c in range(8)],
                      r=[r_hmT, r_w3], w=[rb])
                sc.op("act", lambda e: e.copy(out=lg_all[:, t, :], in_=bk[:, 0:36]), r=[rb], w=[r_lg])
            run_pipelined(p3_tile, NT, 5)
            dump("logits", lg_all[:], r_lg, [128, NT, 36])
            if "x2" in dbg:
                sc.barrier()
                t_ = nc.dram_tensor("dbg_x2", [S, D], F32, kind="ExternalOutput").ap()
                dbg_out["x2"] = t_
                sc.dma("sp", lambda e: e.dma_start(out=t_, in_=out_d), is_out=True)
                t2_ = nc.dram_tensor("dbg_hm", [S, D], BF16, kind="ExternalOutput").ap()
                dbg_out["hm"] = t2_
                sc.dma("sp", lambda e: e.dma_start(out=t2_, in_=hm_d), is_out=True)
            sc.barrier()
            p3s.close()

        if last_phase >= 4:
            p4s = es.enter_context(ExitStack())
            reg_npos = nc.gpsimd.to_reg(NPOS - 1)
            reg_ew = nc.gpsimd.to_reg(32 * 128 - 1)
            r4 = sc.res("r4")

            def T4(name, shape, dt=F32):
                return sb("r4_" + name, shape, dt, p4s)

            def V(fn, r=(), w=(), eng="dve"):
                sc.op(eng, fn, r=[r4, r_lg, r_const] + list(r), w=[r4] + list(w))

            L = lg_all
            GL = L[:, :, 0:4]
            EL = L[:, :, 4:36].rearrange("p t (g j) -> p t g j", g=4)
            gb, goh, ge = T4("gb", [128, NT, 4]), T4("goh", [128, NT, 4]), T4("ge", [128, NT, 4])
            gmax, gm, gsum, gnum, gw = (T4(n, [128, NT]) for n in ("gmax", "gm", "gsum", "gnum", "gw"))
            t48 = T4("t48", [128, NT, 4, 8])
            esel, bsel, eb, oh1, eb2, oh2, ex, t8 = (T4(n, [128, NT, 8]) for n in ("esel", "bsel", "eb", "oh1", "eb2", "oh2", "ex", "t8"))
            m1, m2, em, a1, a2, den, ff = (T4(n, [128, NT]) for n in ("m1", "m2", "em", "a1", "a2", "den", "ff"))
            OH1, OH2 = T4("OH1", [128, NT, 32]), T4("OH2", [128, NT, 32])
            C_bf = T4("C_bf", [128, NT, 32], BF16)
            TT, PP, cA, cB, base, tmp32 = (T4(n, [128, NT, 32]) for n in ("TT", "PP", "cA", "cB", "base", "tmp32"))
            npad_i = T4("npad_i", [128, 32], I32)
            npad, eA, eB, off = (T4(n, [128, 32]) for n in ("npad", "eA", "eB", "off"))
            posf = T4("posf", [128, 2, NT])
            tpos_i = T4("tpos_i", [128, NTS], I32)
            tpos_f, eid_f = T4("tpos_f", [128, NTS]), T4("eid_f", [128, NTS])
            cmp_ = T4("cmp", [128, NTS, 32])
            pidx_i = T4("pidx_i", [128, 1], I32)
            pidx_f = T4("pidx_f", [128, 1])

            def bc(ap2, n):
                return ap2.unsqueeze(2).broadcast_to([128, NT, n])

            bg = b_rt[:, 0:4].unsqueeze(1).broadcast_to([128, NT, 4])
            be = b_rt[:, 4:36].rearrange("p (g j) -> p g j", g=4).unsqueeze(1).broadcast_to([128, NT, 4, 8])
            V(lambda e: e.tensor_tensor(out=gb[:], in0=GL, in1=bg, op=ALU.add))
            V(lambda e: e.tensor_reduce(out=gmax[:], in_=gb[:], axis=AX.X, op=ALU.max))
            V(lambda e: e.tensor_tensor(out=goh[:], in0=gb[:], in1=bc(gmax[:], 4), op=ALU.is_equal))
            V(lambda e: e.tensor_reduce(out=gm[:], in_=GL, axis=AX.X, op=ALU.max))
            V(lambda e: e.tensor_tensor(out=ge[:], in0=GL, in1=bc(gm[:], 4), op=ALU.subtract))
            V(lambda e: e.activation(out=ge[:].rearrange("p t g -> p (t g)"), in_=ge[:].rearrange("p t g -> p (t g)"), func=AF.Exp), eng="act")
            V(lambda e: e.tensor_reduce(out=gsum[:], in_=ge[:], axis=AX.X, op=ALU.add))
            V(lambda e: e.tensor_tensor(out=gb[:], in0=goh[:], in1=ge[:], op=ALU.mult))
            V(lambda e: e.tensor_reduce(out=gnum[:], in_=gb[:], axis=AX.X, op=ALU.add))
            V(lambda e: e.reciprocal(out=gsum[:], in_=gsum[:]))
            V(lambda e: e.tensor_tensor(out=gw[:], in0=gnum[:], in1=gsum[:], op=ALU.mult))
            goh_b = goh[:].unsqueeze(3).broadcast_to([128, NT, 4, 8])
            V(lambda e: e.tensor_tensor(out=t48[:], in0=EL, in1=goh_b, op=ALU.mult))
            V(lambda e: e.tensor_reduce(out=esel[:], in_=t48[:].rearrange("p t g j -> p t j g"), axis=AX.X, op=ALU.add))
            V(lambda e: e.tensor_tensor(out=t48[:], in0=be, in1=goh_b, op=ALU.mult))
            V(lambda e: e.tensor_reduce(out=bsel[:], in_=t48[:].rearrange("p t g j -> p t j g"), axis=AX.X, op=ALU.add))
            V(lambda e: e.tensor_tensor(out=eb[:], in0=esel[:], in1=bsel[:], op=ALU.add))
            V(lambda e: e.tensor_reduce(out=m1[:], in_=eb[:], axis=AX.X, op=ALU.max))
            V(lambda e: e.tensor_tensor(out=oh1[:], in0=eb[:], in1=bc(m1[:], 8), op=ALU.is_equal))
            V(lambda e: e.scalar_tensor_tensor(out=eb2[:].rearrange("p t j -> p (t j)"), in0=oh1[:].rearrange("p t j -> p (t j)"), scalar=-1e30,
                                               in1=eb[:].rearrange("p t j -> p (t j)"), op0=ALU.mult, op1=ALU.add))
            V(lambda e: e.tensor_reduce(out=m2[:], in_=eb2[:], axis=AX.X, op=ALU.max))
            V(lambda e: e.tensor_tensor(out=oh2[:], in0=eb2[:], in1=bc(m2[:], 8), op=ALU.is_equal))
            V(lambda e: e.tensor_reduce(out=em[:], in_=esel[:], axis=AX.X, op=ALU.max))
            V(lambda e: e.tensor_tensor(out=ex[:], in0=esel[:], in1=bc(em[:], 8), op=ALU.subtract))
            V(lambda e: e.activation(out=ex[:].rearrange("p t j -> p (t j)"), in_=ex[:].rearrange("p t j -> p (t j)"), func=AF.Exp), eng="act")
            V(lambda e: e.tensor_tensor(out=t8[:], in0=oh1[:], in1=ex[:], op=ALU.mult))
            V(lambda e: e.tensor_reduce(out=a1[:], in_=t8[:], axis=AX.X, op=ALU.add))
            V(lambda e: e.tensor_tensor(out=t8[:], in0=oh2[:], in1=ex[:], op=ALU.mult))
            V(lambda e: e.tensor_reduce(out=a2[:], in_=t8[:], axis=AX.X, op=ALU.add))
            V(lambda e: e.tensor_tensor(out=den[:], in0=a1[:], in1=a2[:], op=ALU.add))
            V(lambda e: e.reciprocal(out=den[:], in_=den[:]))
            V(lambda e: e.tensor_tensor(out=ff[:], in0=den[:], in1=gw[:], op=ALU.mult))
            V(lambda e: e.tensor_tensor(out=w12[:, 0, :], in0=a1[:], in1=ff[:], op=ALU.mult), w=[r_route])
            V(lambda e: e.tensor_tensor(out=w12[:, 1, :], in0=a2[:], in1=ff[:], op=ALU.mult), w=[r_route])
            V(lambda e: e.tensor_tensor(out=OH1[:].rearrange("p t (g j) -> p t g j", g=4), in0=goh_b,
                                        in1=oh1[:].unsqueeze(2).broadcast_to([128, NT, 4, 8]), op=ALU.mult))
            V(lambda e: e.tensor_tensor(out=OH2[:].rearrange("p t (g j) -> p t g j", g=4), in0=goh_b,
                                        in1=oh2[:].unsqueeze(2).broadcast_to([128, NT, 4, 8]), op=ALU.mult))
            V(lambda e: e.tensor_tensor(out=C_bf[:], in0=OH1[:], in1=OH2[:], op=ALU.add))
            W = NT * 32
            Cf = C_bf[:].rearrange("p t e -> p (t e)")
            TTf = TT[:].rearrange("p t e -> p (t e)")
            PPf = PP[:].rearrange("p t e -> p (t e)")
            for c0 in range(0, W, 512):
                c1 = min(W, c0 + 512)
                bk, rb = nbank()
                sc.pe([lambda e: e.matmul(bk[:, 0:c1 - c0], lhsT=ones_bf[:], rhs=Cf[:, c0:c1], start=True, stop=True)], r=[r4, r_const], w=[rb])
                sc.op("act", lambda e: e.copy(out=TTf[:, c0:c1], in_=bk[:, 0:c1 - c0]), r=[rb], w=[r4])
                bk, rb = nbank()
                sc.pe([lambda e: e.matmul(bk[:, 0:c1 - c0], lhsT=tri[:], rhs=Cf[:, c0:c1], start=True, stop=True)], r=[r4, r_const], w=[rb])
                sc.op("act", lambda e: e.copy(out=PPf[:, c0:c1], in_=bk[:, 0:c1 - c0]), r=[rb], w=[r4])
            V(lambda e: e.tensor_copy(out=cA[:], in_=TT[:]))
            cur, nxt = cA, cB
            s_ = 1
            while s_ < NT:
                V(lambda e: e.tensor_tensor(out=nxt[:, s_:NT, :], in0=cur[:, s_:NT, :], in1=cur[:, 0:NT - s_, :], op=ALU.add))
                V(lambda e: e.tensor_copy(out=nxt[:, 0:s_, :], in_=cur[:, 0:s_, :]))
                cur, nxt = nxt, cur
                s_ *= 2
            incl = cur
            V(lambda e: e.tensor_scalar(out=npad[:], in0=incl[:, NT - 1, :], scalar1=float(TS - 1), scalar2=None, op0=ALU.add))
            V(lambda e: e.tensor_copy(out=npad_i[:], in_=npad[:]))
            V(lambda e: e.tensor_scalar(out=npad_i[:], in0=npad_i[:], scalar1=8, scalar2=8, op0=ALU.arith_shift_right, op1=ALU.logical_shift_left))
            V(lambda e: e.tensor_copy(out=npad[:], in_=npad_i[:]))
            V(lambda e: e.tensor_copy(out=eA[:], in_=npad[:]))
            cur2, nxt2 = eA, eB
            s_ = 1
            while s_ < 32:
                V(lambda e: e.tensor_tensor(out=nxt2[:, s_:32], in0=cur2[:, s_:32], in1=cur2[:, 0:32 - s_], op=ALU.add))
                V(lambda e: e.tensor_copy(out=nxt2[:, 0:s_], in_=cur2[:, 0:s_]))
                cur2, nxt2 = nxt2, cur2
                s_ *= 2
            endI = cur2
            V(lambda e: e.tensor_tensor(out=off[:], in0=endI[:], in1=npad[:], op=ALU.subtract))
            V(lambda e: e.tensor_tensor(out=base[:], in0=incl[:], in1=TT[:], op=ALU.subtract))
            V(lambda e: e.tensor_tensor(out=base[:], in0=base[:], in1=PP[:], op=ALU.add))
            V(lambda e: e.tensor_tensor(out=base[:], in0=base[:], in1=off[:].unsqueeze(1).broadcast_to([128, NT, 32]), op=ALU.add))
            V(lambda e: e.tensor_tensor(out=tmp32[:], in0=OH1[:], in1=base[:], op=ALU.mult))
            V(lambda e: e.tensor_reduce(out=posf[:, 0, :], in_=tmp32[:], axis=AX.X, op=ALU.add))
            V(lambda e: e.tensor_tensor(out=tmp32[:], in0=OH2[:], in1=base[:], op=ALU.mult))
            V(lambda e: e.tensor_reduce(out=posf[:, 1, :], in_=tmp32[:], axis=AX.X, op=ALU.add))
            V(lambda e: e.tensor_copy(out=pos12[:], in_=posf[:]), w=[r_route])
            V(lambda e: e.iota(tpos_i[:], pattern=[[TS, NTS]], base=0, channel_multiplier=0), eng="pool")
            V(lambda e: e.iota(pidx_i[:], pattern=[[0, 1]], base=0, channel_multiplier=1), eng="pool")
            V(lambda e: e.tensor_copy(out=tpos_f[:], in_=tpos_i[:]))
            V(lambda e: e.tensor_copy(out=pidx_f[:], in_=pidx_i[:]))
            V(lambda e: e.tensor_tensor(out=cmp_[:], in0=endI[:].unsqueeze(1).broadcast_to([128, NTS, 32]),
                                        in1=tpos_f[:].unsqueeze(2).broadcast_to([128, NTS, 32]), op=ALU.is_le))
            V(lambda e: e.tensor_reduce(out=eid_f[:], in_=cmp_[:], axis=AX.X, op=ALU.add))
            V(lambda e: e.tensor_scalar(out=eid_f[:], in0=eid_f[:], scalar1=31.0, scalar2=128.0, op0=ALU.min, op1=ALU.mult))
            V(lambda e: e.tensor_scalar(out=eid_f[:], in0=eid_f[:], scalar1=pidx_f[:, 0:1], scalar2=None, op0=ALU.add))
            V(lambda e: e.tensor_copy(out=widx[:], in_=eid_f[:]), w=[r_route])
            dump("pos12", pos12[:], r_route, [128, 2, NT], I32)
            dump("w12", w12[:], r_route, [128, 2, NT])
            dump("widx", widx[:], r_route, [128, NTS], I32)

            hsb = [sb("hsb%d" % i, [128, 1024], BF16, p4s) for i in range(2)]
            r_hsb = mkres("hsb")
            for t in range(NT):
                b = t % 2
                ts_ = slice(t * 128, (t + 1) * 128)
                sc.dma("sp", lambda e: e.dma_start(out=hsb[b][:], in_=hm_d[ts_, :]), r=[r_hmd[t]], w=[r_hsb[b]])
                for k in range(2):
                    sc.dma("pool", lambda e: e.indirect_dma_start(out=xs_d[:, :], out_offset=bass.IndirectOffsetOnAxis(ap=pos12[:, k, t:t + 1], axis=0),
                                                                  in_=hsb[b][:], in_offset=None, bounds_check=reg_npos, oob_is_err=False),
                           r=[r_hsb[b], r_route], w=[r_xs])
            sc.barrier()
            p4s.close()

        if last_phase >= 5:
            p5s = es.enter_context(ExitStack())
            NWB = 5
            wall = [sb("wall%d" % i, [128, 6144], BF16, p5s) for i in range(NWB)]
            wg = [w_[:, 0:2048] for w_ in wall]
            wu = [w_[:, 2048:4096] for w_ in wall]
            wd = [w_[:, 4096:6144] for w_ in wall]
            r_wg = mkres("wall", NWB)
            r_wu = r_wg
            r_wd = r_wg
            xrow = [sb("xrow%d" % i, [128, 1024], BF16, p5s) for i in range(10)]
            r_xrow = mkres("xrow", 10)
            XsT = [sb("XsT%d" % i, [128, 8, TS], BF16, p5s) for i in range(5)]
            r_XsT = mkres("XsT", 5)
            sa = [sb("sa%d" % i, [128, TS], F32, p5s) for i in range(2)]
            r_sa = mkres("sa")
            actT = [sb("actT%d" % i, [128, 2, TS], BF16, p5s) for i in range(5)]
            r_actT = mkres("actT", 5)
            yt = [sb("yt%d" % i, [128, 1024], F32, p5s) for i in range(4)]
            r_yt = mkres("yt", 4)
            r_ys = sc.res("ys_d")
            cnt5 = {"x": 0, "y": 0}
            def p5_tile(tp):
                wb = tp % NWB
                b = tp % 5
                sc.dma("pool", lambda e: e.indirect_dma_start(out=wall[wb][:], out_offset=None, in_=ewb_all[:, :],
                                                              in_offset=bass.IndirectOffsetOnAxis(ap=widx[:, tp:tp + 1], axis=0),
                                                              bounds_check=reg_ew, oob_is_err=False), r=[r_route, r_ewb], w=[r_wg[wb]])
                for s in range(TS // 128):
                    xi = (2 * tp + s) % 10
                    r0_ = tp * TS + s * 128
                    sc.dma("sp", lambda e: e.dma_start(out=xrow[xi][:], in_=xs_d[r0_:r0_ + 128, :]), r=[r_xs], w=[r_xrow[xi]])
                yield
                for s in range(TS // 128):
                    xi = (2 * tp + s) % 10
                    bk, rb = nbank()
                    pv = bk[:].bitcast(BF16).rearrange("p (c n) -> p c n", n=128)
                    sc.pe([(lambda e, c=c: e.transpose(out=pv[:, c, :], in_=xrow[xi][:, c * 128:(c + 1) * 128], identity=ident_bf[:])) for c in range(8)],
                          r=[r_xrow[xi], r_const], w=[rb])
                    sc.op("dve" if s % 2 == 0 else "act",
                          (lambda e: e.tensor_copy(out=XsT[b][:, :, s * 128:(s + 1) * 128], in_=pv)) if s % 2 == 0 else
                          (lambda e: e.copy(out=XsT[b][:, :, s * 128:(s + 1) * 128], in_=pv)), r=[rb], w=[r_XsT[b]])
                yield
                wgv = wg[wb].rearrange("p (c f) -> p c f", c=8)
                wuv = wu[wb].rearrange("p (c f) -> p c f", c=8)
                wdv = wd[wb].rearrange("p (c d) -> p c d", c=2)
                for fc in range(2):
                    bk, rb = nbank()
                    fns = [(lambda e, c=c: e.matmul(bk[:, 0:TS], lhsT=wgv[:, c, fc * 128:(fc + 1) * 128], rhs=XsT[b][:, c, :], start=(c == 0), stop=(c == 7)))
                           for c in range(8)]
                    fns += [(lambda e, c=c: e.matmul(bk[:, TS:2 * TS], lhsT=wuv[:, c, fc * 128:(fc + 1) * 128], rhs=XsT[b][:, c, :], start=(c == 0), stop=(c == 7)))
                            for c in range(8)]
                    sc.pe(fns, r=[r_wg[wb], r_wu[wb], r_XsT[b]], w=[rb])
                    sc.op("act", lambda e: e.activation(out=sa[fc][:], in_=bk[:, 0:TS], func=AF.Silu), r=[rb], w=[r_sa[fc]])
                    sc.op("dve", lambda e: e.tensor_tensor(out=actT[b][:, fc, :], in0=bk[:, TS:2 * TS], in1=sa[fc][:], op=ALU.mult),
                          r=[rb, r_sa[fc]], w=[r_actT[b]])
                    yield
                for s in range(TS // 128):
                    yi = cnt5["y"] % 4
                    cnt5["y"] += 1
                    for half in range(2):
                        bk, rb = nbank()
                        sc.pe([(lambda e, fc=fc: e.matmul(bk[:], lhsT=actT[b][:, fc, s * 128:(s + 1) * 128], rhs=wdv[:, fc, half * 512:(half + 1) * 512],
                                                          start=(fc == 0), stop=(fc == 1))) for fc in range(2)], r=[r_actT[b], r_wd[wb]], w=[rb])
                        if half == 0:
                            sc.op("act", lambda e: e.copy(out=yt[yi][:, 0:512], in_=bk[:]), r=[rb], w=[r_yt[yi]])
                        else:
                            sc.op("dve", lambda e: e.tensor_copy(out=yt[yi][:, 512:1024], in_=bk[:]), r=[rb], w=[r_yt[yi]])
                    r0_ = tp * TS + s * 128
                    sc.dma("sp", lambda e: e.dma_start(out=ys_d[r0_:r0_ + 128, :], in_=yt[yi][:]), r=[r_yt[yi]], w=[r_ys])
                    yield
            run_pipelined(p5_tile, NTS, 5)
            sc.barrier()
            p5s.close()

        if last_phase >= 6:
            p6s = es.enter_context(ExitStack())
            y1 = [sb("y1_%d" % i, [128, 1024], F32, p6s) for i in range(2)]
            y2 = [sb("y2_%d" % i, [128, 1024], F32, p6s) for i in range(2)]
            xo = [sb("xo_%d" % i, [128, 1024], F32, p6s) for i in range(2)]
            r_y1, r_y2, r_xo = mkres("y1"), mkres("y2"), mkres("xo")
            def p6_tile(t):
                b = t % 2
                ts_ = slice(t * 128, (t + 1) * 128)
                for k, (yy, ry) in enumerate(((y1[b], r_y1[b]), (y2[b], r_y2[b]))):
                    sc.dma("pool", lambda e: e.indirect_dma_start(out=yy[:], out_offset=None, in_=ys_d[:, :],
                                                                  in_offset=bass.IndirectOffsetOnAxis(ap=pos12[:, k, t:t + 1], axis=0),
                                                                  bounds_check=reg_npos, oob_is_err=False), r=[r_route, r_ys], w=[ry])
                sc.dma("sp", lambda e: e.dma_start(out=xo[b][:], in_=out_d[ts_, :]), r=[r_x2d[t]], w=[r_xo[b]])
                yield
                sc.op("dve", lambda e: e.scalar_tensor_tensor(out=xo[b][:], in0=y1[b][:], scalar=w12[:, 0, t:t + 1], in1=xo[b][:], op0=ALU.mult, op1=ALU.add),
                      r=[r_y1[b], r_route], w=[r_xo[b]])
                sc.op("pool", lambda e: e.scalar_tensor_tensor(out=xo[b][:], in0=y2[b][:], scalar=w12[:, 1, t:t + 1], in1=xo[b][:], op0=ALU.mult, op1=ALU.add),
                      r=[r_y2[b], r_route], w=[r_xo[b]]) if False else \
                    sc.op("dve", lambda e: e.scalar_tensor_tensor(out=xo[b][:], in0=y2[b][:], scalar=w12[:, 1, t:t + 1], in1=xo[b][:], op0=ALU.mult, op1=ALU.add),
                          r=[r_y2[b], r_route], w=[r_xo[b]])
                sc.dma("sp", lambda e: e.dma_start(out=out_d[ts_, :], in_=xo[b][:]), r=[r_xo[b]], w=[r_x2d[t]], is_out=True)
            run_pipelined(p6_tile, NT, 2)
            sc.barrier()
            p6s.close()

        sc.finish()
    print("program: %d instructions, %d waits" % (sc.n_inst, sc.n_wait))
    return nc, dbg_out


def _perm_rows(w, c):
    n = w.shape[1]
    return np.ascontiguousarray(w.reshape(c, 128, n).transpose(1, 0, 2))


def _consts():
    cst = np.zeros((128, NCST), np.float64)
    j64 = np.arange(64)
    j32 = np.arange(32)
    invR = 10000.0 ** (-j64 / 64.0) / (2 * np.pi)
    invM = 10000.0 ** (-j32 / 32.0) / (2 * np.pi)
    cst[:, 0:64] = invR
    cst[:, 64:128] = invR
    cst[:, 128:160] = invM
    cst[:, 160:192] = invM
    cst[:, 192 + 64:192 + 128] = 0.25
    cst[:, 192 + 160:192 + 192] = 0.25
    h = np.arange(4)
    lg = np.log(1.0 - np.exp2(-5.0 - h))
    p = np.arange(128)[:, None]
    cst[:, 384:388] = np.exp((p + 1.0) * lg[None, :])
    cst[:, 388:392] = np.exp(-(p + 1.0) * lg[None, :]) * (128.0 ** -0.5)
    cst[:, 392:904] = np.repeat(np.exp(128.0 * lg), 128)[None, :]
    return cst.astype(np.float32)


def make_in_maps(inputs, S, n_cores, last_phase=6):
    f = lambda a: np.ascontiguousarray(np.asarray(a), dtype=np.float32)
    l = 0
    shared = {
        "cst": _consts(),
        "w_in": _perm_rows(f(inputs["w_in"][l]), 8),
        "g_attn": np.ascontiguousarray(f(inputs["attn_norm_g"][l]).reshape(8, 128).T),
        "w_uq": _perm_rows(f(inputs["mla_w_uq"][l]), 2),
        "g_qn": np.ascontiguousarray(f(inputs["mla_q_norm_g"][l]).reshape(2, 128).T),
        "w_ukv": f(inputs["mla_w_ukv"][l]),
        "g_kvn": f(inputs["mla_kv_norm_g"][l]).reshape(128, 1),
        "gq": f(inputs["mla_q_qk_g"][l]).reshape(1, 192),
        "gk": f(inputs["mla_k_qk_g"][l]).reshape(1, 192),
        "gn": f(inputs["ret_gn_g"][l]).reshape(1, 512),
        "w_out": _perm_rows(f(inputs["w_out"][l]), 8),
        "g_cross": np.ascontiguousarray(f(inputs["cross_norm_g"][l]).reshape(8, 128).T),
        "g_mem": np.ascontiguousarray(f(inputs["mem_norm_g"][l]).reshape(8, 128).T),
        "cw_q": _perm_rows(f(inputs["cross_w_q"][l]), 8),
        "cw_kv": _perm_rows(f(inputs["cross_w_kv"][l]), 8),
        "cqg": f(inputs["cross_q_qk_g"][l]).reshape(1, 256),
        "ckg": f(inputs["cross_k_qk_g"][l]).reshape(1, 256),
        "cw_o": _perm_rows(f(inputs["cross_w_o"][l]), 8),
        "mg": f(inputs["moe_norm_g"][l]).reshape(1, 1024),
        "w_rt": _perm_rows(np.concatenate([f(inputs["router_w_group"][l]), f(inputs["router_w_expert"][l])], axis=1), 8),
        "b_rt": np.concatenate([f(inputs["router_b_group"][l]), f(inputs["router_b_expert"][l])]).reshape(1, 36),
        "ew_g": np.ascontiguousarray(f(inputs["expert_w_gate"][l]).reshape(32, 8, 128, 256).transpose(0, 2, 1, 3)).reshape(32 * 128, 2048),
        "ew_u": np.ascontiguousarray(f(inputs["expert_w_up"][l]).reshape(32, 8, 128, 256).transpose(0, 2, 1, 3)).reshape(32 * 128, 2048),
        "ew_d": np.ascontiguousarray(f(inputs["expert_w_down"][l]).reshape(32, 2, 128, 1024).transpose(0, 2, 1, 3)).reshape(32 * 128, 2048),
    }
    if last_phase < 5:
        for k in ("ew_g", "ew_u", "ew_d"):
            del shared[k]
    NT = S // 128
    maps = []
    for b in range(n_cores):
        m = dict(shared)
        m["x"] = f(inputs["x"][b])
        m["mem"] = f(inputs["mem"][b])
        m["pos"] = np.ascontiguousarray(np.asarray(inputs["positions"][b]).astype(np.int32).reshape(NT, 128).T)
        maps.append(m)
    return maps


def kernel(**inputs):
    B, S, _ = inputs["x"].shape
    nc, _ = build_program(S)
    maps = make_in_maps(inputs, S, B)
    res = run_bass_kernel_spmd(nc, maps, core_ids=list(range(B)))
    return np.stack([np.asarray(r["out"]) for r in res.results], axis=0).astype(np.float32)
```

```python
import math
from contextlib import ExitStack

import numpy as np
import concourse.bass as bass
import concourse.mybir as mybir
from concourse.bass_utils import run_bass_kernel_spmd

F32 = mybir.dt.float32
BF16 = mybir.dt.bfloat16
I32 = mybir.dt.int32
AF = mybir.ActivationFunctionType
ALU = mybir.AluOpType
AX = mybir.AxisListType

D = 1024
EPS = 1e-6
NDMA = 24
NCST = 904


class Res:
    __slots__ = ("name", "w", "rd", "excl")

    def __init__(self, name, excl=False):
        self.name = name
        self.w = None
        self.rd = []
        self.excl = excl


class Sched:
    ENGS = ("pe", "act", "dve", "pool", "sp")

    def __init__(self, nc, es):
        self.nc = nc
        self.eng = {"pe": nc.tensor, "act": nc.scalar, "dve": nc.vector, "pool": nc.gpsimd, "sp": nc.sync}
        self.sem = {e: es.enter_context(nc.semaphore("c_" + e)) for e in self.ENGS}
        self.cnt = {e: 0 for e in self.ENGS}
        self.waited = {e: {} for e in self.ENGS}
        self.dsem = {q: [es.enter_context(nc.semaphore("d_%s%d" % (q, i))) for i in range(NDMA)] for q in ("sp", "pool", "act")}
        self.dcnt = {q: [0] * NDMA for q in ("sp", "pool", "act")}
        self.dnext = {q: 0 for q in ("sp", "pool", "act")}
        self.semname = {}
        self.out_tokens = []
        self.n_inst = 0
        self.n_wait = 0

    def res(self, name):
        return Res(name)

    def _wait(self, eng, tok):
        sem, val, src = tok
        if src == "pe" and eng == "pe":
            return
        k = id(sem)
        if self.waited[eng].get(k, 0) >= val:
            return
        self.eng[eng].wait_ge(sem, val)
        self.n_wait += 1
        self.waited[eng][k] = val

    def _deps(self, eng, r, w):
        w = list(w) + [x for x in r if x.excl]
        for x in r:
            if x.w is not None:
                self._wait(eng, x.w)
        for x in w:
            if x.w is not None:
                self._wait(eng, x.w)
            for t in x.rd:
                self._wait(eng, t)

    def _commit(self, tok, r, w):
        w = list(w) + [x for x in r if x.excl]
        r = [x for x in r if not x.excl]
        for x in r:
            x.rd = [t for t in x.rd if t[0] is not tok[0]] + [tok]
        for x in w:
            x.w = tok
            x.rd = []

    def op(self, eng, fn, r=(), w=()):
        self._deps(eng, r, w)
        inst = fn(self.eng[eng])
        self.cnt[eng] += 1
        self.n_inst += 1
        inst.then_inc(self.sem[eng], 1)
        tok = (self.sem[eng], self.cnt[eng], eng)
        self.waited[eng][id(self.sem[eng])] = max(self.waited[eng].get(id(self.sem[eng]), 0), 0)
        self._commit(tok, r, w)
        return tok

    def pe(self, fns, r=(), w=()):
        self._deps("pe", r, w)
        inst = None
        for fn in fns:
            inst = fn(self.eng["pe"])
            self.n_inst += 1
        self.cnt["pe"] += 1
        inst.then_inc(self.sem["pe"], 1)
        tok = (self.sem["pe"], self.cnt["pe"], "pe")
        self._commit(tok, r, w)
        return tok

    def dma(self, q, fn, r=(), w=(), is_out=False):
        self._deps(q, r, w)
        i = self.dnext[q] % NDMA
        self.dnext[q] += 1
        sem = self.dsem[q][i]
        if self.dcnt[q][i] > 0:
            self._wait(q, (sem, 16 * self.dcnt[q][i], "dma"))
        inst = fn(self.eng[q])
        self.n_inst += 1
        self.dcnt[q][i] += 1
        inst.then_inc(sem, 16)
        tok = (sem, 16 * self.dcnt[q][i], "dma")
        self._commit(tok, r, w)
        if is_out:
            self.out_tokens.append(tok)
        return tok

    def barrier(self):
        toks = [(self.sem[e], self.cnt[e], e) for e in self.ENGS if self.cnt[e] > 0]
        for q in ("sp", "pool", "act"):
            for i in range(NDMA):
                if self.dcnt[q][i] > 0:
                    toks.append((self.dsem[q][i], 16 * self.dcnt[q][i], "dma"))
        for e in self.ENGS:
            for t in toks:
                if t[2] == e and e != "pe":
                    pass
                self._wait_force(e, t)

    def _wait_force(self, eng, tok):
        sem, val, src = tok
        k = id(sem)
        if self.waited[eng].get(k, 0) >= val:
            return
        self.eng[eng].wait_ge(sem, val)
        self.n_wait += 1
        self.waited[eng][k] = val

    def finish(self):
        for t in self.out_tokens:
            self._wait_force("sp", t)
        self.barrier()


def build_program(S, last_phase=6, dbg=()):
    NT = S // 128
    NB = S // 512
    nc = bass.Bass("TRN2", target_bir_lowering=False)

    def din(name, shape, dt=F32):
        return nc.dram_tensor(name, list(shape), dt, kind="ExternalInput").ap()

    x_d = din("x", [S, D])
    mem_d = din("mem", [256, D])
    pos_d = din("pos", [128, NT], I32)
    cst_d = din("cst", [128, NCST])
    w_in_d = din("w_in", [128, 8, 2496])
    g_attn_d = din("g_attn", [128, 8])
    w_uq_d = din("w_uq", [128, 2, 768])
    g_qn_d = din("g_qn", [128, 2])
    w_ukv_d = din("w_ukv", [128, 1024])
    g_kvn_d = din("g_kvn", [128, 1])
    gq_d = din("gq", [1, 192])
    gk_d = din("gk", [1, 192])
    gn_d = din("gn", [1, 512])
    w_out_d = din("w_out", [128, 8, 1024])
    g_cross_d = din("g_cross", [128, 8])
    g_mem_d = din("g_mem", [128, 8])
    cw_q_d = din("cw_q", [128, 8, 1024])
    cw_kv_d = din("cw_kv", [128, 8, 2048])
    cqg_d = din("cqg", [1, 256])
    ckg_d = din("ckg", [1, 256])
    cw_o_d = din("cw_o", [128, 8, 1024])
    mg_d = din("mg", [1, 1024])
    w_rt_d = din("w_rt", [128, 8, 36])
    b_rt_d = din("b_rt", [1, 36])
    if last_phase >= 5:
        ew_g_d = din("ew_g", [32 * 128, 2048])
        ew_u_d = din("ew_u", [32 * 128, 2048])
        ew_d_d = din("ew_d", [32 * 128, 2048])
    out_d = nc.dram_tensor("out", [S, D], F32, kind="ExternalOutput").ap()

    dbg_out = {}

    with ExitStack() as es:
        sc = Sched(nc, es)

        def sb(name, shape, dt=F32, stack=es):
            return stack.enter_context(nc.sbuf_tensor("s_" + name, list(shape), dt))

        banks = [es.enter_context(nc.psum_tensor("bank%d" % i, [128, 512], F32)) for i in range(8)]
        bank_res = [Res("bank%d" % i, excl=True) for i in range(8)]
        bstate = {"i": 0}

        def nbank():
            i = bstate["i"] % 8
            bstate["i"] += 1
            return banks[i], bank_res[i]

        def dump(name, ap, res, shape, dt=F32):
            if name not in dbg:
                return
            t = nc.dram_tensor("dbg_" + name, list(shape), dt, kind="ExternalOutput").ap()
            dbg_out[name] = t
            sc.dma("sp", lambda e: e.dma_start(out=t, in_=ap), r=[res], w=[], is_out=True)

        def nbank(lo=0, hi=8):
            key = (lo, hi)
            i = lo + bstate.get(key, 0) % (hi - lo)
            bstate[key] = bstate.get(key, 0) + 1
            return banks[i], bank_res[i]

        cst = sb("cst", [128, NCST])
        r_cst = sc.res("cst")
        sc.dma("sp", lambda e: e.dma_start(out=cst[:], in_=cst_d[:, :]), w=[r_cst])
        INVF = cst[:, 0:192]
        OFFS = cst[:, 192:384]
        QDc = cst[:, 384:388]
        KDc = cst[:, 388:392]
        CDEC = cst[:, 392:904]

        ident_bf = sb("ident_bf", [128, 128], BF16)
        ident_f = sb("ident_f", [128, 128], F32)
        maskT = sb("maskT", [128, 128], BF16)
        mask4 = sb("mask4", [128, 4, 128], F32)
        tri = sb("tri", [128, 128], BF16)
        ones_bf = sb("ones_bf", [128, 128], BF16)
        r_const = sc.res("consts")

        def mk_mask(t_ap, pattern, cmp):
            sc.op("pool", lambda e: e.memset(t_ap, 1.0), w=[r_const])
            sc.op("pool", lambda e: e.affine_select(out=t_ap, in_=t_ap, pattern=pattern, compare_op=cmp, fill=0.0,
                                                    base=0, channel_multiplier=-1), r=[r_const], w=[r_const])

        mk_mask(ident_bf[:], [[1, 128]], ALU.is_equal)
        mk_mask(ident_f[:], [[1, 128]], ALU.is_equal)
        mk_mask(maskT[:], [[1, 128]], ALU.is_ge)
        mk_mask(mask4[:], [[0, 4], [1, 128]], ALU.is_ge)
        mk_mask(tri[:], [[1, 128]], ALU.is_gt)
        negm = sb("negm", [128, 128], BF16)
        sc.op("pool", lambda e: e.memset(negm[:], -30000.0), w=[r_const])
        sc.op("pool", lambda e: e.affine_select(out=negm[:], in_=negm[:], pattern=[[-1, 128]], compare_op=ALU.is_gt, fill=0.0,
                                                base=0, channel_multiplier=1), r=[r_const], w=[r_const])
        sc.op("pool", lambda e: e.memset(ones_bf[:], 1.0), w=[r_const])

        def bload(name, src, n, stack=es):
            t = sb(name, [128, n], F32, stack)
            sc.dma("sp", lambda e: e.dma_start(out=t[:], in_=src.partition_broadcast(128)), w=[r_const])
            return t

        def pload(name, src, n, stack=es):
            t = sb(name, [128, n], F32, stack)
            sc.dma("sp", lambda e: e.dma_start(out=t[:], in_=src[:, :]), w=[r_const])
            return t

        gq = bload("gq", gq_d, 192)
        gk = bload("gk", gk_d, 192)
        gn = bload("gn", gn_d, 512)

        pos_i = sb("pos_i", [128, NT], I32)
        pos_f = sb("pos_f", [128, NT])
        SCm = sb("SCm", [128, NT, 64])
        rstd1 = sb("rstd1", [128, NT])
        r_sc = sc.res("SC")
        r_rstd1 = sc.res("rstd1")
        stage = [None, None]
        r_stage = [sc.res("stage%d" % i) for i in range(2)]
        st = {"i": 0}
        scale_engs = ("dve", "act")

        def load_scaled(dst_fn, src_fn, gain_fn, C, N, rdst):
            for c in range(C):
                for n0 in range(0, N, 1024):
                    n1 = min(N, n0 + 1024)
                    i = st["i"] % 2
                    st["i"] += 1
                    stg, rs = stage[i], r_stage[i]
                    sc.dma("sp", lambda e: e.dma_start(out=stg[:, 0:n1 - n0], in_=src_fn(c, n0, n1)), w=[rs])
                    if scale_engs[i] == "act":
                        sc.op("act", lambda e: e.activation(out=dst_fn(c, n0, n1), in_=stg[:, 0:n1 - n0], func=AF.Copy, scale=gain_fn(c)),
                              r=[rs, r_const], w=[rdst])
                    else:
                        sc.op("dve", lambda e: e.tensor_scalar(out=dst_fn(c, n0, n1), in0=stg[:, 0:n1 - n0], scalar1=gain_fn(c), scalar2=None,
                                                               op0=ALU.mult), r=[rs, r_const], w=[rdst])

        def load_cast(dst_fn, src_fn, C, rdst):
            for c in range(C):
                sc.dma("pool", lambda e: e.dma_start(out=dst_fn(c), in_=src_fn(c)), w=[rdst])

        mhalf = sb("mhalf", [128, 8])
        sc.op("pool", lambda e: e.memset(mhalf[:], -0.5), w=[r_const])

        def rstd_from_ss(ss_ap, n, out_ap, r_in, r_out, tmp_ap, r_tmp):
            k = ss_ap.shape[1]
            sc.op("dve", lambda e: e.tensor_scalar(out=tmp_ap, in0=ss_ap, scalar1=1.0 / n, scalar2=EPS, op0=ALU.mult, op1=ALU.add),
                  r=[r_in], w=[r_tmp])
            sc.op("pool", lambda e: e.tensor_tensor(out=out_ap, in0=tmp_ap, in1=mhalf[:, 0:k], op=ALU.pow), r=[r_tmp, r_const], w=[r_out])

        sc.dma("sp", lambda e: e.dma_start(out=pos_i[:], in_=pos_d[:, :]), w=[r_sc])
        sc.op("dve", lambda e: e.tensor_copy(out=pos_f[:], in_=pos_i[:]), r=[r_sc], w=[r_sc])

        rout_d = nc.dram_tensor("rout_s", [S, 512], BF16).ap()
        x1_d = nc.dram_tensor("x1_s", [S, D], F32).ap()
        hm_d = nc.dram_tensor("hm_s", [S, D], BF16).ap()
        TS = 256
        NTS = (2 * S) // TS + 32
        NPOS = NTS * TS
        xs_d = nc.dram_tensor("xs_s", [NPOS, D], BF16).ap()
        ys_d = nc.dram_tensor("ys_s", [NPOS, D], F32).ap()
        r_xs = sc.res("xs_d")

        def run_pipelined(gen_fn, n_items, depth):
            active = []
            nxt = 0
            while nxt < n_items or active:
                if nxt < n_items and len(active) < depth:
                    active.append(gen_fn(nxt))
                    nxt += 1
                for g in list(active):
                    try:
                        next(g)
                    except StopIteration:
                        active.remove(g)

        def mkres(n, k=2):
            return [sc.res("%s%d" % (n, i)) for i in range(k)]

        if last_phase >= 1:
            p1s = es.enter_context(ExitStack())
            import os as _os
            stop = int(_os.environ.get('P1_STOP', '99'))
            SCr = sb("SCr", [128, NT, 128], F32, p1s)
            w_r = sb("w_r", [128, 8, 2048], BF16, p1s)
            r_wr = sc.res("w_r")
            stage[:] = [sb("stage1_%d" % i, [128, 1024], F32, p1s) for i in range(2)]
            g_attn = pload("g_attn", g_attn_d, 8, p1s)
            load_scaled(lambda c, a, b_: w_r[:, c, a:b_], lambda c, a, b_: w_in_d[:, c, 448 + a:448 + b_], lambda c: g_attn[:, c:c + 1], 8, 2048, r_wr)

            NTsc = NT if stop >= 2 else 0
            tr_t = sb("tr_t", [128, 192], F32, p1s)
            tr_i = sb("tr_i", [128, 192], I32, p1s)
            tr_f = sb("tr_f", [128, 192], F32, p1s)
            r_tr = sc.res("tr")
            for t in range(NTsc):
                sc.op("dve", lambda e: e.scalar_tensor_tensor(out=tr_t[:], in0=INVF, scalar=pos_f[:, t:t + 1], in1=OFFS,
                                                              op0=ALU.mult, op1=ALU.add), r=[r_cst, r_sc], w=[r_tr])
                sc.op("dve", lambda e: e.tensor_copy(out=tr_i[:], in_=tr_t[:]), r=[r_tr], w=[r_tr])
                sc.op("dve", lambda e: e.tensor_copy(out=tr_f[:], in_=tr_i[:]), r=[r_tr], w=[r_tr])
                sc.op("dve", lambda e: e.tensor_tensor(out=tr_t[:], in0=tr_t[:], in1=tr_f[:], op=ALU.subtract), r=[r_tr], w=[r_tr])
                sc.op("dve", lambda e: e.scalar_tensor_tensor(out=tr_f[:], in0=tr_t[:], scalar=0.5, in1=tr_t[:],
                                                              op0=ALU.is_gt, op1=ALU.subtract), r=[r_tr], w=[r_tr])
                sc.op("dve", lambda e: e.scalar_tensor_tensor(out=tr_t[:], in0=tr_f[:], scalar=0.5, in1=tr_f[:],
                                                              op0=ALU.is_gt, op1=ALU.subtract), r=[r_tr], w=[r_tr])
                sc.op("act", lambda e: e.activation(out=SCr[:, t, :], in_=tr_t[:, 0:128], func=AF.Sin, scale=6.28318), r=[r_tr], w=[r_sc])
                sc.op("act", lambda e: e.activation(out=SCm[:, t, :], in_=tr_t[:, 128:192], func=AF.Sin, scale=6.28318), r=[r_tr], w=[r_sc])
            dump("SCr", SCr[:], r_sc, [128, NT, 128])
            dump("SCm", SCm[:], r_sc, [128, NT, 64])

            do_pc = last_phase >= 5
            if do_pc:
                ewb_all = nc.dram_tensor("ewb_all", [32 * 128, 6144], BF16).ap()
                ewb_d = [ewb_all[:, k * 2048:(k + 1) * 2048] for k in range(3)]
                ew_src = [ew_g_d, ew_u_d, ew_d_d]
                r_ewb = sc.res("ewb")
                pcs = [sb("pcs%d" % i, [128, 2048], BF16, p1s) for i in range(3)]
                r_pcs = mkres("pcs", 3)
                pc_state = {"i": 0}
                PC_PER_TILE = (96 + NT - 1) // NT

                pend_wb = []

                def precast_flush():
                    while pend_wb:
                        k_, rows, bi = pend_wb.pop(0)
                        sc.dma("sp", lambda e: e.dma_start(out=ewb_d[k_][rows, :], in_=pcs[bi][:]), r=[r_pcs[bi]], w=[r_ewb])

                def precast_step():
                    precast_flush()
                    for _ in range(PC_PER_TILE):
                        i = pc_state["i"]
                        if i >= 96:
                            return
                        pc_state["i"] += 1
                        if len(pend_wb) >= len(pcs):
                            precast_flush()
                        e_, k_ = i // 3, i % 3
                        bi = i % len(pcs)
                        rows = slice(e_ * 128, (e_ + 1) * 128)
                        sc.dma("pool", lambda e: e.dma_start(out=pcs[bi][:], in_=ew_src[k_][rows, :]), w=[r_pcs[bi]])
                        pend_wb.append((k_, rows, bi))

            zt = sb("zt", [128, 2, 1024], BF16, p1s)
            r_zt = sc.res("zt")
            sc.op("pool", lambda e: e.memset(zt[:], 0.0), w=[r_zt])
            ROWS_PER = NPOS // NT

            def zero_step(t):
                if last_phase < 4:
                    return
                for r0_ in range(t * ROWS_PER, (t + 1) * ROWS_PER, 256):
                    sc.dma("sp", lambda e: e.dma_start(out=xs_d[r0_:r0_ + 256, :].rearrange("(p a) d -> p a d", a=2), in_=zt[:]), r=[r_zt], w=[r_xs])

            xt = [sb("xt%d" % i, [128, 1024], F32, p1s) for i in range(4)]
            xb = [sb("xb%d" % i, [128, 1024], BF16, p1s) for i in range(4)]
            xT = [sb("xT%d" % i, [128, 8, 128], BF16, p1s) for i in range(4)]
            junk = sb("junk", [128, 1024], BF16, p1s)
            rq_f = [sb("rq_f%d" % i, [128, 512], F32, p1s) for i in range(4)]
            rk_f = [sb("rk_f%d" % i, [128, 512], F32, p1s) for i in range(4)]
            v_b = [sb("v_b%d" % i, [128, 512], BF16, p1s) for i in range(4)]
            sg = [sb("sg%d" % i, [128, 512], F32, p1s) for i in range(6)]
            sm1 = [sb("sm1_%d" % i, [128, 32], F32, p1s) for i in range(4)]
            rp = [sb("rp%d" % i, [128, 2, 512], F32, p1s) for i in range(2)]
            qp_b = [sb("qp_b%d" % i, [128, 512], BF16, p1s) for i in range(4)]
            kp_b = [sb("kp_b%d" % i, [128, 512], BF16, p1s) for i in range(4)]
            qpT = [sb("qpT%d" % i, [128, 4, 128], BF16, p1s) for i in range(4)]
            kpT = [sb("kpT%d" % i, [128, 4, 128], BF16, p1s) for i in range(4)]
            PT = [sb("PT%d" % i, [128, 4, 128], BF16, p1s) for i in range(3)]
            Tst = sb("Tst", [128, 4, 128], F32, p1s)
            Tst_b = sb("Tst_b", [128, 4, 128], BF16, p1s)
            Ttmp = sb("Ttmp", [128, 4, 128], F32, p1s)
            o_f = [sb("o_f%d" % i, [128, 4, 128], F32, p1s) for i in range(4)]
            bnst = [sb("bnst%d" % i, [128, 4, 6], F32, p1s) for i in range(4)]
            bnag = [sb("bnag%d" % i, [128, 4, 2], F32, p1s) for i in range(4)]
            ro_b = [sb("ro_b%d" % i, [128, 512], BF16, p1s) for i in range(3)]

            r_xt, r_xb, r_xT = mkres("xt", 4), mkres("xb", 4), mkres("xT", 4)
            r_rq, r_rk, r_vb, r_sg, r_sm1 = mkres("rq", 4), mkres("rk", 4), mkres("vb", 4), mkres("sg", 6), mkres("sm1", 4)
            r_rp = mkres("rp")
            r_qpb, r_kpb, r_qpT, r_kpT, r_PT, r_of, r_bn, r_rob = (mkres("qpb", 4), mkres("kpb", 4), mkres("qpT", 4), mkres("kpT", 4), mkres("PT", 3),
                                                                   mkres("of", 4), mkres("bn", 4), mkres("rob", 3))
            r_junk = sc.res("junk")
            r_T = sc.res("Tst")
            r_Tb = sc.res("Tst_b")
            r_Tt = sc.res("Ttmp")
            sc.op("dve", lambda e: e.memset(Tst[:], 0.0), w=[r_T])
            sc.op("dve", lambda e: e.memset(Tst_b[:], 0.0), w=[r_Tb])

            def rope_ret(eng, src, r_src, dst_b, r_dst, t, decay, scr, r_scr):
                cosB = SCr[:, t, 64:128].unsqueeze(1).broadcast_to([128, 8, 64])
                sinB = SCr[:, t, 0:64].unsqueeze(1).broadcast_to([128, 4, 64])
                sv = src[:].rearrange("p (h two d) -> p h two d", h=4, two=2)
                Pv = scr[:, 0, :]
                Qv = scr[:, 1, :].rearrange("p (h two d) -> p h two d", h=4, two=2)
                P4 = scr[:, 0, :].rearrange("p (h two d) -> p h two d", h=4, two=2)
                sc.op(eng, lambda e: e.tensor_tensor(out=Pv.rearrange("p (g d) -> p g d", g=8), in0=src[:].rearrange("p (g d) -> p g d", g=8),
                                                     in1=cosB, op=ALU.mult), r=[r_src, r_sc], w=[r_scr])
                sc.op(eng, lambda e: e.tensor_tensor(out=Qv[:, :, 0, :], in0=sv[:, :, 1, :], in1=sinB, op=ALU.mult), r=[r_src, r_sc], w=[r_scr])
                sc.op(eng, lambda e: e.tensor_tensor(out=Qv[:, :, 1, :], in0=sv[:, :, 0, :], in1=sinB, op=ALU.mult), r=[r_src, r_sc], w=[r_scr])
                sc.op(eng, lambda e: e.tensor_tensor(out=P4[:, :, 0, :], in0=P4[:, :, 0, :], in1=Qv[:, :, 0, :], op=ALU.subtract), r=[r_scr], w=[r_scr])
                sc.op(eng, lambda e: e.tensor_tensor(out=P4[:, :, 1, :], in0=P4[:, :, 1, :], in1=Qv[:, :, 1, :], op=ALU.add), r=[r_scr], w=[r_scr])
                decB = decay.unsqueeze(2).broadcast_to([128, 4, 128])
                sc.op(eng, lambda e: e.tensor_tensor(out=dst_b[:].rearrange("p (h d) -> p h d", h=4), in0=Pv.rearrange("p (h d) -> p h d", h=4),
                                                     in1=decB, op=ALU.mult), r=[r_scr, r_cst], w=[r_dst])

            def p1_tile(t):
                b = t % 4
                b6 = t % 6
                b3 = t % 3
                ts_ = slice(t * 128, (t + 1) * 128)
                s1 = sm1[b]
                sc.dma("sp", lambda e: e.dma_start(out=xt[b][:], in_=x_d[ts_, :]), w=[r_xt[b]])
                if do_pc:
                    precast_step()
                zero_step(t)
                sc.op("act", lambda e: e.activation(out=junk[:], in_=xt[b][:], func=AF.Square, accum_out=s1[:, 0:1]),
                      r=[r_xt[b]], w=[r_junk, r_sm1[b]])
                rstd_from_ss(s1[:, 0:1], 1024.0, rstd1[:, t:t + 1], r_sm1[b], r_rstd1, s1[:, 1:2], r_sm1[b])
                yield
                sc.op("act", lambda e: e.copy(out=xb[b][:], in_=xt[b][:]), r=[r_xt[b]], w=[r_xb[b]])
                bk, rb = nbank()
                pv = bk[:].bitcast(BF16).rearrange("p (c n) -> p c n", n=128)
                sc.pe([(lambda e, c=c: e.transpose(out=pv[:, c, :], in_=xb[b][:, c * 128:(c + 1) * 128], identity=ident_bf[:])) for c in range(8)],
                      r=[r_xb[b], r_const], w=[rb])
                sc.op("dve", lambda e: e.tensor_copy(out=xT[b][:], in_=pv), r=[rb], w=[r_xT[b]])
                rs1 = rstd1[:, t:t + 1]
                yield
                pb = []
                for k4 in range(4):
                    bk, rb = nbank()
                    sc.pe([(lambda e, c=c: e.matmul(bk[:], lhsT=xT[b][:, c, :], rhs=w_r[:, c, k4 * 512:(k4 + 1) * 512], start=(c == 0), stop=(c == 7)))
                           for c in range(8)], r=[r_xT[b], r_wr], w=[rb])
                    pb.append((bk, rb))
                sc.op("act", lambda e: e.activation(out=rq_f[b][:], in_=pb[0][0][:], func=AF.Copy, scale=rs1), r=[pb[0][1], r_rstd1], w=[r_rq[b]])
                sc.op("dve", lambda e: e.tensor_scalar(out=rk_f[b][:], in0=pb[1][0][:], scalar1=rs1, scalar2=None, op0=ALU.mult),
                      r=[pb[1][1], r_rstd1], w=[r_rk[b]])
                sc.op("act", lambda e: e.activation(out=v_b[b][:], in_=pb[2][0][:], func=AF.Copy, scale=rs1), r=[pb[2][1], r_rstd1], w=[r_vb[b]])
                sc.op("act", lambda e: e.activation(out=sg[b6][:], in_=pb[3][0][:], func=AF.Silu, scale=rs1), r=[pb[3][1], r_rstd1], w=[r_sg[b6]])

                yield
                rope_ret("dve", rq_f[b], r_rq[b], qp_b[b], r_qpb[b], t, QDc, rp[0], r_rp[0])
                rope_ret("pool", rk_f[b], r_rk[b], kp_b[b], r_kpb[b], t, KDc, rp[1], r_rp[1])
                yield
                bk, rb = nbank()
                pv = bk[:].bitcast(BF16).rearrange("p (c n) -> p c n", n=128)
                sc.pe([(lambda e, h=h: e.transpose(out=pv[:, h, :], in_=qp_b[b][:, h * 128:(h + 1) * 128], identity=ident_bf[:])) for h in range(4)]
                      + [(lambda e, h=h: e.transpose(out=pv[:, 4 + h, :], in_=kp_b[b][:, h * 128:(h + 1) * 128], identity=ident_bf[:])) for h in range(4)],
                      r=[r_qpb[b], r_kpb[b], r_const], w=[rb])
                sc.op("act", lambda e: e.copy(out=qpT[b][:], in_=pv[:, 0:4, :]), r=[rb], w=[r_qpT[b]])
                sc.op("dve", lambda e: e.tensor_copy(out=kpT[b][:], in_=pv[:, 4:8, :]), r=[rb], w=[r_kpT[b]])
                bk, rb = nbank()
                sv_ = bk[:].rearrange("p (h n) -> p h n", h=4)
                sc.pe([(lambda e, h=h: e.matmul(sv_[:, h, :], lhsT=kpT[b][:, h, :], rhs=qpT[b][:, h, :], start=True, stop=True)) for h in range(4)],
                      r=[r_kpT[b], r_qpT[b]], w=[rb])
                sc.op("dve", lambda e: e.tensor_tensor(out=PT[b3][:], in0=sv_, in1=mask4[:], op=ALU.mult), r=[rb, r_const], w=[r_PT[b3]])
                yield
                bk_o, rb_o = nbank()
                ov = bk_o[:].rearrange("p (h n) -> p h n", h=4)
                fns = []
                for h in range(4):
                    fns.append(lambda e, h=h: e.matmul(ov[:, h, :], lhsT=PT[b3][:, h, :], rhs=v_b[b][:, h * 128:(h + 1) * 128], start=True, stop=False))
                    fns.append(lambda e, h=h: e.matmul(ov[:, h, :], lhsT=qpT[b][:, h, :], rhs=Tst_b[:, h, :], start=False, stop=True))
                sc.pe(fns, r=[r_PT[b3], r_vb[b], r_qpT[b], r_Tb], w=[rb_o])
                bk_s, rb_s = nbank()
                stv = bk_s[:].rearrange("p (h n) -> p h n", h=4)
                sc.pe([(lambda e, h=h: e.matmul(stv[:, h, :], lhsT=kp_b[b][:, h * 128:(h + 1) * 128], rhs=v_b[b][:, h * 128:(h + 1) * 128],
                                                start=True, stop=True)) for h in range(4)], r=[r_kpb[b], r_vb[b]], w=[rb_s])
                sc.op("dve", lambda e: e.tensor_tensor(out=Ttmp[:], in0=stv, in1=Tst[:], op=ALU.add), r=[rb_s, r_T], w=[r_Tt])
                sc.op("pool", lambda e: e.tensor_tensor(out=Tst[:], in0=Ttmp[:], in1=CDEC.rearrange("p (h n) -> p h n", h=4), op=ALU.mult),
                      r=[r_Tt, r_cst], w=[r_T])
                sc.op("pool", lambda e: e.tensor_copy(out=Tst_b[:], in_=Tst[:]), r=[r_T], w=[r_Tb])
                yield
                sc.op("act", lambda e: e.copy(out=o_f[b][:], in_=ov), r=[rb_o], w=[r_of[b]])
                for h in range(4):
                    sc.op("dve", lambda e: e.bn_stats(out=bnst[b][:, h, :], in_=o_f[b][:, h, :]), r=[r_of[b]], w=[r_bn[b]])
                for h in range(4):
                    sc.op("dve", lambda e: e.bn_aggr(out=bnag[b][:, h, :], in_=bnst[b][:, h, :]), r=[r_bn[b]], w=[r_bn[b]])
                sc.op("dve", lambda e: e.tensor_scalar(out=s1[:, 20:24], in0=bnag[b][:, :, 1], scalar1=EPS, scalar2=None, op0=ALU.add),
                      r=[r_bn[b]], w=[r_sm1[b]])
                sc.op("pool", lambda e: e.tensor_tensor(out=s1[:, 24:28], in0=s1[:, 20:24], in1=mhalf[:, 0:4], op=ALU.pow), r=[r_sm1[b], r_const], w=[r_sm1[b]])
                for h in range(4):
                    sc.op("dve", lambda e: e.tensor_scalar(out=o_f[b][:, h, :], in0=o_f[b][:, h, :], scalar1=bnag[b][:, h, 0:1], scalar2=s1[:, 24 + h:25 + h],
                                                           op0=ALU.subtract, op1=ALU.mult), r=[r_of[b], r_bn[b], r_sm1[b]], w=[r_of[b]])
                ofl = o_f[b][:].rearrange("p h n -> p (h n)")
                sc.op("pool", lambda e: e.tensor_tensor(out=ofl, in0=ofl, in1=gn[:], op=ALU.mult), r=[r_of[b], r_const], w=[r_of[b]])
                sc.op("pool", lambda e: e.tensor_tensor(out=ro_b[b3][:], in0=ofl, in1=sg[b6][:], op=ALU.mult), r=[r_of[b], r_sg[b6]], w=[r_rob[b3]])
                r_routd = sc.res("rout_d%d" % t)
                sc.dma("pool", lambda e: e.dma_start(out=rout_d[ts_, :], in_=ro_b[b3][:]), r=[r_rob[b3]], w=[r_routd])
                if t == NT - 1:
                    dump("ro_last", ro_b[b3][:], r_rob[b3], [128, 512], BF16)
            run_pipelined(p1_tile, NT, 4)
            if do_pc:
                while pc_state['i'] < 96:
                    precast_step()
                precast_flush()
            dump("rstd1", rstd1[:], r_rstd1, [128, NT])
            sc.barrier()
            p1s.close()

        if last_phase >= 2:
            p2s = es.enter_context(ExitStack())
            KTn = sb("KTn", [128, 4, S], BF16, p2s)
            KTr = sb("KTr", [128, 2, S], BF16, p2s)
            Vaug = sb("Vaug", [128, NT, 4, 129], BF16, p2s)
            r_KT = [sc.res("KT%d" % t) for t in range(NT)]
            r_V = sc.res("Vaug")
            sc.op("pool", lambda e: e.memset(Vaug[:], 1.0), w=[r_V])
            w_a = sb("w_a", [128, 8, 448], BF16, p2s)
            w_uq = sb("w_uq", [128, 2, 768], BF16, p2s)
            w_ukv = sb("w_ukv", [128, 1024], BF16, p2s)
            w_out = sb("w_out", [128, 8, 1024], BF16, p2s)
            r_w2 = sc.res("w2")
            g_attn2 = pload("g_attn2", g_attn_d, 8, p2s)
            g_qn = pload("g_qn", g_qn_d, 2, p2s)
            g_kvn = pload("g_kvn", g_kvn_d, 1, p2s)
            pw2 = es.enter_context(ExitStack())
            stage[:] = [sb("stage2_%d" % i, [128, 1024], F32, pw2) for i in range(2)]
            load_scaled(lambda c, a, b_: w_a[:, c, a:b_], lambda c, a, b_: w_in_d[:, c, a:b_], lambda c: g_attn2[:, c:c + 1], 8, 448, r_w2)
            load_scaled(lambda c, a, b_: w_uq[:, c, a:b_], lambda c, a, b_: w_uq_d[:, c, a:b_], lambda c: g_qn[:, c:c + 1], 2, 768, r_w2)
            load_scaled(lambda c, a, b_: w_ukv[:, a:b_], lambda c, a, b_: w_ukv_d[:, a:b_], lambda c: g_kvn[:, 0:1], 1, 1024, r_w2)
            load_cast(lambda c: w_out[:, c, :], lambda c: w_out_d[:, c, :], 8, r_w2)
            sc.barrier()
            pw2.close()

            xt = [sb("x2t%d" % i, [128, 1024], F32, p2s) for i in range(2)]
            xb = [sb("x2b%d" % i, [128, 1024], BF16, p2s) for i in range(2)]
            xT = [sb("x2T%d" % i, [128, 8, 128], BF16, p2s) for i in range(2)]
            junk = sb("junk2", [128, 768], F32, p2s)
            cq_f = [sb("cq_f%d" % i, [128, 256], F32, p2s) for i in range(3)]
            cq_b = [sb("cq_b%d" % i, [128, 256], BF16, p2s) for i in range(3)]
            cqT = [sb("cqT%d" % i, [128, 2, 128], BF16, p2s) for i in range(3)]
            ckv_f = [sb("ckv_f%d" % i, [128, 192], F32, p2s) for i in range(3)]
            ckv_b = [sb("ckv_b%d" % i, [128, 128], BF16, p2s) for i in range(3)]
            ckvT = [sb("ckvT%d" % i, [128, 128], BF16, p2s) for i in range(3)]
            kn_f = [sb("kn_f0", [128, 4, 128], F32, p2s)] * 3
            kn_b = [sb("kn_b%d" % i, [128, 4, 128], BF16, p2s) for i in range(2)]
            kpe = [sb("kpe0", [128, 3, 64], F32, p2s)] * 3
            kr = [sb("kr%d" % i, [128, 64], F32, p2s) for i in range(3)]
            krn_b = [sb("krn_b%d" % i, [128, 4, 64], BF16, p2s) for i in range(3)]
            q_f = [sb("q_f%d" % i, [128, 4, 192], F32, p2s) for i in range(2)]
            qn_b = [sb("qn_b%d" % i, [128, 4, 128], BF16, p2s) for i in range(2)]
            qr = [sb("qr0", [128, 4, 4, 64], F32, p2s)] * 3
            qrn_b = [sb("qrn_b%d" % i, [128, 4, 64], BF16, p2s) for i in range(3)]
            sm2 = [sb("sm2_%d" % i, [128, 48], F32, p2s) for i in range(3)]
            QTn = [sb("QTn%d" % i, [128, 4, 512], BF16, p2s) for i in range(2)]
            QTr = [sb("QTr%d" % i, [128, 2, 512], BF16, p2s) for i in range(2)]
            PTt = [sb("PTt%d" % i, [128, 512], BF16, p2s) for i in range(3)]
            a_b = [sb("a_b%d" % i, [128, 4, 512], BF16, p2s) for i in range(2)]
            rcp = [sb("rcp%d" % i, [128, 4], F32, p2s) for i in range(2)]
            r_b = [sb("r_b%d" % i, [128, 512], BF16, p2s) for i in range(2)]
            aoT = [sb("aoT%d" % i, [128, 8, 128], BF16, p2s) for i in range(2)]

            r_xt, r_xb, r_xT = mkres("x2t"), mkres("x2b"), mkres("x2T")
            r_junk = sc.res("junk2")
            r_cq, r_cqb, r_cqT, r_ckv, r_ckvb, r_ckvT = mkres("cq", 3), mkres("cqb", 3), mkres("cqT", 3), mkres("ckv", 3), mkres("ckvb", 3), mkres("ckvT", 3)
            r_kn, r_knb, r_kpe, r_kr, r_krn = mkres("kn", 1) * 3, mkres("knb"), mkres("kpe", 1) * 3, mkres("kr", 3), mkres("krn", 3)
            r_qf, r_qnb, r_qr, r_qrn, r_sm2 = mkres("qf"), mkres("qnb"), mkres("qr", 1) * 3, mkres("qrn", 3), mkres("sm2", 3)
            r_QT = mkres("QT")
            r_PTt = mkres("PTt", 3)
            r_ab, r_rcp, r_rb, r_aoT = mkres("ab"), mkres("rcp"), mkres("rb"), mkres("aoT")
            SCALE = 192.0 ** -0.5

            def prep_tile(t, qb):
                b = t % 3
                bx = t % 2
                ts_ = slice(t * 128, (t + 1) * 128)
                tl = slice((t % 4) * 128, (t % 4 + 1) * 128)
                s2 = sm2[b]
                rs1 = rstd1[:, t:t + 1]
                sc.dma("sp", lambda e: e.dma_start(out=xt[0][:], in_=x_d[ts_, :]), w=[r_xt[0]])
                sc.op("dve", lambda e: e.tensor_copy(out=xb[bx][:], in_=xt[0][:]), r=[r_xt[0]], w=[r_xb[bx]])
                yield
                bk, rb = nbank(4, 8)
                pv = bk[:].bitcast(BF16).rearrange("p (c n) -> p c n", n=128)
                sc.pe([(lambda e, c=c: e.transpose(out=pv[:, c, :], in_=xb[bx][:, c * 128:(c + 1) * 128], identity=ident_bf[:])) for c in range(8)],
                      r=[r_xb[bx], r_const], w=[rb])
                sc.op("dve", lambda e: e.tensor_copy(out=xT[bx][:], in_=pv), r=[rb], w=[r_xT[bx]])
                yield
                bk, rb = nbank(4, 8)
                sc.pe([(lambda e, c=c: e.matmul(bk[:, 0:448], lhsT=xT[bx][:, c, :], rhs=w_a[:, c, :], start=(c == 0), stop=(c == 7))) for c in range(8)],
                      r=[r_xT[bx], r_w2], w=[rb])
                sc.op("act", lambda e: e.activation(out=cq_f[b][:], in_=bk[:, 0:256], func=AF.Copy, scale=rs1), r=[rb, r_rstd1], w=[r_cq[b]])
                sc.op("act", lambda e: e.activation(out=ckv_f[b][:], in_=bk[:, 256:448], func=AF.Copy, scale=rs1), r=[rb, r_rstd1], w=[r_ckv[b]])
                yield
                sc.op("act", lambda e: e.activation(out=junk[:, 0:128], in_=ckv_f[b][:, 0:128], func=AF.Square, accum_out=s2[:, 2:3]), r=[r_ckv[b]], w=[r_junk, r_sm2[b]])
                rstd_from_ss(s2[:, 2:3], 128.0, s2[:, 3:4], r_sm2[b], r_sm2[b], s2[:, 4:5], r_sm2[b])
                sc.op("act", lambda e: e.activation(out=junk[:, 128:192], in_=ckv_f[b][:, 128:192], func=AF.Square, accum_out=s2[:, 5:6]), r=[r_ckv[b]], w=[r_junk, r_sm2[b]])
                sc.op("pool", lambda e: e.tensor_copy(out=ckv_b[b][:], in_=ckv_f[b][:, 0:128]), r=[r_ckv[b]], w=[r_ckvb[b]])
                bk, rb = nbank(4, 8)
                pv = bk[:].bitcast(BF16)
                sc.pe([lambda e: e.transpose(out=pv[:, 0:128], in_=ckv_b[b][:], identity=ident_bf[:])], r=[r_ckvb[b], r_const], w=[rb])
                sc.op("act", lambda e: e.copy(out=ckvT[b][:], in_=pv[:, 0:128]), r=[rb], w=[r_ckvT[b]])
                yield
                for hh in range(2):
                    bk, rb = nbank(4, 8)
                    sc.pe([lambda e: e.matmul(bk[:], lhsT=ckvT[b][:], rhs=w_ukv[:, hh * 512:(hh + 1) * 512], start=True, stop=True)],
                          r=[r_ckvT[b], r_w2], w=[rb])
                    kvv = bk[:].rearrange("p (h two d) -> p h two d", h=2, two=2)
                    sc.op("dve", lambda e: e.tensor_scalar(out=kn_f[b][:, 2 * hh:2 * hh + 2, :], in0=kvv[:, :, 0, :], scalar1=s2[:, 3:4], scalar2=None,
                                                           op0=ALU.mult), r=[rb, r_sm2[b]], w=[r_kn[b]])
                    sc.op("act", lambda e: e.activation(out=Vaug[:, t, 2 * hh:2 * hh + 2, 0:128], in_=kvv[:, :, 1, :], func=AF.Copy, scale=s2[:, 3:4]),
                          r=[rb, r_sm2[b]], w=[r_V])
                yield
                jk = junk[:, 0:512].rearrange("p (h d) -> p h d", h=4)
                sc.op("dve", lambda e: e.tensor_tensor(out=jk, in0=kn_f[b][:], in1=kn_f[b][:], op=ALU.mult), r=[r_kn[b]], w=[r_junk])
                sc.op("dve", lambda e: e.tensor_reduce(out=s2[:, 8:12], in_=jk, axis=AX.X, op=ALU.add), r=[r_junk], w=[r_sm2[b]])
                sc.op("dve", lambda e: e.tensor_scalar(out=s2[:, 8:12], in0=s2[:, 8:12], scalar1=s2[:, 5:6], scalar2=None, op0=ALU.add),
                      r=[r_sm2[b]], w=[r_sm2[b]])
                rstd_from_ss(s2[:, 8:12], 192.0, s2[:, 12:16], r_sm2[b], r_sm2[b], s2[:, 16:20], r_sm2[b])
                for h in range(4):
                    sc.op("dve", lambda e: e.scalar_tensor_tensor(out=kn_b[bx][:, h, :], in0=kn_f[b][:, h, :], scalar=s2[:, 12 + h:13 + h], in1=gk[:, 0:128],
                                                                  op0=ALU.mult, op1=ALU.mult), r=[r_kn[b], r_sm2[b], r_const], w=[r_knb[bx]])
                yield
                kp = kpe[b]
                sc.op("pool", lambda e: e.tensor_tensor(out=kp[:, 0, :], in0=ckv_f[b][:, 128:192], in1=gk[:, 128:192], op=ALU.mult),
                      r=[r_ckv[b], r_const], w=[r_kpe[b]])
                cosM2 = SCm[:, t, 32:64].unsqueeze(1).broadcast_to([128, 2, 32])
                sinM2 = SCm[:, t, 0:32].unsqueeze(1).broadcast_to([128, 2, 32])
                k0 = kp[:, 0, :].rearrange("p (two d) -> p two d", two=2)
                kA = kp[:, 1, :].rearrange("p (two d) -> p two d", two=2)
                kB = kp[:, 2, :].rearrange("p (two d) -> p two d", two=2)
                sc.op("pool", lambda e: e.tensor_tensor(out=kA, in0=k0, in1=cosM2, op=ALU.mult), r=[r_kpe[b], r_sc], w=[r_kpe[b]])
                sc.op("pool", lambda e: e.tensor_tensor(out=kB, in0=k0, in1=sinM2, op=ALU.mult), r=[r_kpe[b], r_sc], w=[r_kpe[b]])
                sc.op("pool", lambda e: e.tensor_tensor(out=kr[b][:, 0:32], in0=kp[:, 1, 0:32], in1=kp[:, 2, 32:64], op=ALU.subtract),
                      r=[r_kpe[b]], w=[r_kr[b]])
                sc.op("pool", lambda e: e.tensor_tensor(out=kr[b][:, 32:64], in0=kp[:, 1, 32:64], in1=kp[:, 2, 0:32], op=ALU.add),
                      r=[r_kpe[b]], w=[r_kr[b]])
                for h in range(4):
                    sc.op("dve", lambda e: e.tensor_scalar(out=krn_b[b][:, h, :], in0=kr[b][:], scalar1=s2[:, 12 + h:13 + h], scalar2=None, op0=ALU.mult),
                          r=[r_kr[b], r_sm2[b]], w=[r_krn[b]])
                bk, rb = nbank(4, 8)
                pv = bk[:].bitcast(BF16).rearrange("p (c n) -> p c n", n=128)
                sc.pe([(lambda e, h=h: e.transpose(out=pv[:, h, :], in_=kn_b[bx][:, h, :], identity=ident_bf[:])) for h in range(4)]
                      + [(lambda e, pr=pr: e.transpose(out=pv[:, 4 + pr, :], in_=krn_b[b][:, 2 * pr:2 * pr + 2, :].rearrange("p a d -> p (a d)"),
                                                       identity=ident_bf[:])) for pr in range(2)],
                      r=[r_knb[bx], r_krn[b], r_const], w=[rb])
                sc.op("act", lambda e: e.copy(out=KTn[:, :, ts_], in_=pv[:, 0:4, :]), r=[rb], w=[r_KT[t]])
                sc.op("dve", lambda e: e.tensor_copy(out=KTr[:, :, ts_], in_=pv[:, 4:6, :]), r=[rb], w=[r_KT[t]])
                yield
                sc.op("act", lambda e: e.activation(out=junk[:, 0:256], in_=cq_f[b][:], func=AF.Square, accum_out=s2[:, 20:21]), r=[r_cq[b]], w=[r_junk, r_sm2[b]])
                rstd_from_ss(s2[:, 20:21], 256.0, s2[:, 21:22], r_sm2[b], r_sm2[b], s2[:, 22:23], r_sm2[b])
                sc.op("pool", lambda e: e.tensor_copy(out=cq_b[b][:], in_=cq_f[b][:]), r=[r_cq[b]], w=[r_cqb[b]])
                bk, rb = nbank(4, 8)
                pv = bk[:].bitcast(BF16).rearrange("p (c n) -> p c n", n=128)
                sc.pe([(lambda e, c=c: e.transpose(out=pv[:, c, :], in_=cq_b[b][:, c * 128:(c + 1) * 128], identity=ident_bf[:])) for c in range(2)],
                      r=[r_cqb[b], r_const], w=[rb])
                sc.op("act", lambda e: e.copy(out=cqT[b][:], in_=pv[:, 0:2, :]), r=[rb], w=[r_cqT[b]])
                yield
                for hh in range(2):
                    bk, rb = nbank(4, 8)
                    sc.pe([(lambda e, c=c: e.matmul(bk[:, 0:384], lhsT=cqT[b][:, c, :], rhs=w_uq[:, c, hh * 384:(hh + 1) * 384], start=(c == 0), stop=(c == 1)))
                           for c in range(2)], r=[r_cqT[b], r_w2], w=[rb])
                    sc.op("act", lambda e: e.activation(out=q_f[bx][:, 2 * hh:2 * hh + 2, :], in_=bk[:, 0:384].rearrange("p (h d) -> p h d", h=2),
                                                        func=AF.Copy, scale=s2[:, 21:22]), r=[rb, r_sm2[b]], w=[r_qf[bx]])
                yield
                jq = junk[:, 0:768].rearrange("p (h d) -> p h d", h=4)
                sc.op("dve", lambda e: e.tensor_tensor(out=jq, in0=q_f[bx][:], in1=q_f[bx][:], op=ALU.mult), r=[r_qf[bx]], w=[r_junk])
                sc.op("dve", lambda e: e.tensor_reduce(out=s2[:, 24:28], in_=jq, axis=AX.X, op=ALU.add), r=[r_junk], w=[r_sm2[b]])
                rstd_from_ss(s2[:, 24:28], 192.0, s2[:, 28:32], r_sm2[b], r_sm2[b], s2[:, 32:36], r_sm2[b])
                for h in range(4):
                    sc.op("dve", lambda e: e.scalar_tensor_tensor(out=qn_b[bx][:, h, :], in0=q_f[bx][:, h, 0:128], scalar=s2[:, 28 + h:29 + h], in1=gq[:, 0:128],
                                                                  op0=ALU.mult, op1=ALU.mult), r=[r_qf[bx], r_sm2[b], r_const], w=[r_qnb[bx]])
                yield
                qq = qr[b]
                gqB = gq[:, 128:192].unsqueeze(1).broadcast_to([128, 4, 64])
                sc.op("pool", lambda e: e.tensor_tensor(out=qq[:, 0, :, :], in0=q_f[bx][:, :, 128:192], in1=gqB, op=ALU.mult), r=[r_qf[bx], r_const], w=[r_qr[b]])
                cosM8 = SCm[:, t, 32:64].unsqueeze(1).broadcast_to([128, 8, 32])
                sinM8 = SCm[:, t, 0:32].unsqueeze(1).broadcast_to([128, 8, 32])
                q0 = qq[:, 0, :, :].rearrange("p h (two d) -> p (h two) d", two=2)
                qA = qq[:, 1, :, :].rearrange("p h (two d) -> p (h two) d", two=2)
                qB = qq[:, 2, :, :].rearrange("p h (two d) -> p (h two) d", two=2)
                sc.op("pool", lambda e: e.tensor_tensor(out=qA, in0=q0, in1=cosM8, op=ALU.mult), r=[r_qr[b], r_sc], w=[r_qr[b]])
                sc.op("pool", lambda e: e.tensor_tensor(out=qB, in0=q0, in1=sinM8, op=ALU.mult), r=[r_qr[b], r_sc], w=[r_qr[b]])
                sc.op("pool", lambda e: e.tensor_tensor(out=qq[:, 3, :, 0:32], in0=qq[:, 1, :, 0:32], in1=qq[:, 2, :, 32:64], op=ALU.subtract),
                      r=[r_qr[b]], w=[r_qr[b]])
                sc.op("pool", lambda e: e.tensor_tensor(out=qq[:, 3, :, 32:64], in0=qq[:, 1, :, 32:64], in1=qq[:, 2, :, 0:32], op=ALU.add),
                      r=[r_qr[b]], w=[r_qr[b]])
                yield
                rsB = s2[:, 28:32].unsqueeze(2).broadcast_to([128, 4, 64])
                sc.op("dve", lambda e: e.tensor_tensor(out=qrn_b[b][:], in0=qq[:, 3, :, :], in1=rsB, op=ALU.mult), r=[r_qr[b], r_sm2[b]], w=[r_qrn[b]])
                bk, rb = nbank(4, 8)
                pv = bk[:].bitcast(BF16).rearrange("p (c n) -> p c n", n=128)
                sc.pe([(lambda e, h=h: e.transpose(out=pv[:, h, :], in_=qn_b[bx][:, h, :], identity=ident_bf[:])) for h in range(4)]
                      + [(lambda e, pr=pr: e.transpose(out=pv[:, 4 + pr, :], in_=qrn_b[b][:, 2 * pr:2 * pr + 2, :].rearrange("p a d -> p (a d)"),
                                                       identity=ident_bf[:])) for pr in range(2)],
                      r=[r_qnb[bx], r_qrn[b], r_const], w=[rb])
                sc.op("act", lambda e: e.copy(out=QTn[qb][:, :, tl], in_=pv[:, 0:4, :]), r=[rb], w=[r_QT[qb]])
                sc.op("dve", lambda e: e.tensor_copy(out=QTr[qb][:, :, tl], in_=pv[:, 4:6, :]), r=[rb], w=[r_QT[qb]])

            def attention_block(i, qb):
                ab = a_b[i % 2]
                its = [(h, j) for h in range(4) for j in range(4 * i + 4)]

                def qk(h, j):
                    pair, hp = h // 2, h % 2
                    psl = slice(hp * 64, (hp + 1) * 64)
                    r0 = max(0, j - 4 * i)
                    n = 512 - r0 * 128
                    ks = slice(j * 128, (j + 1) * 128)
                    bk, rb = nbank(4, 8)
                    diag = j >= 4 * i
                    fq = [lambda e: e.matmul(bk[:, 0:n], lhsT=KTn[:, h, ks], rhs=QTn[qb][:, h, r0 * 128:512], start=True, stop=False),
                          lambda e: e.matmul(bk[:, 0:n], lhsT=KTr[psl, pair, ks], rhs=QTr[qb][psl, pair, r0 * 128:512], start=False, stop=not diag)]
                    if diag:
                        fq.append(lambda e: e.matmul(bk[:, 0:128], lhsT=ident_bf[:], rhs=negm[:], start=False, stop=True))
                    sc.pe(fq, r=[r_KT[j], r_QT[qb], r_const], w=[rb])
                    pi = (h * 64 + j) % 3
                    pt, rpt = PTt[pi], r_PTt[pi]
                    sc.op("act", lambda e: e.activation(out=pt[:, 0:n], in_=bk[:, 0:n], func=AF.Exp, scale=SCALE), r=[rb], w=[rpt])
                    return pt, rpt, r0

                def pv_(h, j, pt, rpt, r0):
                    fns = []
                    for s in range(r0, 4):
                        fns.append(lambda e, s=s: e.matmul(banks[s][:, 0:129], lhsT=pt[:, (s - r0) * 128:(s - r0 + 1) * 128], rhs=Vaug[:, j, h, :],
                                                           start=(j == 0), stop=(j == 4 * i + s)))
                    sc.pe(fns, r=[rpt, r_V, r_KT[j]], w=[bank_res[s] for s in range(r0, 4)])
                    if j >= 4 * i:
                        s = j - 4 * i
                        sc.op("dve", lambda e: e.reciprocal(out=rcp[i % 2][:, s:s + 1], in_=banks[s][:, 128:129]), r=[bank_res[s]], w=[r_rcp[i % 2]])
                        sc.op("dve", lambda e: e.tensor_scalar(out=ab[:, s, h * 128:(h + 1) * 128], in0=banks[s][:, 0:128], scalar1=rcp[i % 2][:, s:s + 1],
                                                               scalar2=None, op0=ALU.mult), r=[bank_res[s], r_rcp[i % 2]], w=[r_ab[i % 2]])

                pend = []
                ystep = max(1, len(its) // 30)
                for k_, (h, j) in enumerate(its):
                    pend.append((h, j) + qk(h, j))
                    if len(pend) > 1:
                        pv_(*pend.pop(0))
                    if k_ % ystep == ystep - 1:
                        yield
                while pend:
                    pv_(*pend.pop(0))

            def out_tile(t):
                i, s = t // 4, t % 4
                ab = a_b[i % 2]
                if True:
                    b = t % 2
                    ts_ = slice(t * 128, (t + 1) * 128)
                    sc.dma("sp", lambda e: e.dma_start(out=r_b[b][:], in_=rout_d[ts_, :]), w=[r_rb[b]])
                    sc.dma("sp", lambda e: e.dma_start(out=x1t[1][:], in_=x_d[ts_, :]), w=[r_x1t[1]])
                    yield
                    bk, rb = nbank(4, 8)
                    pv = bk[:].bitcast(BF16).rearrange("p (c n) -> p c n", n=128)
                    sc.pe([(lambda e, c=c: e.transpose(out=pv[:, c, :], in_=ab[:, s, c * 128:(c + 1) * 128], identity=ident_bf[:])) for c in range(4)]
                          + [(lambda e, c=c: e.transpose(out=pv[:, 4 + c, :], in_=r_b[b][:, c * 128:(c + 1) * 128], identity=ident_bf[:])) for c in range(4)],
                          r=[r_ab[i % 2], r_rb[b], r_const], w=[rb])
                    sc.op("dve", lambda e: e.tensor_copy(out=aoT[b][:], in_=pv), r=[rb], w=[r_aoT[b]])
                    yield
                    for hh in range(2):
                        bk, rb = nbank(4, 8)
                        sc.pe([(lambda e, c=c: e.matmul(bk[:], lhsT=aoT[b][:, c, :], rhs=w_out[:, c, hh * 512:(hh + 1) * 512], start=(c == 0), stop=(c == 7)))
                               for c in range(8)], r=[r_aoT[b], r_w2], w=[rb])
                        sc.op("dve", lambda e: e.tensor_tensor(out=x1t[1][:, hh * 512:(hh + 1) * 512], in0=bk[:], in1=x1t[1][:, hh * 512:(hh + 1) * 512], op=ALU.add),
                              r=[rb], w=[r_x1t[1]])
                    r_x1d = sc.res("x1d")
                    sc.dma("act", lambda e: e.dma_start(out=x1_d[ts_, :], in_=x1t[1][:]), r=[r_x1t[1]], w=[r_x1d])

            def drive(gens):
                gens = list(gens)
                while gens:
                    for g in list(gens):
                        try:
                            next(g)
                        except StopIteration:
                            gens.remove(g)

            x1t = xt
            r_x1t = r_xt
            run_pipelined(lambda t: prep_tile(t, 0), 4, 3)
            for i in range(NB):
                qb = i % 2

                def side(i=i):
                    if i + 1 < NB:
                        act_ = []
                        nx = 4 * (i + 1)
                        while nx < 4 * (i + 2) or act_:
                            if nx < 4 * (i + 2) and len(act_) < 3:
                                act_.append(prep_tile(nx, (i + 1) % 2))
                                nx += 1
                            for g in list(act_):
                                try:
                                    next(g)
                                except StopIteration:
                                    act_.remove(g)
                            yield

                def outs(i=i):
                    if i == 0:
                        return
                    act_ = []
                    nx = 4 * (i - 1)
                    while nx < 4 * i or act_:
                        if nx < 4 * i and len(act_) < 1:
                            act_.append(out_tile(nx))
                            nx += 1
                        for g in list(act_):
                            try:
                                next(g)
                            except StopIteration:
                                act_.remove(g)
                        yield

                drive([attention_block(i, qb), outs(), side()])
            run_pipelined(lambda k_: out_tile(4 * (NB - 1) + k_), 4, 1)
            if "x1" in dbg:
                sc.barrier()
                t_ = nc.dram_tensor("dbg_x1", [S, D], F32, kind="ExternalOutput").ap()
                dbg_out["x1"] = t_
                sc.dma("sp", lambda e: e.dma_start(out=t_, in_=x1_d), is_out=True)
            dump("KTn", KTn[:], r_KT[NT - 1], [128, 4, S], BF16)
            dump("KTr", KTr[:], r_KT[NT - 1], [128, 2, S], BF16)
            dump("Vaug", Vaug[:], r_V, [128, NT, 4, 129], BF16)
            sc.barrier()
            p2s.close()

        if last_phase >= 3:
            p36 = es.enter_context(ExitStack())
            lg_all = sb("lg_all", [128, NT, 36], F32, p36)
            r_lg = sc.res("lg_all")
            pos12 = sb("pos12", [128, 2, NT], I32, p36)
            w12 = sb("w12", [128, 2, NT], F32, p36)
            widx = sb("widx", [128, NTS], I32, p36)
            r_route = sc.res("route")
            b_rt = bload("b_rt", b_rt_d, 36, p36)

            p3s = es.enter_context(ExitStack())
            cw_q = sb("cw_q", [128, 8, 1024], BF16, p3s)
            cw_o = sb("cw_o", [128, 8, 1024], BF16, p3s)
            KcT = sb("KcT", [128, 4, 2, 256], BF16, p3s)
            Vc = sb("Vc", [128, 2, 4, 257], BF16, p3s)
            mg = bload("mg", mg_d, 1024, p3s)
            cqg = bload("cqg", cqg_d, 256, p3s)
            ckg = bload("ckg", ckg_d, 256, p3s)
            stage[:] = [sb("stage3_%d" % i, [128, 1024], F32, p3s) for i in range(2)]
            g_cross = pload("g_cross", g_cross_d, 8, p3s)
            g_mem = pload("g_mem", g_mem_d, 8, p3s)
            w_rt = sb("w_rt", [128, 8, 36], F32, p3s)
            r_w3 = sc.res("w3")
            sc.dma("sp", lambda e: e.dma_start(out=w_rt[:], in_=w_rt_d[:, :, :]), w=[r_w3])
            load_scaled(lambda c, a, b_: cw_q[:, c, a:b_], lambda c, a, b_: cw_q_d[:, c, a:b_], lambda c: g_cross[:, c:c + 1], 8, 1024, r_w3)
            load_cast(lambda c: cw_o[:, c, :], lambda c: cw_o_d[:, c, :], 8, r_w3)
            r_kvc = sc.res("kvc")
            sc.op("pool", lambda e: e.memset(Vc[:], 1.0), w=[r_kvc])

            pm = es.enter_context(ExitStack())
            cw_kv = sb("cw_kv", [128, 8, 2048], BF16, pm)
            r_cwkv = sc.res("cw_kv")
            load_scaled(lambda c, a, b_: cw_kv[:, c, a:b_], lambda c, a, b_: cw_kv_d[:, c, a:b_], lambda c: g_mem[:, c:c + 1], 8, 2048, r_cwkv)
            m_f = sb("m_f", [128, 1024], F32, pm)
            m_b = sb("m_b", [128, 1024], BF16, pm)
            m_T = sb("m_T", [128, 8, 128], BF16, pm)
            kc_f = sb("kc_f", [128, 4, 256], F32, pm)
            kc_sq = sb("kc_sq", [128, 4, 256], F32, pm)
            kc_b = sb("kc_b", [128, 4, 256], BF16, pm)
            sm = sb("sm0", [128, 16], F32, pm)
            r_m = sc.res("m")
            r_sm = sc.res("sm0")
            for mt in range(2):
                sc.dma("sp", lambda e: e.dma_start(out=m_f[:], in_=mem_d[mt * 128:(mt + 1) * 128, :]), w=[r_m])
                sc.op("act", lambda e: e.activation(out=m_b[:], in_=m_f[:], func=AF.Square, accum_out=sm[:, 0:1]), r=[r_m], w=[r_m, r_sm])
                rstd_from_ss(sm[:, 0:1], 1024.0, sm[:, 1:2], r_sm, r_sm, sm[:, 2:3], r_sm)
                sc.op("pool", lambda e: e.tensor_copy(out=m_b[:], in_=m_f[:]), r=[r_m], w=[r_m])
                bk, rb = nbank()
                pv = bk[:].bitcast(BF16).rearrange("p (c n) -> p c n", n=128)
                sc.pe([(lambda e, c=c: e.transpose(out=pv[:, c, :], in_=m_b[:, c * 128:(c + 1) * 128], identity=ident_bf[:])) for c in range(8)],
                      r=[r_m, r_const], w=[rb])
                sc.op("dve", lambda e: e.tensor_copy(out=m_T[:], in_=pv), r=[rb], w=[r_m])
                for nchunk in range(4):
                    bk, rb = nbank()
                    sc.pe([(lambda e, c=c: e.matmul(bk[:], lhsT=m_T[:, c, :], rhs=cw_kv[:, c, nchunk * 512:(nchunk + 1) * 512],
                                                    start=(c == 0), stop=(c == 7))) for c in range(8)], r=[r_m, r_cwkv], w=[rb])
                    if nchunk < 2:
                        sc.op("act", lambda e: e.activation(out=kc_f[:, 2 * nchunk:2 * nchunk + 2, :], in_=bk[:].rearrange("p (h d) -> p h d", h=2),
                                                            func=AF.Copy, scale=sm[:, 1:2]), r=[rb, r_sm], w=[r_m])
                    else:
                        hh = 2 * (nchunk - 2)
                        sc.op("act", lambda e: e.activation(out=Vc[:, mt, hh:hh + 2, 0:256], in_=bk[:].rearrange("p (h d) -> p h d", h=2),
                                                            func=AF.Copy, scale=sm[:, 1:2]), r=[rb, r_sm], w=[r_kvc])
                sc.op("dve", lambda e: e.tensor_tensor(out=kc_sq[:], in0=kc_f[:], in1=kc_f[:], op=ALU.mult), r=[r_m], w=[r_m])
                sc.op("dve", lambda e: e.tensor_reduce(out=sm[:, 4:8], in_=kc_sq[:], axis=AX.X, op=ALU.add), r=[r_m], w=[r_sm])
                rstd_from_ss(sm[:, 4:8], 256.0, sm[:, 8:12], r_sm, r_sm, sm[:, 12:16], r_sm)
                for h in range(4):
                    sc.op("dve", lambda e: e.scalar_tensor_tensor(out=kc_b[:, h, :], in0=kc_f[:, h, :], scalar=sm[:, 8 + h:9 + h], in1=ckg[:],
                                                                  op0=ALU.mult, op1=ALU.mult), r=[r_m, r_sm, r_const], w=[r_m])
                bk, rb = nbank()
                pv = bk[:].bitcast(BF16).rearrange("p (h c n) -> p h c n", h=4, c=2)
                sc.pe([(lambda e, h=h, c=c: e.transpose(out=pv[:, h, c, :], in_=kc_b[:, h, c * 128:(c + 1) * 128], identity=ident_bf[:]))
                       for h in range(4) for c in range(2)], r=[r_m, r_const], w=[rb])
                sc.op("dve", lambda e: e.tensor_copy(out=KcT[:, :, :, mt * 128:(mt + 1) * 128], in_=pv), r=[rb], w=[r_kvc])
            dump("KcT", KcT[:], r_kvc, [128, 4, 2, 256], BF16)
            dump("Vc", Vc[:], r_kvc, [128, 2, 4, 257], BF16)
            sc.barrier()
            pm.close()

            x1t = [sb("x3t%d" % i, [128, 1024], F32, p3s) for i in range(5)]
            xb = [sb("x3b%d" % i, [128, 1024], BF16, p3s) for i in range(5)]
            hcT = [sb("hcT%d" % i, [128, 8, 128], BF16, p3s) for i in range(5)]
            junk = sb("junk3", [128, 1024], F32, p3s)
            qc_f = sb("qc_f", [128, 4, 256], F32, p3s)
            qc_b = sb("qc_b", [128, 4, 256], BF16, p3s)
            qcT = [sb("qcT%d" % i, [128, 4, 2, 128], BF16, p3s) for i in range(5)]
            PTc = [sb("PTc%d" % i, [128, 8, 128], BF16, p3s) for i in range(5)]
            oc_b = sb("oc_b", [128, 4, 256], BF16, p3s)
            ocT = [sb("ocT%d" % i, [128, 8, 128], BF16, p3s) for i in range(5)]
            hm_f = [sb("hm_f%d" % i, [128, 1024], F32, p3s) for i in range(5)]
            hm_b = [sb("hm_b%d" % i, [128, 1024], BF16, p3s) for i in range(5)]
            hmT_f = sb("hmT_f", [128, 8, 128], F32, p3s)
            sm3 = [sb("sm3_%d" % i, [128, 32], F32, p3s) for i in range(5)]
            r_x1t, r_xb, r_hcT = mkres("x3t", 5), mkres("x3b", 5), mkres("hcT", 5)
            r_junk = sc.res("junk3")
            r_qcf, r_qcb = sc.res("qcf"), sc.res("qcb")
            r_qcT, r_PTc, r_ocT, r_hmf, r_hmb, r_sm3 = mkres("qcT", 5), mkres("PTc", 5), mkres("ocT", 5), mkres("hmf", 5), mkres("hmb", 5), mkres("sm3", 5)
            r_ocb = sc.res("ocb")
            r_hmT = sc.res("hmT")
            r_x2d = [sc.res("x2d%d" % t) for t in range(NT)]
            r_hmd = [sc.res("hmd%d" % t) for t in range(NT)]
            CSCALE = 256.0 ** -0.5

            def p3_tile(t):
                b = t % 5
                b3 = t % 5
                ts_ = slice(t * 128, (t + 1) * 128)
                s3 = sm3[b3]
                sc.dma("sp", lambda e: e.dma_start(out=x1t[b3][:], in_=x1_d[ts_, :]), w=[r_x1t[b3]])
                sc.op("act", lambda e: e.activation(out=junk[:], in_=x1t[b3][:], func=AF.Square, accum_out=s3[:, 0:1]), r=[r_x1t[b3]], w=[r_junk, r_sm3[b3]])
                rstd_from_ss(s3[:, 0:1], 1024.0, s3[:, 1:2], r_sm3[b3], r_sm3[b3], s3[:, 2:3], r_sm3[b3])
                sc.op("act", lambda e: e.copy(out=xb[b][:], in_=x1t[b3][:]), r=[r_x1t[b3]], w=[r_xb[b]])
                yield
                bk, rb = nbank()
                pv = bk[:].bitcast(BF16).rearrange("p (c n) -> p c n", n=128)
                sc.pe([(lambda e, c=c: e.transpose(out=pv[:, c, :], in_=xb[b][:, c * 128:(c + 1) * 128], identity=ident_bf[:])) for c in range(8)],
                      r=[r_xb[b], r_const], w=[rb])
                sc.op("dve", lambda e: e.tensor_copy(out=hcT[b][:], in_=pv), r=[rb], w=[r_hcT[b]])
                yield
                for hh in range(2):
                    bk, rb = nbank()
                    sc.pe([(lambda e, c=c: e.matmul(bk[:], lhsT=hcT[b][:, c, :], rhs=cw_q[:, c, hh * 512:(hh + 1) * 512], start=(c == 0), stop=(c == 7)))
                           for c in range(8)], r=[r_hcT[b], r_w3], w=[rb])
                    sc.op("act", lambda e: e.activation(out=qc_f[:, 2 * hh:2 * hh + 2, :], in_=bk[:].rearrange("p (h d) -> p h d", h=2), func=AF.Copy,
                                                        scale=s3[:, 1:2]), r=[rb, r_sm3[b3]], w=[r_qcf])
                yield
                jq = junk[:].rearrange("p (h d) -> p h d", h=4)
                sc.op("dve", lambda e: e.tensor_tensor(out=jq, in0=qc_f[:], in1=qc_f[:], op=ALU.mult), r=[r_qcf], w=[r_junk])
                sc.op("dve", lambda e: e.tensor_reduce(out=s3[:, 4:8], in_=jq, axis=AX.X, op=ALU.add), r=[r_junk], w=[r_sm3[b3]])
                rstd_from_ss(s3[:, 4:8], 256.0, s3[:, 8:12], r_sm3[b3], r_sm3[b3], s3[:, 12:16], r_sm3[b3])
                for h in range(4):
                    sc.op("dve", lambda e: e.scalar_tensor_tensor(out=qc_b[:, h, :], in0=qc_f[:, h, :], scalar=s3[:, 8 + h:9 + h], in1=cqg[:],
                                                                  op0=ALU.mult, op1=ALU.mult), r=[r_qcf, r_sm3[b3], r_const], w=[r_qcb])
                yield
                bk, rb = nbank()
                pv = bk[:].bitcast(BF16).rearrange("p (h c n) -> p h c n", h=4, c=2)
                sc.pe([(lambda e, h=h, c=c: e.transpose(out=pv[:, h, c, :], in_=qc_b[:, h, c * 128:(c + 1) * 128], identity=ident_bf[:]))
                       for h in range(4) for c in range(2)], r=[r_qcb, r_const], w=[rb])
                sc.op("act", lambda e: e.copy(out=qcT[b][:], in_=pv), r=[rb], w=[r_qcT[b]])
                yield
                for hp in range(2):
                    bk, rb = nbank()
                    sv_ = bk[:].rearrange("p (a n) -> p a n", a=4)
                    fns = []
                    for hl in range(2):
                        h = 2 * hp + hl
                        for mc in range(2):
                            for dc in range(2):
                                fns.append(lambda e, h=h, mc=mc, dc=dc, hl=hl: e.matmul(sv_[:, hl * 2 + mc, :], lhsT=KcT[:, h, dc, mc * 128:(mc + 1) * 128],
                                                                                        rhs=qcT[b][:, h, dc, :], start=(dc == 0), stop=(dc == 1)))
                    sc.pe(fns, r=[r_kvc, r_qcT[b]], w=[rb])
                    sc.op("act", lambda e: e.activation(out=PTc[b][:, 4 * hp:4 * hp + 4, :], in_=sv_, func=AF.Exp, scale=CSCALE), r=[rb], w=[r_PTc[b]])
                yield
                for h in range(4):
                    bk, rb = nbank()
                    sc.pe([(lambda e, mc=mc: e.matmul(bk[:, 0:257], lhsT=PTc[b][:, 2 * h + mc, :], rhs=Vc[:, mc, h, :], start=(mc == 0), stop=(mc == 1)))
                           for mc in range(2)], r=[r_PTc[b], r_kvc], w=[rb])
                    sc.op("dve", lambda e: e.reciprocal(out=s3[:, 16 + h:17 + h], in_=bk[:, 256:257]), r=[rb], w=[r_sm3[b3]])
                    sc.op("dve", lambda e: e.tensor_scalar(out=oc_b[:, h, :], in0=bk[:, 0:256], scalar1=s3[:, 16 + h:17 + h], scalar2=None, op0=ALU.mult),
                          r=[rb, r_sm3[b3]], w=[r_ocb])
                yield
                bk, rb = nbank()
                pv = bk[:].bitcast(BF16).rearrange("p (c n) -> p c n", n=128)
                ocf = oc_b[:].rearrange("p h d -> p (h d)")
                sc.pe([(lambda e, c=c: e.transpose(out=pv[:, c, :], in_=ocf[:, c * 128:(c + 1) * 128], identity=ident_bf[:])) for c in range(8)],
                      r=[r_ocb, r_const], w=[rb])
                sc.op("act", lambda e: e.copy(out=ocT[b][:], in_=pv), r=[rb], w=[r_ocT[b]])
                yield
                for hh in range(2):
                    bk, rb = nbank()
                    sc.pe([(lambda e, c=c: e.matmul(bk[:], lhsT=ocT[b][:, c, :], rhs=cw_o[:, c, hh * 512:(hh + 1) * 512], start=(c == 0), stop=(c == 7)))
                           for c in range(8)], r=[r_ocT[b], r_w3], w=[rb])
                    sc.op("dve", lambda e: e.tensor_tensor(out=x1t[b3][:, hh * 512:(hh + 1) * 512], in0=bk[:], in1=x1t[b3][:, hh * 512:(hh + 1) * 512], op=ALU.add),
                          r=[rb], w=[r_x1t[b3]])
                sc.dma("act", lambda e: e.dma_start(out=out_d[ts_, :], in_=x1t[b3][:]), r=[r_x1t[b3]], w=[r_x2d[t]])
                yield
                sc.op("act", lambda e: e.activation(out=junk[:], in_=x1t[b3][:], func=AF.Square, accum_out=s3[:, 20:21]), r=[r_x1t[b3]], w=[r_junk, r_sm3[b3]])
                rstd_from_ss(s3[:, 20:21], 1024.0, s3[:, 21:22], r_sm3[b3], r_sm3[b3], s3[:, 22:23], r_sm3[b3])
                sc.op("dve", lambda e: e.scalar_tensor_tensor(out=hm_f[b][:], in0=x1t[b3][:], scalar=s3[:, 21:22], in1=mg[:], op0=ALU.mult, op1=ALU.mult),
                      r=[r_x1t[b3], r_sm3[b3], r_const], w=[r_hmf[b]])
                sc.op("pool", lambda e: e.tensor_copy(out=hm_b[b][:], in_=hm_f[b][:]), r=[r_hmf[b]], w=[r_hmb[b]])
                sc.dma("pool", lambda e: e.dma_start(out=hm_d[ts_, :], in_=hm_b[b][:]), r=[r_hmb[b]], w=[r_hmd[t]])
                yield
                bkA, rbA = nbank()
                bkB, rbB = nbank()
                sc.pe([(lambda e, c=c: e.transpose(out=(bkA if c < 4 else bkB)[:, (c % 4) * 128:(c % 4 + 1) * 128], in_=hm_f[b][:, c * 128:(c + 1) * 128],
                                                   identity=ident_f[:])) for c in range(8)], r=[r_hmf[b], r_const], w=[rbA, rbB])
                sc.op("dve", lambda e: e.tensor_copy(out=hmT_f[:, 0:4, :], in_=bkA[:].rearrange("p (c n) -> p c n", c=4)), r=[rbA], w=[r_hmT])
                sc.op("act", lambda e: e.copy(out=hmT_f[:, 4:8, :], in_=bkB[:].rearrange("p (c n) -> p c n", c=4)), r=[rbB], w=[r_hmT])
                yield
                bk, rb = nbank()
                sc.pe([(lambda e, c=c: e.matmul(bk[:, 0:36], lhsT=hmT_f[:, c, :], rhs=w_rt[:, c, :], start=(c == 0), stop=(c == 7))) for c in range(8)],
                      r=[r_hmT, r_w3], w=[rb])
                sc.op("act", lambda e: e.copy(out=lg_all[:, t, :], in_=bk[:, 0:36]), r=[rb], w=[r_lg])
            run_pipelined(p3_tile, NT, 5)
            dump("logits", lg_all[:], r_lg, [128, NT, 36])
            if "x2" in dbg:
                sc.barrier()
                t_ = nc.dram_tensor("dbg_x2", [S, D], F32, kind="ExternalOutput").ap()
                dbg_out["x2"] = t_
                sc.dma("sp", lambda e: e.dma_start(out=t_, in_=out_d), is_out=True)
                t2_ = nc.dram_tensor("dbg_hm", [S, D], BF16, kind="ExternalOutput").ap()
                dbg_out["hm"] = t2_
                sc.dma("sp", lambda e: e.dma_start(out=t2_, in_=hm_d), is_out=True)
            sc.barrier()
            p3s.close()

        if last_phase >= 4:
            p4s = es.enter_context(ExitStack())
            reg_npos = nc.gpsimd.to_reg(NPOS - 1)
            reg_ew = nc.gpsimd.to_reg(32 * 128 - 1)
            r4 = sc.res("r4")

            def T4(name, shape, dt=F32):
                return sb("r4_" + name, shape, dt, p4s)

            def V(fn, r=(), w=(), eng="dve"):
                sc.op(eng, fn, r=[r4, r_lg, r_const] + list(r), w=[r4] + list(w))

            L = lg_all
            GL = L[:, :, 0:4]
            EL = L[:, :, 4:36].rearrange("p t (g j) -> p t g j", g=4)
            gb, goh, ge = T4("gb", [128, NT, 4]), T4("goh", [128, NT, 4]), T4("ge", [128, NT, 4])
            gmax, gm, gsum, gnum, gw = (T4(n, [128, NT]) for n in ("gmax", "gm", "gsum", "gnum", "gw"))
            t48 = T4("t48", [128, NT, 4, 8])
            esel, bsel, eb, oh1, eb2, oh2, ex, t8 = (T4(n, [128, NT, 8]) for n in ("esel", "bsel", "eb", "oh1", "eb2", "oh2", "ex", "t8"))
            m1, m2, em, a1, a2, den, ff = (T4(n, [128, NT]) for n in ("m1", "m2", "em", "a1", "a2", "den", "ff"))
            OH1, OH2 = T4("OH1", [128, NT, 32]), T4("OH2", [128, NT, 32])
            C_bf = T4("C_bf", [128, NT, 32], BF16)
            TT, PP, cA, cB, base, tmp32 = (T4(n, [128, NT, 32]) for n in ("TT", "PP", "cA", "cB", "base", "tmp32"))
            npad_i = T4("npad_i", [128, 32], I32)
            npad, eA, eB, off = (T4(n, [128, 32]) for n in ("npad", "eA", "eB", "off"))
            posf = T4("posf", [128, 2, NT])
            tpos_i = T4("tpos_i", [128, NTS], I32)
            tpos_f, eid_f = T4("tpos_f", [128, NTS]), T4("eid_f", [128, NTS])
            cmp_ = T4("cmp", [128, NTS, 32])
            pidx_i = T4("pidx_i", [128, 1], I32)
            pidx_f = T4("pidx_f", [128, 1])

            def bc(ap2, n):
                return ap2.unsqueeze(2).broadcast_to([128, NT, n])

            bg = b_rt[:, 0:4].unsqueeze(1).broadcast_to([128, NT, 4])
            be = b_rt[:, 4:36].rearrange("p (g j) -> p g j", g=4).unsqueeze(1).broadcast_to([128, NT, 4, 8])
            V(lambda e: e.tensor_tensor(out=gb[:], in0=GL, in1=bg, op=ALU.add))
            V(lambda e: e.tensor_reduce(out=gmax[:], in_=gb[:], axis=AX.X, op=ALU.max))
            V(lambda e: e.tensor_tensor(out=goh[:], in0=gb[:], in1=bc(gmax[:], 4), op=ALU.is_equal))
            V(lambda e: e.tensor_reduce(out=gm[:], in_=GL, axis=AX.X, op=ALU.max))
            V(lambda e: e.tensor_tensor(out=ge[:], in0=GL, in1=bc(gm[:], 4), op=ALU.subtract))
            V(lambda e: e.activation(out=ge[:].rearrange("p t g -> p (t g)"), in_=ge[:].rearrange("p t g -> p (t g)"), func=AF.Exp), eng="act")
            V(lambda e: e.tensor_reduce(out=gsum[:], in_=ge[:], axis=AX.X, op=ALU.add))
            V(lambda e: e.tensor_tensor(out=gb[:], in0=goh[:], in1=ge[:], op=ALU.mult))
            V(lambda e: e.tensor_reduce(out=gnum[:], in_=gb[:], axis=AX.X, op=ALU.add))
            V(lambda e: e.reciprocal(out=gsum[:], in_=gsum[:]))
            V(lambda e: e.tensor_tensor(out=gw[:], in0=gnum[:], in1=gsum[:], op=ALU.mult))
            goh_b = goh[:].unsqueeze(3).broadcast_to([128, NT, 4, 8])
            V(lambda e: e.tensor_tensor(out=t48[:], in0=EL, in1=goh_b, op=ALU.mult))
            V(lambda e: e.tensor_reduce(out=esel[:], in_=t48[:].rearrange("p t g j -> p t j g"), axis=AX.X, op=ALU.add))
            V(lambda e: e.tensor_tensor(out=t48[:], in0=be, in1=goh_b, op=ALU.mult))
            V(lambda e: e.tensor_reduce(out=bsel[:], in_=t48[:].rearrange("p t g j -> p t j g"), axis=AX.X, op=ALU.add))
            V(lambda e: e.tensor_tensor(out=eb[:], in0=esel[:], in1=bsel[:], op=ALU.add))
            V(lambda e: e.tensor_reduce(out=m1[:], in_=eb[:], axis=AX.X, op=ALU.max))
            V(lambda e: e.tensor_tensor(out=oh1[:], in0=eb[:], in1=bc(m1[:], 8), op=ALU.is_equal))
            V(lambda e: e.scalar_tensor_tensor(out=eb2[:].rearrange("p t j -> p (t j)"), in0=oh1[:].rearrange("p t j -> p (t j)"), scalar=-1e30,
                                               in1=eb[:].rearrange("p t j -> p (t j)"), op0=ALU.mult, op1=ALU.add))
            V(lambda e: e.tensor_reduce(out=m2[:], in_=eb2[:], axis=AX.X, op=ALU.max))
            V(lambda e: e.tensor_tensor(out=oh2[:], in0=eb2[:], in1=bc(m2[:], 8), op=ALU.is_equal))
            V(lambda e: e.tensor_reduce(out=em[:], in_=esel[:], axis=AX.X, op=ALU.max))
            V(lambda e: e.tensor_tensor(out=ex[:], in0=esel[:], in1=bc(em[:], 8), op=ALU.subtract))
            V(lambda e: e.activation(out=ex[:].rearrange("p t j -> p (t j)"), in_=ex[:].rearrange("p t j -> p (t j)"), func=AF.Exp), eng="act")
            V(lambda e: e.tensor_tensor(out=t8[:], in0=oh1[:], in1=ex[:], op=ALU.mult))
            V(lambda e: e.tensor_reduce(out=a1[:], in_=t8[:], axis=AX.X, op=ALU.add))
            V(lambda e: e.tensor_tensor(out=t8[:], in0=oh2[:], in1=ex[:], op=ALU.mult))
            V(lambda e: e.tensor_reduce(out=a2[:], in_=t8[:], axis=AX.X, op=ALU.add))
            V(lambda e: e.tensor_tensor(out=den[:], in0=a1[:], in1=a2[:], op=ALU.add))
            V(lambda e: e.reciprocal(out=den[:], in_=den[:]))
            V(lambda e: e.tensor_tensor(out=ff[:], in0=den[:], in1=gw[:], op=ALU.mult))
            V(lambda e: e.tensor_tensor(out=w12[:, 0, :], in0=a1[:], in1=ff[:], op=ALU.mult), w=[r_route])
            V(lambda e: e.tensor_tensor(out=w12[:, 1, :], in0=a2[:], in1=ff[:], op=ALU.mult), w=[r_route])
            V(lambda e: e.tensor_tensor(out=OH1[:].rearrange("p t (g j) -> p t g j", g=4), in0=goh_b,
                                        in1=oh1[:].unsqueeze(2).broadcast_to([128, NT, 4, 8]), op=ALU.mult))
            V(lambda e: e.tensor_tensor(out=OH2[:].rearrange("p t (g j) -> p t g j", g=4), in0=goh_b,
                                        in1=oh2[:].unsqueeze(2).broadcast_to([128, NT, 4, 8]), op=ALU.mult))
            V(lambda e: e.tensor_tensor(out=C_bf[:], in0=OH1[:], in1=OH2[:], op=ALU.add))
            W = NT * 32
            Cf = C_bf[:].rearrange("p t e -> p (t e)")
            TTf = TT[:].rearrange("p t e -> p (t e)")
            PPf = PP[:].rearrange("p t e -> p (t e)")
            for c0 in range(0, W, 512):
                c1 = min(W, c0 + 512)
                bk, rb = nbank()
                sc.pe([lambda e: e.matmul(bk[:, 0:c1 - c0], lhsT=ones_bf[:], rhs=Cf[:, c0:c1], start=True, stop=True)], r=[r4, r_const], w=[rb])
                sc.op("act", lambda e: e.copy(out=TTf[:, c0:c1], in_=bk[:, 0:c1 - c0]), r=[rb], w=[r4])
                bk, rb = nbank()
                sc.pe([lambda e: e.matmul(bk[:, 0:c1 - c0], lhsT=tri[:], rhs=Cf[:, c0:c1], start=True, stop=True)], r=[r4, r_const], w=[rb])
                sc.op("act", lambda e: e.copy(out=PPf[:, c0:c1], in_=bk[:, 0:c1 - c0]), r=[rb], w=[r4])
            V(lambda e: e.tensor_copy(out=cA[:], in_=TT[:]))
            cur, nxt = cA, cB
            s_ = 1
            while s_ < NT:
                V(lambda e: e.tensor_tensor(out=nxt[:, s_:NT, :], in0=cur[:, s_:NT, :], in1=cur[:, 0:NT - s_, :], op=ALU.add))
                V(lambda e: e.tensor_copy(out=nxt[:, 0:s_, :], in_=cur[:, 0:s_, :]))
                cur, nxt = nxt, cur
                s_ *= 2
            incl = cur
            V(lambda e: e.tensor_scalar(out=npad[:], in0=incl[:, NT - 1, :], scalar1=float(TS - 1), scalar2=None, op0=ALU.add))
            V(lambda e: e.tensor_copy(out=npad_i[:], in_=npad[:]))
            V(lambda e: e.tensor_scalar(out=npad_i[:], in0=npad_i[:], scalar1=8, scalar2=8, op0=ALU.arith_shift_right, op1=ALU.logical_shift_left))
            V(lambda e: e.tensor_copy(out=npad[:], in_=npad_i[:]))
            V(lambda e: e.tensor_copy(out=eA[:], in_=npad[:]))
            cur2, nxt2 = eA, eB
            s_ = 1
            while s_ < 32:
                V(lambda e: e.tensor_tensor(out=nxt2[:, s_:32], in0=cur2[:, s_:32], in1=cur2[:, 0:32 - s_], op=ALU.add))
                V(lambda e: e.tensor_copy(out=nxt2[:, 0:s_], in_=cur2[:, 0:s_]))
                cur2, nxt2 = nxt2, cur2
                s_ *= 2
            endI = cur2
            V(lambda e: e.tensor_tensor(out=off[:], in0=endI[:], in1=npad[:], op=ALU.subtract))
            V(lambda e: e.tensor_tensor(out=base[:], in0=incl[:], in1=TT[:], op=ALU.subtract))
            V(lambda e: e.tensor_tensor(out=base[:], in0=base[:], in1=PP[:], op=ALU.add))
            V(lambda e: e.tensor_tensor(out=base[:], in0=base[:], in1=off[:].unsqueeze(1).broadcast_to([128, NT, 32]), op=ALU.add))
            V(lambda e: e.tensor_tensor(out=tmp32[:], in0=OH1[:], in1=base[:], op=ALU.mult))
            V(lambda e: e.tensor_reduce(out=posf[:, 0, :], in_=tmp32[:], axis=AX.X, op=ALU.add))
            V(lambda e: e.tensor_tensor(out=tmp32[:], in0=OH2[:], in1=base[:], op=ALU.mult))
            V(lambda e: e.tensor_reduce(out=posf[:, 1, :], in_=tmp32[:], axis=AX.X, op=ALU.add))
            V(lambda e: e.tensor_copy(out=pos12[:], in_=posf[:]), w=[r_route])
            V(lambda e: e.iota(tpos_i[:], pattern=[[TS, NTS]], base=0, channel_multiplier=0), eng="pool")
            V(lambda e: e.iota(pidx_i[:], pattern=[[0, 1]], base=0, channel_multiplier=1), eng="pool")
            V(lambda e: e.tensor_copy(out=tpos_f[:], in_=tpos_i[:]))
            V(lambda e: e.tensor_copy(out=pidx_f[:], in_=pidx_i[:]))
            V(lambda e: e.tensor_tensor(out=cmp_[:], in0=endI[:].unsqueeze(1).broadcast_to([128, NTS, 32]),
                                        in1=tpos_f[:].unsqueeze(2).broadcast_to([128, NTS, 32]), op=ALU.is_le))
            V(lambda e: e.tensor_reduce(out=eid_f[:], in_=cmp_[:], axis=AX.X, op=ALU.add))
            V(lambda e: e.tensor_scalar(out=eid_f[:], in0=eid_f[:], scalar1=31.0, scalar2=128.0, op0=ALU.min, op1=ALU.mult))
            V(lambda e: e.tensor_scalar(out=eid_f[:], in0=eid_f[:], scalar1=pidx_f[:, 0:1], scalar2=None, op0=ALU.add))
            V(lambda e: e.tensor_copy(out=widx[:], in_=eid_f[:]), w=[r_route])
            dump("pos12", pos12[:], r_route, [128, 2, NT], I32)
            dump("w12", w12[:], r_route, [128, 2, NT])
            dump("widx", widx[:], r_route, [128, NTS], I32)

            hsb = [sb("hsb%d" % i, [128, 1024], BF16, p4s) for i in range(2)]
            r_hsb = mkres("hsb")
            for t in range(NT):
                b = t % 2
                ts_ = slice(t * 128, (t + 1) * 128)
                sc.dma("sp", lambda e: e.dma_start(out=hsb[b][:], in_=hm_d[ts_, :]), r=[r_hmd[t]], w=[r_hsb[b]])
                for k in range(2):
                    sc.dma("pool", lambda e: e.indirect_dma_start(out=xs_d[:, :], out_offset=bass.IndirectOffsetOnAxis(ap=pos12[:, k, t:t + 1], axis=0),
                                                                  in_=hsb[b][:], in_offset=None, bounds_check=reg_npos, oob_is_err=False),
                           r=[r_hsb[b], r_route], w=[r_xs])
            sc.barrier()
            p4s.close()

        if last_phase >= 5:
            p5s = es.enter_context(ExitStack())
            NWB = 5
            wall = [sb("wall%d" % i, [128, 6144], BF16, p5s) for i in range(NWB)]
            wg = [w_[:, 0:2048] for w_ in wall]
            wu = [w_[:, 2048:4096] for w_ in wall]
            wd = [w_[:, 4096:6144] for w_ in wall]
            r_wg = mkres("wall", NWB)
            r_wu = r_wg
            r_wd = r_wg
            xrow = [sb("xrow%d" % i, [128, 1024], BF16, p5s) for i in range(10)]
            r_xrow = mkres("xrow", 10)
            XsT = [sb("XsT%d" % i, [128, 8, TS], BF16, p5s) for i in range(5)]
            r_XsT = mkres("XsT", 5)
            sa = [sb("sa%d" % i, [128, TS], F32, p5s) for i in range(2)]
            r_sa = mkres("sa")
            actT = [sb("actT%d" % i, [128, 2, TS], BF16, p5s) for i in range(5)]
            r_actT = mkres("actT", 5)
            yt = [sb("yt%d" % i, [128, 1024], F32, p5s) for i in range(4)]
            r_yt = mkres("yt", 4)
            r_ys = sc.res("ys_d")
            cnt5 = {"x": 0, "y": 0}
            def p5_tile(tp):
                wb = tp % NWB
                b = tp % 5
                sc.dma("pool", lambda e: e.indirect_dma_start(out=wall[wb][:], out_offset=None, in_=ewb_all[:, :],
                                                              in_offset=bass.IndirectOffsetOnAxis(ap=widx[:, tp:tp + 1], axis=0),
                                                              bounds_check=reg_ew, oob_is_err=False), r=[r_route, r_ewb], w=[r_wg[wb]])
                for s in range(TS // 128):
                    xi = (2 * tp + s) % 10
                    r0_ = tp * TS + s * 128
                    sc.dma("sp", lambda e: e.dma_start(out=xrow[xi][:], in_=xs_d[r0_:r0_ + 128, :]), r=[r_xs], w=[r_xrow[xi]])
                yield
                for s in range(TS // 128):
                    xi = (2 * tp + s) % 10
                    bk, rb = nbank()
                    pv = bk[:].bitcast(BF16).rearrange("p (c n) -> p c n", n=128)
                    sc.pe([(lambda e, c=c: e.transpose(out=pv[:, c, :], in_=xrow[xi][:, c * 128:(c + 1) * 128], identity=ident_bf[:])) for c in range(8)],
                          r=[r_xrow[xi], r_const], w=[rb])
                    sc.op("dve" if s % 2 == 0 else "act",
                          (lambda e: e.tensor_copy(out=XsT[b][:, :, s * 128:(s + 1) * 128], in_=pv)) if s % 2 == 0 else
                          (lambda e: e.copy(out=XsT[b][:, :, s * 128:(s + 1) * 128], in_=pv)), r=[rb], w=[r_XsT[b]])
                yield
                wgv = wg[wb].rearrange("p (c f) -> p c f", c=8)
                wuv = wu[wb].rearrange("p (c f) -> p c f", c=8)
                wdv = wd[wb].rearrange("p (c d) -> p c d", c=2)
                for fc in range(2):
                    bk, rb = nbank()
                    fns = [(lambda e, c=c: e.matmul(bk[:, 0:TS], lhsT=wgv[:, c, fc * 128:(fc + 1) * 128], rhs=XsT[b][:, c, :], start=(c == 0), stop=(c == 7)))
                           for c in range(8)]
                    fns += [(lambda e, c=c: e.matmul(bk[:, TS:2 * TS], lhsT=wuv[:, c, fc * 128:(fc + 1) * 128], rhs=XsT[b][:, c, :], start=(c == 0), stop=(c == 7)))
                            for c in range(8)]
                    sc.pe(fns, r=[r_wg[wb], r_wu[wb], r_XsT[b]], w=[rb])
                    sc.op("act", lambda e: e.activation(out=sa[fc][:], in_=bk[:, 0:TS], func=AF.Silu), r=[rb], w=[r_sa[fc]])
                    sc.op("dve", lambda e: e.tensor_tensor(out=actT[b][:, fc, :], in0=bk[:, TS:2 * TS], in1=sa[fc][:], op=ALU.mult),
                          r=[rb, r_sa[fc]], w=[r_actT[b]])
                    yield
                for s in range(TS // 128):
                    yi = cnt5["y"] % 4
                    cnt5["y"] += 1
                    for half in range(2):
                        bk, rb = nbank()
                        sc.pe([(lambda e, fc=fc: e.matmul(bk[:], lhsT=actT[b][:, fc, s * 128:(s + 1) * 128], rhs=wdv[:, fc, half * 512:(half + 1) * 512],
                                                          start=(fc == 0), stop=(fc == 1))) for fc in range(2)], r=[r_actT[b], r_wd[wb]], w=[rb])
                        if half == 0:
                            sc.op("act", lambda e: e.copy(out=yt[yi][:, 0:512], in_=bk[:]), r=[rb], w=[r_yt[yi]])
                        else:
                            sc.op("dve", lambda e: e.tensor_copy(out=yt[yi][:, 512:1024], in_=bk[:]), r=[rb], w=[r_yt[yi]])
                    r0_ = tp * TS + s * 128
                    sc.dma("act", lambda e: e.dma_start(out=ys_d[r0_:r0_ + 128, :], in_=yt[yi][:]), r=[r_yt[yi]], w=[r_ys])
                    yield
            run_pipelined(p5_tile, NTS, 5)
            sc.barrier()
            p5s.close()

        if last_phase >= 6:
            p6s = es.enter_context(ExitStack())
            y1 = [sb("y1_%d" % i, [128, 1024], F32, p6s) for i in range(2)]
            y2 = [sb("y2_%d" % i, [128, 1024], F32, p6s) for i in range(2)]
            xo = [sb("xo_%d" % i, [128, 1024], F32, p6s) for i in range(2)]
            r_y1, r_y2, r_xo = mkres("y1"), mkres("y2"), mkres("xo")
            def p6_tile(t):
                b = t % 2
                ts_ = slice(t * 128, (t + 1) * 128)
                for k, (yy, ry) in enumerate(((y1[b], r_y1[b]), (y2[b], r_y2[b]))):
                    sc.dma("pool", lambda e: e.indirect_dma_start(out=yy[:], out_offset=None, in_=ys_d[:, :],
                                                                  in_offset=bass.IndirectOffsetOnAxis(ap=pos12[:, k, t:t + 1], axis=0),
                                                                  bounds_check=reg_npos, oob_is_err=False), r=[r_route, r_ys], w=[ry])
                sc.dma("sp", lambda e: e.dma_start(out=xo[b][:], in_=out_d[ts_, :]), r=[r_x2d[t]], w=[r_xo[b]])
                yield
                sc.op("dve", lambda e: e.scalar_tensor_tensor(out=xo[b][:], in0=y1[b][:], scalar=w12[:, 0, t:t + 1], in1=xo[b][:], op0=ALU.mult, op1=ALU.add),
                      r=[r_y1[b], r_route], w=[r_xo[b]])
                sc.op("pool", lambda e: e.scalar_tensor_tensor(out=xo[b][:], in0=y2[b][:], scalar=w12[:, 1, t:t + 1], in1=xo[b][:], op0=ALU.mult, op1=ALU.add),
                      r=[r_y2[b], r_route], w=[r_xo[b]]) if False else \
                    sc.op("dve", lambda e: e.scalar_tensor_tensor(out=xo[b][:], in0=y2[b][:], scalar=w12[:, 1, t:t + 1], in1=xo[b][:], op0=ALU.mult, op1=ALU.add),
                          r=[r_y2[b], r_route], w=[r_xo[b]])
                sc.dma("act", lambda e: e.dma_start(out=out_d[ts_, :], in_=xo[b][:]), r=[r_xo[b]], w=[r_x2d[t]], is_out=True)
            run_pipelined(p6_tile, NT, 2)
            sc.barrier()
            p6s.close()

        sc.finish()
    print("program: %d instructions, %d waits" % (sc.n_inst, sc.n_wait))
    return nc, dbg_out


def _perm_rows(w, c):
    n = w.shape[1]
    return np.ascontiguousarray(w.reshape(c, 128, n).transpose(1, 0, 2))


def _consts():
    cst = np.zeros((128, NCST), np.float64)
    j64 = np.arange(64)
    j32 = np.arange(32)
    invR = 10000.0 ** (-j64 / 64.0) / (2 * np.pi)
    invM = 10000.0 ** (-j32 / 32.0) / (2 * np.pi)
    cst[:, 0:64] = invR
    cst[:, 64:128] = invR
    cst[:, 128:160] = invM
    cst[:, 160:192] = invM
    cst[:, 192 + 64:192 + 128] = 0.25
    cst[:, 192 + 160:192 + 192] = 0.25
    h = np.arange(4)
    lg = np.log(1.0 - np.exp2(-5.0 - h))
    p = np.arange(128)[:, None]
    cst[:, 384:388] = np.exp((p + 1.0) * lg[None, :])
    cst[:, 388:392] = np.exp(-(p + 1.0) * lg[None, :]) * (128.0 ** -0.5)
    cst[:, 392:904] = np.repeat(np.exp(128.0 * lg), 128)[None, :]
    return cst.astype(np.float32)


def make_in_maps(inputs, S, n_cores, last_phase=6):
    f = lambda a: np.ascontiguousarray(np.asarray(a), dtype=np.float32)
    l = 0
    shared = {
        "cst": _consts(),
        "w_in": _perm_rows(f(inputs["w_in"][l]), 8),
        "g_attn": np.ascontiguousarray(f(inputs["attn_norm_g"][l]).reshape(8, 128).T),
        "w_uq": _perm_rows(f(inputs["mla_w_uq"][l]), 2),
        "g_qn": np.ascontiguousarray(f(inputs["mla_q_norm_g"][l]).reshape(2, 128).T),
        "w_ukv": f(inputs["mla_w_ukv"][l]),
        "g_kvn": f(inputs["mla_kv_norm_g"][l]).reshape(128, 1),
        "gq": f(inputs["mla_q_qk_g"][l]).reshape(1, 192),
        "gk": f(inputs["mla_k_qk_g"][l]).reshape(1, 192),
        "gn": f(inputs["ret_gn_g"][l]).reshape(1, 512),
        "w_out": _perm_rows(f(inputs["w_out"][l]), 8),
        "g_cross": np.ascontiguousarray(f(inputs["cross_norm_g"][l]).reshape(8, 128).T),
        "g_mem": np.ascontiguousarray(f(inputs["mem_norm_g"][l]).reshape(8, 128).T),
        "cw_q": _perm_rows(f(inputs["cross_w_q"][l]), 8),
        "cw_kv": _perm_rows(f(inputs["cross_w_kv"][l]), 8),
        "cqg": f(inputs["cross_q_qk_g"][l]).reshape(1, 256),
        "ckg": f(inputs["cross_k_qk_g"][l]).reshape(1, 256),
        "cw_o": _perm_rows(f(inputs["cross_w_o"][l]), 8),
        "mg": f(inputs["moe_norm_g"][l]).reshape(1, 1024),
        "w_rt": _perm_rows(np.concatenate([f(inputs["router_w_group"][l]), f(inputs["router_w_expert"][l])], axis=1), 8),
        "b_rt": np.concatenate([f(inputs["router_b_group"][l]), f(inputs["router_b_expert"][l])]).reshape(1, 36),
        "ew_g": np.ascontiguousarray(f(inputs["expert_w_gate"][l]).reshape(32, 8, 128, 256).transpose(0, 2, 1, 3)).reshape(32 * 128, 2048),
        "ew_u": np.ascontiguousarray(f(inputs["expert_w_up"][l]).reshape(32, 8, 128, 256).transpose(0, 2, 1, 3)).reshape(32 * 128, 2048),
        "ew_d": np.ascontiguousarray(f(inputs["expert_w_down"][l]).reshape(32, 2, 128, 1024).transpose(0, 2, 1, 3)).reshape(32 * 128, 2048),
    }
    if last_phase < 5:
        for k in ("ew_g", "ew_u", "ew_d"):
            del shared[k]
    NT = S // 128
    maps = []
    for b in range(n_cores):
        m = dict(shared)
        m["x"] = f(inputs["x"][b])
        m["mem"] = f(inputs["mem"][b])
        m["pos"] = np.ascontiguousarray(np.asarray(inputs["positions"][b]).astype(np.int32).reshape(NT, 128).T)
        maps.append(m)
    return maps


def kernel(**inputs):
    B, S, _ = inputs["x"].shape
    nc, _ = build_program(S)
    maps = make_in_maps(inputs, S, B)
    res = run_bass_kernel_spmd(nc, maps, core_ids=list(range(B)))
    return np.stack([np.asarray(r["out"]) for r in res.results], axis=0).astype(np.float32)
```

```python
import math
from contextlib import ExitStack

import numpy as np
import concourse.bass as bass
import concourse.mybir as mybir
from concourse.bass_utils import run_bass_kernel_spmd

F32 = mybir.dt.float32
BF16 = mybir.dt.bfloat16
I32 = mybir.dt.int32
AF = mybir.ActivationFunctionType
ALU = mybir.AluOpType
AX = mybir.AxisListType

D = 1024
EPS = 1e-6
NDMA = 24
NCST = 904


class Res:
    __slots__ = ("name", "w", "rd", "excl")

    def __init__(self, name, excl=False):
        self.name = name
        self.w = None
        self.rd = []
        self.excl = excl


class Sched:
    ENGS = ("pe", "act", "dve", "pool", "sp")

    def __init__(self, nc, es):
        self.nc = nc
        self.eng = {"pe": nc.tensor, "act": nc.scalar, "dve": nc.vector, "pool": nc.gpsimd, "sp": nc.sync}
        self.sem = {e: es.enter_context(nc.semaphore("c_" + e)) for e in self.ENGS}
        self.cnt = {e: 0 for e in self.ENGS}
        self.waited = {e: {} for e in self.ENGS}
        self.dsem = {q: [es.enter_context(nc.semaphore("d_%s%d" % (q, i))) for i in range(NDMA)] for q in ("sp", "pool", "act")}
        self.dcnt = {q: [0] * NDMA for q in ("sp", "pool", "act")}
        self.dnext = {q: 0 for q in ("sp", "pool", "act")}
        self.semname = {}
        self.out_tokens = []
        self.n_inst = 0
        self.n_wait = 0

    def res(self, name):
        return Res(name)

    def _wait(self, eng, tok):
        sem, val, src = tok
        if src == "pe" and eng == "pe":
            return
        k = id(sem)
        if self.waited[eng].get(k, 0) >= val:
            return
        self.eng[eng].wait_ge(sem, val)
        self.n_wait += 1
        self.waited[eng][k] = val

    def _deps(self, eng, r, w):
        w = list(w) + [x for x in r if x.excl]
        for x in r:
            if x.w is not None:
                self._wait(eng, x.w)
        for x in w:
            if x.w is not None:
                self._wait(eng, x.w)
            for t in x.rd:
                self._wait(eng, t)

    def _commit(self, tok, r, w):
        w = list(w) + [x for x in r if x.excl]
        r = [x for x in r if not x.excl]
        for x in r:
            x.rd = [t for t in x.rd if t[0] is not tok[0]] + [tok]
        for x in w:
            x.w = tok
            x.rd = []

    def op(self, eng, fn, r=(), w=()):
        self._deps(eng, r, w)
        inst = fn(self.eng[eng])
        self.cnt[eng] += 1
        self.n_inst += 1
        inst.then_inc(self.sem[eng], 1)
        tok = (self.sem[eng], self.cnt[eng], eng)
        self.waited[eng][id(self.sem[eng])] = max(self.waited[eng].get(id(self.sem[eng]), 0), 0)
        self._commit(tok, r, w)
        return tok

    def pe(self, fns, r=(), w=()):
        self._deps("pe", r, w)
        inst = None
        for fn in fns:
            inst = fn(self.eng["pe"])
            self.n_inst += 1
        self.cnt["pe"] += 1
        inst.then_inc(self.sem["pe"], 1)
        tok = (self.sem["pe"], self.cnt["pe"], "pe")
        self._commit(tok, r, w)
        return tok

    def dma(self, q, fn, r=(), w=(), is_out=False):
        self._deps(q, r, w)
        i = self.dnext[q] % NDMA
        self.dnext[q] += 1
        sem = self.dsem[q][i]
        if self.dcnt[q][i] > 0:
            self._wait(q, (sem, 16 * self.dcnt[q][i], "dma"))
        inst = fn(self.eng[q])
        self.n_inst += 1
        self.dcnt[q][i] += 1
        inst.then_inc(sem, 16)
        tok = (sem, 16 * self.dcnt[q][i], "dma")
        self._commit(tok, r, w)
        if is_out:
            self.out_tokens.append(tok)
        return tok

    def barrier(self):
        toks = [(self.sem[e], self.cnt[e], e) for e in self.ENGS if self.cnt[e] > 0]
        for q in ("sp", "pool", "act"):
            for i in range(NDMA):
                if self.dcnt[q][i] > 0:
                    toks.append((self.dsem[q][i], 16 * self.dcnt[q][i], "dma"))
        for e in self.ENGS:
            for t in toks:
                if t[2] == e and e != "pe":
                    pass
                self._wait_force(e, t)

    def _wait_force(self, eng, tok):
        sem, val, src = tok
        k = id(sem)
        if self.waited[eng].get(k, 0) >= val:
            return
        self.eng[eng].wait_ge(sem, val)
        self.n_wait += 1
        self.waited[eng][k] = val

    def finish(self):
        for t in self.out_tokens:
            self._wait_force("sp", t)
        self.barrier()


def build_program(S, last_phase=6, dbg=()):
    NT = S // 128
    NB = S // 512
    nc = bass.Bass("TRN2", target_bir_lowering=False)

    def din(name, shape, dt=F32):
        return nc.dram_tensor(name, list(shape), dt, kind="ExternalInput").ap()

    x_d = din("x", [S, D])
    mem_d = din("mem", [256, D])
    pos_d = din("pos", [128, NT], I32)
    cst_d = din("cst", [128, NCST])
    w_in_d = din("w_in", [128, 8, 2496])
    g_attn_d = din("g_attn", [128, 8])
    w_uq_d = din("w_uq", [128, 2, 768])
    g_qn_d = din("g_qn", [128, 2])
    w_ukv_d = din("w_ukv", [128, 1024])
    g_kvn_d = din("g_kvn", [128, 1])
    gq_d = din("gq", [1, 192])
    gk_d = din("gk", [1, 192])
    gn_d = din("gn", [1, 512])
    w_out_d = din("w_out", [128, 8, 1024])
    g_cross_d = din("g_cross", [128, 8])
    g_mem_d = din("g_mem", [128, 8])
    cw_q_d = din("cw_q", [128, 8, 1024])
    cw_kv_d = din("cw_kv", [128, 8, 2048])
    cqg_d = din("cqg", [1, 256])
    ckg_d = din("ckg", [1, 256])
    cw_o_d = din("cw_o", [128, 8, 1024])
    mg_d = din("mg", [1, 1024])
    w_rt_d = din("w_rt", [128, 8, 36])
    b_rt_d = din("b_rt", [1, 36])
    if last_phase >= 5:
        ew_g_d = din("ew_g", [32 * 128, 2048])
        ew_u_d = din("ew_u", [32 * 128, 2048])
        ew_d_d = din("ew_d", [32 * 128, 2048])
    out_d = nc.dram_tensor("out", [S, D], F32, kind="ExternalOutput").ap()

    dbg_out = {}

    with ExitStack() as es:
        sc = Sched(nc, es)

        def sb(name, shape, dt=F32, stack=es):
            return stack.enter_context(nc.sbuf_tensor("s_" + name, list(shape), dt))

        banks = [es.enter_context(nc.psum_tensor("bank%d" % i, [128, 512], F32)) for i in range(8)]
        bank_res = [Res("bank%d" % i, excl=True) for i in range(8)]
        bstate = {"i": 0}

        def nbank():
            i = bstate["i"] % 8
            bstate["i"] += 1
            return banks[i], bank_res[i]

        def dump(name, ap, res, shape, dt=F32):
            if name not in dbg:
                return
            t = nc.dram_tensor("dbg_" + name, list(shape), dt, kind="ExternalOutput").ap()
            dbg_out[name] = t
            sc.dma("sp", lambda e: e.dma_start(out=t, in_=ap), r=[res], w=[], is_out=True)

        def nbank(lo=0, hi=8):
            key = (lo, hi)
            i = lo + bstate.get(key, 0) % (hi - lo)
            bstate[key] = bstate.get(key, 0) + 1
            return banks[i], bank_res[i]

        cst = sb("cst", [128, NCST])
        r_cst = sc.res("cst")
        sc.dma("sp", lambda e: e.dma_start(out=cst[:], in_=cst_d[:, :]), w=[r_cst])
        INVF = cst[:, 0:192]
        OFFS = cst[:, 192:384]
        QDc = cst[:, 384:388]
        KDc = cst[:, 388:392]
        CDEC = cst[:, 392:904]

        ident_bf = sb("ident_bf", [128, 128], BF16)
        ident_f = sb("ident_f", [128, 128], F32)
        maskT = sb("maskT", [128, 128], BF16)
        mask4 = sb("mask4", [128, 4, 128], F32)
        tri = sb("tri", [128, 128], BF16)
        ones_bf = sb("ones_bf", [128, 128], BF16)
        r_const = sc.res("consts")

        def mk_mask(t_ap, pattern, cmp):
            sc.op("pool", lambda e: e.memset(t_ap, 1.0), w=[r_const])
            sc.op("pool", lambda e: e.affine_select(out=t_ap, in_=t_ap, pattern=pattern, compare_op=cmp, fill=0.0,
                                                    base=0, channel_multiplier=-1), r=[r_const], w=[r_const])

        mk_mask(ident_bf[:], [[1, 128]], ALU.is_equal)
        mk_mask(ident_f[:], [[1, 128]], ALU.is_equal)
        mk_mask(maskT[:], [[1, 128]], ALU.is_ge)
        mk_mask(mask4[:], [[0, 4], [1, 128]], ALU.is_ge)
        mk_mask(tri[:], [[1, 128]], ALU.is_gt)
        negm = sb("negm", [128, 128], BF16)
        sc.op("pool", lambda e: e.memset(negm[:], -30000.0), w=[r_const])
        sc.op("pool", lambda e: e.affine_select(out=negm[:], in_=negm[:], pattern=[[-1, 128]], compare_op=ALU.is_gt, fill=0.0,
                                                base=0, channel_multiplier=1), r=[r_const], w=[r_const])
        sc.op("pool", lambda e: e.memset(ones_bf[:], 1.0), w=[r_const])

        def bload(name, src, n, stack=es):
            t = sb(name, [128, n], F32, stack)
            sc.dma("sp", lambda e: e.dma_start(out=t[:], in_=src.partition_broadcast(128)), w=[r_const])
            return t

        def pload(name, src, n, stack=es):
            t = sb(name, [128, n], F32, stack)
            sc.dma("sp", lambda e: e.dma_start(out=t[:], in_=src[:, :]), w=[r_const])
            return t

        gq = bload("gq", gq_d, 192)
        gk = bload("gk", gk_d, 192)
        gn = bload("gn", gn_d, 512)

        pos_i = sb("pos_i", [128, NT], I32)
        pos_f = sb("pos_f", [128, NT])
        SCm = sb("SCm", [128, NT, 64])
        rstd1 = sb("rstd1", [128, NT])
        r_sc = sc.res("SC")
        r_sct = [sc.res("SCt%d" % t) for t in range(NT)]
        r_rstd1 = sc.res("rstd1")
        stage = [None, None]
        r_stage = [sc.res("stage%d" % i) for i in range(2)]
        st = {"i": 0}
        scale_engs = ("dve", "act")

        def load_scaled(dst_fn, src_fn, gain_fn, C, N, rdst):
            for c in range(C):
                for n0 in range(0, N, 1024):
                    n1 = min(N, n0 + 1024)
                    i = st["i"] % 2
                    st["i"] += 1
                    stg, rs = stage[i], r_stage[i]
                    sc.dma("sp", lambda e: e.dma_start(out=stg[:, 0:n1 - n0], in_=src_fn(c, n0, n1)), w=[rs])
                    if scale_engs[i] == "act":
                        sc.op("act", lambda e: e.activation(out=dst_fn(c, n0, n1), in_=stg[:, 0:n1 - n0], func=AF.Copy, scale=gain_fn(c)),
                              r=[rs, r_const], w=[rdst])
                    else:
                        sc.op("dve", lambda e: e.tensor_scalar(out=dst_fn(c, n0, n1), in0=stg[:, 0:n1 - n0], scalar1=gain_fn(c), scalar2=None,
                                                               op0=ALU.mult), r=[rs, r_const], w=[rdst])

        def load_cast(dst_fn, src_fn, C, rdst):
            for c in range(C):
                sc.dma("pool", lambda e: e.dma_start(out=dst_fn(c), in_=src_fn(c)), w=[rdst])

        mhalf = sb("mhalf", [128, 8])
        sc.op("pool", lambda e: e.memset(mhalf[:], -0.5), w=[r_const])

        def rstd_from_ss(ss_ap, n, out_ap, r_in, r_out, tmp_ap, r_tmp):
            k = ss_ap.shape[1]
            sc.op("dve", lambda e: e.tensor_scalar(out=tmp_ap, in0=ss_ap, scalar1=1.0 / n, scalar2=EPS, op0=ALU.mult, op1=ALU.add),
                  r=[r_in], w=[r_tmp])
            sc.op("pool", lambda e: e.tensor_tensor(out=out_ap, in0=tmp_ap, in1=mhalf[:, 0:k], op=ALU.pow), r=[r_tmp, r_const], w=[r_out])

        sc.dma("sp", lambda e: e.dma_start(out=pos_i[:], in_=pos_d[:, :]), w=[r_sc])
        sc.op("dve", lambda e: e.tensor_copy(out=pos_f[:], in_=pos_i[:]), r=[r_sc], w=[r_sc])

        rout_d = nc.dram_tensor("rout_s", [S, 512], BF16).ap()
        x1_d = nc.dram_tensor("x1_s", [S, D], F32).ap()
        hm_d = nc.dram_tensor("hm_s", [S, D], BF16).ap()
        TS = 256
        NTS = (2 * S) // TS + 32
        NPOS = NTS * TS
        xs_d = nc.dram_tensor("xs_s", [NPOS, D], BF16).ap()
        ys_d = nc.dram_tensor("ys_s", [NPOS, D], F32).ap()
        r_xs = sc.res("xs_d")

        def run_pipelined(gen_fn, n_items, depth):
            active = []
            nxt = 0
            while nxt < n_items or active:
                if nxt < n_items and len(active) < depth:
                    active.append(gen_fn(nxt))
                    nxt += 1
                for g in list(active):
                    try:
                        next(g)
                    except StopIteration:
                        active.remove(g)

        def mkres(n, k=2):
            return [sc.res("%s%d" % (n, i)) for i in range(k)]

        if last_phase >= 1:
            p1s = es.enter_context(ExitStack())
            import os as _os
            stop = int(_os.environ.get('P1_STOP', '99'))
            SCr = sb("SCr", [128, NT, 128], F32, p1s)
            w_r = sb("w_r", [128, 8, 2048], BF16, p1s)
            r_wr = sc.res("w_r")
            stage[:] = [sb("stage1_%d" % i, [128, 1024], F32, p1s) for i in range(2)]
            g_attn = pload("g_attn", g_attn_d, 8, p1s)
            load_scaled(lambda c, a, b_: w_r[:, c, a:b_], lambda c, a, b_: w_in_d[:, c, 448 + a:448 + b_], lambda c: g_attn[:, c:c + 1], 8, 2048, r_wr)

            NTsc = NT if stop >= 2 else 0
            tr_t = sb("tr_t", [128, 192], F32, p1s)
            tr_i = sb("tr_i", [128, 192], I32, p1s)
            tr_f = sb("tr_f", [128, 192], F32, p1s)
            r_tr = sc.res("tr")
            def sc_tables(t):
                sc.op("dve", lambda e: e.scalar_tensor_tensor(out=tr_t[:], in0=INVF, scalar=pos_f[:, t:t + 1], in1=OFFS,
                                                              op0=ALU.mult, op1=ALU.add), r=[r_cst, r_sc], w=[r_tr])
                sc.op("dve", lambda e: e.tensor_copy(out=tr_i[:], in_=tr_t[:]), r=[r_tr], w=[r_tr])
                sc.op("dve", lambda e: e.tensor_copy(out=tr_f[:], in_=tr_i[:]), r=[r_tr], w=[r_tr])
                sc.op("dve", lambda e: e.tensor_tensor(out=tr_t[:], in0=tr_t[:], in1=tr_f[:], op=ALU.subtract), r=[r_tr], w=[r_tr])
                sc.op("dve", lambda e: e.scalar_tensor_tensor(out=tr_f[:], in0=tr_t[:], scalar=0.5, in1=tr_t[:],
                                                              op0=ALU.is_gt, op1=ALU.subtract), r=[r_tr], w=[r_tr])
                sc.op("dve", lambda e: e.scalar_tensor_tensor(out=tr_t[:], in0=tr_f[:], scalar=0.5, in1=tr_f[:],
                                                              op0=ALU.is_gt, op1=ALU.subtract), r=[r_tr], w=[r_tr])
                sc.op("act", lambda e: e.activation(out=SCr[:, t, :], in_=tr_t[:, 0:128], func=AF.Sin, scale=6.28318), r=[r_tr], w=[r_sct[t]])
                sc.op("act", lambda e: e.activation(out=SCm[:, t, :], in_=tr_t[:, 128:192], func=AF.Sin, scale=6.28318), r=[r_tr], w=[r_sct[t]])
            dump("SCr", SCr[:], r_sct[NT - 1], [128, NT, 128])
            dump("SCm", SCm[:], r_sct[NT - 1], [128, NT, 64])

            do_pc = last_phase >= 5
            if do_pc:
                ewb_all = nc.dram_tensor("ewb_all", [32 * 128, 6144], BF16).ap()
                ewb_d = [ewb_all[:, k * 2048:(k + 1) * 2048] for k in range(3)]
                ew_src = [ew_g_d, ew_u_d, ew_d_d]
                r_ewb = sc.res("ewb")
                pcs = [sb("pcs%d" % i, [128, 2048], BF16, p1s) for i in range(3)]
                r_pcs = mkres("pcs", 3)
                pc_state = {"i": 0}
                PC_PER_TILE = (96 + NT - 1) // NT

                pend_wb = []

                def precast_flush():
                    while pend_wb:
                        k_, rows, bi = pend_wb.pop(0)
                        sc.dma("sp", lambda e: e.dma_start(out=ewb_d[k_][rows, :], in_=pcs[bi][:]), r=[r_pcs[bi]], w=[])

                def precast_step():
                    precast_flush()
                    for _ in range(PC_PER_TILE):
                        i = pc_state["i"]
                        if i >= 96:
                            return
                        pc_state["i"] += 1
                        if len(pend_wb) >= len(pcs):
                            precast_flush()
                        e_, k_ = i // 3, i % 3
                        bi = i % len(pcs)
                        rows = slice(e_ * 128, (e_ + 1) * 128)
                        sc.dma("pool", lambda e: e.dma_start(out=pcs[bi][:], in_=ew_src[k_][rows, :]), w=[r_pcs[bi]])
                        pend_wb.append((k_, rows, bi))

            zt = sb("zt", [128, 2, 1024], BF16, p1s)
            r_zt = sc.res("zt")
            sc.op("pool", lambda e: e.memset(zt[:], 0.0), w=[r_zt])
            ROWS_PER = NPOS // NT

            def zero_step(t):
                if last_phase < 4:
                    return
                for r0_ in range(t * ROWS_PER, (t + 1) * ROWS_PER, 256):
                    sc.dma("sp", lambda e: e.dma_start(out=xs_d[r0_:r0_ + 256, :].rearrange("(p a) d -> p a d", a=2), in_=zt[:]), r=[r_zt], w=[])

            xt = [sb("xt%d" % i, [128, 1024], F32, p1s) for i in range(4)]
            xb = [sb("xb%d" % i, [128, 1024], BF16, p1s) for i in range(4)]
            xT = [sb("xT%d" % i, [128, 8, 128], BF16, p1s) for i in range(4)]
            junk = sb("junk", [128, 1024], BF16, p1s)
            rq_f = [sb("rq_f%d" % i, [128, 512], F32, p1s) for i in range(4)]
            rk_f = [sb("rk_f%d" % i, [128, 512], F32, p1s) for i in range(4)]
            v_b = [sb("v_b%d" % i, [128, 512], BF16, p1s) for i in range(4)]
            sg = [sb("sg%d" % i, [128, 512], F32, p1s) for i in range(6)]
            sm1 = [sb("sm1_%d" % i, [128, 32], F32, p1s) for i in range(4)]
            rp = [sb("rp%d" % i, [128, 2, 512], F32, p1s) for i in range(2)]
            qp_b = [sb("qp_b%d" % i, [128, 512], BF16, p1s) for i in range(4)]
            kp_b = [sb("kp_b%d" % i, [128, 512], BF16, p1s) for i in range(4)]
            qpT = [sb("qpT%d" % i, [128, 4, 128], BF16, p1s) for i in range(4)]
            kpT = [sb("kpT%d" % i, [128, 4, 128], BF16, p1s) for i in range(4)]
            PT = [sb("PT%d" % i, [128, 4, 128], BF16, p1s) for i in range(3)]
            Tst = sb("Tst", [128, 4, 128], F32, p1s)
            Tst_b = sb("Tst_b", [128, 4, 128], BF16, p1s)
            Ttmp = sb("Ttmp", [128, 4, 128], F32, p1s)
            o_f = [sb("o_f%d" % i, [128, 4, 128], F32, p1s) for i in range(4)]
            bnst = [sb("bnst%d" % i, [128, 4, 6], F32, p1s) for i in range(4)]
            bnag = [sb("bnag%d" % i, [128, 4, 2], F32, p1s) for i in range(4)]
            ro_b = [sb("ro_b%d" % i, [128, 512], BF16, p1s) for i in range(3)]

            r_xt, r_xb, r_xT = mkres("xt", 4), mkres("xb", 4), mkres("xT", 4)
            r_rq, r_rk, r_vb, r_sg, r_sm1 = mkres("rq", 4), mkres("rk", 4), mkres("vb", 4), mkres("sg", 6), mkres("sm1", 4)
            r_rp = mkres("rp")
            r_qpb, r_kpb, r_qpT, r_kpT, r_PT, r_of, r_bn, r_rob = (mkres("qpb", 4), mkres("kpb", 4), mkres("qpT", 4), mkres("kpT", 4), mkres("PT", 3),
                                                                   mkres("of", 4), mkres("bn", 4), mkres("rob", 3))
            r_junk = sc.res("junk")
            r_T = sc.res("Tst")
            r_Tb = sc.res("Tst_b")
            r_Tt = sc.res("Ttmp")
            sc.op("dve", lambda e: e.memset(Tst[:], 0.0), w=[r_T])
            sc.op("dve", lambda e: e.memset(Tst_b[:], 0.0), w=[r_Tb])

            def rope_ret(eng, src, r_src, dst_b, r_dst, t, decay, scr, r_scr):
                cosB = SCr[:, t, 64:128].unsqueeze(1).broadcast_to([128, 8, 64])
                sinB = SCr[:, t, 0:64].unsqueeze(1).broadcast_to([128, 4, 64])
                sv = src[:].rearrange("p (h two d) -> p h two d", h=4, two=2)
                Pv = scr[:, 0, :]
                Qv = scr[:, 1, :].rearrange("p (h two d) -> p h two d", h=4, two=2)
                P4 = scr[:, 0, :].rearrange("p (h two d) -> p h two d", h=4, two=2)
                sc.op(eng, lambda e: e.tensor_tensor(out=Pv.rearrange("p (g d) -> p g d", g=8), in0=src[:].rearrange("p (g d) -> p g d", g=8),
                                                     in1=cosB, op=ALU.mult), r=[r_src, r_sct[t]], w=[r_scr])
                sc.op(eng, lambda e: e.tensor_tensor(out=Qv[:, :, 0, :], in0=sv[:, :, 1, :], in1=sinB, op=ALU.mult), r=[r_src, r_sct[t]], w=[r_scr])
                sc.op(eng, lambda e: e.tensor_tensor(out=Qv[:, :, 1, :], in0=sv[:, :, 0, :], in1=sinB, op=ALU.mult), r=[r_src, r_sct[t]], w=[r_scr])
                sc.op(eng, lambda e: e.tensor_tensor(out=P4[:, :, 0, :], in0=P4[:, :, 0, :], in1=Qv[:, :, 0, :], op=ALU.subtract), r=[r_scr], w=[r_scr])
                sc.op(eng, lambda e: e.tensor_tensor(out=P4[:, :, 1, :], in0=P4[:, :, 1, :], in1=Qv[:, :, 1, :], op=ALU.add), r=[r_scr], w=[r_scr])
                decB = decay.unsqueeze(2).broadcast_to([128, 4, 128])
                sc.op(eng, lambda e: e.tensor_tensor(out=dst_b[:].rearrange("p (h d) -> p h d", h=4), in0=Pv.rearrange("p (h d) -> p h d", h=4),
                                                     in1=decB, op=ALU.mult), r=[r_scr, r_cst], w=[r_dst])

            def p1_tile(t):
                b = t % 4
                b6 = t % 6
                b3 = t % 3
                ts_ = slice(t * 128, (t + 1) * 128)
                s1 = sm1[b]
                sc.dma("sp", lambda e: e.dma_start(out=xt[b][:], in_=x_d[ts_, :]), w=[r_xt[b]])
                sc_tables(t)
                if do_pc:
                    precast_step()
                zero_step(t)
                sc.op("act", lambda e: e.activation(out=junk[:], in_=xt[b][:], func=AF.Square, accum_out=s1[:, 0:1]),
                      r=[r_xt[b]], w=[r_junk, r_sm1[b]])
                rstd_from_ss(s1[:, 0:1], 1024.0, rstd1[:, t:t + 1], r_sm1[b], r_rstd1, s1[:, 1:2], r_sm1[b])
                yield
                sc.op("act", lambda e: e.copy(out=xb[b][:], in_=xt[b][:]), r=[r_xt[b]], w=[r_xb[b]])
                bk, rb = nbank()
                pv = bk[:].bitcast(BF16).rearrange("p (c n) -> p c n", n=128)
                sc.pe([(lambda e, c=c: e.transpose(out=pv[:, c, :], in_=xb[b][:, c * 128:(c + 1) * 128], identity=ident_bf[:])) for c in range(8)],
                      r=[r_xb[b], r_const], w=[rb])
                sc.op("dve", lambda e: e.tensor_copy(out=xT[b][:], in_=pv), r=[rb], w=[r_xT[b]])
                rs1 = rstd1[:, t:t + 1]
                yield
                pb = []
                for k4 in range(4):
                    bk, rb = nbank()
                    sc.pe([(lambda e, c=c: e.matmul(bk[:], lhsT=xT[b][:, c, :], rhs=w_r[:, c, k4 * 512:(k4 + 1) * 512], start=(c == 0), stop=(c == 7)))
                           for c in range(8)], r=[r_xT[b], r_wr], w=[rb])
                    pb.append((bk, rb))
                sc.op("act", lambda e: e.activation(out=rq_f[b][:], in_=pb[0][0][:], func=AF.Copy, scale=rs1), r=[pb[0][1], r_rstd1], w=[r_rq[b]])
                sc.op("dve", lambda e: e.tensor_scalar(out=rk_f[b][:], in0=pb[1][0][:], scalar1=rs1, scalar2=None, op0=ALU.mult),
                      r=[pb[1][1], r_rstd1], w=[r_rk[b]])
                sc.op("act", lambda e: e.activation(out=v_b[b][:], in_=pb[2][0][:], func=AF.Copy, scale=rs1), r=[pb[2][1], r_rstd1], w=[r_vb[b]])
                sc.op("act", lambda e: e.activation(out=sg[b6][:], in_=pb[3][0][:], func=AF.Silu, scale=rs1), r=[pb[3][1], r_rstd1], w=[r_sg[b6]])

                yield
                rope_ret("dve", rq_f[b], r_rq[b], qp_b[b], r_qpb[b], t, QDc, rp[0], r_rp[0])
                rope_ret("pool", rk_f[b], r_rk[b], kp_b[b], r_kpb[b], t, KDc, rp[1], r_rp[1])
                yield
                bk, rb = nbank()
                pv = bk[:].bitcast(BF16).rearrange("p (c n) -> p c n", n=128)
                sc.pe([(lambda e, h=h: e.transpose(out=pv[:, h, :], in_=qp_b[b][:, h * 128:(h + 1) * 128], identity=ident_bf[:])) for h in range(4)]
                      + [(lambda e, h=h: e.transpose(out=pv[:, 4 + h, :], in_=kp_b[b][:, h * 128:(h + 1) * 128], identity=ident_bf[:])) for h in range(4)],
                      r=[r_qpb[b], r_kpb[b], r_const], w=[rb])
                sc.op("act", lambda e: e.copy(out=qpT[b][:], in_=pv[:, 0:4, :]), r=[rb], w=[r_qpT[b]])
                sc.op("dve", lambda e: e.tensor_copy(out=kpT[b][:], in_=pv[:, 4:8, :]), r=[rb], w=[r_kpT[b]])
                bk, rb = nbank()
                sv_ = bk[:].rearrange("p (h n) -> p h n", h=4)
                sc.pe([(lambda e, h=h: e.matmul(sv_[:, h, :], lhsT=kpT[b][:, h, :], rhs=qpT[b][:, h, :], start=True, stop=True)) for h in range(4)],
                      r=[r_kpT[b], r_qpT[b]], w=[rb])
                sc.op("dve", lambda e: e.tensor_tensor(out=PT[b3][:], in0=sv_, in1=mask4[:], op=ALU.mult), r=[rb, r_const], w=[r_PT[b3]])
                yield
                bk_o, rb_o = nbank()
                ov = bk_o[:].rearrange("p (h n) -> p h n", h=4)
                fns = []
                for h in range(4):
                    fns.append(lambda e, h=h: e.matmul(ov[:, h, :], lhsT=PT[b3][:, h, :], rhs=v_b[b][:, h * 128:(h + 1) * 128], start=True, stop=False))
                    fns.append(lambda e, h=h: e.matmul(ov[:, h, :], lhsT=qpT[b][:, h, :], rhs=Tst_b[:, h, :], start=False, stop=True))
                sc.pe(fns, r=[r_PT[b3], r_vb[b], r_qpT[b], r_Tb], w=[rb_o])
                bk_s, rb_s = nbank()
                stv = bk_s[:].rearrange("p (h n) -> p h n", h=4)
                sc.pe([(lambda e, h=h: e.matmul(stv[:, h, :], lhsT=kp_b[b][:, h * 128:(h + 1) * 128], rhs=v_b[b][:, h * 128:(h + 1) * 128],
                                                start=True, stop=True)) for h in range(4)], r=[r_kpb[b], r_vb[b]], w=[rb_s])
                sc.op("dve", lambda e: e.tensor_tensor(out=Ttmp[:], in0=stv, in1=Tst[:], op=ALU.add), r=[rb_s, r_T], w=[r_Tt])
                sc.op("pool", lambda e: e.tensor_tensor(out=Tst[:], in0=Ttmp[:], in1=CDEC.rearrange("p (h n) -> p h n", h=4), op=ALU.mult),
                      r=[r_Tt, r_cst], w=[r_T])
                sc.op("pool", lambda e: e.tensor_copy(out=Tst_b[:], in_=Tst[:]), r=[r_T], w=[r_Tb])
                yield
                sc.op("act", lambda e: e.copy(out=o_f[b][:], in_=ov), r=[rb_o], w=[r_of[b]])
                for h in range(4):
                    sc.op("dve", lambda e: e.bn_stats(out=bnst[b][:, h, :], in_=o_f[b][:, h, :]), r=[r_of[b]], w=[r_bn[b]])
                for h in range(4):
                    sc.op("dve", lambda e: e.bn_aggr(out=bnag[b][:, h, :], in_=bnst[b][:, h, :]), r=[r_bn[b]], w=[r_bn[b]])
                sc.op("dve", lambda e: e.tensor_scalar(out=s1[:, 20:24], in0=bnag[b][:, :, 1], scalar1=EPS, scalar2=None, op0=ALU.add),
                      r=[r_bn[b]], w=[r_sm1[b]])
                sc.op("pool", lambda e: e.tensor_tensor(out=s1[:, 24:28], in0=s1[:, 20:24], in1=mhalf[:, 0:4], op=ALU.pow), r=[r_sm1[b], r_const], w=[r_sm1[b]])
                for h in range(4):
                    sc.op("dve", lambda e: e.tensor_scalar(out=o_f[b][:, h, :], in0=o_f[b][:, h, :], scalar1=bnag[b][:, h, 0:1], scalar2=s1[:, 24 + h:25 + h],
                                                           op0=ALU.subtract, op1=ALU.mult), r=[r_of[b], r_bn[b], r_sm1[b]], w=[r_of[b]])
                ofl = o_f[b][:].rearrange("p h n -> p (h n)")
                sc.op("pool", lambda e: e.tensor_tensor(out=ofl, in0=ofl, in1=gn[:], op=ALU.mult), r=[r_of[b], r_const], w=[r_of[b]])
                sc.op("pool", lambda e: e.tensor_tensor(out=ro_b[b3][:], in0=ofl, in1=sg[b6][:], op=ALU.mult), r=[r_of[b], r_sg[b6]], w=[r_rob[b3]])
                r_routd = sc.res("rout_d%d" % t)
                sc.dma("pool", lambda e: e.dma_start(out=rout_d[ts_, :], in_=ro_b[b3][:]), r=[r_rob[b3]], w=[r_routd])
                if t == NT - 1:
                    dump("ro_last", ro_b[b3][:], r_rob[b3], [128, 512], BF16)
            run_pipelined(p1_tile, NT, 4)
            if do_pc:
                while pc_state['i'] < 96:
                    precast_step()
                precast_flush()
            dump("rstd1", rstd1[:], r_rstd1, [128, NT])
            sc.barrier()
            p1s.close()

        if last_phase >= 2:
            p2s = es.enter_context(ExitStack())
            KTn = sb("KTn", [128, 4, S], BF16, p2s)
            KTr = sb("KTr", [128, 2, S], BF16, p2s)
            Vaug = sb("Vaug", [128, NT, 4, 129], BF16, p2s)
            r_KT = [sc.res("KT%d" % t) for t in range(NT)]
            r_Vt = [sc.res("Vaug%d" % t) for t in range(NT)]
            sc.op("pool", lambda e: e.memset(Vaug[:], 1.0), w=r_Vt)
            w_a = sb("w_a", [128, 8, 448], BF16, p2s)
            w_uq = sb("w_uq", [128, 2, 768], BF16, p2s)
            w_ukv = sb("w_ukv", [128, 1024], BF16, p2s)
            w_out = sb("w_out", [128, 8, 1024], BF16, p2s)
            r_w2 = sc.res("w2")
            g_attn2 = pload("g_attn2", g_attn_d, 8, p2s)
            g_qn = pload("g_qn", g_qn_d, 2, p2s)
            g_kvn = pload("g_kvn", g_kvn_d, 1, p2s)
            pw2 = es.enter_context(ExitStack())
            stage[:] = [sb("stage2_%d" % i, [128, 1024], F32, pw2) for i in range(2)]
            load_scaled(lambda c, a, b_: w_a[:, c, a:b_], lambda c, a, b_: w_in_d[:, c, a:b_], lambda c: g_attn2[:, c:c + 1], 8, 448, r_w2)
            load_scaled(lambda c, a, b_: w_uq[:, c, a:b_], lambda c, a, b_: w_uq_d[:, c, a:b_], lambda c: g_qn[:, c:c + 1], 2, 768, r_w2)
            load_scaled(lambda c, a, b_: w_ukv[:, a:b_], lambda c, a, b_: w_ukv_d[:, a:b_], lambda c: g_kvn[:, 0:1], 1, 1024, r_w2)
            load_cast(lambda c: w_out[:, c, :], lambda c: w_out_d[:, c, :], 8, r_w2)
            sc.barrier()
            pw2.close()

            xt = [sb("x2t%d" % i, [128, 1024], F32, p2s) for i in range(2)]
            xb = [sb("x2b%d" % i, [128, 1024], BF16, p2s) for i in range(2)]
            xT = [sb("x2T%d" % i, [128, 8, 128], BF16, p2s) for i in range(2)]
            junk = sb("junk2", [128, 768], F32, p2s)
            cq_f = [sb("cq_f%d" % i, [128, 256], F32, p2s) for i in range(3)]
            cq_b = [sb("cq_b%d" % i, [128, 256], BF16, p2s) for i in range(3)]
            cqT = [sb("cqT%d" % i, [128, 2, 128], BF16, p2s) for i in range(3)]
            ckv_f = [sb("ckv_f%d" % i, [128, 192], F32, p2s) for i in range(3)]
            ckv_b = [sb("ckv_b%d" % i, [128, 128], BF16, p2s) for i in range(3)]
            ckvT = [sb("ckvT%d" % i, [128, 128], BF16, p2s) for i in range(3)]
            kn_f = [sb("kn_f0", [128, 4, 128], F32, p2s)] * 3
            kn_b = [sb("kn_b%d" % i, [128, 4, 128], BF16, p2s) for i in range(2)]
            kpe = [sb("kpe0", [128, 3, 64], F32, p2s)] * 3
            kr = [sb("kr%d" % i, [128, 64], F32, p2s) for i in range(3)]
            krn_b = [sb("krn_b%d" % i, [128, 4, 64], BF16, p2s) for i in range(3)]
            q_f = [sb("q_f%d" % i, [128, 4, 192], F32, p2s) for i in range(2)]
            qn_b = [sb("qn_b%d" % i, [128, 4, 128], BF16, p2s) for i in range(2)]
            qr = [sb("qr0", [128, 4, 4, 64], F32, p2s)] * 3
            qrn_b = [sb("qrn_b%d" % i, [128, 4, 64], BF16, p2s) for i in range(3)]
            sm2 = [sb("sm2_%d" % i, [128, 48], F32, p2s) for i in range(3)]
            QTn = [sb("QTn%d" % i, [128, 4, 512], BF16, p2s) for i in range(2)]
            QTr = [sb("QTr%d" % i, [128, 2, 512], BF16, p2s) for i in range(2)]
            PTt = [sb("PTt%d" % i, [128, 512], BF16, p2s) for i in range(3)]
            a_b = [sb("a_b%d" % i, [128, 4, 512], BF16, p2s) for i in range(2)]
            rcp = [sb("rcp%d" % i, [128, 4], F32, p2s) for i in range(2)]
            r_b = [sb("r_b%d" % i, [128, 512], BF16, p2s) for i in range(2)]
            aoT = [sb("aoT%d" % i, [128, 8, 128], BF16, p2s) for i in range(2)]

            r_xt, r_xb, r_xT = mkres("x2t"), mkres("x2b"), mkres("x2T")
            r_junk = sc.res("junk2")
            r_cq, r_cqb, r_cqT, r_ckv, r_ckvb, r_ckvT = mkres("cq", 3), mkres("cqb", 3), mkres("cqT", 3), mkres("ckv", 3), mkres("ckvb", 3), mkres("ckvT", 3)
            r_kn, r_knb, r_kpe, r_kr, r_krn = mkres("kn", 1) * 3, mkres("knb"), mkres("kpe", 1) * 3, mkres("kr", 3), mkres("krn", 3)
            r_qf, r_qnb, r_qr, r_qrn, r_sm2 = mkres("qf"), mkres("qnb"), mkres("qr", 1) * 3, mkres("qrn", 3), mkres("sm2", 3)
            r_QT = mkres("QT")
            r_PTt = mkres("PTt", 3)
            r_ab, r_rcp, r_rb, r_aoT = mkres("ab"), mkres("rcp"), mkres("rb"), mkres("aoT")
            SCALE = 192.0 ** -0.5

            def prep_tile(t, qb):
                b = t % 3
                bx = t % 2
                ts_ = slice(t * 128, (t + 1) * 128)
                tl = slice((t % 4) * 128, (t % 4 + 1) * 128)
                s2 = sm2[b]
                rs1 = rstd1[:, t:t + 1]
                sc.dma("sp", lambda e: e.dma_start(out=xt[0][:], in_=x_d[ts_, :]), w=[r_xt[0]])
                sc.op("dve", lambda e: e.tensor_copy(out=xb[bx][:], in_=xt[0][:]), r=[r_xt[0]], w=[r_xb[bx]])
                yield
                bk, rb = nbank(4, 8)
                pv = bk[:].bitcast(BF16).rearrange("p (c n) -> p c n", n=128)
                sc.pe([(lambda e, c=c: e.transpose(out=pv[:, c, :], in_=xb[bx][:, c * 128:(c + 1) * 128], identity=ident_bf[:])) for c in range(8)],
                      r=[r_xb[bx], r_const], w=[rb])
                sc.op("dve", lambda e: e.tensor_copy(out=xT[bx][:], in_=pv), r=[rb], w=[r_xT[bx]])
                yield
                bk, rb = nbank(4, 8)
                sc.pe([(lambda e, c=c: e.matmul(bk[:, 0:448], lhsT=xT[bx][:, c, :], rhs=w_a[:, c, :], start=(c == 0), stop=(c == 7))) for c in range(8)],
                      r=[r_xT[bx], r_w2], w=[rb])
                sc.op("act", lambda e: e.activation(out=cq_f[b][:], in_=bk[:, 0:256], func=AF.Copy, scale=rs1), r=[rb, r_rstd1], w=[r_cq[b]])
                sc.op("act", lambda e: e.activation(out=ckv_f[b][:], in_=bk[:, 256:448], func=AF.Copy, scale=rs1), r=[rb, r_rstd1], w=[r_ckv[b]])
                yield
                sc.op("act", lambda e: e.activation(out=junk[:, 0:128], in_=ckv_f[b][:, 0:128], func=AF.Square, accum_out=s2[:, 2:3]), r=[r_ckv[b]], w=[r_junk, r_sm2[b]])
                rstd_from_ss(s2[:, 2:3], 128.0, s2[:, 3:4], r_sm2[b], r_sm2[b], s2[:, 4:5], r_sm2[b])
                sc.op("act", lambda e: e.activation(out=junk[:, 128:192], in_=ckv_f[b][:, 128:192], func=AF.Square, accum_out=s2[:, 5:6]), r=[r_ckv[b]], w=[r_junk, r_sm2[b]])
                sc.op("pool", lambda e: e.tensor_copy(out=ckv_b[b][:], in_=ckv_f[b][:, 0:128]), r=[r_ckv[b]], w=[r_ckvb[b]])
                bk, rb = nbank(4, 8)
                pv = bk[:].bitcast(BF16)
                sc.pe([lambda e: e.transpose(out=pv[:, 0:128], in_=ckv_b[b][:], identity=ident_bf[:])], r=[r_ckvb[b], r_const], w=[rb])
                sc.op("act", lambda e: e.copy(out=ckvT[b][:], in_=pv[:, 0:128]), r=[rb], w=[r_ckvT[b]])
                yield
                for hh in range(2):
                    bk, rb = nbank(4, 8)
                    sc.pe([lambda e: e.matmul(bk[:], lhsT=ckvT[b][:], rhs=w_ukv[:, hh * 512:(hh + 1) * 512], start=True, stop=True)],
                          r=[r_ckvT[b], r_w2], w=[rb])
                    kvv = bk[:].rearrange("p (h two d) -> p h two d", h=2, two=2)
                    sc.op("dve", lambda e: e.tensor_scalar(out=kn_f[b][:, 2 * hh:2 * hh + 2, :], in0=kvv[:, :, 0, :], scalar1=s2[:, 3:4], scalar2=None,
                                                           op0=ALU.mult), r=[rb, r_sm2[b]], w=[r_kn[b]])
                    sc.op("act", lambda e: e.activation(out=Vaug[:, t, 2 * hh:2 * hh + 2, 0:128], in_=kvv[:, :, 1, :], func=AF.Copy, scale=s2[:, 3:4]),
                          r=[rb, r_sm2[b]], w=[r_Vt[t]])
                yield
                jk = junk[:, 0:512].rearrange("p (h d) -> p h d", h=4)
                sc.op("dve", lambda e: e.tensor_tensor(out=jk, in0=kn_f[b][:], in1=kn_f[b][:], op=ALU.mult), r=[r_kn[b]], w=[r_junk])
                sc.op("dve", lambda e: e.tensor_reduce(out=s2[:, 8:12], in_=jk, axis=AX.X, op=ALU.add), r=[r_junk], w=[r_sm2[b]])
                sc.op("dve", lambda e: e.tensor_scalar(out=s2[:, 8:12], in0=s2[:, 8:12], scalar1=s2[:, 5:6], scalar2=None, op0=ALU.add),
                      r=[r_sm2[b]], w=[r_sm2[b]])
                rstd_from_ss(s2[:, 8:12], 192.0, s2[:, 12:16], r_sm2[b], r_sm2[b], s2[:, 16:20], r_sm2[b])
                for h in range(4):
                    sc.op("dve", lambda e: e.scalar_tensor_tensor(out=kn_b[bx][:, h, :], in0=kn_f[b][:, h, :], scalar=s2[:, 12 + h:13 + h], in1=gk[:, 0:128],
                                                                  op0=ALU.mult, op1=ALU.mult), r=[r_kn[b], r_sm2[b], r_const], w=[r_knb[bx]])
                yield
                kp = kpe[b]
                sc.op("pool", lambda e: e.tensor_tensor(out=kp[:, 0, :], in0=ckv_f[b][:, 128:192], in1=gk[:, 128:192], op=ALU.mult),
                      r=[r_ckv[b], r_const], w=[r_kpe[b]])
                cosM2 = SCm[:, t, 32:64].unsqueeze(1).broadcast_to([128, 2, 32])
                sinM2 = SCm[:, t, 0:32].unsqueeze(1).broadcast_to([128, 2, 32])
                k0 = kp[:, 0, :].rearrange("p (two d) -> p two d", two=2)
                kA = kp[:, 1, :].rearrange("p (two d) -> p two d", two=2)
                kB = kp[:, 2, :].rearrange("p (two d) -> p two d", two=2)
                sc.op("pool", lambda e: e.tensor_tensor(out=kA, in0=k0, in1=cosM2, op=ALU.mult), r=[r_kpe[b], r_sct[t]], w=[r_kpe[b]])
                sc.op("pool", lambda e: e.tensor_tensor(out=kB, in0=k0, in1=sinM2, op=ALU.mult), r=[r_kpe[b], r_sct[t]], w=[r_kpe[b]])
                sc.op("pool", lambda e: e.tensor_tensor(out=kr[b][:, 0:32], in0=kp[:, 1, 0:32], in1=kp[:, 2, 32:64], op=ALU.subtract),
                      r=[r_kpe[b]], w=[r_kr[b]])
                sc.op("pool", lambda e: e.tensor_tensor(out=kr[b][:, 32:64], in0=kp[:, 1, 32:64], in1=kp[:, 2, 0:32], op=ALU.add),
                      r=[r_kpe[b]], w=[r_kr[b]])
                for h in range(4):
                    sc.op("dve", lambda e: e.tensor_scalar(out=krn_b[b][:, h, :], in0=kr[b][:], scalar1=s2[:, 12 + h:13 + h], scalar2=None, op0=ALU.mult),
                          r=[r_kr[b], r_sm2[b]], w=[r_krn[b]])
                bk, rb = nbank(4, 8)
                pv = bk[:].bitcast(BF16).rearrange("p (c n) -> p c n", n=128)
                sc.pe([(lambda e, h=h: e.transpose(out=pv[:, h, :], in_=kn_b[bx][:, h, :], identity=ident_bf[:])) for h in range(4)]
                      + [(lambda e, pr=pr: e.transpose(out=pv[:, 4 + pr, :], in_=krn_b[b][:, 2 * pr:2 * pr + 2, :].rearrange("p a d -> p (a d)"),
                                                       identity=ident_bf[:])) for pr in range(2)],
                      r=[r_knb[bx], r_krn[b], r_const], w=[rb])
                sc.op("act", lambda e: e.copy(out=KTn[:, :, ts_], in_=pv[:, 0:4, :]), r=[rb], w=[r_KT[t]])
                sc.op("dve", lambda e: e.tensor_copy(out=KTr[:, :, ts_], in_=pv[:, 4:6, :]), r=[rb], w=[r_KT[t]])
                yield
                sc.op("act", lambda e: e.activation(out=junk[:, 0:256], in_=cq_f[b][:], func=AF.Square, accum_out=s2[:, 20:21]), r=[r_cq[b]], w=[r_junk, r_sm2[b]])
                rstd_from_ss(s2[:, 20:21], 256.0, s2[:, 21:22], r_sm2[b], r_sm2[b], s2[:, 22:23], r_sm2[b])
                sc.op("pool", lambda e: e.tensor_copy(out=cq_b[b][:], in_=cq_f[b][:]), r=[r_cq[b]], w=[r_cqb[b]])
                bk, rb = nbank(4, 8)
                pv = bk[:].bitcast(BF16).rearrange("p (c n) -> p c n", n=128)
                sc.pe([(lambda e, c=c: e.transpose(out=pv[:, c, :], in_=cq_b[b][:, c * 128:(c + 1) * 128], identity=ident_bf[:])) for c in range(2)],
                      r=[r_cqb[b], r_const], w=[rb])
                sc.op("act", lambda e: e.copy(out=cqT[b][:], in_=pv[:, 0:2, :]), r=[rb], w=[r_cqT[b]])
                yield
                for hh in range(2):
                    bk, rb = nbank(4, 8)
                    sc.pe([(lambda e, c=c: e.matmul(bk[:, 0:384], lhsT=cqT[b][:, c, :], rhs=w_uq[:, c, hh * 384:(hh + 1) * 384], start=(c == 0), stop=(c == 1)))
                           for c in range(2)], r=[r_cqT[b], r_w2], w=[rb])
                    sc.op("act", lambda e: e.activation(out=q_f[bx][:, 2 * hh:2 * hh + 2, :], in_=bk[:, 0:384].rearrange("p (h d) -> p h d", h=2),
                                                        func=AF.Copy, scale=s2[:, 21:22]), r=[rb, r_sm2[b]], w=[r_qf[bx]])
                yield
                jq = junk[:, 0:768].rearrange("p (h d) -> p h d", h=4)
                sc.op("dve", lambda e: e.tensor_tensor(out=jq, in0=q_f[bx][:], in1=q_f[bx][:], op=ALU.mult), r=[r_qf[bx]], w=[r_junk])
                sc.op("dve", lambda e: e.tensor_reduce(out=s2[:, 24:28], in_=jq, axis=AX.X, op=ALU.add), r=[r_junk], w=[r_sm2[b]])
                rstd_from_ss(s2[:, 24:28], 192.0, s2[:, 28:32], r_sm2[b], r_sm2[b], s2[:, 32:36], r_sm2[b])
                for h in range(4):
                    sc.op("dve", lambda e: e.scalar_tensor_tensor(out=qn_b[bx][:, h, :], in0=q_f[bx][:, h, 0:128], scalar=s2[:, 28 + h:29 + h], in1=gq[:, 0:128],
                                                                  op0=ALU.mult, op1=ALU.mult), r=[r_qf[bx], r_sm2[b], r_const], w=[r_qnb[bx]])
                yield
                qq = qr[b]
                gqB = gq[:, 128:192].unsqueeze(1).broadcast_to([128, 4, 64])
                sc.op("pool", lambda e: e.tensor_tensor(out=qq[:, 0, :, :], in0=q_f[bx][:, :, 128:192], in1=gqB, op=ALU.mult), r=[r_qf[bx], r_const], w=[r_qr[b]])
                cosM8 = SCm[:, t, 32:64].unsqueeze(1).broadcast_to([128, 8, 32])
                sinM8 = SCm[:, t, 0:32].unsqueeze(1).broadcast_to([128, 8, 32])
                q0 = qq[:, 0, :, :].rearrange("p h (two d) -> p (h two) d", two=2)
                qA = qq[:, 1, :, :].rearrange("p h (two d) -> p (h two) d", two=2)
                qB = qq[:, 2, :, :].rearrange("p h (two d) -> p (h two) d", two=2)
                sc.op("pool", lambda e: e.tensor_tensor(out=qA, in0=q0, in1=cosM8, op=ALU.mult), r=[r_qr[b], r_sct[t]], w=[r_qr[b]])
                sc.op("pool", lambda e: e.tensor_tensor(out=qB, in0=q0, in1=sinM8, op=ALU.mult), r=[r_qr[b], r_sct[t]], w=[r_qr[b]])
                sc.op("pool", lambda e: e.tensor_tensor(out=qq[:, 3, :, 0:32], in0=qq[:, 1, :, 0:32], in1=qq[:, 2, :, 32:64], op=ALU.subtract),
                      r=[r_qr[b]], w=[r_qr[b]])
                sc.op("pool", lambda e: e.tensor_tensor(out=qq[:, 3, :, 32:64], in0=qq[:, 1, :, 32:64], in1=qq[:, 2, :, 0:32], op=ALU.add),
                      r=[r_qr[b]], w=[r_qr[b]])
                yield
                rsB = s2[:, 28:32].unsqueeze(2).broadcast_to([128, 4, 64])
                sc.op("dve", lambda e: e.tensor_tensor(out=qrn_b[b][:], in0=qq[:, 3, :, :], in1=rsB, op=ALU.mult), r=[r_qr[b], r_sm2[b]], w=[r_qrn[b]])
                bk, rb = nbank(4, 8)
                pv = bk[:].bitcast(BF16).rearrange("p (c n) -> p c n", n=128)
                sc.pe([(lambda e, h=h: e.transpose(out=pv[:, h, :], in_=qn_b[bx][:, h, :], identity=ident_bf[:])) for h in range(4)]
                      + [(lambda e, pr=pr: e.transpose(out=pv[:, 4 + pr, :], in_=qrn_b[b][:, 2 * pr:2 * pr + 2, :].rearrange("p a d -> p (a d)"),
                                                       identity=ident_bf[:])) for pr in range(2)],
                      r=[r_qnb[bx], r_qrn[b], r_const], w=[rb])
                sc.op("act", lambda e: e.copy(out=QTn[qb][:, :, tl], in_=pv[:, 0:4, :]), r=[rb], w=[r_QT[qb]])
                sc.op("dve", lambda e: e.tensor_copy(out=QTr[qb][:, :, tl], in_=pv[:, 4:6, :]), r=[rb], w=[r_QT[qb]])

            def attention_block(i, qb):
                ab = a_b[i % 2]
                its = [(h, j) for h in range(4) for j in range(4 * i + 4)]

                def qk(h, j):
                    pair, hp = h // 2, h % 2
                    psl = slice(hp * 64, (hp + 1) * 64)
                    r0 = max(0, j - 4 * i)
                    n = 512 - r0 * 128
                    ks = slice(j * 128, (j + 1) * 128)
                    bk, rb = nbank(4, 8)
                    diag = j >= 4 * i
                    fq = [lambda e: e.matmul(bk[:, 0:n], lhsT=KTn[:, h, ks], rhs=QTn[qb][:, h, r0 * 128:512], start=True, stop=False),
                          lambda e: e.matmul(bk[:, 0:n], lhsT=KTr[psl, pair, ks], rhs=QTr[qb][psl, pair, r0 * 128:512], start=False, stop=not diag)]
                    if diag:
                        fq.append(lambda e: e.matmul(bk[:, 0:128], lhsT=ident_bf[:], rhs=negm[:], start=False, stop=True))
                    sc.pe(fq, r=[r_KT[j], r_QT[qb], r_const], w=[rb])
                    pi = (h * 64 + j) % 3
                    pt, rpt = PTt[pi], r_PTt[pi]
                    sc.op("act", lambda e: e.activation(out=pt[:, 0:n], in_=bk[:, 0:n], func=AF.Exp, scale=SCALE), r=[rb], w=[rpt])
                    return pt, rpt, r0

                def pv_(h, j, pt, rpt, r0):
                    fns = []
                    for s in range(r0, 4):
                        fns.append(lambda e, s=s: e.matmul(banks[s][:, 0:129], lhsT=pt[:, (s - r0) * 128:(s - r0 + 1) * 128], rhs=Vaug[:, j, h, :],
                                                           start=(j == 0), stop=(j == 4 * i + s)))
                    sc.pe(fns, r=[rpt, r_Vt[j], r_KT[j]], w=[bank_res[s] for s in range(r0, 4)])
                    if j >= 4 * i:
                        s = j - 4 * i
                        sc.op("dve", lambda e: e.reciprocal(out=rcp[i % 2][:, s:s + 1], in_=banks[s][:, 128:129]), r=[bank_res[s]], w=[r_rcp[i % 2]])
                        sc.op("dve", lambda e: e.tensor_scalar(out=ab[:, s, h * 128:(h + 1) * 128], in0=banks[s][:, 0:128], scalar1=rcp[i % 2][:, s:s + 1],
                                                               scalar2=None, op0=ALU.mult), r=[bank_res[s], r_rcp[i % 2]], w=[r_ab[i % 2]])

                pend = []
                ystep = max(1, len(its) // 30)
                for k_, (h, j) in enumerate(its):
                    pend.append((h, j) + qk(h, j))
                    if len(pend) > 1:
                        pv_(*pend.pop(0))
                    if k_ % ystep == ystep - 1:
                        yield
                while pend:
                    pv_(*pend.pop(0))

            def out_tile(t):
                i, s = t // 4, t % 4
                ab = a_b[i % 2]
                if True:
                    b = t % 2
                    ts_ = slice(t * 128, (t + 1) * 128)
                    sc.dma("sp", lambda e: e.dma_start(out=r_b[b][:], in_=rout_d[ts_, :]), w=[r_rb[b]])
                    sc.dma("sp", lambda e: e.dma_start(out=x1t[1][:], in_=x_d[ts_, :]), w=[r_x1t[1]])
                    yield
                    bk, rb = nbank(4, 8)
                    pv = bk[:].bitcast(BF16).rearrange("p (c n) -> p c n", n=128)
                    sc.pe([(lambda e, c=c: e.transpose(out=pv[:, c, :], in_=ab[:, s, c * 128:(c + 1) * 128], identity=ident_bf[:])) for c in range(4)]
                          + [(lambda e, c=c: e.transpose(out=pv[:, 4 + c, :], in_=r_b[b][:, c * 128:(c + 1) * 128], identity=ident_bf[:])) for c in range(4)],
                          r=[r_ab[i % 2], r_rb[b], r_const], w=[rb])
                    sc.op("dve", lambda e: e.tensor_copy(out=aoT[b][:], in_=pv), r=[rb], w=[r_aoT[b]])
                    yield
                    for hh in range(2):
                        bk, rb = nbank(4, 8)
                        sc.pe([(lambda e, c=c: e.matmul(bk[:], lhsT=aoT[b][:, c, :], rhs=w_out[:, c, hh * 512:(hh + 1) * 512], start=(c == 0), stop=(c == 7)))
                               for c in range(8)], r=[r_aoT[b], r_w2], w=[rb])
                        sc.op("dve", lambda e: e.tensor_tensor(out=x1t[1][:, hh * 512:(hh + 1) * 512], in0=bk[:], in1=x1t[1][:, hh * 512:(hh + 1) * 512], op=ALU.add),
                              r=[rb], w=[r_x1t[1]])
                    r_x1d = sc.res("x1d")
                    sc.dma("act", lambda e: e.dma_start(out=x1_d[ts_, :], in_=x1t[1][:]), r=[r_x1t[1]], w=[r_x1d])

            def drive(gens):
                gens = list(gens)
                while gens:
                    for g in list(gens):
                        try:
                            next(g)
                        except StopIteration:
                            gens.remove(g)

            x1t = xt
            r_x1t = r_xt
            run_pipelined(lambda t: prep_tile(t, 0), 4, 3)
            for i in range(NB):
                qb = i % 2

                def side(i=i):
                    if i + 1 < NB:
                        act_ = []
                        nx = 4 * (i + 1)
                        while nx < 4 * (i + 2) or act_:
                            if nx < 4 * (i + 2) and len(act_) < 3:
                                act_.append(prep_tile(nx, (i + 1) % 2))
                                nx += 1
                            for g in list(act_):
                                try:
                                    next(g)
                                except StopIteration:
                                    act_.remove(g)
                            yield

                def outs(i=i):
                    if i == 0:
                        return
                    act_ = []
                    nx = 4 * (i - 1)
                    while nx < 4 * i or act_:
                        if nx < 4 * i and len(act_) < 1:
                            act_.append(out_tile(nx))
                            nx += 1
                        for g in list(act_):
                            try:
                                next(g)
                            except StopIteration:
                                act_.remove(g)
                        yield

                drive([attention_block(i, qb), outs(), side()])
            run_pipelined(lambda k_: out_tile(4 * (NB - 1) + k_), 4, 1)
            if "x1" in dbg:
                sc.barrier()
                t_ = nc.dram_tensor("dbg_x1", [S, D], F32, kind="ExternalOutput").ap()
                dbg_out["x1"] = t_
                sc.dma("sp", lambda e: e.dma_start(out=t_, in_=x1_d), is_out=True)
            dump("KTn", KTn[:], r_KT[NT - 1], [128, 4, S], BF16)
            dump("KTr", KTr[:], r_KT[NT - 1], [128, 2, S], BF16)
            dump("Vaug", Vaug[:], r_Vt[NT - 1], [128, NT, 4, 129], BF16)
            sc.barrier()
            p2s.close()

        if last_phase >= 3:
            p36 = es.enter_context(ExitStack())
            lg_all = sb("lg_all", [128, NT, 36], F32, p36)
            r_lg = sc.res("lg_all")
            pos12 = sb("pos12", [128, 2, NT], I32, p36)
            w12 = sb("w12", [128, 2, NT], F32, p36)
            widx = sb("widx", [128, NTS], I32, p36)
            r_route = sc.res("route")
            b_rt = bload("b_rt", b_rt_d, 36, p36)

            p3s = es.enter_context(ExitStack())
            cw_q = sb("cw_q", [128, 8, 1024], BF16, p3s)
            cw_o = sb("cw_o", [128, 8, 1024], BF16, p3s)
            KcT = sb("KcT", [128, 4, 2, 256], BF16, p3s)
            Vc = sb("Vc", [128, 2, 4, 257], BF16, p3s)
            mg = bload("mg", mg_d, 1024, p3s)
            cqg = bload("cqg", cqg_d, 256, p3s)
            ckg = bload("ckg", ckg_d, 256, p3s)
            stage[:] = [sb("stage3_%d" % i, [128, 1024], F32, p3s) for i in range(2)]
            g_cross = pload("g_cross", g_cross_d, 8, p3s)
            g_mem = pload("g_mem", g_mem_d, 8, p3s)
            w_rt = sb("w_rt", [128, 8, 36], F32, p3s)
            r_w3 = sc.res("w3")
            sc.dma("sp", lambda e: e.dma_start(out=w_rt[:], in_=w_rt_d[:, :, :]), w=[r_w3])
            load_scaled(lambda c, a, b_: cw_q[:, c, a:b_], lambda c, a, b_: cw_q_d[:, c, a:b_], lambda c: g_cross[:, c:c + 1], 8, 1024, r_w3)
            load_cast(lambda c: cw_o[:, c, :], lambda c: cw_o_d[:, c, :], 8, r_w3)
            r_kvc = sc.res("kvc")
            sc.op("pool", lambda e: e.memset(Vc[:], 1.0), w=[r_kvc])

            pm = es.enter_context(ExitStack())
            cw_kv = sb("cw_kv", [128, 8, 2048], BF16, pm)
            r_cwkv = sc.res("cw_kv")
            load_scaled(lambda c, a, b_: cw_kv[:, c, a:b_], lambda c, a, b_: cw_kv_d[:, c, a:b_], lambda c: g_mem[:, c:c + 1], 8, 2048, r_cwkv)
            m_f = sb("m_f", [128, 1024], F32, pm)
            m_b = sb("m_b", [128, 1024], BF16, pm)
            m_T = sb("m_T", [128, 8, 128], BF16, pm)
            kc_f = sb("kc_f", [128, 4, 256], F32, pm)
            kc_sq = sb("kc_sq", [128, 4, 256], F32, pm)
            kc_b = sb("kc_b", [128, 4, 256], BF16, pm)
            sm = sb("sm0", [128, 16], F32, pm)
            r_m = sc.res("m")
            r_sm = sc.res("sm0")
            for mt in range(2):
                sc.dma("sp", lambda e: e.dma_start(out=m_f[:], in_=mem_d[mt * 128:(mt + 1) * 128, :]), w=[r_m])
                sc.op("act", lambda e: e.activation(out=m_b[:], in_=m_f[:], func=AF.Square, accum_out=sm[:, 0:1]), r=[r_m], w=[r_m, r_sm])
                rstd_from_ss(sm[:, 0:1], 1024.0, sm[:, 1:2], r_sm, r_sm, sm[:, 2:3], r_sm)
                sc.op("pool", lambda e: e.tensor_copy(out=m_b[:], in_=m_f[:]), r=[r_m], w=[r_m])
                bk, rb = nbank()
                pv = bk[:].bitcast(BF16).rearrange("p (c n) -> p c n", n=128)
                sc.pe([(lambda e, c=c: e.transpose(out=pv[:, c, :], in_=m_b[:, c * 128:(c + 1) * 128], identity=ident_bf[:])) for c in range(8)],
                      r=[r_m, r_const], w=[rb])
                sc.op("dve", lambda e: e.tensor_copy(out=m_T[:], in_=pv), r=[rb], w=[r_m])
                for nchunk in range(4):
                    bk, rb = nbank()
                    sc.pe([(lambda e, c=c: e.matmul(bk[:], lhsT=m_T[:, c, :], rhs=cw_kv[:, c, nchunk * 512:(nchunk + 1) * 512],
                                                    start=(c == 0), stop=(c == 7))) for c in range(8)], r=[r_m, r_cwkv], w=[rb])
                    if nchunk < 2:
                        sc.op("act", lambda e: e.activation(out=kc_f[:, 2 * nchunk:2 * nchunk + 2, :], in_=bk[:].rearrange("p (h d) -> p h d", h=2),
                                                            func=AF.Copy, scale=sm[:, 1:2]), r=[rb, r_sm], w=[r_m])
                    else:
                        hh = 2 * (nchunk - 2)
                        sc.op("act", lambda e: e.activation(out=Vc[:, mt, hh:hh + 2, 0:256], in_=bk[:].rearrange("p (h d) -> p h d", h=2),
                                                            func=AF.Copy, scale=sm[:, 1:2]), r=[rb, r_sm], w=[r_kvc])
                sc.op("dve", lambda e: e.tensor_tensor(out=kc_sq[:], in0=kc_f[:], in1=kc_f[:], op=ALU.mult), r=[r_m], w=[r_m])
                sc.op("dve", lambda e: e.tensor_reduce(out=sm[:, 4:8], in_=kc_sq[:], axis=AX.X, op=ALU.add), r=[r_m], w=[r_sm])
                rstd_from_ss(sm[:, 4:8], 256.0, sm[:, 8:12], r_sm, r_sm, sm[:, 12:16], r_sm)
                for h in range(4):
                    sc.op("dve", lambda e: e.scalar_tensor_tensor(out=kc_b[:, h, :], in0=kc_f[:, h, :], scalar=sm[:, 8 + h:9 + h], in1=ckg[:],
                                                                  op0=ALU.mult, op1=ALU.mult), r=[r_m, r_sm, r_const], w=[r_m])
                bk, rb = nbank()
                pv = bk[:].bitcast(BF16).rearrange("p (h c n) -> p h c n", h=4, c=2)
                sc.pe([(lambda e, h=h, c=c: e.transpose(out=pv[:, h, c, :], in_=kc_b[:, h, c * 128:(c + 1) * 128], identity=ident_bf[:]))
                       for h in range(4) for c in range(2)], r=[r_m, r_const], w=[rb])
                sc.op("dve", lambda e: e.tensor_copy(out=KcT[:, :, :, mt * 128:(mt + 1) * 128], in_=pv), r=[rb], w=[r_kvc])
            dump("KcT", KcT[:], r_kvc, [128, 4, 2, 256], BF16)
            dump("Vc", Vc[:], r_kvc, [128, 2, 4, 257], BF16)
            sc.barrier()
            pm.close()

            x1t = [sb("x3t%d" % i, [128, 1024], F32, p3s) for i in range(5)]
            xb = [sb("x3b%d" % i, [128, 1024], BF16, p3s) for i in range(5)]
            hcT = [sb("hcT%d" % i, [128, 8, 128], BF16, p3s) for i in range(5)]
            junk = sb("junk3", [128, 1024], F32, p3s)
            qc_f = sb("qc_f", [128, 4, 256], F32, p3s)
            qc_b = sb("qc_b", [128, 4, 256], BF16, p3s)
            qcT = [sb("qcT%d" % i, [128, 4, 2, 128], BF16, p3s) for i in range(5)]
            PTc = [sb("PTc%d" % i, [128, 8, 128], BF16, p3s) for i in range(5)]
            oc_b = sb("oc_b", [128, 4, 256], BF16, p3s)
            ocT = [sb("ocT%d" % i, [128, 8, 128], BF16, p3s) for i in range(5)]
            hm_f = [sb("hm_f%d" % i, [128, 1024], F32, p3s) for i in range(5)]
            hm_b = [sb("hm_b%d" % i, [128, 1024], BF16, p3s) for i in range(5)]
            hmT_f = sb("hmT_f", [128, 8, 128], F32, p3s)
            sm3 = [sb("sm3_%d" % i, [128, 32], F32, p3s) for i in range(5)]
            r_x1t, r_xb, r_hcT = mkres("x3t", 5), mkres("x3b", 5), mkres("hcT", 5)
            r_junk = sc.res("junk3")
            r_qcf, r_qcb = sc.res("qcf"), sc.res("qcb")
            r_qcT, r_PTc, r_ocT, r_hmf, r_hmb, r_sm3 = mkres("qcT", 5), mkres("PTc", 5), mkres("ocT", 5), mkres("hmf", 5), mkres("hmb", 5), mkres("sm3", 5)
            r_ocb = sc.res("ocb")
            r_hmT = sc.res("hmT")
            r_x2d = [sc.res("x2d%d" % t) for t in range(NT)]
            r_hmd = [sc.res("hmd%d" % t) for t in range(NT)]
            CSCALE = 256.0 ** -0.5

            def p3_tile(t):
                b = t % 5
                b3 = t % 5
                ts_ = slice(t * 128, (t + 1) * 128)
                s3 = sm3[b3]
                sc.dma("sp", lambda e: e.dma_start(out=x1t[b3][:], in_=x1_d[ts_, :]), w=[r_x1t[b3]])
                sc.op("act", lambda e: e.activation(out=junk[:], in_=x1t[b3][:], func=AF.Square, accum_out=s3[:, 0:1]), r=[r_x1t[b3]], w=[r_junk, r_sm3[b3]])
                rstd_from_ss(s3[:, 0:1], 1024.0, s3[:, 1:2], r_sm3[b3], r_sm3[b3], s3[:, 2:3], r_sm3[b3])
                sc.op("act", lambda e: e.copy(out=xb[b][:], in_=x1t[b3][:]), r=[r_x1t[b3]], w=[r_xb[b]])
                yield
                bk, rb = nbank()
                pv = bk[:].bitcast(BF16).rearrange("p (c n) -> p c n", n=128)
                sc.pe([(lambda e, c=c: e.transpose(out=pv[:, c, :], in_=xb[b][:, c * 128:(c + 1) * 128], identity=ident_bf[:])) for c in range(8)],
                      r=[r_xb[b], r_const], w=[rb])
                sc.op("dve", lambda e: e.tensor_copy(out=hcT[b][:], in_=pv), r=[rb], w=[r_hcT[b]])
                yield
                for hh in range(2):
                    bk, rb = nbank()
                    sc.pe([(lambda e, c=c: e.matmul(bk[:], lhsT=hcT[b][:, c, :], rhs=cw_q[:, c, hh * 512:(hh + 1) * 512], start=(c == 0), stop=(c == 7)))
                           for c in range(8)], r=[r_hcT[b], r_w3], w=[rb])
                    sc.op("act", lambda e: e.activation(out=qc_f[:, 2 * hh:2 * hh + 2, :], in_=bk[:].rearrange("p (h d) -> p h d", h=2), func=AF.Copy,
                                                        scale=s3[:, 1:2]), r=[rb, r_sm3[b3]], w=[r_qcf])
                yield
                jq = junk[:].rearrange("p (h d) -> p h d", h=4)
                sc.op("dve", lambda e: e.tensor_tensor(out=jq, in0=qc_f[:], in1=qc_f[:], op=ALU.mult), r=[r_qcf], w=[r_junk])
                sc.op("dve", lambda e: e.tensor_reduce(out=s3[:, 4:8], in_=jq, axis=AX.X, op=ALU.add), r=[r_junk], w=[r_sm3[b3]])
                rstd_from_ss(s3[:, 4:8], 256.0, s3[:, 8:12], r_sm3[b3], r_sm3[b3], s3[:, 12:16], r_sm3[b3])
                for h in range(4):
                    sc.op("dve", lambda e: e.scalar_tensor_tensor(out=qc_b[:, h, :], in0=qc_f[:, h, :], scalar=s3[:, 8 + h:9 + h], in1=cqg[:],
                                                                  op0=ALU.mult, op1=ALU.mult), r=[r_qcf, r_sm3[b3], r_const], w=[r_qcb])
                yield
                bk, rb = nbank()
                pv = bk[:].bitcast(BF16).rearrange("p (h c n) -> p h c n", h=4, c=2)
                sc.pe([(lambda e, h=h, c=c: e.transpose(out=pv[:, h, c, :], in_=qc_b[:, h, c * 128:(c + 1) * 128], identity=ident_bf[:]))
                       for h in range(4) for c in range(2)], r=[r_qcb, r_const], w=[rb])
                sc.op("act", lambda e: e.copy(out=qcT[b][:], in_=pv), r=[rb], w=[r_qcT[b]])
                yield
                for hp in range(2):
                    bk, rb = nbank()
                    sv_ = bk[:].rearrange("p (a n) -> p a n", a=4)
                    fns = []
                    for hl in range(2):
                        h = 2 * hp + hl
                        for mc in range(2):
                            for dc in range(2):
                                fns.append(lambda e, h=h, mc=mc, dc=dc, hl=hl: e.matmul(sv_[:, hl * 2 + mc, :], lhsT=KcT[:, h, dc, mc * 128:(mc + 1) * 128],
                                                                                        rhs=qcT[b][:, h, dc, :], start=(dc == 0), stop=(dc == 1)))
                    sc.pe(fns, r=[r_kvc, r_qcT[b]], w=[rb])
                    sc.op("act", lambda e: e.activation(out=PTc[b][:, 4 * hp:4 * hp + 4, :], in_=sv_, func=AF.Exp, scale=CSCALE), r=[rb], w=[r_PTc[b]])
                yield
                for h in range(4):
                    bk, rb = nbank()
                    sc.pe([(lambda e, mc=mc: e.matmul(bk[:, 0:257], lhsT=PTc[b][:, 2 * h + mc, :], rhs=Vc[:, mc, h, :], start=(mc == 0), stop=(mc == 1)))
                           for mc in range(2)], r=[r_PTc[b], r_kvc], w=[rb])
                    sc.op("dve", lambda e: e.reciprocal(out=s3[:, 16 + h:17 + h], in_=bk[:, 256:257]), r=[rb], w=[r_sm3[b3]])
                    sc.op("dve", lambda e: e.tensor_scalar(out=oc_b[:, h, :], in0=bk[:, 0:256], scalar1=s3[:, 16 + h:17 + h], scalar2=None, op0=ALU.mult),
                          r=[rb, r_sm3[b3]], w=[r_ocb])
                yield
                bk, rb = nbank()
                pv = bk[:].bitcast(BF16).rearrange("p (c n) -> p c n", n=128)
                ocf = oc_b[:].rearrange("p h d -> p (h d)")
                sc.pe([(lambda e, c=c: e.transpose(out=pv[:, c, :], in_=ocf[:, c * 128:(c + 1) * 128], identity=ident_bf[:])) for c in range(8)],
                      r=[r_ocb, r_const], w=[rb])
                sc.op("act", lambda e: e.copy(out=ocT[b][:], in_=pv), r=[rb], w=[r_ocT[b]])
                yield
                for hh in range(2):
                    bk, rb = nbank()
                    sc.pe([(lambda e, c=c: e.matmul(bk[:], lhsT=ocT[b][:, c, :], rhs=cw_o[:, c, hh * 512:(hh + 1) * 512], start=(c == 0), stop=(c == 7)))
                           for c in range(8)], r=[r_ocT[b], r_w3], w=[rb])
                    sc.op("dve", lambda e: e.tensor_tensor(out=x1t[b3][:, hh * 512:(hh + 1) * 512], in0=bk[:], in1=x1t[b3][:, hh * 512:(hh + 1) * 512], op=ALU.add),
                          r=[rb], w=[r_x1t[b3]])
                sc.dma("act", lambda e: e.dma_start(out=out_d[ts_, :], in_=x1t[b3][:]), r=[r_x1t[b3]], w=[r_x2d[t]])
                yield
                sc.op("act", lambda e: e.activation(out=junk[:], in_=x1t[b3][:], func=AF.Square, accum_out=s3[:, 20:21]), r=[r_x1t[b3]], w=[r_junk, r_sm3[b3]])
                rstd_from_ss(s3[:, 20:21], 1024.0, s3[:, 21:22], r_sm3[b3], r_sm3[b3], s3[:, 22:23], r_sm3[b3])
                sc.op("dve", lambda e: e.scalar_tensor_tensor(out=hm_f[b][:], in0=x1t[b3][:], scalar=s3[:, 21:22], in1=mg[:], op0=ALU.mult, op1=ALU.mult),
                      r=[r_x1t[b3], r_sm3[b3], r_const], w=[r_hmf[b]])
                sc.op("pool", lambda e: e.tensor_copy(out=hm_b[b][:], in_=hm_f[b][:]), r=[r_hmf[b]], w=[r_hmb[b]])
                sc.dma("pool", lambda e: e.dma_start(out=hm_d[ts_, :], in_=hm_b[b][:]), r=[r_hmb[b]], w=[r_hmd[t]])
                yield
                bkA, rbA = nbank()
                bkB, rbB = nbank()
                sc.pe([(lambda e, c=c: e.transpose(out=(bkA if c < 4 else bkB)[:, (c % 4) * 128:(c % 4 + 1) * 128], in_=hm_f[b][:, c * 128:(c + 1) * 128],
                                                   identity=ident_f[:])) for c in range(8)], r=[r_hmf[b], r_const], w=[rbA, rbB])
                sc.op("dve", lambda e: e.tensor_copy(out=hmT_f[:, 0:4, :], in_=bkA[:].rearrange("p (c n) -> p c n", c=4)), r=[rbA], w=[r_hmT])
                sc.op("act", lambda e: e.copy(out=hmT_f[:, 4:8, :], in_=bkB[:].rearrange("p (c n) -> p c n", c=4)), r=[rbB], w=[r_hmT])
                yield
                bk, rb = nbank()
                sc.pe([(lambda e, c=c: e.matmul(bk[:, 0:36], lhsT=hmT_f[:, c, :], rhs=w_rt[:, c, :], start=(c == 0), stop=(c == 7))) for c in range(8)],
                      r=[r_hmT, r_w3], w=[rb])
                sc.op("act", lambda e: e.copy(out=lg_all[:, t, :], in_=bk[:, 0:36]), r=[rb], w=[r_lg])
            run_pipelined(p3_tile, NT, 5)
            dump("logits", lg_all[:], r_lg, [128, NT, 36])
            if "x2" in dbg:
                sc.barrier()
                t_ = nc.dram_tensor("dbg_x2", [S, D], F32, kind="ExternalOutput").ap()
                dbg_out["x2"] = t_
                sc.dma("sp", lambda e: e.dma_start(out=t_, in_=out_d), is_out=True)
                t2_ = nc.dram_tensor("dbg_hm", [S, D], BF16, kind="ExternalOutput").ap()
                dbg_out["hm"] = t2_
                sc.dma("sp", lambda e: e.dma_start(out=t2_, in_=hm_d), is_out=True)
            sc.barrier()
            p3s.close()

        if last_phase >= 4:
            p4s = es.enter_context(ExitStack())
            reg_npos = nc.gpsimd.to_reg(NPOS - 1)
            reg_ew = nc.gpsimd.to_reg(32 * 128 - 1)
            r4 = sc.res("r4")

            def T4(name, shape, dt=F32):
                return sb("r4_" + name, shape, dt, p4s)

            def V(fn, r=(), w=(), eng="dve"):
                sc.op(eng, fn, r=[r4, r_lg, r_const] + list(r), w=[r4] + list(w))

            L = lg_all
            GL = L[:, :, 0:4]
            EL = L[:, :, 4:36].rearrange("p t (g j) -> p t g j", g=4)
            gb, goh, ge = T4("gb", [128, NT, 4]), T4("goh", [128, NT, 4]), T4("ge", [128, NT, 4])
            gmax, gm, gsum, gnum, gw = (T4(n, [128, NT]) for n in ("gmax", "gm", "gsum", "gnum", "gw"))
            t48 = T4("t48", [128, NT, 4, 8])
            esel, bsel, eb, oh1, eb2, oh2, ex, t8 = (T4(n, [128, NT, 8]) for n in ("esel", "bsel", "eb", "oh1", "eb2", "oh2", "ex", "t8"))
            m1, m2, em, a1, a2, den, ff = (T4(n, [128, NT]) for n in ("m1", "m2", "em", "a1", "a2", "den", "ff"))
            OH1, OH2 = T4("OH1", [128, NT, 32]), T4("OH2", [128, NT, 32])
            C_bf = T4("C_bf", [128, NT, 32], BF16)
            TT, PP, cA, cB, base, tmp32 = (T4(n, [128, NT, 32]) for n in ("TT", "PP", "cA", "cB", "base", "tmp32"))
            npad_i = T4("npad_i", [128, 32], I32)
            npad, eA, eB, off = (T4(n, [128, 32]) for n in ("npad", "eA", "eB", "off"))
            posf = T4("posf", [128, 2, NT])
            tpos_i = T4("tpos_i", [128, NTS], I32)
            tpos_f, eid_f = T4("tpos_f", [128, NTS]), T4("eid_f", [128, NTS])
            cmp_ = T4("cmp", [128, NTS, 32])
            pidx_i = T4("pidx_i", [128, 1], I32)
            pidx_f = T4("pidx_f", [128, 1])

            def bc(ap2, n):
                return ap2.unsqueeze(2).broadcast_to([128, NT, n])

            bg = b_rt[:, 0:4].unsqueeze(1).broadcast_to([128, NT, 4])
            be = b_rt[:, 4:36].rearrange("p (g j) -> p g j", g=4).unsqueeze(1).broadcast_to([128, NT, 4, 8])
            V(lambda e: e.tensor_tensor(out=gb[:], in0=GL, in1=bg, op=ALU.add))
            V(lambda e: e.tensor_reduce(out=gmax[:], in_=gb[:], axis=AX.X, op=ALU.max))
            V(lambda e: e.tensor_tensor(out=goh[:], in0=gb[:], in1=bc(gmax[:], 4), op=ALU.is_equal))
            V(lambda e: e.tensor_reduce(out=gm[:], in_=GL, axis=AX.X, op=ALU.max))
            V(lambda e: e.tensor_tensor(out=ge[:], in0=GL, in1=bc(gm[:], 4), op=ALU.subtract))
            V(lambda e: e.activation(out=ge[:].rearrange("p t g -> p (t g)"), in_=ge[:].rearrange("p t g -> p (t g)"), func=AF.Exp), eng="act")
            V(lambda e: e.tensor_reduce(out=gsum[:], in_=ge[:], axis=AX.X, op=ALU.add))
            V(lambda e: e.tensor_tensor(out=gb[:], in0=goh[:], in1=ge[:], op=ALU.mult))
            V(lambda e: e.tensor_reduce(out=gnum[:], in_=gb[:], axis=AX.X, op=ALU.add))
            V(lambda e: e.reciprocal(out=gsum[:], in_=gsum[:]))
            V(lambda e: e.tensor_tensor(out=gw[:], in0=gnum[:], in1=gsum[:], op=ALU.mult))
            goh_b = goh[:].unsqueeze(3).broadcast_to([128, NT, 4, 8])
            V(lambda e: e.tensor_tensor(out=t48[:], in0=EL, in1=goh_b, op=ALU.mult))
            V(lambda e: e.tensor_reduce(out=esel[:], in_=t48[:].rearrange("p t g j -> p t j g"), axis=AX.X, op=ALU.add))
            V(lambda e: e.tensor_tensor(out=t48[:], in0=be, in1=goh_b, op=ALU.mult))
            V(lambda e: e.tensor_reduce(out=bsel[:], in_=t48[:].rearrange("p t g j -> p t j g"), axis=AX.X, op=ALU.add))
            V(lambda e: e.tensor_tensor(out=eb[:], in0=esel[:], in1=bsel[:], op=ALU.add))
            V(lambda e: e.tensor_reduce(out=m1[:], in_=eb[:], axis=AX.X, op=ALU.max))
            V(lambda e: e.tensor_tensor(out=oh1[:], in0=eb[:], in1=bc(m1[:], 8), op=ALU.is_equal))
            V(lambda e: e.scalar_tensor_tensor(out=eb2[:].rearrange("p t j -> p (t j)"), in0=oh1[:].rearrange("p t j -> p (t j)"), scalar=-1e30,
                                               in1=eb[:].rearrange("p t j -> p (t j)"), op0=ALU.mult, op1=ALU.add))
            V(lambda e: e.tensor_reduce(out=m2[:], in_=eb2[:], axis=AX.X, op=ALU.max))
            V(lambda e: e.tensor_tensor(out=oh2[:], in0=eb2[:], in1=bc(m2[:], 8), op=ALU.is_equal))
            V(lambda e: e.tensor_reduce(out=em[:], in_=esel[:], axis=AX.X, op=ALU.max))
            V(lambda e: e.tensor_tensor(out=ex[:], in0=esel[:], in1=bc(em[:], 8), op=ALU.subtract))
            V(lambda e: e.activation(out=ex[:].rearrange("p t j -> p (t j)"), in_=ex[:].rearrange("p t j -> p (t j)"), func=AF.Exp), eng="act")
            V(lambda e: e.tensor_tensor(out=t8[:], in0=oh1[:], in1=ex[:], op=ALU.mult))
            V(lambda e: e.tensor_reduce(out=a1[:], in_=t8[:], axis=AX.X, op=ALU.add))
            V(lambda e: e.tensor_tensor(out=t8[:], in0=oh2[:], in1=ex[:], op=ALU.mult))
            V(lambda e: e.tensor_reduce(out=a2[:], in_=t8[:], axis=AX.X, op=ALU.add))
            V(lambda e: e.tensor_tensor(out=den[:], in0=a1[:], in1=a2[:], op=ALU.add))
            V(lambda e: e.reciprocal(out=den[:], in_=den[:]))
            V(lambda e: e.tensor_tensor(out=ff[:], in0=den[:], in1=gw[:], op=ALU.mult))
            V(lambda e: e.tensor_tensor(out=w12[:, 0, :], in0=a1[:], in1=ff[:], op=ALU.mult), w=[r_route])
            V(lambda e: e.tensor_tensor(out=w12[:, 1, :], in0=a2[:], in1=ff[:], op=ALU.mult), w=[r_route])
            V(lambda e: e.tensor_tensor(out=OH1[:].rearrange("p t (g j) -> p t g j", g=4), in0=goh_b,
                                        in1=oh1[:].unsqueeze(2).broadcast_to([128, NT, 4, 8]), op=ALU.mult))
            V(lambda e: e.tensor_tensor(out=OH2[:].rearrange("p t (g j) -> p t g j", g=4), in0=goh_b,
                                        in1=oh2[:].unsqueeze(2).broadcast_to([128, NT, 4, 8]), op=ALU.mult))
            V(lambda e: e.tensor_tensor(out=C_bf[:], in0=OH1[:], in1=OH2[:], op=ALU.add))
            W = NT * 32
            Cf = C_bf[:].rearrange("p t e -> p (t e)")
            TTf = TT[:].rearrange("p t e -> p (t e)")
            PPf = PP[:].rearrange("p t e -> p (t e)")
            for c0 in range(0, W, 512):
                c1 = min(W, c0 + 512)
                bk, rb = nbank()
                sc.pe([lambda e: e.matmul(bk[:, 0:c1 - c0], lhsT=ones_bf[:], rhs=Cf[:, c0:c1], start=True, stop=True)], r=[r4, r_const], w=[rb])
                sc.op("act", lambda e: e.copy(out=TTf[:, c0:c1], in_=bk[:, 0:c1 - c0]), r=[rb], w=[r4])
                bk, rb = nbank()
                sc.pe([lambda e: e.matmul(bk[:, 0:c1 - c0], lhsT=tri[:], rhs=Cf[:, c0:c1], start=True, stop=True)], r=[r4, r_const], w=[rb])
                sc.op("act", lambda e: e.copy(out=PPf[:, c0:c1], in_=bk[:, 0:c1 - c0]), r=[rb], w=[r4])
            V(lambda e: e.tensor_copy(out=cA[:], in_=TT[:]))
            cur, nxt = cA, cB
            s_ = 1
            while s_ < NT:
                V(lambda e: e.tensor_tensor(out=nxt[:, s_:NT, :], in0=cur[:, s_:NT, :], in1=cur[:, 0:NT - s_, :], op=ALU.add))
                V(lambda e: e.tensor_copy(out=nxt[:, 0:s_, :], in_=cur[:, 0:s_, :]))
                cur, nxt = nxt, cur
                s_ *= 2
            incl = cur
            V(lambda e: e.tensor_scalar(out=npad[:], in0=incl[:, NT - 1, :], scalar1=float(TS - 1), scalar2=None, op0=ALU.add))
            V(lambda e: e.tensor_copy(out=npad_i[:], in_=npad[:]))
            V(lambda e: e.tensor_scalar(out=npad_i[:], in0=npad_i[:], scalar1=8, scalar2=8, op0=ALU.arith_shift_right, op1=ALU.logical_shift_left))
            V(lambda e: e.tensor_copy(out=npad[:], in_=npad_i[:]))
            V(lambda e: e.tensor_copy(out=eA[:], in_=npad[:]))
            cur2, nxt2 = eA, eB
            s_ = 1
            while s_ < 32:
                V(lambda e: e.tensor_tensor(out=nxt2[:, s_:32], in0=cur2[:, s_:32], in1=cur2[:, 0:32 - s_], op=ALU.add))
                V(lambda e: e.tensor_copy(out=nxt2[:, 0:s_], in_=cur2[:, 0:s_]))
                cur2, nxt2 = nxt2, cur2
                s_ *= 2
            endI = cur2
            V(lambda e: e.tensor_tensor(out=off[:], in0=endI[:], in1=npad[:], op=ALU.subtract))
            V(lambda e: e.tensor_tensor(out=base[:], in0=incl[:], in1=TT[:], op=ALU.subtract))
            V(lambda e: e.tensor_tensor(out=base[:], in0=base[:], in1=PP[:], op=ALU.add))
            V(lambda e: e.tensor_tensor(out=base[:], in0=base[:], in1=off[:].unsqueeze(1).broadcast_to([128, NT, 32]), op=ALU.add))
            V(lambda e: e.tensor_tensor(out=tmp32[:], in0=OH1[:], in1=base[:], op=ALU.mult))
            V(lambda e: e.tensor_reduce(out=posf[:, 0, :], in_=tmp32[:], axis=AX.X, op=ALU.add))
            V(lambda e: e.tensor_tensor(out=tmp32[:], in0=OH2[:], in1=base[:], op=ALU.mult))
            V(lambda e: e.tensor_reduce(out=posf[:, 1, :], in_=tmp32[:], axis=AX.X, op=ALU.add))
            V(lambda e: e.tensor_copy(out=pos12[:], in_=posf[:]), w=[r_route])
            V(lambda e: e.iota(tpos_i[:], pattern=[[TS, NTS]], base=0, channel_multiplier=0), eng="pool")
            V(lambda e: e.iota(pidx_i[:], pattern=[[0, 1]], base=0, channel_multiplier=1), eng="pool")
            V(lambda e: e.tensor_copy(out=tpos_f[:], in_=tpos_i[:]))
            V(lambda e: e.tensor_copy(out=pidx_f[:], in_=pidx_i[:]))
            V(lambda e: e.tensor_tensor(out=cmp_[:], in0=endI[:].unsqueeze(1).broadcast_to([128, NTS, 32]),
                                        in1=tpos_f[:].unsqueeze(2).broadcast_to([128, NTS, 32]), op=ALU.is_le))
            V(lambda e: e.tensor_reduce(out=eid_f[:], in_=cmp_[:], axis=AX.X, op=ALU.add))
            V(lambda e: e.tensor_scalar(out=eid_f[:], in0=eid_f[:], scalar1=31.0, scalar2=128.0, op0=ALU.min, op1=ALU.mult))
            V(lambda e: e.tensor_scalar(out=eid_f[:], in0=eid_f[:], scalar1=pidx_f[:, 0:1], scalar2=None, op0=ALU.add))
            V(lambda e: e.tensor_copy(out=widx[:], in_=eid_f[:]), w=[r_route])
            dump("pos12", pos12[:], r_route, [128, 2, NT], I32)
            dump("w12", w12[:], r_route, [128, 2, NT])
            dump("widx", widx[:], r_route, [128, NTS], I32)

            hsb = [sb("hsb%d" % i, [128, 1024], BF16, p4s) for i in range(2)]
            r_hsb = mkres("hsb")
            for t in range(NT):
                b = t % 2
                ts_ = slice(t * 128, (t + 1) * 128)
                sc.dma("sp", lambda e: e.dma_start(out=hsb[b][:], in_=hm_d[ts_, :]), r=[r_hmd[t]], w=[r_hsb[b]])
                for k in range(2):
                    sc.dma("pool", lambda e: e.indirect_dma_start(out=xs_d[:, :], out_offset=bass.IndirectOffsetOnAxis(ap=pos12[:, k, t:t + 1], axis=0),
                                                                  in_=hsb[b][:], in_offset=None, bounds_check=reg_npos, oob_is_err=False),
                           r=[r_hsb[b], r_route], w=[])
            sc.barrier()
            p4s.close()

        if last_phase >= 5:
            p5s = es.enter_context(ExitStack())
            NWB = 5
            wall = [sb("wall%d" % i, [128, 6144], BF16, p5s) for i in range(NWB)]
            wg = [w_[:, 0:2048] for w_ in wall]
            wu = [w_[:, 2048:4096] for w_ in wall]
            wd = [w_[:, 4096:6144] for w_ in wall]
            r_wg = mkres("wall", NWB)
            r_wu = r_wg
            r_wd = r_wg
            xrow = [sb("xrow%d" % i, [128, 1024], BF16, p5s) for i in range(10)]
            r_xrow = mkres("xrow", 10)
            XsT = [sb("XsT%d" % i, [128, 8, TS], BF16, p5s) for i in range(5)]
            r_XsT = mkres("XsT", 5)
            sa = [sb("sa%d" % i, [128, TS], F32, p5s) for i in range(2)]
            r_sa = mkres("sa")
            actT = [sb("actT%d" % i, [128, 2, TS], BF16, p5s) for i in range(5)]
            r_actT = mkres("actT", 5)
            yt = [sb("yt%d" % i, [128, 1024], F32, p5s) for i in range(4)]
            r_yt = mkres("yt", 4)
            r_ys = sc.res("ys_d")
            cnt5 = {"x": 0, "y": 0}
            def p5_tile(tp):
                wb = tp % NWB
                b = tp % 5
                sc.dma("pool", lambda e: e.indirect_dma_start(out=wall[wb][:], out_offset=None, in_=ewb_all[:, :],
                                                              in_offset=bass.IndirectOffsetOnAxis(ap=widx[:, tp:tp + 1], axis=0),
                                                              bounds_check=reg_ew, oob_is_err=False), r=[r_route, r_ewb], w=[r_wg[wb]])
                for s in range(TS // 128):
                    xi = (2 * tp + s) % 10
                    r0_ = tp * TS + s * 128
                    sc.dma("sp", lambda e: e.dma_start(out=xrow[xi][:], in_=xs_d[r0_:r0_ + 128, :]), r=[r_xs], w=[r_xrow[xi]])
                yield
                for s in range(TS // 128):
                    xi = (2 * tp + s) % 10
                    bk, rb = nbank()
                    pv = bk[:].bitcast(BF16).rearrange("p (c n) -> p c n", n=128)
                    sc.pe([(lambda e, c=c: e.transpose(out=pv[:, c, :], in_=xrow[xi][:, c * 128:(c + 1) * 128], identity=ident_bf[:])) for c in range(8)],
                          r=[r_xrow[xi], r_const], w=[rb])
                    sc.op("dve" if s % 2 == 0 else "act",
                          (lambda e: e.tensor_copy(out=XsT[b][:, :, s * 128:(s + 1) * 128], in_=pv)) if s % 2 == 0 else
                          (lambda e: e.copy(out=XsT[b][:, :, s * 128:(s + 1) * 128], in_=pv)), r=[rb], w=[r_XsT[b]])
                yield
                wgv = wg[wb].rearrange("p (c f) -> p c f", c=8)
                wuv = wu[wb].rearrange("p (c f) -> p c f", c=8)
                wdv = wd[wb].rearrange("p (c d) -> p c d", c=2)
                for fc in range(2):
                    bk, rb = nbank()
                    fns = [(lambda e, c=c: e.matmul(bk[:, 0:TS], lhsT=wgv[:, c, fc * 128:(fc + 1) * 128], rhs=XsT[b][:, c, :], start=(c == 0), stop=(c == 7)))
                           for c in range(8)]
                    fns += [(lambda e, c=c: e.matmul(bk[:, TS:2 * TS], lhsT=wuv[:, c, fc * 128:(fc + 1) * 128], rhs=XsT[b][:, c, :], start=(c == 0), stop=(c == 7)))
                            for c in range(8)]
                    sc.pe(fns, r=[r_wg[wb], r_wu[wb], r_XsT[b]], w=[rb])
                    sc.op("act", lambda e: e.activation(out=sa[fc][:], in_=bk[:, 0:TS], func=AF.Silu), r=[rb], w=[r_sa[fc]])
                    sc.op("dve", lambda e: e.tensor_tensor(out=actT[b][:, fc, :], in0=bk[:, TS:2 * TS], in1=sa[fc][:], op=ALU.mult),
                          r=[rb, r_sa[fc]], w=[r_actT[b]])
                    yield
                for s in range(TS // 128):
                    yi = cnt5["y"] % 4
                    cnt5["y"] += 1
                    for half in range(2):
                        bk, rb = nbank()
                        sc.pe([(lambda e, fc=fc: e.matmul(bk[:], lhsT=actT[b][:, fc, s * 128:(s + 1) * 128], rhs=wdv[:, fc, half * 512:(half + 1) * 512],
                                                          start=(fc == 0), stop=(fc == 1))) for fc in range(2)], r=[r_actT[b], r_wd[wb]], w=[rb])
                        if half == 0:
                            sc.op("act", lambda e: e.copy(out=yt[yi][:, 0:512], in_=bk[:]), r=[rb], w=[r_yt[yi]])
                        else:
                            sc.op("dve", lambda e: e.tensor_copy(out=yt[yi][:, 512:1024], in_=bk[:]), r=[rb], w=[r_yt[yi]])
                    r0_ = tp * TS + s * 128
                    sc.dma("act", lambda e: e.dma_start(out=ys_d[r0_:r0_ + 128, :], in_=yt[yi][:]), r=[r_yt[yi]], w=[])
                    yield
            run_pipelined(p5_tile, NTS, 5)
            sc.barrier()
            p5s.close()

        if last_phase >= 6:
            p6s = es.enter_context(ExitStack())
            y1 = [sb("y1_%d" % i, [128, 1024], F32, p6s) for i in range(2)]
            y2 = [sb("y2_%d" % i, [128, 1024], F32, p6s) for i in range(2)]
            xo = [sb("xo_%d" % i, [128, 1024], F32, p6s) for i in range(2)]
            r_y1, r_y2, r_xo = mkres("y1"), mkres("y2"), mkres("xo")
            def p6_tile(t):
                b = t % 2
                ts_ = slice(t * 128, (t + 1) * 128)
                for k, (yy, ry) in enumerate(((y1[b], r_y1[b]), (y2[b], r_y2[b]))):
                    sc.dma("pool", lambda e: e.indirect_dma_start(out=yy[:], out_offset=None, in_=ys_d[:, :],
                                                                  in_offset=bass.IndirectOffsetOnAxis(ap=pos12[:, k, t:t + 1], axis=0),
                                                                  bounds_check=reg_npos, oob_is_err=False), r=[r_route, r_ys], w=[ry])
                sc.dma("sp", lambda e: e.dma_start(out=xo[b][:], in_=out_d[ts_, :]), r=[r_x2d[t]], w=[r_xo[b]])
                yield
                sc.op("dve", lambda e: e.scalar_tensor_tensor(out=xo[b][:], in0=y1[b][:], scalar=w12[:, 0, t:t + 1], in1=xo[b][:], op0=ALU.mult, op1=ALU.add),
                      r=[r_y1[b], r_route], w=[r_xo[b]])
                sc.op("pool", lambda e: e.scalar_tensor_tensor(out=xo[b][:], in0=y2[b][:], scalar=w12[:, 1, t:t + 1], in1=xo[b][:], op0=ALU.mult, op1=ALU.add),
                      r=[r_y2[b], r_route], w=[r_xo[b]]) if False else \
                    sc.op("dve", lambda e: e.scalar_tensor_tensor(out=xo[b][:], in0=y2[b][:], scalar=w12[:, 1, t:t + 1], in1=xo[b][:], op0=ALU.mult, op1=ALU.add),
                          r=[r_y2[b], r_route], w=[r_xo[b]])
                sc.dma("act", lambda e: e.dma_start(out=out_d[ts_, :], in_=xo[b][:]), r=[r_xo[b]], w=[r_x2d[t]], is_out=True)
            run_pipelined(p6_tile, NT, 2)
            sc.barrier()
            p6s.close()

        sc.finish()
    print("program: %d instructions, %d waits" % (sc.n_inst, sc.n_wait))
    return nc, dbg_out


def _perm_rows(w, c):
    n = w.shape[1]
    return np.ascontiguousarray(w.reshape(c, 128, n).transpose(1, 0, 2))


def _consts():
    cst = np.zeros((128, NCST), np.float64)
    j64 = np.arange(64)
    j32 = np.arange(32)
    invR = 10000.0 ** (-j64 / 64.0) / (2 * np.pi)
    invM = 10000.0 ** (-j32 / 32.0) / (2 * np.pi)
    cst[:, 0:64] = invR
    cst[:, 64:128] = invR
    cst[:, 128:160] = invM
    cst[:, 160:192] = invM
    cst[:, 192 + 64:192 + 128] = 0.25
    cst[:, 192 + 160:192 + 192] = 0.25
    h = np.arange(4)
    lg = np.log(1.0 - np.exp2(-5.0 - h))
    p = np.arange(128)[:, None]
    cst[:, 384:388] = np.exp((p + 1.0) * lg[None, :])
    cst[:, 388:392] = np.exp(-(p + 1.0) * lg[None, :]) * (128.0 ** -0.5)
    cst[:, 392:904] = np.repeat(np.exp(128.0 * lg), 128)[None, :]
    return cst.astype(np.float32)


def make_in_maps(inputs, S, n_cores, last_phase=6):
    f = lambda a: np.ascontiguousarray(np.asarray(a), dtype=np.float32)
    l = 0
    shared = {
        "cst": _consts(),
        "w_in": _perm_rows(f(inputs["w_in"][l]), 8),
        "g_attn": np.ascontiguousarray(f(inputs["attn_norm_g"][l]).reshape(8, 128).T),
        "w_uq": _perm_rows(f(inputs["mla_w_uq"][l]), 2),
        "g_qn": np.ascontiguousarray(f(inputs["mla_q_norm_g"][l]).reshape(2, 128).T),
        "w_ukv": f(inputs["mla_w_ukv"][l]),
        "g_kvn": f(inputs["mla_kv_norm_g"][l]).reshape(128, 1),
        "gq": f(inputs["mla_q_qk_g"][l]).reshape(1, 192),
        "gk": f(inputs["mla_k_qk_g"][l]).reshape(1, 192),
        "gn": f(inputs["ret_gn_g"][l]).reshape(1, 512),
        "w_out": _perm_rows(f(inputs["w_out"][l]), 8),
        "g_cross": np.ascontiguousarray(f(inputs["cross_norm_g"][l]).reshape(8, 128).T),
        "g_mem": np.ascontiguousarray(f(inputs["mem_norm_g"][l]).reshape(8, 128).T),
        "cw_q": _perm_rows(f(inputs["cross_w_q"][l]), 8),
        "cw_kv": _perm_rows(f(inputs["cross_w_kv"][l]), 8),
        "cqg": f(inputs["cross_q_qk_g"][l]).reshape(1, 256),
        "ckg": f(inputs["cross_k_qk_g"][l]).reshape(1, 256),
        "cw_o": _perm_rows(f(inputs["cross_w_o"][l]), 8),
        "mg": f(inputs["moe_norm_g"][l]).reshape(1, 1024),
        "w_rt": _perm_rows(np.concatenate([f(inputs["router_w_group"][l]), f(inputs["router_w_expert"][l])], axis=1), 8),
        "b_rt": np.concatenate([f(inputs["router_b_group"][l]), f(inputs["router_b_expert"][l])]).reshape(1, 36),
        "ew_g": np.ascontiguousarray(f(inputs["expert_w_gate"][l]).reshape(32, 8, 128, 256).transpose(0, 2, 1, 3)).reshape(32 * 128, 2048),
        "ew_u": np.ascontiguousarray(f(inputs["expert_w_up"][l]).reshape(32, 8, 128, 256).transpose(0, 2, 1, 3)).reshape(32 * 128, 2048),
        "ew_d": np.ascontiguousarray(f(inputs["expert_w_down"][l]).reshape(32, 2, 128, 1024).transpose(0, 2, 1, 3)).reshape(32 * 128, 2048),
    }
    if last_phase < 5:
        for k in ("ew_g", "ew_u", "ew_d"):
            del shared[k]
    NT = S // 128
    maps = []
    for b in range(n_cores):
        m = dict(shared)
        m["x"] = f(inputs["x"][b])
        m["mem"] = f(inputs["mem"][b])
        m["pos"] = np.ascontiguousarray(np.asarray(inputs["positions"][b]).astype(np.int32).reshape(NT, 128).T)
        maps.append(m)
    return maps


def kernel(**inputs):
    B, S, _ = inputs["x"].shape
    nc, _ = build_program(S)
    maps = make_in_maps(inputs, S, B)
    res = run_bass_kernel_spmd(nc, maps, core_ids=list(range(B)))
    return np.stack([np.asarray(r["out"]) for r in res.results], axis=0).astype(np.float32)
```

```python
import math
from contextlib import ExitStack

import numpy as np
import concourse.bass as bass
import concourse.mybir as mybir
from concourse.bass_utils import run_bass_kernel_spmd

F32 = mybir.dt.float32
BF16 = mybir.dt.bfloat16
I32 = mybir.dt.int32
AF = mybir.ActivationFunctionType
ALU = mybir.AluOpType
AX = mybir.AxisListType

D = 1024
EPS = 1e-6
NDMA = 24
NCST = 904


class Res:
    __slots__ = ("name", "w", "rd", "excl")

    def __init__(self, name, excl=False):
        self.name = name
        self.w = None
        self.rd = []
        self.excl = excl


class Sched:
    ENGS = ("pe", "act", "dve", "pool", "sp")

    def __init__(self, nc, es):
        self.nc = nc
        self.eng = {"pe": nc.tensor, "act": nc.scalar, "dve": nc.vector, "pool": nc.gpsimd, "sp": nc.sync}
        self.sem = {e: es.enter_context(nc.semaphore("c_" + e)) for e in self.ENGS}
        self.cnt = {e: 0 for e in self.ENGS}
        self.waited = {e: {} for e in self.ENGS}
        self.dsem = {q: [es.enter_context(nc.semaphore("d_%s%d" % (q, i))) for i in range(NDMA)] for q in ("sp", "pool", "act")}
        self.dcnt = {q: [0] * NDMA for q in ("sp", "pool", "act")}
        self.dnext = {q: 0 for q in ("sp", "pool", "act")}
        self.semname = {}
        self.out_tokens = []
        self.n_inst = 0
        self.n_wait = 0

    def res(self, name):
        return Res(name)

    def _wait(self, eng, tok):
        sem, val, src = tok
        if src == "pe" and eng == "pe":
            return
        k = id(sem)
        if self.waited[eng].get(k, 0) >= val:
            return
        self.eng[eng].wait_ge(sem, val)
        self.n_wait += 1
        self.waited[eng][k] = val

    def _deps(self, eng, r, w):
        w = list(w) + [x for x in r if x.excl]
        for x in r:
            if x.w is not None:
                self._wait(eng, x.w)
        for x in w:
            if x.w is not None:
                self._wait(eng, x.w)
            for t in x.rd:
                self._wait(eng, t)

    def _commit(self, tok, r, w):
        w = list(w) + [x for x in r if x.excl]
        r = [x for x in r if not x.excl]
        for x in r:
            x.rd = [t for t in x.rd if t[0] is not tok[0]] + [tok]
        for x in w:
            x.w = tok
            x.rd = []

    def op(self, eng, fn, r=(), w=()):
        self._deps(eng, r, w)
        inst = fn(self.eng[eng])
        self.cnt[eng] += 1
        self.n_inst += 1
        inst.then_inc(self.sem[eng], 1)
        tok = (self.sem[eng], self.cnt[eng], eng)
        self.waited[eng][id(self.sem[eng])] = max(self.waited[eng].get(id(self.sem[eng]), 0), 0)
        self._commit(tok, r, w)
        return tok

    def pe(self, fns, r=(), w=()):
        self._deps("pe", r, w)
        inst = None
        for fn in fns:
            inst = fn(self.eng["pe"])
            self.n_inst += 1
        self.cnt["pe"] += 1
        inst.then_inc(self.sem["pe"], 1)
        tok = (self.sem["pe"], self.cnt["pe"], "pe")
        self._commit(tok, r, w)
        return tok

    def dma(self, q, fn, r=(), w=(), is_out=False):
        self._deps(q, r, w)
        i = self.dnext[q] % NDMA
        self.dnext[q] += 1
        sem = self.dsem[q][i]
        if self.dcnt[q][i] > 0:
            self._wait(q, (sem, 16 * self.dcnt[q][i], "dma"))
        inst = fn(self.eng[q])
        self.n_inst += 1
        self.dcnt[q][i] += 1
        inst.then_inc(sem, 16)
        tok = (sem, 16 * self.dcnt[q][i], "dma")
        self._commit(tok, r, w)
        if is_out:
            self.out_tokens.append(tok)
        return tok

    def barrier(self):
        toks = [(self.sem[e], self.cnt[e], e) for e in self.ENGS if self.cnt[e] > 0]
        for q in ("sp", "pool", "act"):
            for i in range(NDMA):
                if self.dcnt[q][i] > 0:
                    toks.append((self.dsem[q][i], 16 * self.dcnt[q][i], "dma"))
        for e in self.ENGS:
            for t in toks:
                if t[2] == e and e != "pe":
                    pass
                self._wait_force(e, t)

    def _wait_force(self, eng, tok):
        sem, val, src = tok
        k = id(sem)
        if self.waited[eng].get(k, 0) >= val:
            return
        self.eng[eng].wait_ge(sem, val)
        self.n_wait += 1
        self.waited[eng][k] = val

    def finish(self):
        for t in self.out_tokens:
            self._wait_force("sp", t)
        self.barrier()


def build_program(S, last_phase=6, dbg=()):
    NT = S // 128
    NB = S // 512
    nc = bass.Bass("TRN2", target_bir_lowering=False)

    def din(name, shape, dt=F32):
        return nc.dram_tensor(name, list(shape), dt, kind="ExternalInput").ap()

    x_d = din("x", [S, D])
    mem_d = din("mem", [256, D])
    pos_d = din("pos", [128, NT], I32)
    cst_d = din("cst", [128, NCST])
    w_in_d = din("w_in", [128, 8, 2496])
    g_attn_d = din("g_attn", [128, 8])
    w_uq_d = din("w_uq", [128, 2, 768])
    g_qn_d = din("g_qn", [128, 2])
    w_ukv_d = din("w_ukv", [128, 1024])
    g_kvn_d = din("g_kvn", [128, 1])
    gq_d = din("gq", [1, 192])
    gk_d = din("gk", [1, 192])
    gn_d = din("gn", [1, 512])
    w_out_d = din("w_out", [128, 8, 1024])
    g_cross_d = din("g_cross", [128, 8])
    g_mem_d = din("g_mem", [128, 8])
    cw_q_d = din("cw_q", [128, 8, 1024])
    cw_kv_d = din("cw_kv", [128, 8, 2048])
    cqg_d = din("cqg", [1, 256])
    ckg_d = din("ckg", [1, 256])
    cw_o_d = din("cw_o", [128, 8, 1024])
    mg_d = din("mg", [1, 1024])
    w_rt_d = din("w_rt", [128, 8, 36])
    b_rt_d = din("b_rt", [1, 36])
    if last_phase >= 5:
        ew_g_d = din("ew_g", [32 * 128, 2048])
        ew_u_d = din("ew_u", [32 * 128, 2048])
        ew_d_d = din("ew_d", [32 * 128, 2048])
    out_d = nc.dram_tensor("out", [S, D], F32, kind="ExternalOutput").ap()

    dbg_out = {}

    with ExitStack() as es:
        sc = Sched(nc, es)

        def sb(name, shape, dt=F32, stack=es):
            return stack.enter_context(nc.sbuf_tensor("s_" + name, list(shape), dt))

        banks = [es.enter_context(nc.psum_tensor("bank%d" % i, [128, 512], F32)) for i in range(8)]
        bank_res = [Res("bank%d" % i, excl=True) for i in range(8)]
        bstate = {"i": 0}

        def nbank():
            i = bstate["i"] % 8
            bstate["i"] += 1
            return banks[i], bank_res[i]

        def dump(name, ap, res, shape, dt=F32):
            if name not in dbg:
                return
            t = nc.dram_tensor("dbg_" + name, list(shape), dt, kind="ExternalOutput").ap()
            dbg_out[name] = t
            sc.dma("sp", lambda e: e.dma_start(out=t, in_=ap), r=[res], w=[], is_out=True)

        def nbank(lo=0, hi=8):
            key = (lo, hi)
            i = lo + bstate.get(key, 0) % (hi - lo)
            bstate[key] = bstate.get(key, 0) + 1
            return banks[i], bank_res[i]

        cst = sb("cst", [128, NCST])
        r_cst = sc.res("cst")
        sc.dma("sp", lambda e: e.dma_start(out=cst[:], in_=cst_d[:, :]), w=[r_cst])
        INVF = cst[:, 0:192]
        OFFS = cst[:, 192:384]
        QDc = cst[:, 384:388]
        KDc = cst[:, 388:392]
        CDEC = cst[:, 392:904]

        ident_bf = sb("ident_bf", [128, 128], BF16)
        ident_f = sb("ident_f", [128, 128], F32)
        maskT = sb("maskT", [128, 128], BF16)
        mask4 = sb("mask4", [128, 4, 128], F32)
        tri = sb("tri", [128, 128], BF16)
        ones_bf = sb("ones_bf", [128, 128], BF16)
        r_const = sc.res("consts")

        def mk_mask(t_ap, pattern, cmp):
            sc.op("pool", lambda e: e.memset(t_ap, 1.0), w=[r_const])
            sc.op("pool", lambda e: e.affine_select(out=t_ap, in_=t_ap, pattern=pattern, compare_op=cmp, fill=0.0,
                                                    base=0, channel_multiplier=-1), r=[r_const], w=[r_const])

        mk_mask(ident_bf[:], [[1, 128]], ALU.is_equal)
        mk_mask(ident_f[:], [[1, 128]], ALU.is_equal)
        mk_mask(maskT[:], [[1, 128]], ALU.is_ge)
        mk_mask(mask4[:], [[0, 4], [1, 128]], ALU.is_ge)
        mk_mask(tri[:], [[1, 128]], ALU.is_gt)
        negm = sb("negm", [128, 128], BF16)
        sc.op("pool", lambda e: e.memset(negm[:], -30000.0), w=[r_const])
        sc.op("pool", lambda e: e.affine_select(out=negm[:], in_=negm[:], pattern=[[-1, 128]], compare_op=ALU.is_gt, fill=0.0,
                                                base=0, channel_multiplier=1), r=[r_const], w=[r_const])
        sc.op("pool", lambda e: e.memset(ones_bf[:], 1.0), w=[r_const])

        def bload(name, src, n, stack=es):
            t = sb(name, [128, n], F32, stack)
            sc.dma("sp", lambda e: e.dma_start(out=t[:], in_=src.partition_broadcast(128)), w=[r_const])
            return t

        def pload(name, src, n, stack=es):
            t = sb(name, [128, n], F32, stack)
            sc.dma("sp", lambda e: e.dma_start(out=t[:], in_=src[:, :]), w=[r_const])
            return t

        gq = bload("gq", gq_d, 192)
        gk = bload("gk", gk_d, 192)
        gn = bload("gn", gn_d, 512)

        pos_i = sb("pos_i", [128, NT], I32)
        pos_f = sb("pos_f", [128, NT])
        SCm = sb("SCm", [128, NT, 64])
        rstd1 = sb("rstd1", [128, NT])
        r_sc = sc.res("SC")
        r_sct = [sc.res("SCt%d" % t) for t in range(NT)]
        r_rstd1 = sc.res("rstd1")
        r_rstd1t = [sc.res("rstd1_%d" % t) for t in range(NT)]
        stage = [None, None]
        r_stage = [sc.res("stage%d" % i) for i in range(2)]
        st = {"i": 0}
        scale_engs = ("dve", "act")

        def load_scaled(dst_fn, src_fn, gain_fn, C, N, rdst):
            for c in range(C):
                for n0 in range(0, N, 1024):
                    n1 = min(N, n0 + 1024)
                    i = st["i"] % 2
                    st["i"] += 1
                    stg, rs = stage[i], r_stage[i]
                    sc.dma("sp", lambda e: e.dma_start(out=stg[:, 0:n1 - n0], in_=src_fn(c, n0, n1)), w=[rs])
                    if scale_engs[i] == "act":
                        sc.op("act", lambda e: e.activation(out=dst_fn(c, n0, n1), in_=stg[:, 0:n1 - n0], func=AF.Copy, scale=gain_fn(c)),
                              r=[rs, r_const], w=[rdst])
                    else:
                        sc.op("dve", lambda e: e.tensor_scalar(out=dst_fn(c, n0, n1), in0=stg[:, 0:n1 - n0], scalar1=gain_fn(c), scalar2=None,
                                                               op0=ALU.mult), r=[rs, r_const], w=[rdst])

        def load_cast(dst_fn, src_fn, C, rdst):
            for c in range(C):
                sc.dma("pool", lambda e: e.dma_start(out=dst_fn(c), in_=src_fn(c)), w=[rdst])

        mhalf = sb("mhalf", [128, 8])
        sc.op("pool", lambda e: e.memset(mhalf[:], -0.5), w=[r_const])

        def rstd_from_ss(ss_ap, n, out_ap, r_in, r_out, tmp_ap, r_tmp):
            k = ss_ap.shape[1]
            sc.op("dve", lambda e: e.tensor_scalar(out=tmp_ap, in0=ss_ap, scalar1=1.0 / n, scalar2=EPS, op0=ALU.mult, op1=ALU.add),
                  r=[r_in], w=[r_tmp])
            sc.op("pool", lambda e: e.tensor_tensor(out=out_ap, in0=tmp_ap, in1=mhalf[:, 0:k], op=ALU.pow), r=[r_tmp, r_const], w=[r_out])

        sc.dma("sp", lambda e: e.dma_start(out=pos_i[:], in_=pos_d[:, :]), w=[r_sc])
        sc.op("dve", lambda e: e.tensor_copy(out=pos_f[:], in_=pos_i[:]), r=[r_sc], w=[r_sc])

        rout_d = nc.dram_tensor("rout_s", [S, 512], BF16).ap()
        x1_d = nc.dram_tensor("x1_s", [S, D], F32).ap()
        hm_d = nc.dram_tensor("hm_s", [S, D], BF16).ap()
        TS = 256
        NTS = (2 * S) // TS + 32
        NPOS = NTS * TS
        xs_d = nc.dram_tensor("xs_s", [NPOS, D], BF16).ap()
        ys_d = nc.dram_tensor("ys_s", [NPOS, D], F32).ap()
        r_xs = sc.res("xs_d")

        def run_pipelined(gen_fn, n_items, depth):
            active = []
            nxt = 0
            while nxt < n_items or active:
                if nxt < n_items and len(active) < depth:
                    active.append(gen_fn(nxt))
                    nxt += 1
                for g in list(active):
                    try:
                        next(g)
                    except StopIteration:
                        active.remove(g)

        def mkres(n, k=2):
            return [sc.res("%s%d" % (n, i)) for i in range(k)]

        if last_phase >= 1:
            p1s = es.enter_context(ExitStack())
            import os as _os
            stop = int(_os.environ.get('P1_STOP', '99'))
            SCr = sb("SCr", [128, NT, 128], F32, p1s)
            w_r = sb("w_r", [128, 8, 2048], BF16, p1s)
            r_wr = sc.res("w_r")
            stage[:] = [sb("stage1_%d" % i, [128, 1024], F32, p1s) for i in range(2)]
            g_attn = pload("g_attn", g_attn_d, 8, p1s)
            load_scaled(lambda c, a, b_: w_r[:, c, a:b_], lambda c, a, b_: w_in_d[:, c, 448 + a:448 + b_], lambda c: g_attn[:, c:c + 1], 8, 2048, r_wr)

            NTsc = NT if stop >= 2 else 0
            tr_t = sb("tr_t", [128, 192], F32, p1s)
            tr_i = sb("tr_i", [128, 192], I32, p1s)
            tr_f = sb("tr_f", [128, 192], F32, p1s)
            r_tr = sc.res("tr")
            def sc_tables(t):
                sc.op("dve", lambda e: e.scalar_tensor_tensor(out=tr_t[:], in0=INVF, scalar=pos_f[:, t:t + 1], in1=OFFS,
                                                              op0=ALU.mult, op1=ALU.add), r=[r_cst, r_sc], w=[r_tr])
                sc.op("dve", lambda e: e.tensor_copy(out=tr_i[:], in_=tr_t[:]), r=[r_tr], w=[r_tr])
                sc.op("dve", lambda e: e.tensor_copy(out=tr_f[:], in_=tr_i[:]), r=[r_tr], w=[r_tr])
                sc.op("dve", lambda e: e.tensor_tensor(out=tr_t[:], in0=tr_t[:], in1=tr_f[:], op=ALU.subtract), r=[r_tr], w=[r_tr])
                sc.op("dve", lambda e: e.scalar_tensor_tensor(out=tr_f[:], in0=tr_t[:], scalar=0.5, in1=tr_t[:],
                                                              op0=ALU.is_gt, op1=ALU.subtract), r=[r_tr], w=[r_tr])
                sc.op("dve", lambda e: e.scalar_tensor_tensor(out=tr_t[:], in0=tr_f[:], scalar=0.5, in1=tr_f[:],
                                                              op0=ALU.is_gt, op1=ALU.subtract), r=[r_tr], w=[r_tr])
                sc.op("act", lambda e: e.activation(out=SCr[:, t, :], in_=tr_t[:, 0:128], func=AF.Sin, scale=6.28318), r=[r_tr], w=[r_sct[t]])
                sc.op("act", lambda e: e.activation(out=SCm[:, t, :], in_=tr_t[:, 128:192], func=AF.Sin, scale=6.28318), r=[r_tr], w=[r_sct[t]])
            dump("SCr", SCr[:], r_sct[NT - 1], [128, NT, 128])
            dump("SCm", SCm[:], r_sct[NT - 1], [128, NT, 64])

            do_pc = last_phase >= 5
            if do_pc:
                ewb_all = nc.dram_tensor("ewb_all", [32 * 128, 6144], BF16).ap()
                ewb_d = [ewb_all[:, k * 2048:(k + 1) * 2048] for k in range(3)]
                ew_src = [ew_g_d, ew_u_d, ew_d_d]
                r_ewb = sc.res("ewb")
                pcs = [sb("pcs%d" % i, [128, 2048], BF16, p1s) for i in range(3)]
                r_pcs = mkres("pcs", 3)
                pc_state = {"i": 0}
                PC_PER_TILE = (96 + NT - 1) // NT

                pend_wb = []

                def precast_flush():
                    while pend_wb:
                        k_, rows, bi = pend_wb.pop(0)
                        sc.dma("sp", lambda e: e.dma_start(out=ewb_d[k_][rows, :], in_=pcs[bi][:]), r=[r_pcs[bi]], w=[])

                def precast_step():
                    precast_flush()
                    for _ in range(PC_PER_TILE):
                        i = pc_state["i"]
                        if i >= 96:
                            return
                        pc_state["i"] += 1
                        if len(pend_wb) >= len(pcs):
                            precast_flush()
                        e_, k_ = i // 3, i % 3
                        bi = i % len(pcs)
                        rows = slice(e_ * 128, (e_ + 1) * 128)
                        sc.dma("pool", lambda e: e.dma_start(out=pcs[bi][:], in_=ew_src[k_][rows, :]), w=[r_pcs[bi]])
                        pend_wb.append((k_, rows, bi))

            zt = sb("zt", [128, 2, 1024], BF16, p1s)
            r_zt = sc.res("zt")
            sc.op("pool", lambda e: e.memset(zt[:], 0.0), w=[r_zt])
            ROWS_PER = NPOS // NT

            def zero_step(t):
                if last_phase < 4:
                    return
                for r0_ in range(t * ROWS_PER, (t + 1) * ROWS_PER, 256):
                    sc.dma("sp", lambda e: e.dma_start(out=xs_d[r0_:r0_ + 256, :].rearrange("(p a) d -> p a d", a=2), in_=zt[:]), r=[r_zt], w=[])

            xt = [sb("xt%d" % i, [128, 1024], F32, p1s) for i in range(4)]
            xb = [sb("xb%d" % i, [128, 1024], BF16, p1s) for i in range(4)]
            xT = [sb("xT%d" % i, [128, 8, 128], BF16, p1s) for i in range(4)]
            junk = sb("junk", [128, 1024], BF16, p1s)
            rq_f = [sb("rq_f%d" % i, [128, 512], F32, p1s) for i in range(4)]
            rk_f = [sb("rk_f%d" % i, [128, 512], F32, p1s) for i in range(4)]
            v_b = [sb("v_b%d" % i, [128, 512], BF16, p1s) for i in range(4)]
            sg = [sb("sg%d" % i, [128, 512], F32, p1s) for i in range(6)]
            sm1 = [sb("sm1_%d" % i, [128, 32], F32, p1s) for i in range(4)]
            rp = [sb("rp%d" % i, [128, 2, 512], F32, p1s) for i in range(2)]
            qp_b = [sb("qp_b%d" % i, [128, 512], BF16, p1s) for i in range(4)]
            kp_b = [sb("kp_b%d" % i, [128, 512], BF16, p1s) for i in range(4)]
            qpT = [sb("qpT%d" % i, [128, 4, 128], BF16, p1s) for i in range(4)]
            kpT = [sb("kpT%d" % i, [128, 4, 128], BF16, p1s) for i in range(4)]
            PT = [sb("PT%d" % i, [128, 4, 128], BF16, p1s) for i in range(3)]
            Tst = sb("Tst", [128, 4, 128], F32, p1s)
            Tst_b = sb("Tst_b", [128, 4, 128], BF16, p1s)
            Ttmp = sb("Ttmp", [128, 4, 128], F32, p1s)
            o_f = [sb("o_f%d" % i, [128, 4, 128], F32, p1s) for i in range(4)]
            bnst = [sb("bnst%d" % i, [128, 4, 6], F32, p1s) for i in range(4)]
            bnag = [sb("bnag%d" % i, [128, 4, 2], F32, p1s) for i in range(4)]
            ro_b = [sb("ro_b%d" % i, [128, 512], BF16, p1s) for i in range(3)]

            r_xt, r_xb, r_xT = mkres("xt", 4), mkres("xb", 4), mkres("xT", 4)
            r_rq, r_rk, r_vb, r_sg, r_sm1 = mkres("rq", 4), mkres("rk", 4), mkres("vb", 4), mkres("sg", 6), mkres("sm1", 4)
            r_rp = mkres("rp")
            r_qpb, r_kpb, r_qpT, r_kpT, r_PT, r_of, r_bn, r_rob = (mkres("qpb", 4), mkres("kpb", 4), mkres("qpT", 4), mkres("kpT", 4), mkres("PT", 3),
                                                                   mkres("of", 4), mkres("bn", 4), mkres("rob", 3))
            r_junk = sc.res("junk")
            r_T = sc.res("Tst")
            r_Tb = sc.res("Tst_b")
            r_Tt = sc.res("Ttmp")
            sc.op("dve", lambda e: e.memset(Tst[:], 0.0), w=[r_T])
            sc.op("dve", lambda e: e.memset(Tst_b[:], 0.0), w=[r_Tb])

            def rope_ret(eng, src, r_src, dst_b, r_dst, t, decay, scr, r_scr):
                cosB = SCr[:, t, 64:128].unsqueeze(1).broadcast_to([128, 8, 64])
                sinB = SCr[:, t, 0:64].unsqueeze(1).broadcast_to([128, 4, 64])
                sv = src[:].rearrange("p (h two d) -> p h two d", h=4, two=2)
                Pv = scr[:, 0, :]
                Qv = scr[:, 1, :].rearrange("p (h two d) -> p h two d", h=4, two=2)
                P4 = scr[:, 0, :].rearrange("p (h two d) -> p h two d", h=4, two=2)
                sc.op(eng, lambda e: e.tensor_tensor(out=Pv.rearrange("p (g d) -> p g d", g=8), in0=src[:].rearrange("p (g d) -> p g d", g=8),
                                                     in1=cosB, op=ALU.mult), r=[r_src, r_sct[t]], w=[r_scr])
                sc.op(eng, lambda e: e.tensor_tensor(out=Qv[:, :, 0, :], in0=sv[:, :, 1, :], in1=sinB, op=ALU.mult), r=[r_src, r_sct[t]], w=[r_scr])
                sc.op(eng, lambda e: e.tensor_tensor(out=Qv[:, :, 1, :], in0=sv[:, :, 0, :], in1=sinB, op=ALU.mult), r=[r_src, r_sct[t]], w=[r_scr])
                sc.op(eng, lambda e: e.tensor_tensor(out=P4[:, :, 0, :], in0=P4[:, :, 0, :], in1=Qv[:, :, 0, :], op=ALU.subtract), r=[r_scr], w=[r_scr])
                sc.op(eng, lambda e: e.tensor_tensor(out=P4[:, :, 1, :], in0=P4[:, :, 1, :], in1=Qv[:, :, 1, :], op=ALU.add), r=[r_scr], w=[r_scr])
                decB = decay.unsqueeze(2).broadcast_to([128, 4, 128])
                sc.op(eng, lambda e: e.tensor_tensor(out=dst_b[:].rearrange("p (h d) -> p h d", h=4), in0=Pv.rearrange("p (h d) -> p h d", h=4),
                                                     in1=decB, op=ALU.mult), r=[r_scr, r_cst], w=[r_dst])

            def p1_tile(t):
                b = t % 4
                b6 = t % 6
                b3 = t % 3
                ts_ = slice(t * 128, (t + 1) * 128)
                s1 = sm1[b]
                sc.dma("sp", lambda e: e.dma_start(out=xt[b][:], in_=x_d[ts_, :]), w=[r_xt[b]])
                sc_tables(t)
                if do_pc:
                    precast_step()
                zero_step(t)
                sc.op("act", lambda e: e.activation(out=junk[:], in_=xt[b][:], func=AF.Square, accum_out=s1[:, 0:1]),
                      r=[r_xt[b]], w=[r_junk, r_sm1[b]])
                rstd_from_ss(s1[:, 0:1], 1024.0, rstd1[:, t:t + 1], r_sm1[b], r_rstd1t[t], s1[:, 1:2], r_sm1[b])
                yield
                sc.op("act", lambda e: e.copy(out=xb[b][:], in_=xt[b][:]), r=[r_xt[b]], w=[r_xb[b]])
                bk, rb = nbank()
                pv = bk[:].bitcast(BF16).rearrange("p (c n) -> p c n", n=128)
                sc.pe([(lambda e, c=c: e.transpose(out=pv[:, c, :], in_=xb[b][:, c * 128:(c + 1) * 128], identity=ident_bf[:])) for c in range(8)],
                      r=[r_xb[b], r_const], w=[rb])
                sc.op("dve", lambda e: e.tensor_copy(out=xT[b][:], in_=pv), r=[rb], w=[r_xT[b]])
                rs1 = rstd1[:, t:t + 1]
                yield
                pb = []
                for k4 in range(4):
                    bk, rb = nbank()
                    sc.pe([(lambda e, c=c: e.matmul(bk[:], lhsT=xT[b][:, c, :], rhs=w_r[:, c, k4 * 512:(k4 + 1) * 512], start=(c == 0), stop=(c == 7)))
                           for c in range(8)], r=[r_xT[b], r_wr], w=[rb])
                    pb.append((bk, rb))
                sc.op("act", lambda e: e.activation(out=rq_f[b][:], in_=pb[0][0][:], func=AF.Copy, scale=rs1), r=[pb[0][1], r_rstd1t[t]], w=[r_rq[b]])
                sc.op("dve", lambda e: e.tensor_scalar(out=rk_f[b][:], in0=pb[1][0][:], scalar1=rs1, scalar2=None, op0=ALU.mult),
                      r=[pb[1][1], r_rstd1t[t]], w=[r_rk[b]])
                sc.op("act", lambda e: e.activation(out=v_b[b][:], in_=pb[2][0][:], func=AF.Copy, scale=rs1), r=[pb[2][1], r_rstd1t[t]], w=[r_vb[b]])
                sc.op("act", lambda e: e.activation(out=sg[b6][:], in_=pb[3][0][:], func=AF.Silu, scale=rs1), r=[pb[3][1], r_rstd1t[t]], w=[r_sg[b6]])

                yield
                rope_ret("dve", rq_f[b], r_rq[b], qp_b[b], r_qpb[b], t, QDc, rp[0], r_rp[0])
                rope_ret("pool", rk_f[b], r_rk[b], kp_b[b], r_kpb[b], t, KDc, rp[1], r_rp[1])
                yield
                bk, rb = nbank()
                pv = bk[:].bitcast(BF16).rearrange("p (c n) -> p c n", n=128)
                sc.pe([(lambda e, h=h: e.transpose(out=pv[:, h, :], in_=qp_b[b][:, h * 128:(h + 1) * 128], identity=ident_bf[:])) for h in range(4)]
                      + [(lambda e, h=h: e.transpose(out=pv[:, 4 + h, :], in_=kp_b[b][:, h * 128:(h + 1) * 128], identity=ident_bf[:])) for h in range(4)],
                      r=[r_qpb[b], r_kpb[b], r_const], w=[rb])
                sc.op("act", lambda e: e.copy(out=qpT[b][:], in_=pv[:, 0:4, :]), r=[rb], w=[r_qpT[b]])
                sc.op("dve", lambda e: e.tensor_copy(out=kpT[b][:], in_=pv[:, 4:8, :]), r=[rb], w=[r_kpT[b]])
                bk, rb = nbank()
                sv_ = bk[:].rearrange("p (h n) -> p h n", h=4)
                sc.pe([(lambda e, h=h: e.matmul(sv_[:, h, :], lhsT=kpT[b][:, h, :], rhs=qpT[b][:, h, :], start=True, stop=True)) for h in range(4)],
                      r=[r_kpT[b], r_qpT[b]], w=[rb])
                sc.op("dve", lambda e: e.tensor_tensor(out=PT[b3][:], in0=sv_, in1=mask4[:], op=ALU.mult), r=[rb, r_const], w=[r_PT[b3]])
                yield
                bk_o, rb_o = nbank()
                ov = bk_o[:].rearrange("p (h n) -> p h n", h=4)
                fns = []
                for h in range(4):
                    fns.append(lambda e, h=h: e.matmul(ov[:, h, :], lhsT=PT[b3][:, h, :], rhs=v_b[b][:, h * 128:(h + 1) * 128], start=True, stop=False))
                    fns.append(lambda e, h=h: e.matmul(ov[:, h, :], lhsT=qpT[b][:, h, :], rhs=Tst_b[:, h, :], start=False, stop=True))
                sc.pe(fns, r=[r_PT[b3], r_vb[b], r_qpT[b], r_Tb], w=[rb_o])
                bk_s, rb_s = nbank()
                stv = bk_s[:].rearrange("p (h n) -> p h n", h=4)
                sc.pe([(lambda e, h=h: e.matmul(stv[:, h, :], lhsT=kp_b[b][:, h * 128:(h + 1) * 128], rhs=v_b[b][:, h * 128:(h + 1) * 128],
                                                start=True, stop=True)) for h in range(4)], r=[r_kpb[b], r_vb[b]], w=[rb_s])
                sc.op("dve", lambda e: e.tensor_tensor(out=Ttmp[:], in0=stv, in1=Tst[:], op=ALU.add), r=[rb_s, r_T], w=[r_Tt])
                sc.op("pool", lambda e: e.tensor_tensor(out=Tst[:], in0=Ttmp[:], in1=CDEC.rearrange("p (h n) -> p h n", h=4), op=ALU.mult),
                      r=[r_Tt, r_cst], w=[r_T])
                sc.op("pool", lambda e: e.tensor_copy(out=Tst_b[:], in_=Tst[:]), r=[r_T], w=[r_Tb])
                yield
                sc.op("act", lambda e: e.copy(out=o_f[b][:], in_=ov), r=[rb_o], w=[r_of[b]])
                for h in range(4):
                    sc.op("dve", lambda e: e.bn_stats(out=bnst[b][:, h, :], in_=o_f[b][:, h, :]), r=[r_of[b]], w=[r_bn[b]])
                for h in range(4):
                    sc.op("dve", lambda e: e.bn_aggr(out=bnag[b][:, h, :], in_=bnst[b][:, h, :]), r=[r_bn[b]], w=[r_bn[b]])
                sc.op("dve", lambda e: e.tensor_scalar(out=s1[:, 20:24], in0=bnag[b][:, :, 1], scalar1=EPS, scalar2=None, op0=ALU.add),
                      r=[r_bn[b]], w=[r_sm1[b]])
                sc.op("pool", lambda e: e.tensor_tensor(out=s1[:, 24:28], in0=s1[:, 20:24], in1=mhalf[:, 0:4], op=ALU.pow), r=[r_sm1[b], r_const], w=[r_sm1[b]])
                for h in range(4):
                    sc.op("dve", lambda e: e.tensor_scalar(out=o_f[b][:, h, :], in0=o_f[b][:, h, :], scalar1=bnag[b][:, h, 0:1], scalar2=s1[:, 24 + h:25 + h],
                                                           op0=ALU.subtract, op1=ALU.mult), r=[r_of[b], r_bn[b], r_sm1[b]], w=[r_of[b]])
                ofl = o_f[b][:].rearrange("p h n -> p (h n)")
                sc.op("pool", lambda e: e.tensor_tensor(out=ofl, in0=ofl, in1=gn[:], op=ALU.mult), r=[r_of[b], r_const], w=[r_of[b]])
                sc.op("pool", lambda e: e.tensor_tensor(out=ro_b[b3][:], in0=ofl, in1=sg[b6][:], op=ALU.mult), r=[r_of[b], r_sg[b6]], w=[r_rob[b3]])
                r_routd = sc.res("rout_d%d" % t)
                sc.dma("pool", lambda e: e.dma_start(out=rout_d[ts_, :], in_=ro_b[b3][:]), r=[r_rob[b3]], w=[r_routd])
                if t == NT - 1:
                    dump("ro_last", ro_b[b3][:], r_rob[b3], [128, 512], BF16)
            run_pipelined(p1_tile, NT, 4)
            if do_pc:
                while pc_state['i'] < 96:
                    precast_step()
                precast_flush()
            dump("rstd1", rstd1[:], r_rstd1t[NT - 1], [128, NT])
            sc.barrier()
            p1s.close()

        if last_phase >= 2:
            p2s = es.enter_context(ExitStack())
            KTn = sb("KTn", [128, 4, S], BF16, p2s)
            KTr = sb("KTr", [128, 2, S], BF16, p2s)
            Vaug = sb("Vaug", [128, NT, 4, 129], BF16, p2s)
            r_KT = [sc.res("KT%d" % t) for t in range(NT)]
            r_Vt = [sc.res("Vaug%d" % t) for t in range(NT)]
            sc.op("pool", lambda e: e.memset(Vaug[:], 1.0), w=r_Vt)
            w_a = sb("w_a", [128, 8, 448], BF16, p2s)
            w_uq = sb("w_uq", [128, 2, 768], BF16, p2s)
            w_ukv = sb("w_ukv", [128, 1024], BF16, p2s)
            w_out = sb("w_out", [128, 8, 1024], BF16, p2s)
            r_w2 = sc.res("w2")
            g_attn2 = pload("g_attn2", g_attn_d, 8, p2s)
            g_qn = pload("g_qn", g_qn_d, 2, p2s)
            g_kvn = pload("g_kvn", g_kvn_d, 1, p2s)
            pw2 = es.enter_context(ExitStack())
            stage[:] = [sb("stage2_%d" % i, [128, 1024], F32, pw2) for i in range(2)]
            load_scaled(lambda c, a, b_: w_a[:, c, a:b_], lambda c, a, b_: w_in_d[:, c, a:b_], lambda c: g_attn2[:, c:c + 1], 8, 448, r_w2)
            load_scaled(lambda c, a, b_: w_uq[:, c, a:b_], lambda c, a, b_: w_uq_d[:, c, a:b_], lambda c: g_qn[:, c:c + 1], 2, 768, r_w2)
            load_scaled(lambda c, a, b_: w_ukv[:, a:b_], lambda c, a, b_: w_ukv_d[:, a:b_], lambda c: g_kvn[:, 0:1], 1, 1024, r_w2)
            load_cast(lambda c: w_out[:, c, :], lambda c: w_out_d[:, c, :], 8, r_w2)
            sc.barrier()
            pw2.close()

            xt = [sb("x2t%d" % i, [128, 1024], F32, p2s) for i in range(2)]
            xb = [sb("x2b%d" % i, [128, 1024], BF16, p2s) for i in range(2)]
            xT = [sb("x2T%d" % i, [128, 8, 128], BF16, p2s) for i in range(2)]
            junk = sb("junk2", [128, 768], F32, p2s)
            junkA = sb("junk2a", [128, 256], BF16, p2s)
            r_junkA = sc.res("junk2a")
            cq_f = [sb("cq_f%d" % i, [128, 256], F32, p2s) for i in range(3)]
            cq_b = [sb("cq_b%d" % i, [128, 256], BF16, p2s) for i in range(3)]
            cqT = [sb("cqT%d" % i, [128, 2, 128], BF16, p2s) for i in range(3)]
            ckv_f = [sb("ckv_f%d" % i, [128, 192], F32, p2s) for i in range(3)]
            ckv_b = [sb("ckv_b%d" % i, [128, 128], BF16, p2s) for i in range(3)]
            ckvT = [sb("ckvT%d" % i, [128, 128], BF16, p2s) for i in range(3)]
            kn_f = [sb("kn_f0", [128, 4, 128], F32, p2s)] * 3
            kn_b = [sb("kn_b%d" % i, [128, 4, 128], BF16, p2s) for i in range(2)]
            kpe = [sb("kpe0", [128, 3, 64], F32, p2s)] * 3
            kr = [sb("kr%d" % i, [128, 64], F32, p2s) for i in range(3)]
            krn_b = [sb("krn_b%d" % i, [128, 4, 64], BF16, p2s) for i in range(3)]
            q_f = [sb("q_f%d" % i, [128, 4, 192], F32, p2s) for i in range(2)]
            qn_b = [sb("qn_b%d" % i, [128, 4, 128], BF16, p2s) for i in range(2)]
            qr = [sb("qr0", [128, 4, 4, 64], F32, p2s)] * 3
            qrn_b = [sb("qrn_b%d" % i, [128, 4, 64], BF16, p2s) for i in range(3)]
            sm2 = [sb("sm2_%d" % i, [128, 48], F32, p2s) for i in range(3)]
            QTn = [sb("QTn%d" % i, [128, 4, 512], BF16, p2s) for i in range(2)]
            QTr = [sb("QTr%d" % i, [128, 2, 512], BF16, p2s) for i in range(2)]
            PTt = [sb("PTt%d" % i, [128, 512], BF16, p2s) for i in range(3)]
            a_b = [sb("a_b%d" % i, [128, 4, 512], BF16, p2s) for i in range(2)]
            rcp = [sb("rcp%d" % i, [128, 4], F32, p2s) for i in range(2)]
            r_b = [sb("r_b%d" % i, [128, 512], BF16, p2s) for i in range(2)]
            aoT = [sb("aoT%d" % i, [128, 8, 128], BF16, p2s) for i in range(2)]

            r_xt, r_xb, r_xT = mkres("x2t"), mkres("x2b"), mkres("x2T")
            r_junk = sc.res("junk2")
            r_cq, r_cqb, r_cqT, r_ckv, r_ckvb, r_ckvT = mkres("cq", 3), mkres("cqb", 3), mkres("cqT", 3), mkres("ckv", 3), mkres("ckvb", 3), mkres("ckvT", 3)
            r_kn, r_knb, r_kpe, r_kr, r_krn = mkres("kn", 1) * 3, mkres("knb"), mkres("kpe", 1) * 3, mkres("kr", 3), mkres("krn", 3)
            r_qf, r_qnb, r_qr, r_qrn, r_sm2 = mkres("qf"), mkres("qnb"), mkres("qr", 1) * 3, mkres("qrn", 3), mkres("sm2", 3)
            r_QT = mkres("QT")
            r_PTt = mkres("PTt", 3)
            r_ab, r_rcp, r_rb, r_aoT = mkres("ab"), mkres("rcp"), mkres("rb"), mkres("aoT")
            SCALE = 192.0 ** -0.5

            def prep_tile(t, qb):
                b = t % 3
                bx = t % 2
                ts_ = slice(t * 128, (t + 1) * 128)
                tl = slice((t % 4) * 128, (t % 4 + 1) * 128)
                s2 = sm2[b]
                rs1 = rstd1[:, t:t + 1]
                sc.dma("sp", lambda e: e.dma_start(out=xt[0][:], in_=x_d[ts_, :]), w=[r_xt[0]])
                sc.op("dve", lambda e: e.tensor_copy(out=xb[bx][:], in_=xt[0][:]), r=[r_xt[0]], w=[r_xb[bx]])
                yield
                bk, rb = nbank(4, 8)
                pv = bk[:].bitcast(BF16).rearrange("p (c n) -> p c n", n=128)
                sc.pe([(lambda e, c=c: e.transpose(out=pv[:, c, :], in_=xb[bx][:, c * 128:(c + 1) * 128], identity=ident_bf[:])) for c in range(8)],
                      r=[r_xb[bx], r_const], w=[rb])
                sc.op("dve", lambda e: e.tensor_copy(out=xT[bx][:], in_=pv), r=[rb], w=[r_xT[bx]])
                yield
                bk, rb = nbank(4, 8)
                sc.pe([(lambda e, c=c: e.matmul(bk[:, 0:448], lhsT=xT[bx][:, c, :], rhs=w_a[:, c, :], start=(c == 0), stop=(c == 7))) for c in range(8)],
                      r=[r_xT[bx], r_w2], w=[rb])
                sc.op("act", lambda e: e.activation(out=cq_f[b][:], in_=bk[:, 0:256], func=AF.Copy, scale=rs1), r=[rb, r_rstd1t[t]], w=[r_cq[b]])
                sc.op("act", lambda e: e.activation(out=ckv_f[b][:], in_=bk[:, 256:448], func=AF.Copy, scale=rs1), r=[rb, r_rstd1t[t]], w=[r_ckv[b]])
                yield
                sc.op("act", lambda e: e.activation(out=junkA[:, 0:128], in_=ckv_f[b][:, 0:128], func=AF.Square, accum_out=s2[:, 2:3]), r=[r_ckv[b]], w=[r_junkA, r_sm2[b]])
                rstd_from_ss(s2[:, 2:3], 128.0, s2[:, 3:4], r_sm2[b], r_sm2[b], s2[:, 4:5], r_sm2[b])
                sc.op("act", lambda e: e.activation(out=junkA[:, 128:192], in_=ckv_f[b][:, 128:192], func=AF.Square, accum_out=s2[:, 5:6]), r=[r_ckv[b]], w=[r_junkA, r_sm2[b]])
                sc.op("pool", lambda e: e.tensor_copy(out=ckv_b[b][:], in_=ckv_f[b][:, 0:128]), r=[r_ckv[b]], w=[r_ckvb[b]])
                bk, rb = nbank(4, 8)
                pv = bk[:].bitcast(BF16)
                sc.pe([lambda e: e.transpose(out=pv[:, 0:128], in_=ckv_b[b][:], identity=ident_bf[:])], r=[r_ckvb[b], r_const], w=[rb])
                sc.op("act", lambda e: e.copy(out=ckvT[b][:], in_=pv[:, 0:128]), r=[rb], w=[r_ckvT[b]])
                yield
                for hh in range(2):
                    bk, rb = nbank(4, 8)
                    sc.pe([lambda e: e.matmul(bk[:], lhsT=ckvT[b][:], rhs=w_ukv[:, hh * 512:(hh + 1) * 512], start=True, stop=True)],
                          r=[r_ckvT[b], r_w2], w=[rb])
                    kvv = bk[:].rearrange("p (h two d) -> p h two d", h=2, two=2)
                    sc.op("dve", lambda e: e.tensor_scalar(out=kn_f[b][:, 2 * hh:2 * hh + 2, :], in0=kvv[:, :, 0, :], scalar1=s2[:, 3:4], scalar2=None,
                                                           op0=ALU.mult), r=[rb, r_sm2[b]], w=[r_kn[b]])
                    sc.op("act", lambda e: e.activation(out=Vaug[:, t, 2 * hh:2 * hh + 2, 0:128], in_=kvv[:, :, 1, :], func=AF.Copy, scale=s2[:, 3:4]),
                          r=[rb, r_sm2[b]], w=[r_Vt[t]])
                yield
                jk = junk[:, 0:512].rearrange("p (h d) -> p h d", h=4)
                sc.op("dve", lambda e: e.tensor_tensor(out=jk, in0=kn_f[b][:], in1=kn_f[b][:], op=ALU.mult), r=[r_kn[b]], w=[r_junk])
                sc.op("dve", lambda e: e.tensor_reduce(out=s2[:, 8:12], in_=jk, axis=AX.X, op=ALU.add), r=[r_junk], w=[r_sm2[b]])
                sc.op("dve", lambda e: e.tensor_scalar(out=s2[:, 8:12], in0=s2[:, 8:12], scalar1=s2[:, 5:6], scalar2=None, op0=ALU.add),
                      r=[r_sm2[b]], w=[r_sm2[b]])
                rstd_from_ss(s2[:, 8:12], 192.0, s2[:, 12:16], r_sm2[b], r_sm2[b], s2[:, 16:20], r_sm2[b])
                for h in range(4):
                    sc.op("dve", lambda e: e.scalar_tensor_tensor(out=kn_b[bx][:, h, :], in0=kn_f[b][:, h, :], scalar=s2[:, 12 + h:13 + h], in1=gk[:, 0:128],
                                                                  op0=ALU.mult, op1=ALU.mult), r=[r_kn[b], r_sm2[b], r_const], w=[r_knb[bx]])
                yield
                kp = kpe[b]
                sc.op("pool", lambda e: e.tensor_tensor(out=kp[:, 0, :], in0=ckv_f[b][:, 128:192], in1=gk[:, 128:192], op=ALU.mult),
                      r=[r_ckv[b], r_const], w=[r_kpe[b]])
                cosM2 = SCm[:, t, 32:64].unsqueeze(1).broadcast_to([128, 2, 32])
                sinM2 = SCm[:, t, 0:32].unsqueeze(1).broadcast_to([128, 2, 32])
                k0 = kp[:, 0, :].rearrange("p (two d) -> p two d", two=2)
                kA = kp[:, 1, :].rearrange("p (two d) -> p two d", two=2)
                kB = kp[:, 2, :].rearrange("p (two d) -> p two d", two=2)
                sc.op("pool", lambda e: e.tensor_tensor(out=kA, in0=k0, in1=cosM2, op=ALU.mult), r=[r_kpe[b], r_sct[t]], w=[r_kpe[b]])
                sc.op("pool", lambda e: e.tensor_tensor(out=kB, in0=k0, in1=sinM2, op=ALU.mult), r=[r_kpe[b], r_sct[t]], w=[r_kpe[b]])
                sc.op("pool", lambda e: e.tensor_tensor(out=kr[b][:, 0:32], in0=kp[:, 1, 0:32], in1=kp[:, 2, 32:64], op=ALU.subtract),
                      r=[r_kpe[b]], w=[r_kr[b]])
                sc.op("pool", lambda e: e.tensor_tensor(out=kr[b][:, 32:64], in0=kp[:, 1, 32:64], in1=kp[:, 2, 0:32], op=ALU.add),
                      r=[r_kpe[b]], w=[r_kr[b]])
                for h in range(4):
                    sc.op("dve", lambda e: e.tensor_scalar(out=krn_b[b][:, h, :], in0=kr[b][:], scalar1=s2[:, 12 + h:13 + h], scalar2=None, op0=ALU.mult),
                          r=[r_kr[b], r_sm2[b]], w=[r_krn[b]])
                bk, rb = nbank(4, 8)
                pv = bk[:].bitcast(BF16).rearrange("p (c n) -> p c n", n=128)
                sc.pe([(lambda e, h=h: e.transpose(out=pv[:, h, :], in_=kn_b[bx][:, h, :], identity=ident_bf[:])) for h in range(4)]
                      + [(lambda e, pr=pr: e.transpose(out=pv[:, 4 + pr, :], in_=krn_b[b][:, 2 * pr:2 * pr + 2, :].rearrange("p a d -> p (a d)"),
                                                       identity=ident_bf[:])) for pr in range(2)],
                      r=[r_knb[bx], r_krn[b], r_const], w=[rb])
                sc.op("act", lambda e: e.copy(out=KTn[:, :, ts_], in_=pv[:, 0:4, :]), r=[rb], w=[r_KT[t]])
                sc.op("dve", lambda e: e.tensor_copy(out=KTr[:, :, ts_], in_=pv[:, 4:6, :]), r=[rb], w=[r_KT[t]])
                yield
                sc.op("act", lambda e: e.activation(out=junkA[:, 0:256], in_=cq_f[b][:], func=AF.Square, accum_out=s2[:, 20:21]), r=[r_cq[b]], w=[r_junkA, r_sm2[b]])
                rstd_from_ss(s2[:, 20:21], 256.0, s2[:, 21:22], r_sm2[b], r_sm2[b], s2[:, 22:23], r_sm2[b])
                sc.op("pool", lambda e: e.tensor_copy(out=cq_b[b][:], in_=cq_f[b][:]), r=[r_cq[b]], w=[r_cqb[b]])
                bk, rb = nbank(4, 8)
                pv = bk[:].bitcast(BF16).rearrange("p (c n) -> p c n", n=128)
                sc.pe([(lambda e, c=c: e.transpose(out=pv[:, c, :], in_=cq_b[b][:, c * 128:(c + 1) * 128], identity=ident_bf[:])) for c in range(2)],
                      r=[r_cqb[b], r_const], w=[rb])
                sc.op("act", lambda e: e.copy(out=cqT[b][:], in_=pv[:, 0:2, :]), r=[rb], w=[r_cqT[b]])
                yield
                for hh in range(2):
                    bk, rb = nbank(4, 8)
                    sc.pe([(lambda e, c=c: e.matmul(bk[:, 0:384], lhsT=cqT[b][:, c, :], rhs=w_uq[:, c, hh * 384:(hh + 1) * 384], start=(c == 0), stop=(c == 1)))
                           for c in range(2)], r=[r_cqT[b], r_w2], w=[rb])
                    sc.op("act", lambda e: e.activation(out=q_f[bx][:, 2 * hh:2 * hh + 2, :], in_=bk[:, 0:384].rearrange("p (h d) -> p h d", h=2),
                                                        func=AF.Copy, scale=s2[:, 21:22]), r=[rb, r_sm2[b]], w=[r_qf[bx]])
                yield
                jq = junk[:, 0:768].rearrange("p (h d) -> p h d", h=4)
                sc.op("dve", lambda e: e.tensor_tensor(out=jq, in0=q_f[bx][:], in1=q_f[bx][:], op=ALU.mult), r=[r_qf[bx]], w=[r_junk])
                sc.op("dve", lambda e: e.tensor_reduce(out=s2[:, 24:28], in_=jq, axis=AX.X, op=ALU.add), r=[r_junk], w=[r_sm2[b]])
                rstd_from_ss(s2[:, 24:28], 192.0, s2[:, 28:32], r_sm2[b], r_sm2[b], s2[:, 32:36], r_sm2[b])
                for h in range(4):
                    sc.op("dve", lambda e: e.scalar_tensor_tensor(out=qn_b[bx][:, h, :], in0=q_f[bx][:, h, 0:128], scalar=s2[:, 28 + h:29 + h], in1=gq[:, 0:128],
                                                                  op0=ALU.mult, op1=ALU.mult), r=[r_qf[bx], r_sm2[b], r_const], w=[r_qnb[bx]])
                yield
                qq = qr[b]
                gqB = gq[:, 128:192].unsqueeze(1).broadcast_to([128, 4, 64])
                sc.op("pool", lambda e: e.tensor_tensor(out=qq[:, 0, :, :], in0=q_f[bx][:, :, 128:192], in1=gqB, op=ALU.mult), r=[r_qf[bx], r_const], w=[r_qr[b]])
                cosM8 = SCm[:, t, 32:64].unsqueeze(1).broadcast_to([128, 8, 32])
                sinM8 = SCm[:, t, 0:32].unsqueeze(1).broadcast_to([128, 8, 32])
                q0 = qq[:, 0, :, :].rearrange("p h (two d) -> p (h two) d", two=2)
                qA = qq[:, 1, :, :].rearrange("p h (two d) -> p (h two) d", two=2)
                qB = qq[:, 2, :, :].rearrange("p h (two d) -> p (h two) d", two=2)
                sc.op("pool", lambda e: e.tensor_tensor(out=qA, in0=q0, in1=cosM8, op=ALU.mult), r=[r_qr[b], r_sct[t]], w=[r_qr[b]])
                sc.op("pool", lambda e: e.tensor_tensor(out=qB, in0=q0, in1=sinM8, op=ALU.mult), r=[r_qr[b], r_sct[t]], w=[r_qr[b]])
                sc.op("pool", lambda e: e.tensor_tensor(out=qq[:, 3, :, 0:32], in0=qq[:, 1, :, 0:32], in1=qq[:, 2, :, 32:64], op=ALU.subtract),
                      r=[r_qr[b]], w=[r_qr[b]])
                sc.op("pool", lambda e: e.tensor_tensor(out=qq[:, 3, :, 32:64], in0=qq[:, 1, :, 32:64], in1=qq[:, 2, :, 0:32], op=ALU.add),
                      r=[r_qr[b]], w=[r_qr[b]])
                yield
                rsB = s2[:, 28:32].unsqueeze(2).broadcast_to([128, 4, 64])
                sc.op("dve", lambda e: e.tensor_tensor(out=qrn_b[b][:], in0=qq[:, 3, :, :], in1=rsB, op=ALU.mult), r=[r_qr[b], r_sm2[b]], w=[r_qrn[b]])
                bk, rb = nbank(4, 8)
                pv = bk[:].bitcast(BF16).rearrange("p (c n) -> p c n", n=128)
                sc.pe([(lambda e, h=h: e.transpose(out=pv[:, h, :], in_=qn_b[bx][:, h, :], identity=ident_bf[:])) for h in range(4)]
                      + [(lambda e, pr=pr: e.transpose(out=pv[:, 4 + pr, :], in_=qrn_b[b][:, 2 * pr:2 * pr + 2, :].rearrange("p a d -> p (a d)"),
                                                       identity=ident_bf[:])) for pr in range(2)],
                      r=[r_qnb[bx], r_qrn[b], r_const], w=[rb])
                sc.op("act", lambda e: e.copy(out=QTn[qb][:, :, tl], in_=pv[:, 0:4, :]), r=[rb], w=[r_QT[qb]])
                sc.op("dve", lambda e: e.tensor_copy(out=QTr[qb][:, :, tl], in_=pv[:, 4:6, :]), r=[rb], w=[r_QT[qb]])

            def attention_block(i, qb):
                ab = a_b[i % 2]
                its = [(h, j) for h in range(4) for j in range(4 * i + 4)]

                def qk(h, j):
                    pair, hp = h // 2, h % 2
                    psl = slice(hp * 64, (hp + 1) * 64)
                    r0 = max(0, j - 4 * i)
                    n = 512 - r0 * 128
                    ks = slice(j * 128, (j + 1) * 128)
                    bk, rb = nbank(4, 8)
                    diag = j >= 4 * i
                    fq = [lambda e: e.matmul(bk[:, 0:n], lhsT=KTn[:, h, ks], rhs=QTn[qb][:, h, r0 * 128:512], start=True, stop=False),
                          lambda e: e.matmul(bk[:, 0:n], lhsT=KTr[psl, pair, ks], rhs=QTr[qb][psl, pair, r0 * 128:512], start=False, stop=not diag)]
                    if diag:
                        fq.append(lambda e: e.matmul(bk[:, 0:128], lhsT=ident_bf[:], rhs=negm[:], start=False, stop=True))
                    sc.pe(fq, r=[r_KT[j], r_QT[qb], r_const], w=[rb])
                    pi = ptc["i"] % 3
                    ptc["i"] += 1
                    pt, rpt = PTt[pi], r_PTt[pi]
                    sc.op("act", lambda e: e.activation(out=pt[:, 0:n], in_=bk[:, 0:n], func=AF.Exp, scale=SCALE), r=[rb], w=[rpt])
                    return pt, rpt, r0

                def pv_(h, j, pt, rpt, r0):
                    fns = []
                    for s in range(r0, 4):
                        fns.append(lambda e, s=s: e.matmul(banks[s][:, 0:129], lhsT=pt[:, (s - r0) * 128:(s - r0 + 1) * 128], rhs=Vaug[:, j, h, :],
                                                           start=(j == 0), stop=(j == 4 * i + s)))
                    sc.pe(fns, r=[rpt, r_Vt[j], r_KT[j]], w=[bank_res[s] for s in range(r0, 4)])
                    if j >= 4 * i:
                        s = j - 4 * i
                        sc.op("dve", lambda e: e.reciprocal(out=rcp[i % 2][:, s:s + 1], in_=banks[s][:, 128:129]), r=[bank_res[s]], w=[r_rcp[i % 2]])
                        sc.op("dve", lambda e: e.tensor_scalar(out=ab[:, s, h * 128:(h + 1) * 128], in0=banks[s][:, 0:128], scalar1=rcp[i % 2][:, s:s + 1],
                                                               scalar2=None, op0=ALU.mult), r=[bank_res[s], r_rcp[i % 2]], w=[r_ab[i % 2]])

                pend = []
                ptc = {"i": 0}
                ystep = max(1, len(its) // 30)
                for k_, (h, j) in enumerate(its):
                    pend.append((h, j) + qk(h, j))
                    if len(pend) > 2:
                        pv_(*pend.pop(0))
                    if k_ % ystep == ystep - 1:
                        yield
                while pend:
                    pv_(*pend.pop(0))

            def out_tile(t):
                i, s = t // 4, t % 4
                ab = a_b[i % 2]
                if True:
                    b = t % 2
                    ts_ = slice(t * 128, (t + 1) * 128)
                    sc.dma("sp", lambda e: e.dma_start(out=r_b[b][:], in_=rout_d[ts_, :]), w=[r_rb[b]])
                    sc.dma("sp", lambda e: e.dma_start(out=x1t[1][:], in_=x_d[ts_, :]), w=[r_x1t[1]])
                    yield
                    bk, rb = nbank(4, 8)
                    pv = bk[:].bitcast(BF16).rearrange("p (c n) -> p c n", n=128)
                    sc.pe([(lambda e, c=c: e.transpose(out=pv[:, c, :], in_=ab[:, s, c * 128:(c + 1) * 128], identity=ident_bf[:])) for c in range(4)]
                          + [(lambda e, c=c: e.transpose(out=pv[:, 4 + c, :], in_=r_b[b][:, c * 128:(c + 1) * 128], identity=ident_bf[:])) for c in range(4)],
                          r=[r_ab[i % 2], r_rb[b], r_const], w=[rb])
                    sc.op("dve", lambda e: e.tensor_copy(out=aoT[b][:], in_=pv), r=[rb], w=[r_aoT[b]])
                    yield
                    for hh in range(2):
                        bk, rb = nbank(4, 8)
                        sc.pe([(lambda e, c=c: e.matmul(bk[:], lhsT=aoT[b][:, c, :], rhs=w_out[:, c, hh * 512:(hh + 1) * 512], start=(c == 0), stop=(c == 7)))
                               for c in range(8)], r=[r_aoT[b], r_w2], w=[rb])
                        sc.op("dve", lambda e: e.tensor_tensor(out=x1t[1][:, hh * 512:(hh + 1) * 512], in0=bk[:], in1=x1t[1][:, hh * 512:(hh + 1) * 512], op=ALU.add),
                              r=[rb], w=[r_x1t[1]])
                    r_x1d = sc.res("x1d")
                    sc.dma("act", lambda e: e.dma_start(out=x1_d[ts_, :], in_=x1t[1][:]), r=[r_x1t[1]], w=[r_x1d])

            def drive(gens):
                gens = list(gens)
                while gens:
                    for g in list(gens):
                        try:
                            next(g)
                        except StopIteration:
                            gens.remove(g)

            x1t = xt
            r_x1t = r_xt
            run_pipelined(lambda t: prep_tile(t, 0), 4, 3)
            for i in range(NB):
                qb = i % 2

                def side(i=i):
                    if i + 1 < NB:
                        act_ = []
                        nx = 4 * (i + 1)
                        while nx < 4 * (i + 2) or act_:
                            if nx < 4 * (i + 2) and len(act_) < 3:
                                act_.append(prep_tile(nx, (i + 1) % 2))
                                nx += 1
                            for g in list(act_):
                                try:
                                    next(g)
                                except StopIteration:
                                    act_.remove(g)
                            yield

                def outs(i=i):
                    if i == 0:
                        return
                    act_ = []
                    nx = 4 * (i - 1)
                    while nx < 4 * i or act_:
                        if nx < 4 * i and len(act_) < 1:
                            act_.append(out_tile(nx))
                            nx += 1
                        for g in list(act_):
                            try:
                                next(g)
                            except StopIteration:
                                act_.remove(g)
                        yield

                drive([attention_block(i, qb), outs(), side()])
            run_pipelined(lambda k_: out_tile(4 * (NB - 1) + k_), 4, 1)
            if "x1" in dbg:
                sc.barrier()
                t_ = nc.dram_tensor("dbg_x1", [S, D], F32, kind="ExternalOutput").ap()
                dbg_out["x1"] = t_
                sc.dma("sp", lambda e: e.dma_start(out=t_, in_=x1_d), is_out=True)
            dump("KTn", KTn[:], r_KT[NT - 1], [128, 4, S], BF16)
            dump("KTr", KTr[:], r_KT[NT - 1], [128, 2, S], BF16)
            dump("Vaug", Vaug[:], r_Vt[NT - 1], [128, NT, 4, 129], BF16)
            sc.barrier()
            p2s.close()

        if last_phase >= 3:
            p36 = es.enter_context(ExitStack())
            lg_all = sb("lg_all", [128, NT, 36], F32, p36)
            r_lg = sc.res("lg_all")
            pos12 = sb("pos12", [128, 2, NT], I32, p36)
            w12 = sb("w12", [128, 2, NT], F32, p36)
            widx = sb("widx", [128, NTS], I32, p36)
            r_route = sc.res("route")
            b_rt = bload("b_rt", b_rt_d, 36, p36)

            p3s = es.enter_context(ExitStack())
            cw_q = sb("cw_q", [128, 8, 1024], BF16, p3s)
            cw_o = sb("cw_o", [128, 8, 1024], BF16, p3s)
            KcT = sb("KcT", [128, 4, 2, 256], BF16, p3s)
            Vc = sb("Vc", [128, 2, 4, 257], BF16, p3s)
            mg = bload("mg", mg_d, 1024, p3s)
            cqg = bload("cqg", cqg_d, 256, p3s)
            ckg = bload("ckg", ckg_d, 256, p3s)
            stage[:] = [sb("stage3_%d" % i, [128, 1024], F32, p3s) for i in range(2)]
            g_cross = pload("g_cross", g_cross_d, 8, p3s)
            g_mem = pload("g_mem", g_mem_d, 8, p3s)
            w_rt = sb("w_rt", [128, 8, 36], F32, p3s)
            r_w3 = sc.res("w3")
            sc.dma("sp", lambda e: e.dma_start(out=w_rt[:], in_=w_rt_d[:, :, :]), w=[r_w3])
            load_scaled(lambda c, a, b_: cw_q[:, c, a:b_], lambda c, a, b_: cw_q_d[:, c, a:b_], lambda c: g_cross[:, c:c + 1], 8, 1024, r_w3)
            load_cast(lambda c: cw_o[:, c, :], lambda c: cw_o_d[:, c, :], 8, r_w3)
            r_kvc = sc.res("kvc")
            sc.op("pool", lambda e: e.memset(Vc[:], 1.0), w=[r_kvc])

            pm = es.enter_context(ExitStack())
            cw_kv = sb("cw_kv", [128, 8, 2048], BF16, pm)
            r_cwkv = sc.res("cw_kv")
            load_scaled(lambda c, a, b_: cw_kv[:, c, a:b_], lambda c, a, b_: cw_kv_d[:, c, a:b_], lambda c: g_mem[:, c:c + 1], 8, 2048, r_cwkv)
            m_f = sb("m_f", [128, 1024], F32, pm)
            m_b = sb("m_b", [128, 1024], BF16, pm)
            m_T = sb("m_T", [128, 8, 128], BF16, pm)
            kc_f = sb("kc_f", [128, 4, 256], F32, pm)
            kc_sq = sb("kc_sq", [128, 4, 256], F32, pm)
            kc_b = sb("kc_b", [128, 4, 256], BF16, pm)
            sm = sb("sm0", [128, 16], F32, pm)
            r_m = sc.res("m")
            r_sm = sc.res("sm0")
            for mt in range(2):
                sc.dma("sp", lambda e: e.dma_start(out=m_f[:], in_=mem_d[mt * 128:(mt + 1) * 128, :]), w=[r_m])
                sc.op("act", lambda e: e.activation(out=m_b[:], in_=m_f[:], func=AF.Square, accum_out=sm[:, 0:1]), r=[r_m], w=[r_m, r_sm])
                rstd_from_ss(sm[:, 0:1], 1024.0, sm[:, 1:2], r_sm, r_sm, sm[:, 2:3], r_sm)
                sc.op("pool", lambda e: e.tensor_copy(out=m_b[:], in_=m_f[:]), r=[r_m], w=[r_m])
                bk, rb = nbank()
                pv = bk[:].bitcast(BF16).rearrange("p (c n) -> p c n", n=128)
                sc.pe([(lambda e, c=c: e.transpose(out=pv[:, c, :], in_=m_b[:, c * 128:(c + 1) * 128], identity=ident_bf[:])) for c in range(8)],
                      r=[r_m, r_const], w=[rb])
                sc.op("dve", lambda e: e.tensor_copy(out=m_T[:], in_=pv), r=[rb], w=[r_m])
                for nchunk in range(4):
                    bk, rb = nbank()
                    sc.pe([(lambda e, c=c: e.matmul(bk[:], lhsT=m_T[:, c, :], rhs=cw_kv[:, c, nchunk * 512:(nchunk + 1) * 512],
                                                    start=(c == 0), stop=(c == 7))) for c in range(8)], r=[r_m, r_cwkv], w=[rb])
                    if nchunk < 2:
                        sc.op("act", lambda e: e.activation(out=kc_f[:, 2 * nchunk:2 * nchunk + 2, :], in_=bk[:].rearrange("p (h d) -> p h d", h=2),
                                                            func=AF.Copy, scale=sm[:, 1:2]), r=[rb, r_sm], w=[r_m])
                    else:
                        hh = 2 * (nchunk - 2)
                        sc.op("act", lambda e: e.activation(out=Vc[:, mt, hh:hh + 2, 0:256], in_=bk[:].rearrange("p (h d) -> p h d", h=2),
                                                            func=AF.Copy, scale=sm[:, 1:2]), r=[rb, r_sm], w=[r_kvc])
                sc.op("dve", lambda e: e.tensor_tensor(out=kc_sq[:], in0=kc_f[:], in1=kc_f[:], op=ALU.mult), r=[r_m], w=[r_m])
                sc.op("dve", lambda e: e.tensor_reduce(out=sm[:, 4:8], in_=kc_sq[:], axis=AX.X, op=ALU.add), r=[r_m], w=[r_sm])
                rstd_from_ss(sm[:, 4:8], 256.0, sm[:, 8:12], r_sm, r_sm, sm[:, 12:16], r_sm)
                for h in range(4):
                    sc.op("dve", lambda e: e.scalar_tensor_tensor(out=kc_b[:, h, :], in0=kc_f[:, h, :], scalar=sm[:, 8 + h:9 + h], in1=ckg[:],
                                                                  op0=ALU.mult, op1=ALU.mult), r=[r_m, r_sm, r_const], w=[r_m])
                bk, rb = nbank()
                pv = bk[:].bitcast(BF16).rearrange("p (h c n) -> p h c n", h=4, c=2)
                sc.pe([(lambda e, h=h, c=c: e.transpose(out=pv[:, h, c, :], in_=kc_b[:, h, c * 128:(c + 1) * 128], identity=ident_bf[:]))
                       for h in range(4) for c in range(2)], r=[r_m, r_const], w=[rb])
                sc.op("dve", lambda e: e.tensor_copy(out=KcT[:, :, :, mt * 128:(mt + 1) * 128], in_=pv), r=[rb], w=[r_kvc])
            dump("KcT", KcT[:], r_kvc, [128, 4, 2, 256], BF16)
            dump("Vc", Vc[:], r_kvc, [128, 2, 4, 257], BF16)
            sc.barrier()
            pm.close()

            x1t = [sb("x3t%d" % i, [128, 1024], F32, p3s) for i in range(5)]
            xb = [sb("x3b%d" % i, [128, 1024], BF16, p3s) for i in range(5)]
            hcT = [sb("hcT%d" % i, [128, 8, 128], BF16, p3s) for i in range(5)]
            junk = sb("junk3", [128, 1024], F32, p3s)
            junkA = sb("junk3a", [128, 1024], BF16, p3s)
            r_junkA = sc.res("junk3a")
            qc_f = sb("qc_f", [128, 4, 256], F32, p3s)
            qc_b = sb("qc_b", [128, 4, 256], BF16, p3s)
            qcT = [sb("qcT%d" % i, [128, 4, 2, 128], BF16, p3s) for i in range(5)]
            PTc = [sb("PTc%d" % i, [128, 8, 128], BF16, p3s) for i in range(5)]
            oc_b = sb("oc_b", [128, 4, 256], BF16, p3s)
            ocT = [sb("ocT%d" % i, [128, 8, 128], BF16, p3s) for i in range(5)]
            hm_f = [sb("hm_f%d" % i, [128, 1024], F32, p3s) for i in range(5)]
            hm_b = [sb("hm_b%d" % i, [128, 1024], BF16, p3s) for i in range(5)]
            hmT_f = sb("hmT_f", [128, 8, 128], F32, p3s)
            sm3 = [sb("sm3_%d" % i, [128, 32], F32, p3s) for i in range(5)]
            r_x1t, r_xb, r_hcT = mkres("x3t", 5), mkres("x3b", 5), mkres("hcT", 5)
            r_junk = sc.res("junk3")
            r_qcf, r_qcb = sc.res("qcf"), sc.res("qcb")
            r_qcT, r_PTc, r_ocT, r_hmf, r_hmb, r_sm3 = mkres("qcT", 5), mkres("PTc", 5), mkres("ocT", 5), mkres("hmf", 5), mkres("hmb", 5), mkres("sm3", 5)
            r_ocb = sc.res("ocb")
            r_hmT = sc.res("hmT")
            r_x2d = [sc.res("x2d%d" % t) for t in range(NT)]
            r_hmd = [sc.res("hmd%d" % t) for t in range(NT)]
            CSCALE = 256.0 ** -0.5

            def p3_tile(t):
                b = t % 5
                b3 = t % 5
                ts_ = slice(t * 128, (t + 1) * 128)
                s3 = sm3[b3]
                sc.dma("sp", lambda e: e.dma_start(out=x1t[b3][:], in_=x1_d[ts_, :]), w=[r_x1t[b3]])
                sc.op("act", lambda e: e.activation(out=junkA[:], in_=x1t[b3][:], func=AF.Square, accum_out=s3[:, 0:1]), r=[r_x1t[b3]], w=[r_junkA, r_sm3[b3]])
                rstd_from_ss(s3[:, 0:1], 1024.0, s3[:, 1:2], r_sm3[b3], r_sm3[b3], s3[:, 2:3], r_sm3[b3])
                sc.op("act", lambda e: e.copy(out=xb[b][:], in_=x1t[b3][:]), r=[r_x1t[b3]], w=[r_xb[b]])
                yield
                bk, rb = nbank()
                pv = bk[:].bitcast(BF16).rearrange("p (c n) -> p c n", n=128)
                sc.pe([(lambda e, c=c: e.transpose(out=pv[:, c, :], in_=xb[b][:, c * 128:(c + 1) * 128], identity=ident_bf[:])) for c in range(8)],
                      r=[r_xb[b], r_const], w=[rb])
                sc.op("dve", lambda e: e.tensor_copy(out=hcT[b][:], in_=pv), r=[rb], w=[r_hcT[b]])
                yield
                for hh in range(2):
                    bk, rb = nbank()
                    sc.pe([(lambda e, c=c: e.matmul(bk[:], lhsT=hcT[b][:, c, :], rhs=cw_q[:, c, hh * 512:(hh + 1) * 512], start=(c == 0), stop=(c == 7)))
                           for c in range(8)], r=[r_hcT[b], r_w3], w=[rb])
                    sc.op("act", lambda e: e.activation(out=qc_f[:, 2 * hh:2 * hh + 2, :], in_=bk[:].rearrange("p (h d) -> p h d", h=2), func=AF.Copy,
                                                        scale=s3[:, 1:2]), r=[rb, r_sm3[b3]], w=[r_qcf])
                yield
                jq = junk[:].rearrange("p (h d) -> p h d", h=4)
                sc.op("dve", lambda e: e.tensor_tensor(out=jq, in0=qc_f[:], in1=qc_f[:], op=ALU.mult), r=[r_qcf], w=[r_junk])
                sc.op("dve", lambda e: e.tensor_reduce(out=s3[:, 4:8], in_=jq, axis=AX.X, op=ALU.add), r=[r_junk], w=[r_sm3[b3]])
                rstd_from_ss(s3[:, 4:8], 256.0, s3[:, 8:12], r_sm3[b3], r_sm3[b3], s3[:, 12:16], r_sm3[b3])
                for h in range(4):
                    sc.op("dve", lambda e: e.scalar_tensor_tensor(out=qc_b[:, h, :], in0=qc_f[:, h, :], scalar=s3[:, 8 + h:9 + h], in1=cqg[:],
                                                                  op0=ALU.mult, op1=ALU.mult), r=[r_qcf, r_sm3[b3], r_const], w=[r_qcb])
                yield
                bk, rb = nbank()
                pv = bk[:].bitcast(BF16).rearrange("p (h c n) -> p h c n", h=4, c=2)
                sc.pe([(lambda e, h=h, c=c: e.transpose(out=pv[:, h, c, :], in_=qc_b[:, h, c * 128:(c + 1) * 128], identity=ident_bf[:]))
                       for h in range(4) for c in range(2)], r=[r_qcb, r_const], w=[rb])
                sc.op("act", lambda e: e.copy(out=qcT[b][:], in_=pv), r=[rb], w=[r_qcT[b]])
                yield
                for hp in range(2):
                    bk, rb = nbank()
                    sv_ = bk[:].rearrange("p (a n) -> p a n", a=4)
                    fns = []
                    for hl in range(2):
                        h = 2 * hp + hl
                        for mc in range(2):
                            for dc in range(2):
                                fns.append(lambda e, h=h, mc=mc, dc=dc, hl=hl: e.matmul(sv_[:, hl * 2 + mc, :], lhsT=KcT[:, h, dc, mc * 128:(mc + 1) * 128],
                                                                                        rhs=qcT[b][:, h, dc, :], start=(dc == 0), stop=(dc == 1)))
                    sc.pe(fns, r=[r_kvc, r_qcT[b]], w=[rb])
                    sc.op("act", lambda e: e.activation(out=PTc[b][:, 4 * hp:4 * hp + 4, :], in_=sv_, func=AF.Exp, scale=CSCALE), r=[rb], w=[r_PTc[b]])
                yield
                for h in range(4):
                    bk, rb = nbank()
                    sc.pe([(lambda e, mc=mc: e.matmul(bk[:, 0:257], lhsT=PTc[b][:, 2 * h + mc, :], rhs=Vc[:, mc, h, :], start=(mc == 0), stop=(mc == 1)))
                           for mc in range(2)], r=[r_PTc[b], r_kvc], w=[rb])
                    sc.op("dve", lambda e: e.reciprocal(out=s3[:, 16 + h:17 + h], in_=bk[:, 256:257]), r=[rb], w=[r_sm3[b3]])
                    sc.op("dve", lambda e: e.tensor_scalar(out=oc_b[:, h, :], in0=bk[:, 0:256], scalar1=s3[:, 16 + h:17 + h], scalar2=None, op0=ALU.mult),
                          r=[rb, r_sm3[b3]], w=[r_ocb])
                yield
                bk, rb = nbank()
                pv = bk[:].bitcast(BF16).rearrange("p (c n) -> p c n", n=128)
                ocf = oc_b[:].rearrange("p h d -> p (h d)")
                sc.pe([(lambda e, c=c: e.transpose(out=pv[:, c, :], in_=ocf[:, c * 128:(c + 1) * 128], identity=ident_bf[:])) for c in range(8)],
                      r=[r_ocb, r_const], w=[rb])
                sc.op("act", lambda e: e.copy(out=ocT[b][:], in_=pv), r=[rb], w=[r_ocT[b]])
                yield
                for hh in range(2):
                    bk, rb = nbank()
                    sc.pe([(lambda e, c=c: e.matmul(bk[:], lhsT=ocT[b][:, c, :], rhs=cw_o[:, c, hh * 512:(hh + 1) * 512], start=(c == 0), stop=(c == 7)))
                           for c in range(8)], r=[r_ocT[b], r_w3], w=[rb])
                    sc.op("dve", lambda e: e.tensor_tensor(out=x1t[b3][:, hh * 512:(hh + 1) * 512], in0=bk[:], in1=x1t[b3][:, hh * 512:(hh + 1) * 512], op=ALU.add),
                          r=[rb], w=[r_x1t[b3]])
                sc.dma("act", lambda e: e.dma_start(out=out_d[ts_, :], in_=x1t[b3][:]), r=[r_x1t[b3]], w=[r_x2d[t]])
                yield
                sc.op("act", lambda e: e.activation(out=junkA[:], in_=x1t[b3][:], func=AF.Square, accum_out=s3[:, 20:21]), r=[r_x1t[b3]], w=[r_junkA, r_sm3[b3]])
                rstd_from_ss(s3[:, 20:21], 1024.0, s3[:, 21:22], r_sm3[b3], r_sm3[b3], s3[:, 22:23], r_sm3[b3])
                sc.op("dve", lambda e: e.scalar_tensor_tensor(out=hm_f[b][:], in0=x1t[b3][:], scalar=s3[:, 21:22], in1=mg[:], op0=ALU.mult, op1=ALU.mult),
                      r=[r_x1t[b3], r_sm3[b3], r_const], w=[r_hmf[b]])
                sc.op("pool", lambda e: e.tensor_copy(out=hm_b[b][:], in_=hm_f[b][:]), r=[r_hmf[b]], w=[r_hmb[b]])
                sc.dma("pool", lambda e: e.dma_start(out=hm_d[ts_, :], in_=hm_b[b][:]), r=[r_hmb[b]], w=[r_hmd[t]])
                yield
                bkA, rbA = nbank()
                bkB, rbB = nbank()
                sc.pe([(lambda e, c=c: e.transpose(out=(bkA if c < 4 else bkB)[:, (c % 4) * 128:(c % 4 + 1) * 128], in_=hm_f[b][:, c * 128:(c + 1) * 128],
                                                   identity=ident_f[:])) for c in range(8)], r=[r_hmf[b], r_const], w=[rbA, rbB])
                sc.op("dve", lambda e: e.tensor_copy(out=hmT_f[:, 0:4, :], in_=bkA[:].rearrange("p (c n) -> p c n", c=4)), r=[rbA], w=[r_hmT])
                sc.op("act", lambda e: e.copy(out=hmT_f[:, 4:8, :], in_=bkB[:].rearrange("p (c n) -> p c n", c=4)), r=[rbB], w=[r_hmT])
                yield
                bk, rb = nbank()
                sc.pe([(lambda e, c=c: e.matmul(bk[:, 0:36], lhsT=hmT_f[:, c, :], rhs=w_rt[:, c, :], start=(c == 0), stop=(c == 7))) for c in range(8)],
                      r=[r_hmT, r_w3], w=[rb])
                sc.op("act", lambda e: e.copy(out=lg_all[:, t, :], in_=bk[:, 0:36]), r=[rb], w=[r_lg])
            run_pipelined(p3_tile, NT, 5)
            dump("logits", lg_all[:], r_lg, [128, NT, 36])
            if "x2" in dbg:
                sc.barrier()
                t_ = nc.dram_tensor("dbg_x2", [S, D], F32, kind="ExternalOutput").ap()
                dbg_out["x2"] = t_
                sc.dma("sp", lambda e: e.dma_start(out=t_, in_=out_d), is_out=True)
                t2_ = nc.dram_tensor("dbg_hm", [S, D], BF16, kind="ExternalOutput").ap()
                dbg_out["hm"] = t2_
                sc.dma("sp", lambda e: e.dma_start(out=t2_, in_=hm_d), is_out=True)
            sc.barrier()
            p3s.close()

        if last_phase >= 4:
            p4s = es.enter_context(ExitStack())
            reg_npos = nc.gpsimd.to_reg(NPOS - 1)
            reg_ew = nc.gpsimd.to_reg(32 * 128 - 1)
            r4 = sc.res("r4")

            def T4(name, shape, dt=F32):
                return sb("r4_" + name, shape, dt, p4s)

            def V(fn, r=(), w=(), eng="dve"):
                sc.op(eng, fn, r=[r4, r_lg, r_const] + list(r), w=[r4] + list(w))

            L = lg_all
            GL = L[:, :, 0:4]
            EL = L[:, :, 4:36].rearrange("p t (g j) -> p t g j", g=4)
            gb, goh, ge = T4("gb", [128, NT, 4]), T4("goh", [128, NT, 4]), T4("ge", [128, NT, 4])
            gmax, gm, gsum, gnum, gw = (T4(n, [128, NT]) for n in ("gmax", "gm", "gsum", "gnum", "gw"))
            t48 = T4("t48", [128, NT, 4, 8])
            esel, bsel, eb, oh1, eb2, oh2, ex, t8 = (T4(n, [128, NT, 8]) for n in ("esel", "bsel", "eb", "oh1", "eb2", "oh2", "ex", "t8"))
            m1, m2, em, a1, a2, den, ff = (T4(n, [128, NT]) for n in ("m1", "m2", "em", "a1", "a2", "den", "ff"))
            OH1, OH2 = T4("OH1", [128, NT, 32]), T4("OH2", [128, NT, 32])
            C_bf = T4("C_bf", [128, NT, 32], BF16)
            TT, PP, cA, cB, base, tmp32 = (T4(n, [128, NT, 32]) for n in ("TT", "PP", "cA", "cB", "base", "tmp32"))
            npad_i = T4("npad_i", [128, 32], I32)
            npad, eA, eB, off = (T4(n, [128, 32]) for n in ("npad", "eA", "eB", "off"))
            posf = T4("posf", [128, 2, NT])
            tpos_i = T4("tpos_i", [128, NTS], I32)
            tpos_f, eid_f = T4("tpos_f", [128, NTS]), T4("eid_f", [128, NTS])
            cmp_ = T4("cmp", [128, NTS, 32])
            pidx_i = T4("pidx_i", [128, 1], I32)
            pidx_f = T4("pidx_f", [128, 1])

            def bc(ap2, n):
                return ap2.unsqueeze(2).broadcast_to([128, NT, n])

            bg = b_rt[:, 0:4].unsqueeze(1).broadcast_to([128, NT, 4])
            be = b_rt[:, 4:36].rearrange("p (g j) -> p g j", g=4).unsqueeze(1).broadcast_to([128, NT, 4, 8])
            V(lambda e: e.tensor_tensor(out=gb[:], in0=GL, in1=bg, op=ALU.add))
            V(lambda e: e.tensor_reduce(out=gmax[:], in_=gb[:], axis=AX.X, op=ALU.max))
            V(lambda e: e.tensor_tensor(out=goh[:], in0=gb[:], in1=bc(gmax[:], 4), op=ALU.is_equal))
            V(lambda e: e.tensor_reduce(out=gm[:], in_=GL, axis=AX.X, op=ALU.max))
            V(lambda e: e.tensor_tensor(out=ge[:], in0=GL, in1=bc(gm[:], 4), op=ALU.subtract))
            V(lambda e: e.activation(out=ge[:].rearrange("p t g -> p (t g)"), in_=ge[:].rearrange("p t g -> p (t g)"), func=AF.Exp), eng="act")
            V(lambda e: e.tensor_reduce(out=gsum[:], in_=ge[:], axis=AX.X, op=ALU.add))
            V(lambda e: e.tensor_tensor(out=gb[:], in0=goh[:], in1=ge[:], op=ALU.mult))
            V(lambda e: e.tensor_reduce(out=gnum[:], in_=gb[:], axis=AX.X, op=ALU.add))
            V(lambda e: e.reciprocal(out=gsum[:], in_=gsum[:]))
            V(lambda e: e.tensor_tensor(out=gw[:], in0=gnum[:], in1=gsum[:], op=ALU.mult))
            goh_b = goh[:].unsqueeze(3).broadcast_to([128, NT, 4, 8])
            V(lambda e: e.tensor_tensor(out=t48[:], in0=EL, in1=goh_b, op=ALU.mult))
            V(lambda e: e.tensor_reduce(out=esel[:], in_=t48[:].rearrange("p t g j -> p t j g"), axis=AX.X, op=ALU.add))
            V(lambda e: e.tensor_tensor(out=t48[:], in0=be, in1=goh_b, op=ALU.mult))
            V(lambda e: e.tensor_reduce(out=bsel[:], in_=t48[:].rearrange("p t g j -> p t j g"), axis=AX.X, op=ALU.add))
            V(lambda e: e.tensor_tensor(out=eb[:], in0=esel[:], in1=bsel[:], op=ALU.add))
            V(lambda e: e.tensor_reduce(out=m1[:], in_=eb[:], axis=AX.X, op=ALU.max))
            V(lambda e: e.tensor_tensor(out=oh1[:], in0=eb[:], in1=bc(m1[:], 8), op=ALU.is_equal))
            V(lambda e: e.scalar_tensor_tensor(out=eb2[:].rearrange("p t j -> p (t j)"), in0=oh1[:].rearrange("p t j -> p (t j)"), scalar=-1e30,
                                               in1=eb[:].rearrange("p t j -> p (t j)"), op0=ALU.mult, op1=ALU.add))
            V(lambda e: e.tensor_reduce(out=m2[:], in_=eb2[:], axis=AX.X, op=ALU.max))
            V(lambda e: e.tensor_tensor(out=oh2[:], in0=eb2[:], in1=bc(m2[:], 8), op=ALU.is_equal))
            V(lambda e: e.tensor_reduce(out=em[:], in_=esel[:], axis=AX.X, op=ALU.max))
            V(lambda e: e.tensor_tensor(out=ex[:], in0=esel[:], in1=bc(em[:], 8), op=ALU.subtract))
            V(lambda e: e.activation(out=ex[:].rearrange("p t j -> p (t j)"), in_=ex[:].rearrange("p t j -> p (t j)"), func=AF.Exp), eng="act")
            V(lambda e: e.tensor_tensor(out=t8[:], in0=oh1[:], in1=ex[:], op=ALU.mult))
            V(lambda e: e.tensor_reduce(out=a1[:], in_=t8[:], axis=AX.X, op=ALU.add))
            V(lambda e: e.tensor_tensor(out=t8[:], in0=oh2[:], in1=ex[:], op=ALU.mult))
            V(lambda e: e.tensor_reduce(out=a2[:], in_=t8[:], axis=AX.X, op=ALU.add))
            V(lambda e: e.tensor_tensor(out=den[:], in0=a1[:], in1=a2[:], op=ALU.add))
            V(lambda e: e.reciprocal(out=den[:], in_=den[:]))
            V(lambda e: e.tensor_tensor(out=ff[:], in0=den[:], in1=gw[:], op=ALU.mult))
            V(lambda e: e.tensor_tensor(out=w12[:, 0, :], in0=a1[:], in1=ff[:], op=ALU.mult), w=[r_route])
            V(lambda e: e.tensor_tensor(out=w12[:, 1, :], in0=a2[:], in1=ff[:], op=ALU.mult), w=[r_route])
            V(lambda e: e.tensor_tensor(out=OH1[:].rearrange("p t (g j) -> p t g j", g=4), in0=goh_b,
                                        in1=oh1[:].unsqueeze(2).broadcast_to([128, NT, 4, 8]), op=ALU.mult))
            V(lambda e: e.tensor_tensor(out=OH2[:].rearrange("p t (g j) -> p t g j", g=4), in0=goh_b,
                                        in1=oh2[:].unsqueeze(2).broadcast_to([128, NT, 4, 8]), op=ALU.mult))
            V(lambda e: e.tensor_tensor(out=C_bf[:], in0=OH1[:], in1=OH2[:], op=ALU.add))
            W = NT * 32
            Cf = C_bf[:].rearrange("p t e -> p (t e)")
            TTf = TT[:].rearrange("p t e -> p (t e)")
            PPf = PP[:].rearrange("p t e -> p (t e)")
            for c0 in range(0, W, 512):
                c1 = min(W, c0 + 512)
                bk, rb = nbank()
                sc.pe([lambda e: e.matmul(bk[:, 0:c1 - c0], lhsT=ones_bf[:], rhs=Cf[:, c0:c1], start=True, stop=True)], r=[r4, r_const], w=[rb])
                sc.op("act", lambda e: e.copy(out=TTf[:, c0:c1], in_=bk[:, 0:c1 - c0]), r=[rb], w=[r4])
                bk, rb = nbank()
                sc.pe([lambda e: e.matmul(bk[:, 0:c1 - c0], lhsT=tri[:], rhs=Cf[:, c0:c1], start=True, stop=True)], r=[r4, r_const], w=[rb])
                sc.op("act", lambda e: e.copy(out=PPf[:, c0:c1], in_=bk[:, 0:c1 - c0]), r=[rb], w=[r4])
            V(lambda e: e.tensor_copy(out=cA[:], in_=TT[:]))
            cur, nxt = cA, cB
            s_ = 1
            while s_ < NT:
                V(lambda e: e.tensor_tensor(out=nxt[:, s_:NT, :], in0=cur[:, s_:NT, :], in1=cur[:, 0:NT - s_, :], op=ALU.add))
                V(lambda e: e.tensor_copy(out=nxt[:, 0:s_, :], in_=cur[:, 0:s_, :]))
                cur, nxt = nxt, cur
                s_ *= 2
            incl = cur
            V(lambda e: e.tensor_scalar(out=npad[:], in0=incl[:, NT - 1, :], scalar1=float(TS - 1), scalar2=None, op0=ALU.add))
            V(lambda e: e.tensor_copy(out=npad_i[:], in_=npad[:]))
            V(lambda e: e.tensor_scalar(out=npad_i[:], in0=npad_i[:], scalar1=8, scalar2=8, op0=ALU.arith_shift_right, op1=ALU.logical_shift_left))
            V(lambda e: e.tensor_copy(out=npad[:], in_=npad_i[:]))
            V(lambda e: e.tensor_copy(out=eA[:], in_=npad[:]))
            cur2, nxt2 = eA, eB
            s_ = 1
            while s_ < 32:
                V(lambda e: e.tensor_tensor(out=nxt2[:, s_:32], in0=cur2[:, s_:32], in1=cur2[:, 0:32 - s_], op=ALU.add))
                V(lambda e: e.tensor_copy(out=nxt2[:, 0:s_], in_=cur2[:, 0:s_]))
                cur2, nxt2 = nxt2, cur2
                s_ *= 2
            endI = cur2
            V(lambda e: e.tensor_tensor(out=off[:], in0=endI[:], in1=npad[:], op=ALU.subtract))
            V(lambda e: e.tensor_tensor(out=base[:], in0=incl[:], in1=TT[:], op=ALU.subtract))
            V(lambda e: e.tensor_tensor(out=base[:], in0=base[:], in1=PP[:], op=ALU.add))
            V(lambda e: e.tensor_tensor(out=base[:], in0=base[:], in1=off[:].unsqueeze(1).broadcast_to([128, NT, 32]), op=ALU.add))
            V(lambda e: e.tensor_tensor(out=tmp32[:], in0=OH1[:], in1=base[:], op=ALU.mult))
            V(lambda e: e.tensor_reduce(out=posf[:, 0, :], in_=tmp32[:], axis=AX.X, op=ALU.add))
            V(lambda e: e.tensor_tensor(out=tmp32[:], in0=OH2[:], in1=base[:], op=ALU.mult))
            V(lambda e: e.tensor_reduce(out=posf[:, 1, :], in_=tmp32[:], axis=AX.X, op=ALU.add))
            V(lambda e: e.tensor_copy(out=pos12[:], in_=posf[:]), w=[r_route])
            V(lambda e: e.iota(tpos_i[:], pattern=[[TS, NTS]], base=0, channel_multiplier=0), eng="pool")
            V(lambda e: e.iota(pidx_i[:], pattern=[[0, 1]], base=0, channel_multiplier=1), eng="pool")
            V(lambda e: e.tensor_copy(out=tpos_f[:], in_=tpos_i[:]))
            V(lambda e: e.tensor_copy(out=pidx_f[:], in_=pidx_i[:]))
            V(lambda e: e.tensor_tensor(out=cmp_[:], in0=endI[:].unsqueeze(1).broadcast_to([128, NTS, 32]),
                                        in1=tpos_f[:].unsqueeze(2).broadcast_to([128, NTS, 32]), op=ALU.is_le))
            V(lambda e: e.tensor_reduce(out=eid_f[:], in_=cmp_[:], axis=AX.X, op=ALU.add))
            V(lambda e: e.tensor_scalar(out=eid_f[:], in0=eid_f[:], scalar1=31.0, scalar2=128.0, op0=ALU.min, op1=ALU.mult))
            V(lambda e: e.tensor_scalar(out=eid_f[:], in0=eid_f[:], scalar1=pidx_f[:, 0:1], scalar2=None, op0=ALU.add))
            V(lambda e: e.tensor_copy(out=widx[:], in_=eid_f[:]), w=[r_route])
            dump("pos12", pos12[:], r_route, [128, 2, NT], I32)
            dump("w12", w12[:], r_route, [128, 2, NT])
            dump("widx", widx[:], r_route, [128, NTS], I32)

            hsb = [sb("hsb%d" % i, [128, 1024], BF16, p4s) for i in range(2)]
            r_hsb = mkres("hsb")
            for t in range(NT):
                b = t % 2
                ts_ = slice(t * 128, (t + 1) * 128)
                sc.dma("sp", lambda e: e.dma_start(out=hsb[b][:], in_=hm_d[ts_, :]), r=[r_hmd[t]], w=[r_hsb[b]])
                for k in range(2):
                    sc.dma("pool", lambda e: e.indirect_dma_start(out=xs_d[:, :], out_offset=bass.IndirectOffsetOnAxis(ap=pos12[:, k, t:t + 1], axis=0),
                                                                  in_=hsb[b][:], in_offset=None, bounds_check=reg_npos, oob_is_err=False),
                           r=[r_hsb[b], r_route], w=[])
            sc.barrier()
            p4s.close()

        if last_phase >= 5:
            p5s = es.enter_context(ExitStack())
            NWB = 5
            wall = [sb("wall%d" % i, [128, 6144], BF16, p5s) for i in range(NWB)]
            wg = [w_[:, 0:2048] for w_ in wall]
            wu = [w_[:, 2048:4096] for w_ in wall]
            wd = [w_[:, 4096:6144] for w_ in wall]
            r_wg = mkres("wall", NWB)
            r_wu = r_wg
            r_wd = r_wg
            xrow = [sb("xrow%d" % i, [128, 1024], BF16, p5s) for i in range(10)]
            r_xrow = mkres("xrow", 10)
            XsT = [sb("XsT%d" % i, [128, 8, TS], BF16, p5s) for i in range(5)]
            r_XsT = mkres("XsT", 5)
            sa = [sb("sa%d" % i, [128, TS], F32, p5s) for i in range(2)]
            r_sa = mkres("sa")
            actT = [sb("actT%d" % i, [128, 2, TS], BF16, p5s) for i in range(5)]
            r_actT = mkres("actT", 5)
            yt = [sb("yt%d" % i, [128, 1024], F32, p5s) for i in range(4)]
            r_yt = mkres("yt", 4)
            r_ys = sc.res("ys_d")
            cnt5 = {"x": 0, "y": 0}
            def p5_tile(tp):
                wb = tp % NWB
                b = tp % 5
                sc.dma("pool", lambda e: e.indirect_dma_start(out=wall[wb][:], out_offset=None, in_=ewb_all[:, :],
                                                              in_offset=bass.IndirectOffsetOnAxis(ap=widx[:, tp:tp + 1], axis=0),
                                                              bounds_check=reg_ew, oob_is_err=False), r=[r_route, r_ewb], w=[r_wg[wb]])
                for s in range(TS // 128):
                    xi = (2 * tp + s) % 10
                    r0_ = tp * TS + s * 128
                    sc.dma("sp", lambda e: e.dma_start(out=xrow[xi][:], in_=xs_d[r0_:r0_ + 128, :]), r=[r_xs], w=[r_xrow[xi]])
                yield
                for s in range(TS // 128):
                    xi = (2 * tp + s) % 10
                    bk, rb = nbank()
                    pv = bk[:].bitcast(BF16).rearrange("p (c n) -> p c n", n=128)
                    sc.pe([(lambda e, c=c: e.transpose(out=pv[:, c, :], in_=xrow[xi][:, c * 128:(c + 1) * 128], identity=ident_bf[:])) for c in range(8)],
                          r=[r_xrow[xi], r_const], w=[rb])
                    sc.op("dve" if s % 2 == 0 else "act",
                          (lambda e: e.tensor_copy(out=XsT[b][:, :, s * 128:(s + 1) * 128], in_=pv)) if s % 2 == 0 else
                          (lambda e: e.copy(out=XsT[b][:, :, s * 128:(s + 1) * 128], in_=pv)), r=[rb], w=[r_XsT[b]])
                yield
                wgv = wg[wb].rearrange("p (c f) -> p c f", c=8)
                wuv = wu[wb].rearrange("p (c f) -> p c f", c=8)
                wdv = wd[wb].rearrange("p (c d) -> p c d", c=2)
                for fc in range(2):
                    bk, rb = nbank()
                    fns = [(lambda e, c=c: e.matmul(bk[:, 0:TS], lhsT=wgv[:, c, fc * 128:(fc + 1) * 128], rhs=XsT[b][:, c, :], start=(c == 0), stop=(c == 7)))
                           for c in range(8)]
                    fns += [(lambda e, c=c: e.matmul(bk[:, TS:2 * TS], lhsT=wuv[:, c, fc * 128:(fc + 1) * 128], rhs=XsT[b][:, c, :], start=(c == 0), stop=(c == 7)))
                            for c in range(8)]
                    sc.pe(fns, r=[r_wg[wb], r_wu[wb], r_XsT[b]], w=[rb])
                    sc.op("act", lambda e: e.activation(out=sa[fc][:], in_=bk[:, 0:TS], func=AF.Silu), r=[rb], w=[r_sa[fc]])
                    sc.op("dve", lambda e: e.tensor_tensor(out=actT[b][:, fc, :], in0=bk[:, TS:2 * TS], in1=sa[fc][:], op=ALU.mult),
                          r=[rb, r_sa[fc]], w=[r_actT[b]])
                    yield
                for s in range(TS // 128):
                    yi = cnt5["y"] % 4
                    cnt5["y"] += 1
                    for half in range(2):
                        bk, rb = nbank()
                        sc.pe([(lambda e, fc=fc: e.matmul(bk[:], lhsT=actT[b][:, fc, s * 128:(s + 1) * 128], rhs=wdv[:, fc, half * 512:(half + 1) * 512],
                                                          start=(fc == 0), stop=(fc == 1))) for fc in range(2)], r=[r_actT[b], r_wd[wb]], w=[rb])
                        if half == 0:
                            sc.op("act", lambda e: e.copy(out=yt[yi][:, 0:512], in_=bk[:]), r=[rb], w=[r_yt[yi]])
                        else:
                            sc.op("dve", lambda e: e.tensor_copy(out=yt[yi][:, 512:1024], in_=bk[:]), r=[rb], w=[r_yt[yi]])
                    r0_ = tp * TS + s * 128
                    sc.dma("act", lambda e: e.dma_start(out=ys_d[r0_:r0_ + 128, :], in_=yt[yi][:]), r=[r_yt[yi]], w=[])
                    yield
            run_pipelined(p5_tile, NTS, 5)
            sc.barrier()
            p5s.close()

        if last_phase >= 6:
            p6s = es.enter_context(ExitStack())
            y1 = [sb("y1_%d" % i, [128, 1024], F32, p6s) for i in range(2)]
            y2 = [sb("y2_%d" % i, [128, 1024], F32, p6s) for i in range(2)]
            xo = [sb("xo_%d" % i, [128, 1024], F32, p6s) for i in range(2)]
            r_y1, r_y2, r_xo = mkres("y1"), mkres("y2"), mkres("xo")
            def p6_tile(t):
                b = t % 2
                ts_ = slice(t * 128, (t + 1) * 128)
                for k, (yy, ry) in enumerate(((y1[b], r_y1[b]), (y2[b], r_y2[b]))):
                    sc.dma("pool", lambda e: e.indirect_dma_start(out=yy[:], out_offset=None, in_=ys_d[:, :],
                                                                  in_offset=bass.IndirectOffsetOnAxis(ap=pos12[:, k, t:t + 1], axis=0),
                                                                  bounds_check=reg_npos, oob_is_err=False), r=[r_route, r_ys], w=[ry])
                sc.dma("sp", lambda e: e.dma_start(out=xo[b][:], in_=out_d[ts_, :]), r=[r_x2d[t]], w=[r_xo[b]])
                yield
                sc.op("dve", lambda e: e.scalar_tensor_tensor(out=xo[b][:], in0=y1[b][:], scalar=w12[:, 0, t:t + 1], in1=xo[b][:], op0=ALU.mult, op1=ALU.add),
                      r=[r_y1[b], r_route], w=[r_xo[b]])
                sc.op("pool", lambda e: e.scalar_tensor_tensor(out=xo[b][:], in0=y2[b][:], scalar=w12[:, 1, t:t + 1], in1=xo[b][:], op0=ALU.mult, op1=ALU.add),
                      r=[r_y2[b], r_route], w=[r_xo[b]]) if False else \
                    sc.op("dve", lambda e: e.scalar_tensor_tensor(out=xo[b][:], in0=y2[b][:], scalar=w12[:, 1, t:t + 1], in1=xo[b][:], op0=ALU.mult, op1=ALU.add),
                          r=[r_y2[b], r_route], w=[r_xo[b]])
                sc.dma("act", lambda e: e.dma_start(out=out_d[ts_, :], in_=xo[b][:]), r=[r_xo[b]], w=[r_x2d[t]], is_out=True)
            run_pipelined(p6_tile, NT, 2)
            sc.barrier()
            p6s.close()

        sc.finish()
    print("program: %d instructions, %d waits" % (sc.n_inst, sc.n_wait))
    return nc, dbg_out


def _perm_rows(w, c):
    n = w.shape[1]
    return np.ascontiguousarray(w.reshape(c, 128, n).transpose(1, 0, 2))


def _consts():
    cst = np.zeros((128, NCST), np.float64)
    j64 = np.arange(64)
    j32 = np.arange(32)
    invR = 10000.0 ** (-j64 / 64.0) / (2 * np.pi)
    invM = 10000.0 ** (-j32 / 32.0) / (2 * np.pi)
    cst[:, 0:64] = invR
    cst[:, 64:128] = invR
    cst[:, 128:160] = invM
    cst[:, 160:192] = invM
    cst[:, 192 + 64:192 + 128] = 0.25
    cst[:, 192 + 160:192 + 192] = 0.25
    h = np.arange(4)
    lg = np.log(1.0 - np.exp2(-5.0 - h))
    p = np.arange(128)[:, None]
    cst[:, 384:388] = np.exp((p + 1.0) * lg[None, :])
    cst[:, 388:392] = np.exp(-(p + 1.0) * lg[None, :]) * (128.0 ** -0.5)
    cst[:, 392:904] = np.repeat(np.exp(128.0 * lg), 128)[None, :]
    return cst.astype(np.float32)


def make_in_maps(inputs, S, n_cores, last_phase=6):
    f = lambda a: np.ascontiguousarray(np.asarray(a), dtype=np.float32)
    l = 0
    shared = {
        "cst": _consts(),
        "w_in": _perm_rows(f(inputs["w_in"][l]), 8),
        "g_attn": np.ascontiguousarray(f(inputs["attn_norm_g"][l]).reshape(8, 128).T),
        "w_uq": _perm_rows(f(inputs["mla_w_uq"][l]), 2),
        "g_qn": np.ascontiguousarray(f(inputs["mla_q_norm_g"][l]).reshape(2, 128).T),
        "w_ukv": f(inputs["mla_w_ukv"][l]),
        "g_kvn": f(inputs["mla_kv_norm_g"][l]).reshape(128, 1),
        "gq": f(inputs["mla_q_qk_g"][l]).reshape(1, 192),
        "gk": f(inputs["mla_k_qk_g"][l]).reshape(1, 192),
        "gn": f(inputs["ret_gn_g"][l]).reshape(1, 512),
        "w_out": _perm_rows(f(inputs["w_out"][l]), 8),
        "g_cross": np.ascontiguousarray(f(inputs["cross_norm_g"][l]).reshape(8, 128).T),
        "g_mem": np.ascontiguousarray(f(inputs["mem_norm_g"][l]).reshape(8, 128).T),
        "cw_q": _perm_rows(f(inputs["cross_w_q"][l]), 8),
        "cw_kv": _perm_rows(f(inputs["cross_w_kv"][l]), 8),
        "cqg": f(inputs["cross_q_qk_g"][l]).reshape(1, 256),
        "ckg": f(inputs["cross_k_qk_g"][l]).reshape(1, 256),
        "cw_o": _perm_rows(f(inputs["cross_w_o"][l]), 8),
        "mg": f(inputs["moe_norm_g"][l]).reshape(1, 1024),
        "w_rt": _perm_rows(np.concatenate([f(inputs["router_w_group"][l]), f(inputs["router_w_expert"][l])], axis=1), 8),
        "b_rt": np.concatenate([f(inputs["router_b_group"][l]), f(inputs["router_b_expert"][l])]).reshape(1, 36),
        "ew_g": np.ascontiguousarray(f(inputs["expert_w_gate"][l]).reshape(32, 8, 128, 256).transpose(0, 2, 1, 3)).reshape(32 * 128, 2048),
        "ew_u": np.ascontiguousarray(f(inputs["expert_w_up"][l]).reshape(32, 8, 128, 256).transpose(0, 2, 1, 3)).reshape(32 * 128, 2048),
        "ew_d": np.ascontiguousarray(f(inputs["expert_w_down"][l]).reshape(32, 2, 128, 1024).transpose(0, 2, 1, 3)).reshape(32 * 128, 2048),
    }
    if last_phase < 5:
        for k in ("ew_g", "ew_u", "ew_d"):
            del shared[k]
    NT = S // 128
    maps = []
    for b in range(n_cores):
        m = dict(shared)
        m["x"] = f(inputs["x"][b])
        m["mem"] = f(inputs["mem"][b])
        m["pos"] = np.ascontiguousarray(np.asarray(inputs["positions"][b]).astype(np.int32).reshape(NT, 128).T)
        maps.append(m)
    return maps


def kernel(**inputs):
    B, S, _ = inputs["x"].shape
    nc, _ = build_program(S)
    maps = make_in_maps(inputs, S, B)
    res = run_bass_kernel_spmd(nc, maps, core_ids=list(range(B)))
    return np.stack([np.asarray(r["out"]) for r in res.results], axis=0).astype(np.float32)
```
